# Optimizing a Trainium2 kernel written in Bass

```python
import jax
import jax.numpy as jnp
from jax import lax
import numpy as np

D_MODEL = 1024
BATCH = 2
SEQ = 16384
DEPTH = 2

CTX_LEN = 256
GRID_W = 64
BLOCK = 128
HEAD_DIM = 64
ROPE_THETA = 10000.0
EPS = 1e-6
ML_HEADS = 4
ML_DIM = 128
ML_WIDTH = ML_HEADS * ML_DIM
WIN_Q_HEADS = 8
WIN_KV_HEADS = 2
WINDOW = 128
WIN_WIDTH = WIN_Q_HEADS * HEAD_DIM
WIN_KV_WIDTH = WIN_KV_HEADS * HEAD_DIM
GLB_Q_HEADS = 8
GLB_KV_HEADS = 2
GLB_WIDTH = GLB_Q_HEADS * HEAD_DIM
GLB_KV_WIDTH = GLB_KV_HEADS * HEAD_DIM
N_BRANCH = 3
BRANCH_WIDTH = 512
D_FF = ((8 * D_MODEL + 3 * 256 - 1) // (3 * 256)) * 256
SPLITS = (ML_WIDTH, ML_WIDTH, ML_WIDTH, ML_WIDTH, 2 * ML_HEADS, 2 * ML_HEADS,
          WIN_WIDTH, WIN_KV_WIDTH, WIN_KV_WIDTH,
          GLB_WIDTH, GLB_KV_WIDTH, GLB_KV_WIDTH,
          N_BRANCH * D_MODEL)
D_IN = sum(SPLITS)
ML_F_OFF = 4 * ML_WIDTH + 2 * ML_HEADS

kernel_name = 'hybrid_mlstm_swa_axial_gqa_dit'


def rmsnorm(x, g):
    xf = x.astype(jnp.float32)
    y = xf * lax.rsqrt(jnp.mean(xf * xf, axis=-1, keepdims=True) + EPS)
    return (y * g.astype(jnp.float32)).astype(x.dtype)


def modulate(h, shift, scale):
    return h * (1 + scale) + shift


def split_cols(p):
    idx = np.cumsum(SPLITS)[:-1].tolist()
    return jnp.split(p, idx, axis=-1)


def split_heads(a, n_heads):
    b, t, _ = a.shape
    return a.reshape(b, t, n_heads, -1).transpose(0, 2, 1, 3)


def merge_heads(a):
    b, h, t, d = a.shape
    return a.transpose(0, 2, 1, 3).reshape(b, t, h * d)


def axial_angles(n_tok):
    rows = n_tok // GRID_W
    row = jnp.broadcast_to(jnp.arange(rows)[:, None], (rows, GRID_W)).reshape(-1)
    col = jnp.broadcast_to(jnp.arange(GRID_W)[None, :], (rows, GRID_W)).reshape(-1)
    half = HEAD_DIM // 2
    inv = ROPE_THETA ** (-jnp.arange(0, half, 2, dtype=jnp.float32) / half)
    return row.astype(jnp.float32)[:, None] * inv, col.astype(jnp.float32)[:, None] * inv


def rope_1d(x, ang):
    x1, x2 = jnp.split(x, 2, axis=-1)
    cos = jnp.cos(ang).astype(x.dtype)
    sin = jnp.sin(ang).astype(x.dtype)
    return jnp.concatenate([x1 * cos - x2 * sin, x1 * sin + x2 * cos], axis=-1)


def rope_axial(x, ang_row, ang_col):
    xr, xc = jnp.split(x, 2, axis=-1)
    return jnp.concatenate([rope_1d(xr, ang_row), rope_1d(xc, ang_col)], axis=-1)


def zero_state(b, h, d):
    return (jnp.zeros((b, h, d, d), jnp.float32), jnp.zeros((b, h, d), jnp.float32),
            jnp.zeros((b, h), jnp.float32))


def mlstm_scan(q, k, v, ig, lf, state):
    b, h, t, d = q.shape
    nc = t // BLOCK
    out_dtype = q.dtype

    def to_chunks(a):
        a = a.astype(jnp.float32)
        return jnp.moveaxis(a.reshape(b, h, nc, BLOCK, *a.shape[3:]), 2, 0)

    lower = jnp.tril(jnp.ones((BLOCK, BLOCK), bool))
    scale = d ** -0.5

    def step(carry, xs):
        C, n, m = carry
        qc, kc, vc, ic, fc = xs
        qc = qc * scale
        bcum = jnp.cumsum(fc, axis=-1)
        dlog = jnp.where(lower, bcum[..., :, None] - bcum[..., None, :] + ic[..., None, :], -jnp.inf)
        inter = bcum + m[..., None]
        m_t = jnp.maximum(inter, jnp.max(dlog, axis=-1))
        w = jnp.exp(dlog - m_t[..., None])
        a = jnp.exp(inter - m_t)
        s = jnp.einsum('bhtd,bhsd->bhts', qc, kc) * w
        num = a[..., None] * jnp.einsum('bhtd,bhde->bhte', qc, C) + jnp.einsum('bhts,bhse->bhte', s, vc)
        den = a * jnp.einsum('bhtd,bhd->bht', qc, n) + jnp.sum(s, axis=-1)
        hc = num / jnp.maximum(jnp.abs(den), jnp.exp(-m_t))[..., None]
        b_end = bcum[..., -1]
        wlog = b_end[..., None] - bcum + ic
        m_new = jnp.maximum(b_end + m, jnp.max(wlog, axis=-1))
        a_c = jnp.exp(b_end + m - m_new)
        wk = jnp.exp(wlog - m_new[..., None])
        C_new = a_c[..., None, None] * C + jnp.einsum('bhs,bhsd,bhse->bhde', wk, kc, vc)
        n_new = a_c[..., None] * n + jnp.einsum('bhs,bhsd->bhd', wk, kc)
        return (C_new, n_new, m_new), hc

    xs = (to_chunks(q), to_chunks(k), to_chunks(v), to_chunks(ig), to_chunks(lf))
    state, hs = lax.scan(step, state, xs)
    return jnp.moveaxis(hs, 0, 2).reshape(b, h, t, d).astype(out_dtype), state


def flip_if(a, rev):
    return jnp.flip(a, axis=2) if rev else a


def gate_dirs(a):
    b, t, _ = a.shape
    return a.reshape(b, t, 2, ML_HEADS).transpose(2, 0, 3, 1)


def mlstm_branch(lat, ctx, norm_g, ctx_out):
    def prep(parts):
        q, k, v, _, ig, fg = parts
        return (split_heads(q, ML_HEADS), split_heads(k, ML_HEADS), split_heads(v, ML_HEADS),
                gate_dirs(ig), jax.nn.log_sigmoid(gate_dirs(fg).astype(jnp.float32)))

    lq, lk, lv, lig, llf = prep(lat)
    cq, ck, cv, cig, clf = prep(ctx)
    b = lq.shape[0]
    h_lat, h_ctx = [], []
    for dr, rev in enumerate((False, True)):
        hc, st = mlstm_scan(flip_if(cq, rev), flip_if(ck, rev), flip_if(cv, rev),
                            flip_if(cig[dr], rev), flip_if(clf[dr], rev), zero_state(b, ML_HEADS, ML_DIM))
        hl, _ = mlstm_scan(flip_if(lq, rev), flip_if(lk, rev), flip_if(lv, rev),
                           flip_if(lig[dr], rev), flip_if(llf[dr], rev), st)
        h_ctx.append(flip_if(hc, rev))
        h_lat.append(flip_if(hl, rev))

    def finish(hs, o):
        return merge_heads(rmsnorm(hs[0] + hs[1], norm_g[:, None, :])) * jax.nn.sigmoid(o)

    y_lat = finish(h_lat, lat[3])
    y_ctx = finish(h_ctx, ctx[3]) if ctx_out else None
    return y_lat, y_ctx


def window_attention(q, k, v, k_ctx, v_ctx, sink):
    b, hq, n_tok, d = q.shape
    g = k.shape[1]
    r = hq // g
    nb = n_tok // BLOCK
    n_ctx = k_ctx.shape[2]
    scale = d ** -0.5
    qb = jnp.moveaxis(q.reshape(b, g, r, nb, BLOCK, d), 3, 0)

    def band(a):
        ap = jnp.pad(a, ((0, 0), (0, 0), (BLOCK, BLOCK), (0, 0))).reshape(b, g, nb + 2, BLOCK, d)
        a3 = jnp.concatenate([ap[:, :, :-2], ap[:, :, 1:-1], ap[:, :, 2:]], axis=3)
        return jnp.moveaxis(a3, 2, 0)

    kb, vb = band(k), band(v)
    qpos = jnp.arange(n_tok).reshape(nb, BLOCK)
    kpos = jnp.arange(nb)[:, None] * BLOCK - BLOCK + jnp.arange(3 * BLOCK)[None, :]
    valid = ((jnp.abs(qpos[:, :, None] - kpos[:, None, :]) <= WINDOW)
             & (kpos[:, None, :] >= 0) & (kpos[:, None, :] < n_tok))
    sink_g = sink.astype(jnp.float32).reshape(1, g, r, 1, 1)

    def one_block(args):
        qblk, kblk, vblk, ok = args
        s_loc = jnp.where(ok, jnp.einsum('bgrqd,bgkd->bgrqk', qblk, kblk).astype(jnp.float32) * scale, -jnp.inf)
        s_ctx = jnp.einsum('bgrqd,bgkd->bgrqk', qblk, k_ctx).astype(jnp.float32) * scale
        s_snk = jnp.broadcast_to(sink_g, s_ctx.shape[:-1] + (1,))
        p = jax.nn.softmax(jnp.concatenate([s_loc, s_ctx, s_snk], axis=-1), axis=-1).astype(v.dtype)
        return (jnp.einsum('bgrqk,bgkd->bgrqd', p[..., :3 * BLOCK], vblk)
                + jnp.einsum('bgrqk,bgkd->bgrqd', p[..., 3 * BLOCK:3 * BLOCK + n_ctx], v_ctx))

    o = lax.map(one_block, (qb, kb, vb, valid))
    return jnp.moveaxis(o, 0, 3).reshape(b, hq, n_tok, d)


def global_attention(q, k_all, v_all):
    b, hq, n_tok, d = q.shape
    g = k_all.shape[1]
    r = hq // g
    nb = n_tok // BLOCK
    scale = d ** -0.5
    qb = jnp.moveaxis(q.reshape(b, g, r, nb, BLOCK, d), 3, 0)

    def one_block(qblk):
        s = jnp.einsum('bgrqd,bgkd->bgrqk', qblk, k_all).astype(jnp.float32) * scale
        p = jax.nn.softmax(s, axis=-1).astype(v_all.dtype)
        return jnp.einsum('bgrqk,bgkd->bgrqd', p, v_all)

    o = lax.map(one_block, qb)
    return jnp.moveaxis(o, 0, 3).reshape(b, hq, n_tok, d)


def ctx_attention(q, k, v, sink):
    b, hq, n_ctx, d = q.shape
    g = k.shape[1]
    r = hq // g
    s = jnp.einsum('bgrqd,bgkd->bgrqk', q.reshape(b, g, r, n_ctx, d), k).astype(jnp.float32) * d ** -0.5
    if sink is not None:
        s_snk = jnp.broadcast_to(sink.astype(jnp.float32).reshape(1, g, r, 1, 1), s.shape[:-1] + (1,))
        s = jnp.concatenate([s, s_snk], axis=-1)
    p = jax.nn.softmax(s, axis=-1)[..., :n_ctx].astype(v.dtype)
    return jnp.einsum('bgrqk,bgkd->bgrqd', p, v).reshape(b, hq, n_ctx, d)


def merge_branches(ys, gate_logits, w_br, w_o):
    gates = jnp.split(gate_logits, N_BRANCH, axis=-1)
    merged = jax.nn.sigmoid(gates[0]) * (ys[0] @ w_br[0])
    for i in range(1, N_BRANCH):
        merged = merged + jax.nn.sigmoid(gates[i]) * (ys[i] @ w_br[i])
    return merged @ w_o


def token_mixers(hx, hc, w_in, b_in, ml_norm_g, win_sink, qn_g, kn_g, w_br, w_o, ang_row, ang_col, ctx_out):
    px = split_cols(hx @ w_in + b_in)
    pc = split_cols(hc @ w_in + b_in)
    ya_x, ya_c = mlstm_branch(px[0:6], pc[0:6], ml_norm_g, ctx_out)
    wq_x = rope_axial(split_heads(px[6], WIN_Q_HEADS), ang_row, ang_col)
    wk_x = rope_axial(split_heads(px[7], WIN_KV_HEADS), ang_row, ang_col)
    wv_x = split_heads(px[8], WIN_KV_HEADS)
    wk_c = split_heads(pc[7], WIN_KV_HEADS)
    wv_c = split_heads(pc[8], WIN_KV_HEADS)
    yb_x = merge_heads(window_attention(wq_x, wk_x, wv_x, wk_c, wv_c, win_sink))
    gq_x = rope_axial(rmsnorm(split_heads(px[9], GLB_Q_HEADS), qn_g), ang_row, ang_col)
    gk_x = rope_axial(rmsnorm(split_heads(px[10], GLB_KV_HEADS), kn_g), ang_row, ang_col)
    gk_c = rmsnorm(split_heads(pc[10], GLB_KV_HEADS), kn_g)
    gv_c = split_heads(pc[11], GLB_KV_HEADS)
    k_all = jnp.concatenate([gk_c, gk_x], axis=2)
    v_all = jnp.concatenate([gv_c, split_heads(px[11], GLB_KV_HEADS)], axis=2)
    yc_x = merge_heads(global_attention(gq_x, k_all, v_all))
    out_x = merge_branches((ya_x, yb_x, yc_x), px[12], w_br, w_o)
    if not ctx_out:
        return out_x, None
    yb_c = merge_heads(ctx_attention(split_heads(pc[6], WIN_Q_HEADS), wk_c, wv_c, win_sink))
    yc_c = merge_heads(ctx_attention(rmsnorm(split_heads(pc[9], GLB_Q_HEADS), qn_g), gk_c, gv_c, None))
    out_c = merge_branches((ya_c, yb_c, yc_c), pc[12], w_br, w_o)
    return out_x, out_c


def swiglu(h, w1, w3, w2):
    return (jax.nn.silu(h @ w1) * (h @ w3)) @ w2


def setup_inputs(seed: int = 0) -> dict:
    key = jax.random.key(seed)
    ks = jax.random.split(key, 24)
    f32 = jnp.float32

    def lin(k, shape, fan_in):
        return jax.random.normal(k, shape, f32) * fan_in ** -0.5

    def gain(k, shape):
        return 1.0 + 0.05 * jax.random.normal(k, shape, f32)

    b_in = 0.02 * jax.random.normal(ks[8], (DEPTH, D_IN), f32)
    f_bias = 3.0 + 3.0 * jax.random.uniform(ks[9], (DEPTH, 2 * ML_HEADS), f32)
    b_in = b_in.at[:, ML_F_OFF:ML_F_OFF + 2 * ML_HEADS].set(f_bias)
    return {
        'x': jax.random.normal(ks[0], (BATCH, SEQ, D_MODEL), f32),
        'c': jax.random.normal(ks[1], (BATCH, D_MODEL), f32),
        'ctx': jax.random.normal(ks[2], (BATCH, CTX_LEN, D_MODEL), f32),
        'c_ctx': jax.random.normal(ks[3], (D_MODEL,), f32),
        'w_mod': lin(ks[4], (DEPTH, D_MODEL, 6 * D_MODEL), D_MODEL),
        'b_mod': 0.02 * jax.random.normal(ks[5], (DEPTH, 6 * D_MODEL), f32),
        'norm1_g': gain(ks[6], (DEPTH, D_MODEL)),
        'w_in': lin(ks[7], (DEPTH, D_MODEL, D_IN), D_MODEL),
        'b_in': b_in,
        'ml_norm_g': gain(ks[10], (DEPTH, ML_HEADS, ML_DIM)),
        'win_sink': jax.random.normal(ks[11], (DEPTH, WIN_Q_HEADS), f32),
        'qn_g': gain(ks[12], (DEPTH, HEAD_DIM)),
        'kn_g': gain(ks[13], (DEPTH, HEAD_DIM)),
        'w_br': lin(ks[14], (DEPTH, N_BRANCH, BRANCH_WIDTH, D_MODEL), BRANCH_WIDTH),
        'w_o': lin(ks[15], (DEPTH, D_MODEL, D_MODEL), D_MODEL),
        'norm2_g': gain(ks[16], (DEPTH, D_MODEL)),
        'w_ff1': lin(ks[17], (DEPTH, D_MODEL, D_FF), D_MODEL),
        'w_ff3': lin(ks[18], (DEPTH, D_MODEL, D_FF), D_MODEL),
        'w_ff2': lin(ks[19], (DEPTH, D_FF, D_MODEL), D_FF),
        'final_g': gain(ks[20], (D_MODEL,)),
    }


def reference(x, c, ctx, c_ctx, w_mod, b_mod, norm1_g, w_in, b_in, ml_norm_g, win_sink, qn_g, kn_g,
              w_br, w_o, norm2_g, w_ff1, w_ff3, w_ff2, final_g):
    ang_row, ang_col = axial_angles(x.shape[1])
    xc = ctx
    for l in range(DEPTH):
        ctx_out = l < DEPTH - 1
        mod_x = jnp.split((jax.nn.silu(c) @ w_mod[l] + b_mod[l])[:, None, :], 6, axis=-1)
        mod_c = jnp.split(jax.nn.silu(c_ctx) @ w_mod[l] + b_mod[l], 6, axis=-1)
        hx = modulate(rmsnorm(x, norm1_g[l]), mod_x[0], mod_x[1])
        hc = modulate(rmsnorm(xc, norm1_g[l]), mod_c[0], mod_c[1])
        out_x, out_c = token_mixers(hx, hc, w_in[l], b_in[l], ml_norm_g[l], win_sink[l], qn_g[l], kn_g[l],
                                    w_br[l], w_o[l], ang_row, ang_col, ctx_out)
        x = x + mod_x[2] * out_x
        hx = modulate(rmsnorm(x, norm2_g[l]), mod_x[3], mod_x[4])
        x = x + mod_x[5] * swiglu(hx, w_ff1[l], w_ff3[l], w_ff2[l])
        if ctx_out:
            xc = xc + mod_c[2] * out_c
            hc = modulate(rmsnorm(xc, norm2_g[l]), mod_c[3], mod_c[4])
            xc = xc + mod_c[5] * swiglu(hc, w_ff1[l], w_ff3[l], w_ff2[l])
    return rmsnorm(x, final_g)
```

```python
import contextlib
import numpy as np
import ml_dtypes
import concourse.bass as bass
import concourse.mybir as mybir
from concourse.bass_utils import run_bass_kernel_spmd

F32 = mybir.dt.float32
BF16 = mybir.dt.bfloat16
AF = mybir.ActivationFunctionType
ALU = mybir.AluOpType
AX = mybir.AxisListType

D = 1024
KC = 8
DFF = 2816
NFC = 22
EPS = 1e-6
NFM = 52
TMW = 1808
DEPTH = 2
SEQ = 16384
CTX = 256


_ALLBUFS = []


class Buf:
    __slots__ = ("ws", "rs", "name")

    def __init__(self, name=""):
        self.ws = []
        self.rs = []
        self.name = name
        _ALLBUFS.append(self)


class Op:
    __slots__ = ("eng", "fn", "deps", "dma", "signal", "cnt", "idx")

    def __init__(self, eng, fn, dma):
        self.eng = eng
        self.fn = fn
        self.dma = dma
        self.deps = []
        self.signal = False
        self.cnt = 0
        self.idx = 0


ENGS = ("pe", "act", "dve", "pool", "sp")
EPOCH = 20000
NDMA = 8


class Sched:
    def __init__(self, nc, strict_same=True):
        self.nc = nc
        self.ops = {e: [] for e in ENGS}
        self.strict_same = strict_same
        self.pending_dma = {e: [] for e in ENGS}
        self.last_real = {e: None for e in ENGS}

    def add(self, eng, fn, reads=(), writes=(), dma=False, force=()):
        op = Op(eng, fn, dma)
        deps = []
        for b in reads:
            deps.extend(b.ws)
        acc = []
        for b in writes:
            a = dma and len(b.ws) > 0 and all(x.dma for x in b.ws) and not b.rs
            acc.append(a)
            if not a:
                deps.extend(b.ws)
                deps.extend(b.rs)
        for b in reads:
            b.rs.append(op)
        for b, a in zip(writes, acc):
            if a:
                b.ws.append(op)
            else:
                b.ws = [op]
                b.rs = []
        best = {}
        dmadeps = []
        seen = set()
        for d in list(deps) + list(force):
            if d is op or d is None:
                continue
            if d.dma:
                if id(d) not in seen:
                    seen.add(id(d))
                    dmadeps.append(d)
            else:
                if d.eng == eng and (eng == "pe" or not self.strict_same) and d not in force:
                    continue
                cur = best.get(d.eng)
                if cur is None or d.idx > cur.idx:
                    best[d.eng] = d
        op.deps = list(best.values()) + dmadeps
        for d in op.deps:
            d.signal = True
        op.idx = len(self.ops[eng])
        self.ops[eng].append(op)
        if dma:
            self.pending_dma[eng].append(op)
        elif fn is not None:
            self.last_real[eng] = op
        return op

    def dma(self, out, in_, reads=(), writes=(), q="sp"):
        return self.add(q, lambda e: e.dma_start(out=out, in_=in_), reads, writes, dma=True)

    def barrier(self):
        firsts = []
        for e in ENGS:
            f = [self.last_real[e]] + self.pending_dma[e]
            self.pending_dma[e] = []
            firsts.append(self.add(e, None, force=[x for x in f if x is not None]))
        for e in ENGS:
            self.add(e, None, force=firsts)
        for b in _ALLBUFS:
            b.ws = []
            b.rs = []

    def emit(self):
        nc = self.nc
        sems = {}
        for e in ENGS:
            n = 0
            k = 0
            for op in self.ops[e]:
                if op.dma:
                    op.cnt = k
                    k += 1
                elif op.signal:
                    op.cnt = n
                    n += 1
            nep = (n + EPOCH - 1) // EPOCH
            sems[e] = [nc.alloc_semaphore(name=f"c_{e}_{i}") for i in range(nep)]
            sems[e + "_dma"] = [nc.alloc_semaphore(name=f"d_{e}_{i}") for i in range(min(NDMA, k))]

        def run_engine(e, h):
            waited = {x: -1 for x in ENGS}
            dma_done = set()
            for op in self.ops[e]:
                for d in op.deps:
                    if d.dma:
                        key = (d.eng, d.cnt)
                        if key in dma_done:
                            continue
                        h.wait_ge(sems[d.eng + "_dma"][d.cnt % NDMA], 16 * (d.cnt // NDMA + 1))
                        dma_done.add(key)
                    else:
                        if waited[d.eng] >= d.cnt:
                            continue
                        h.wait_ge(sems[d.eng][d.cnt // EPOCH], d.cnt % EPOCH + 1)
                        waited[d.eng] = d.cnt
                if op.dma:
                    s = sems[e + "_dma"][op.cnt % NDMA]
                    if op.cnt >= NDMA:
                        key = (e, op.cnt - NDMA)
                        if key not in dma_done:
                            h.wait_ge(s, 16 * (op.cnt // NDMA))
                            dma_done.add(key)
                    op.fn(h).then_inc(s, 16)
                elif op.fn is None:
                    if op.signal:
                        h.sem_inc(sems[e][op.cnt // EPOCH], 1)
                else:
                    ins = op.fn(h)
                    if op.signal:
                        ins.then_inc(sems[e][op.cnt // EPOCH], 1)
            k = sum(1 for op in self.ops[e] if op.dma)
            for j in range(min(NDMA, k)):
                last = ((k - 1 - j) // NDMA) * NDMA + j
                if (e, last) not in dma_done:
                    h.wait_ge(sems[e + "_dma"][j], 16 * (last // NDMA + 1))

        with nc.Block() as block:
            @block.sync
            def _(h):
                run_engine("sp", h)

            @block.tensor
            def _(h):
                run_engine("pe", h)

            @block.scalar
            def _(h):
                run_engine("act", h)

            @block.vector
            def _(h):
                run_engine("dve", h)

            @block.gpsimd
            def _(h):
                run_engine("pool", h)


_UID = [0]


class Ring:
    def __init__(self, nc, stack, name, shape, dtype, n):
        _UID[0] += 1
        name = f"{name}u{_UID[0]}"
        self.t = [stack.enter_context(nc.sbuf_tensor(f"{name}_{i}", shape, dtype)) for i in range(n)]
        self.b = [Buf(f"{name}_{i}") for i in range(n)]
        self.i = 0

    def next(self):
        i = self.i
        self.i = (i + 1) % len(self.t)
        return self.t[i], self.b[i]


def build(NLB, depth=DEPTH, dbg=False):
    assert NLB % 4 == 0
    NB = NLB + 2
    T = NB * 128
    tiles = [(0, 256)] + [(256 + 512 * i, 512) for i in range(NLB // 4)]
    nc = bass.Bass("TRN2", target_bir_lowering=False)
    S = Sched(nc)

    def din(name, shape, dt=F32):
        return nc.dram_tensor(name, list(shape), dt, kind="ExternalInput").ap()

    def dscr(name, shape, dt):
        return nc.dram_tensor(name, list(shape), dt, kind=("ExternalOutput" if dbg else "Internal")).ap()

    xT0 = din("xT0", [D, T])
    cT = din("cT", [128, KC, 2])
    cosd = din("cos", [128, T])
    sind = din("sins", [128, T])
    fing = din("fing", [128, KC])
    L = []
    for l in range(depth):
        L.append(dict(
            wmod=din(f"wmod{l}", [48, 128, KC, 128]), bmod=din(f"bmod{l}", [128, 48]),
            n1g=din(f"n1g{l}", [128, KC]), n2g=din(f"n2g{l}", [128, KC]),
            wfm=din(f"wfm{l}", [NFM, 128, KC, 128]), bfm=din(f"bfm{l}", [128, NFM]),
            wtm=din(f"wtm{l}", [128, KC, TMW]), btm=din(f"btm{l}", [128, TMW]),
            gml=din(f"gml{l}", [128, 512]), sink=din(f"sink{l}", [128, 2, 512]),
            qng=din(f"qng{l}", [128, 2]), kng=din(f"kng{l}", [128, 2]),
            wbr0=din(f"wbr0{l}", [8, 128, 4, 128]), wbr1=din(f"wbr1{l}", [8, 64, 8, 128]),
            wbr2=din(f"wbr2{l}", [8, 64, 8, 128]), wo=din(f"wo{l}", [8, 128, KC, 128]),
            w1=din(f"w1{l}", [NFC, 128, KC, 128]), w3=din(f"w3{l}", [NFC, 128, KC, 128]),
            w2=din(f"w2{l}", [8, 128, NFC, 128]),
        ))
    yT = nc.dram_tensor("yT", [D, NLB * 128], F32, kind="ExternalOutput").ap()

    xs = dscr("xs", [D, T], F32)
    b_wmod = dscr("b_wmod", [48, 128, KC * 128], BF16)
    b_wfm = dscr("b_wfm", [NFM, 128, KC * 128], BF16)
    b_wtm = dscr("b_wtm", [128, KC * TMW], BF16)
    b_wbr0 = dscr("b_wbr0", [8, 128, 4 * 128], BF16)
    b_wbr1 = dscr("b_wbr1", [8, 64, 8 * 128], BF16)
    b_wbr2 = dscr("b_wbr2", [8, 64, 8 * 128], BF16)
    b_wo = dscr("b_wo", [8, 128, KC * 128], BF16)
    b_w1 = dscr("b_w1", [NFC, 128, KC * 128], BF16)
    b_w3 = dscr("b_w3", [NFC, 128, KC * 128], BF16)
    b_w2 = dscr("b_w2", [8, 128, NFC * 128], BF16)
    mqT = dscr("mqT", [512, T], BF16)
    mkT = dscr("mkT", [512, T], BF16)
    wqT = dscr("wqT", [512, T], BF16)
    wkT = dscr("wkT", [128, T], BF16)
    gqT = dscr("gqT", [512, T], BF16)
    gkT = dscr("gkT", [128, T], BF16)
    bgT = dscr("bgT", [3072, T], BF16)
    mk_tm = dscr("mk_tm", [T, 512], BF16)
    mv_tm = dscr("mv_tm", [T, 512], BF16)
    mo_tm = dscr("mo_tm", [T, 512], BF16)
    wgv_tm = dscr("wgv_tm", [T, 256], BF16)
    gat_tm = dscr("gat_tm", [T, 16], F32)
    hb_tm = dscr("hb_tm", [T, 512], F32)
    yaT = dscr("yaT", [512, T], BF16)
    ybT = dscr("ybT", [64, 8, T], BF16)
    ycT = dscr("ycT", [64, 8, T], BF16)
    DB = {k: Buf(k) for k in ["xs", "w", "proj", "hb", "ya", "yb", "yc"]}

    stack0 = contextlib.ExitStack()
    psum2 = [nc.alloc_psum_tensor(f"dbank{i}", [128, 1024], F32) for i in range(4)]
    psum = [psum2[i // 2][:, (i % 2) * 512:(i % 2 + 1) * 512] for i in range(8)]
    psb = [Buf(f"bank{i}") for i in range(8)]

    def sb(stack, name, shape, dt):
        _UID[0] += 1
        return stack.enter_context(nc.sbuf_tensor(f"{name}u{_UID[0]}", shape, dt))

    c_f32 = sb(stack0, "c_f32", [128, 128], F32)
    ones_f = sb(stack0, "ones_f", [128, 128], F32)
    tri_f = sb(stack0, "tri_f", [128, 128], F32)
    tri_b = sb(stack0, "tri_b", [128, 128], F32)
    m_le = sb(stack0, "m_le", [128, 128], BF16)
    m_ge = sb(stack0, "m_ge", [128, 128], BF16)
    onesm = sb(stack0, "onesm", [128, 128], BF16)
    bd64 = sb(stack0, "bd64", [128, 128], BF16)
    ident = sb(stack0, "ident", [128, 128], BF16)
    eps_sb = sb(stack0, "eps_sb", [128, 1], F32)
    CB = Buf("consts")

    def mk_consts():
        S.add("pool", lambda e: e.memset(ones_f[:], 1.0), writes=[CB])
        S.add("pool", lambda e: e.memset(eps_sb[:], EPS), writes=[CB])
        S.add("pool", lambda e: e.memset(onesm[:], 1.0 / 1024.0), writes=[CB])
        S.add("pool", lambda e: e.affine_select(out=tri_f[:], in_=ones_f[:], pattern=[[1, 128]],
                                                compare_op=ALU.is_ge, fill=0.0, base=0, channel_multiplier=-1),
              reads=[CB], writes=[CB])
        S.add("pool", lambda e: e.affine_select(out=tri_b[:], in_=ones_f[:], pattern=[[-1, 128]],
                                                compare_op=ALU.is_ge, fill=0.0, base=0, channel_multiplier=1),
              reads=[CB], writes=[CB])
        S.add("pool", lambda e: e.tensor_copy(out=m_le[:], in_=tri_f[:]), reads=[CB], writes=[CB])
        S.add("pool", lambda e: e.tensor_copy(out=m_ge[:], in_=tri_b[:]), reads=[CB], writes=[CB])
        S.add("pool", lambda e: e.tensor_tensor(out=ident[:], in0=tri_f[:], in1=tri_b[:], op=ALU.mult),
              reads=[CB], writes=[CB])
        S.add("pool", lambda e: e.memset(bd64[:], 0.0), reads=[CB], writes=[CB])
        S.add("pool", lambda e: e.memset(bd64[0:64, 0:64], 1.0 / 64.0), reads=[CB], writes=[CB])
        S.add("pool", lambda e: e.memset(bd64[64:128, 64:128], 1.0 / 64.0), reads=[CB], writes=[CB])

    mk_consts()

    cast_i = [0]

    def cast_pack(stack, src, dst, nchunk, P, F):
        pieces = []
        for c in range(nchunk):
            f0 = 0
            while f0 < F:
                fw = min(2048, F - f0)
                pieces.append((c, f0, fw))
                f0 += fw
        for (c, f0, fw) in pieces:
            a, ab = cast_f.next()
            o, ob = cast_o.next()
            S.dma(a[0:P, 0:fw], srcflat(src, c)[:, f0:f0 + fw], reads=[], writes=[ab], q="sp")
            eng = ("dve", "pool", "act")[cast_i[0] % 3]
            cast_i[0] += 1
            if eng == "act":
                S.add("act", lambda e, a=a, o=o, fw=fw: e.copy(out=o[0:P, 0:fw], in_=a[0:P, 0:fw]), [ab], [ob])
            else:
                S.add(eng, lambda e, a=a, o=o, fw=fw: e.tensor_copy(out=o[0:P, 0:fw], in_=a[0:P, 0:fw]), [ab], [ob])
            S.dma(dst[c][:, f0:f0 + fw], o[0:P, 0:fw], reads=[ob], writes=[DB["w"]], q="pool")

    def srcflat(src, c):
        v = src[c]
        if len(v.shape) == 3:
            return v.rearrange("p a b -> p (a b)")
        return v

    for l in range(depth):
        W = L[l]
        ctx_out = l < depth - 1
        xin = xT0 if l == 0 else xs
        xin_v = xin.rearrange("(k p) t -> p k t", p=128)
        xs_v = xs.rearrange("(k p) t -> p k t", p=128)

        with contextlib.ExitStack() as st:
            cast_f = Ring(nc, st, "cast_f", [128, 2048], F32, 3)
            cast_o = Ring(nc, st, "cast_o", [128, 2048], BF16, 3)
            cast_pack(st, W["wmod"], b_wmod, 48, 128, KC * 128)
            cast_pack(st, W["wfm"], b_wfm, NFM, 128, KC * 128)
            cast_pack(st, W["wtm"].rearrange("p (o a) b -> o p a b", o=1), b_wtm.rearrange("(o p) f -> o p f", o=1), 1, 128, KC * TMW)
            cast_pack(st, W["wbr0"], b_wbr0, 8, 128, 4 * 128)
            cast_pack(st, W["wbr1"], b_wbr1, 8, 64, 8 * 128)
            cast_pack(st, W["wbr2"], b_wbr2, 8, 64, 8 * 128)
            cast_pack(st, W["wo"], b_wo, 8, 128, KC * 128)
            cast_pack(st, W["w1"], b_w1, NFC, 128, KC * 128)
            cast_pack(st, W["w3"], b_w3, NFC, 128, KC * 128)
            cast_pack(st, W["w2"], b_w2, 8, 128, NFC * 128)
            S.barrier()

        lay = contextlib.ExitStack()
        MOD = sb(lay, "MOD", [128, 48, 2], F32)
        G1 = sb(lay, "G1", [128, KC, 2], F32)
        G2 = sb(lay, "G2", [128, KC, 2], F32)
        MB = Buf("MOD")
        with contextlib.ExitStack() as st:
            c_sb = sb(st, "c_sb", [128, KC, 2], F32)
            sc = sb(st, "sc", [128, KC, 2], BF16)
            bm = sb(st, "bm", [128, 48], F32)
            ng = sb(st, "ng", [128, 2, KC], F32)
            wr = Ring(nc, st, "wmodc", [128, KC, 128], BF16, 4)
            tb = Buf("m_tmp")
            S.dma(c_sb[:], cT, writes=[tb])
            S.dma(bm[:], W["bmod"], writes=[tb])
            S.dma(ng[:, 0, :], W["n1g"], writes=[tb])
            S.dma(ng[:, 1, :], W["n2g"], writes=[tb])
            S.add("act", lambda e: e.activation(out=sc[:], in_=c_sb[:], func=AF.Silu), [tb], [tb])
            pm = psum[0]
            for mc in range(48):
                w_, wb = wr.next()
                S.dma(w_[:].rearrange("p a b -> p (a b)"), b_wmod[mc], reads=[DB["w"]], writes=[wb])
                for kc in range(KC):
                    S.add("pe", lambda e, w_=w_, kc=kc, mc=mc: e.matmul(
                        pm[:, 2 * mc:2 * mc + 2], lhsT=w_[:, kc, :], rhs=sc[:, kc, :], start=(kc == 0), stop=(kc == KC - 1)),
                        [wb, tb], [psb[0]])
            S.add("dve", lambda e: e.tensor_tensor(
                out=MOD[:], in0=pm[:, 0:96].rearrange("p (m w) -> p m w", w=2),
                in1=bm[:].unsqueeze(2).to_broadcast([128, 48, 2]), op=ALU.add), [psb[0], tb], [MB])
            S.add("dve", lambda e: e.scalar_tensor_tensor(
                out=G1[:], in0=MOD[:, 8:16, :], scalar=1.0, in1=ng[:, 0, :].unsqueeze(2).to_broadcast([128, KC, 2]),
                op0=ALU.add, op1=ALU.mult), [MB, tb], [MB])
            S.add("dve", lambda e: e.scalar_tensor_tensor(
                out=G2[:], in0=MOD[:, 32:40, :], scalar=1.0, in1=ng[:, 1, :].unsqueeze(2).to_broadcast([128, KC, 2]),
                op0=ALU.add, op1=ALU.mult), [MB, tb], [MB])
            if dbg and l == 0:
                dbg_mod = dscr("dbg_mod", [128, 96], F32)
                dbg_g1 = dscr("dbg_g1", [128, 16], F32)
                S.dma(dbg_mod, MOD[:].rearrange("p a b -> p (a b)"), reads=[MB])
                S.dma(dbg_g1, G1[:].rearrange("p a b -> p (a b)"), reads=[MB])
            S.barrier()

        with contextlib.ExitStack() as st:
            wtm_sb = sb(st, "wtm_sb", [128, KC, TMW], BF16)
            btm_sb = sb(st, "btm_sb", [128, TMW], F32)
            bfm_sb = sb(st, "bfm_sb", [128, NFM], F32)
            qng_sb = sb(st, "qng_sb", [128, 2], F32)
            kng_sb = sb(st, "kng_sb", [128, 2], F32)
            AB = Buf("a_const")
            S.dma(wtm_sb[:].rearrange("p a b -> p (a b)"), b_wtm, reads=[DB["w"]], writes=[AB])
            S.dma(btm_sb[:], W["btm"], writes=[AB])
            S.dma(bfm_sb[:], W["bfm"], writes=[AB])
            S.dma(qng_sb[:], W["qng"], writes=[AB])
            S.dma(kng_sb[:], W["kng"], writes=[AB])
            xr = Ring(nc, st, "a_x", [128, KC, 512], F32, 2)
            sqr = Ring(nc, st, "a_sq", [128, KC, 512], BF16, 1)
            hr = Ring(nc, st, "a_h", [128, KC, 512], BF16, 2)
            rsr = Ring(nc, st, "a_rs", [128, 512], F32, 2)
            tmpr = Ring(nc, st, "a_tmp", [128, 512], F32, 3)
            wcr = Ring(nc, st, "a_wc", [128, KC, 128], BF16, 6)
            csr = Ring(nc, st, "a_cs", [128, 2, 512], F32, 2)
            f32r = Ring(nc, st, "a_f", [128, 512], F32, 8)
            bfr = Ring(nc, st, "a_b", [128, 512], BF16, 6)
            tmo = Ring(nc, st, "a_tmo", [128, 512], BF16, 4)
            gto = Ring(nc, st, "a_gto", [128, 16], F32, 2)
            pi = [0]

            def pbank(lo, n):
                i = lo + pi[0] % n
                pi[0] += 1
                return psum[i], psb[i]

            def proj(c, h_, hb, tt):
                w_, wb = wcr.next()
                S.dma(w_[:].rearrange("p a b -> p (a b)"), b_wfm[c], reads=[DB["w"]], writes=[wb], q="pool")
                p_, pb = pbank(1, 4)
                for kc in range(KC):
                    S.add("pe", lambda e, w_=w_, kc=kc, p_=p_: e.matmul(
                        p_[:, 0:tt], lhsT=w_[:, kc, :], rhs=h_[:, kc, 0:tt], start=(kc == 0), stop=(kc == KC - 1)),
                        [wb, hb], [pb])
                return p_, pb

            def store(dst, row0, t0, tt, o_, ob):
                S.dma(dst[row0:row0 + 128, t0:t0 + tt], o_[:, 0:tt], reads=[ob], writes=[DB["proj"]], q="sp")

            dbg_h = dscr("dbg_h", [D, T], BF16) if (dbg and l == 0) else None

            def a_tile(t0, tt):
                wsel = 1 if t0 == 0 else 0
                x_, xb = xr.next()
                S.dma(x_[:, :, 0:tt], xin_v[:, :, t0:t0 + tt], reads=[DB["xs"]], writes=[xb])
                cs_, csb = csr.next()
                S.dma(cs_[:, 0, 0:tt], cosd[:, t0:t0 + tt], writes=[csb], q="pool")
                S.dma(cs_[:, 1, 0:tt], sind[:, t0:t0 + tt], writes=[csb], q="pool")
                sq_, sqb = sqr.next()
                S.add("act", lambda e, x_=x_, sq_=sq_: e.activation(out=sq_[:, :, 0:tt], in_=x_[:, :, 0:tt], func=AF.Square),
                      [xb], [sqb])
                pm, pmb = psum[0], psb[0]
                for kc in range(KC):
                    S.add("pe", lambda e, kc=kc, sq_=sq_: e.matmul(pm[:, 0:tt], lhsT=onesm[:], rhs=sq_[:, kc, 0:tt],
                                                                   start=(kc == 0), stop=(kc == KC - 1)), [sqb, CB], [pmb])
                rs_, rsb = rsr.next()
                S.add("act", lambda e, rs_=rs_: e.activation(out=rs_[:, 0:tt], in_=pm[:, 0:tt], func=AF.Ln, bias=eps_sb[:, 0:1], scale=1.0),
                      [pmb, CB], [rsb])
                S.add("act", lambda e, rs_=rs_: e.activation(out=rs_[:, 0:tt], in_=rs_[:, 0:tt], func=AF.Exp, scale=-0.5), [rsb], [rsb])
                h_, hb = hr.next()
                for kc in range(KC):
                    tm_, tmb = tmpr.next()
                    S.add("dve" if kc % 2 == 0 else "pool", lambda e, kc=kc, tm_=tm_, x_=x_, rs_=rs_: e.tensor_tensor(
                        out=tm_[:, 0:tt], in0=x_[:, kc, 0:tt], in1=rs_[:, 0:tt], op=ALU.mult), [xb, rsb], [tmb])
                    S.add("act", lambda e, kc=kc, tm_=tm_, h_=h_: e.activation(
                        out=h_[:, kc, 0:tt], in_=tm_[:, 0:tt], func=AF.Identity,
                        scale=G1[:, kc, wsel:wsel + 1], bias=MOD[:, kc, wsel:wsel + 1]), [tmb, MB], [hb])
                if dbg and l == 0:
                    S.dma(dbg_h.rearrange("(k p) t -> p k t", p=128)[:, :, t0:t0 + tt], h_[:, :, 0:tt], reads=[hb])
                for c in range(8):
                    p_, pb = proj(c, h_, hb, tt)
                    o_, ob = bfr.next()
                    S.add("act", lambda e, p_=p_, o_=o_, c=c: e.activation(
                        out=o_[:, 0:tt], in_=p_[:, 0:tt], func=AF.Identity, bias=bfm_sb[:, c:c + 1], scale=1.0), [pb, AB], [ob])
                    store(mqT if c < 4 else mkT, (c % 4) * 128, t0, tt, o_, ob)
                for (ca, cb_, dst, row0, norm) in (
                        [(8 + j, 12 + j, wqT, j * 128, None) for j in range(4)] + [(16, 17, wkT, 0, None)] +
                        [(18 + j, 22 + j, gqT, j * 128, qng_sb) for j in range(4)] + [(26, 27, gkT, 0, kng_sb)]):
                    pa, pab = proj(ca, h_, hb, tt)
                    pb2, pbb = proj(cb_, h_, hb, tt)
                    q1, q1b = f32r.next()
                    q2, q2b = f32r.next()
                    if norm is None:
                        S.add("dve", lambda e, pa=pa, q1=q1, ca=ca, cs_=cs_: e.scalar_tensor_tensor(
                            out=q1[:, 0:tt], in0=pa[:, 0:tt], scalar=bfm_sb[:, ca:ca + 1], in1=cs_[:, 0, 0:tt],
                            op0=ALU.add, op1=ALU.mult), [pab, AB, csb], [q1b])
                        S.add("dve", lambda e, pb2=pb2, q2=q2, cb_=cb_, cs_=cs_: e.scalar_tensor_tensor(
                            out=q2[:, 0:tt], in0=pb2[:, 0:tt], scalar=bfm_sb[:, cb_:cb_ + 1], in1=cs_[:, 1, 0:tt],
                            op0=ALU.add, op1=ALU.mult), [pbb, AB, csb], [q2b])
                        o_, ob = bfr.next()
                        S.add("pool", lambda e, q1=q1, q2=q2, o_=o_: e.tensor_tensor(
                            out=o_[:, 0:tt], in0=q1[:, 0:tt], in1=q2[:, 0:tt], op=ALU.add), [q1b, q2b], [ob])
                    else:
                        qa, qab = f32r.next()
                        qs, qsb = f32r.next()
                        S.add("act", lambda e, pa=pa, qa=qa, ca=ca: e.activation(
                            out=qa[:, 0:tt], in_=pa[:, 0:tt], func=AF.Identity, bias=bfm_sb[:, ca:ca + 1], scale=1.0),
                            [pab, AB], [qab])
                        S.add("act", lambda e, pb2=pb2, qs=qs, cb_=cb_: e.activation(
                            out=qs[:, 0:tt], in_=pb2[:, 0:tt], func=AF.Identity, bias=bfm_sb[:, cb_:cb_ + 1], scale=1.0),
                            [pbb, AB], [qsb])
                        s2, s2b = bfr.next()
                        S.add("pool", lambda e, qa=qa, s2=s2: e.tensor_tensor(
                            out=s2[:, 0:tt], in0=qa[:, 0:tt], in1=qa[:, 0:tt], op=ALU.mult), [qab], [s2b])
                        pn, pnb = pbank(5, 2)
                        S.add("pe", lambda e, pn=pn, s2=s2: e.matmul(pn[:, 0:tt], lhsT=bd64[:], rhs=s2[:, 0:tt],
                                                                    start=True, stop=True), [s2b, CB], [pnb])
                        rn, rnb = f32r.next()
                        S.add("act", lambda e, pn=pn, rn=rn: e.activation(
                            out=rn[:, 0:tt], in_=pn[:, 0:tt], func=AF.Ln, bias=eps_sb[:, 0:1], scale=1.0), [pnb, CB], [rnb])
                        S.add("act", lambda e, rn=rn: e.activation(out=rn[:, 0:tt], in_=rn[:, 0:tt], func=AF.Exp, scale=-0.5),
                              [rnb], [rnb])
                        S.add("dve", lambda e, qa=qa, q1=q1, cs_=cs_, norm=norm: e.scalar_tensor_tensor(
                            out=q1[:, 0:tt], in0=qa[:, 0:tt], scalar=norm[:, 0:1], in1=cs_[:, 0, 0:tt],
                            op0=ALU.mult, op1=ALU.mult), [qab, AB, csb], [q1b])
                        S.add("dve", lambda e, qs=qs, q2=q2, cs_=cs_, norm=norm: e.scalar_tensor_tensor(
                            out=q2[:, 0:tt], in0=qs[:, 0:tt], scalar=norm[:, 1:2], in1=cs_[:, 1, 0:tt],
                            op0=ALU.mult, op1=ALU.mult), [qsb, AB, csb], [q2b])
                        S.add("dve", lambda e, q1=q1, q2=q2: e.tensor_tensor(
                            out=q1[:, 0:tt], in0=q1[:, 0:tt], in1=q2[:, 0:tt], op=ALU.add), [q1b, q2b], [q1b])
                        o_, ob = bfr.next()
                        S.add("pool", lambda e, q1=q1, rn=rn, o_=o_: e.tensor_tensor(
                            out=o_[:, 0:tt], in0=q1[:, 0:tt], in1=rn[:, 0:tt], op=ALU.mult), [q1b, rnb], [ob])
                    store(dst, row0, t0, tt, o_, ob)
                for c in range(28, 52):
                    p_, pb = proj(c, h_, hb, tt)
                    o_, ob = bfr.next()
                    S.add("act", lambda e, p_=p_, o_=o_, c=c: e.activation(
                        out=o_[:, 0:tt], in_=p_[:, 0:tt], func=AF.Sigmoid, bias=bfm_sb[:, c:c + 1], scale=1.0), [pb, AB], [ob])
                    store(bgT, (c - 28) * 128, t0, tt, o_, ob)
                for blk in range(tt // 128):
                    r0 = t0 + blk * 128
                    for gi, (c0, cw, dst, dcol) in enumerate(((0, 512, mk_tm, 0), (512, 512, mv_tm, 0), (1024, 512, mo_tm, 0),
                                                              (1536, 256, wgv_tm, 0), (1792, 16, gat_tm, 0))):
                        p_, pb = pbank(5, 3)
                        for kc in range(KC):
                            S.add("pe", lambda e, p_=p_, kc=kc, c0=c0, cw=cw, blk=blk: e.matmul(
                                p_[:, 0:cw], lhsT=h_[:, kc, blk * 128:(blk + 1) * 128], rhs=wtm_sb[:, kc, c0:c0 + cw],
                                start=(kc == 0), stop=(kc == KC - 1)), [hb, AB], [pb])
                        if gi == 4:
                            o_, ob = gto.next()
                            S.add("dve", lambda e, p_=p_, o_=o_, c0=c0, cw=cw: e.tensor_tensor(
                                out=o_[:, 0:cw], in0=p_[:, 0:cw], in1=btm_sb[:, c0:c0 + cw], op=ALU.add), [pb, AB], [ob])
                        else:
                            o_, ob = tmo.next()
                            S.add("dve", lambda e, p_=p_, o_=o_, c0=c0, cw=cw: e.tensor_tensor(
                                out=o_[:, 0:cw], in0=p_[:, 0:cw], in1=btm_sb[:, c0:c0 + cw], op=ALU.add), [pb, AB], [ob])
                            if gi == 2:
                                S.add("act", lambda e, o_=o_, cw=cw: e.activation(out=o_[:, 0:cw], in_=o_[:, 0:cw], func=AF.Sigmoid),
                                      [ob], [ob])
                        S.dma(dst[r0:r0 + 128, 0:cw], o_[:, 0:cw], reads=[ob], writes=[DB["proj"]], q="sp")

            for (t0_, tt_) in tiles:
                a_tile(t0_, tt_)
            S.barrier()

        with contextlib.ExitStack() as st:
            E_st = sb(st, "E_st", [128, 8, 129], F32)
            C_bf = sb(st, "C_bf", [128, 8, 129], BF16)
            ebp = sb(st, "ebp", [128, 8], F32)
            gml_sb = sb(st, "gml_sb", [128, 512], F32)
            SB_ = [Buf(f"st{g}") for g in range(8)]
            EBP = Buf("ebp")
            GB = Buf("gml")
            S.dma(gml_sb[:], W["gml"], writes=[GB])
            S.add("pool", lambda e: e.memset(E_st[:], 0.0), writes=SB_)
            S.add("pool", lambda e: e.memset(C_bf[:], 0.0), writes=SB_)
            S.add("pool", lambda e: e.memset(ebp[:], 1.0), writes=[EBP])
            qTr = Ring(nc, st, "b_qT", [128, 4, 128], BF16, 3)
            kTr = Ring(nc, st, "b_kT", [128, 4, 128], BF16, 3)
            ktr = Ring(nc, st, "b_kt", [128, 512], BF16, 3)
            vtr = Ring(nc, st, "b_vt", [128, 4, 129], BF16, 3)
            gtr = Ring(nc, st, "b_gt", [128, 16], F32, 3)
            for (v_, vb_) in zip(vtr.t, vtr.b):
                S.add("pool", lambda e, v_=v_: e.memset(v_[:], 1.0), writes=[vb_])
            smr = Ring(nc, st, "b_sm", [128, 128], BF16, 4)
            vur = Ring(nc, st, "b_vu", [128, 129], BF16, 4)
            g1r = Ring(nc, st, "b_g1", [128, 32], F32, 3)
            smallr = Ring(nc, st, "b_sml", [128, 4], F32, 8)
            hfr = Ring(nc, st, "b_hf", [128, 512], F32, 3)
            hbr = Ring(nc, st, "b_hb", [128, 512], F32, 2)
            mor = Ring(nc, st, "b_mo", [128, 512], BF16, 2)
            ybr = Ring(nc, st, "b_y", [128, 512], BF16, 2)
            yTr = Ring(nc, st, "b_yT", [128, 4, 128], BF16, 2)
            mq_v = mqT.rearrange("(h d) t -> d h t", d=128)
            mk_v = mkT.rearrange("(h d) t -> d h t", d=128)
            ya_v = yaT.rearrange("(h d) t -> d h t", d=128)
            scale_ml = 128.0 ** -0.5
            bi = [0]

            def bbank(lo, n):
                i = lo + bi[0] % n
                bi[0] += 1
                return psum[i], psb[i]

            def b_block(dr, c):
                    t0 = c * 128
                    need_out = ctx_out or c >= 2
                    q_, qb = qTr.next()
                    k_, kb = kTr.next()
                    kt_, ktb = ktr.next()
                    vt_, vtb = vtr.next()
                    gt_, gtb = gtr.next()
                    S.dma(q_[:], mq_v[:, :, t0:t0 + 128], reads=[DB["proj"]], writes=[qb])
                    S.dma(k_[:], mk_v[:, :, t0:t0 + 128], reads=[DB["proj"]], writes=[kb])
                    S.dma(kt_[:], mk_tm[t0:t0 + 128, :], reads=[DB["proj"]], writes=[ktb], q="pool")
                    S.dma(vt_[:, :, 0:128], mv_tm[t0:t0 + 128, :].rearrange("t (h e) -> t h e", e=128), reads=[DB["proj"]],
                          writes=[vtb], q="pool")
                    S.dma(gt_[:], gat_tm[t0:t0 + 128, :], reads=[DB["proj"]], writes=[gtb])
                    g1, g1b = g1r.next()
                    S.add("act", lambda e, gt_=gt_, g1=g1: e.activation(
                        out=g1[:, 16:20], in_=gt_[:, 8 + dr * 4:12 + dr * 4], func=AF.Exp, scale=-1.0), [gtb], [g1b])
                    S.add("act", lambda e, g1=g1: e.activation(out=g1[:, 0:4], in_=g1[:, 16:20], func=AF.Ln, bias=1.0, scale=1.0),
                          [g1b], [g1b])
                    S.add("dve", lambda e, g1=g1: e.tensor_scalar(out=g1[:, 0:4], in0=g1[:, 0:4], scalar1=-1.0, scalar2=None,
                                                                  op0=ALU.mult), [g1b], [g1b])
                    pg, pgb = psum[0], psb[0]
                    tri = tri_b if dr == 1 else tri_f
                    S.add("pe", lambda e, g1=g1, tri=tri: e.matmul(pg[:, 0:4], lhsT=tri[:], rhs=g1[:, 0:4], start=True, stop=True),
                          [g1b, CB], [pgb])
                    S.add("pe", lambda e, g1=g1: e.matmul(pg[:, 4:8], lhsT=ones_f[:], rhs=g1[:, 0:4], start=True, stop=True),
                          [g1b, CB], [pgb])
                    S.add("dve", lambda e, g1=g1, gt_=gt_: e.tensor_tensor(
                        out=g1[:, 16:20], in0=gt_[:, dr * 4:dr * 4 + 4], in1=pg[:, 0:4], op=ALU.subtract), [gtb, pgb, g1b], [g1b])
                    S.add("act", lambda e, g1=g1: e.activation(out=g1[:, 4:8], in_=g1[:, 16:20], func=AF.Exp), [g1b], [g1b])
                    S.add("act", lambda e, g1=g1: e.activation(out=g1[:, 8:16], in_=pg[:, 0:8], func=AF.Exp), [pgb, g1b], [g1b])
                    S.add("dve", lambda e, g1=g1: e.tensor_scalar(out=g1[:, 8:12], in0=g1[:, 8:12], scalar1=scale_ml, scalar2=None,
                                                                  op0=ALU.mult), [g1b], [g1b])
                    if need_out:
                        hf, hfb = hfr.next()
                    for h in range(4):
                        g = dr * 4 + h
                        ps_, psb_ = bbank(1, 2)
                        S.add("pe", lambda e, ps_=ps_, k_=k_, q_=q_, h=h: e.matmul(
                            ps_[:, 0:128], lhsT=k_[:, h, :], rhs=q_[:, h, :], start=True, stop=True), [kb, qb], [psb_])
                        sm_, smb = smr.next()
                        msk = m_ge if dr == 1 else m_le
                        S.add("dve", lambda e, ps_=ps_, sm_=sm_, msk=msk: e.tensor_tensor(
                            out=sm_[:], in0=ps_[:, 0:128], in1=msk[:], op=ALU.mult), [psb_, CB], [smb])
                        vu_, vub = vur.next()
                        S.add("act", lambda e, vu_=vu_, vt_=vt_, g1=g1, h=h: e.activation(
                            out=vu_[:], in_=vt_[:, h, :], func=AF.Identity, scale=g1[:, 4 + h:5 + h]), [vtb, g1b], [vub])
                        if need_out:
                            ph, phb = bbank(3, 2)
                            S.add("pe", lambda e, ph=ph, sm_=sm_, vu_=vu_: e.matmul(
                                ph[:, 0:129], lhsT=sm_[:], rhs=vu_[:], start=True, stop=False), [smb, vub], [phb])
                            S.add("pe", lambda e, ph=ph, q_=q_, h=h, g=g: e.matmul(
                                ph[:, 0:129], lhsT=q_[:, h, :], rhs=C_bf[:, g, :], start=False, stop=True), [qb, SB_[g]], [phb])
                            sm4, sm4b = smallr.next()
                            S.add("act", lambda e, ph=ph, sm4=sm4, g1=g1, h=h: e.activation(
                                out=sm4[:, 0:1], in_=ph[:, 128:129], func=AF.Abs, scale=g1[:, 8 + h:9 + h]), [phb, g1b], [sm4b])
                            S.add("dve", lambda e, sm4=sm4: e.tensor_scalar(
                                out=sm4[:, 0:1], in0=sm4[:, 0:1], scalar1=1.0, scalar2=None, op0=ALU.max), [sm4b], [sm4b])
                            S.add("dve", lambda e, sm4=sm4: e.reciprocal(out=sm4[:, 2:3], in_=sm4[:, 0:1]), [sm4b], [sm4b])
                            S.add("dve", lambda e, sm4=sm4, g1=g1, h=h: e.tensor_tensor(
                                out=sm4[:, 1:2], in0=g1[:, 8 + h:9 + h], in1=sm4[:, 2:3], op=ALU.mult), [sm4b, g1b], [sm4b])
                            S.add("act", lambda e, ph=ph, hf=hf, sm4=sm4, h=h: e.activation(
                                out=hf[:, h * 128:(h + 1) * 128], in_=ph[:, 0:128], func=AF.Identity, scale=sm4[:, 1:2]),
                                [phb, sm4b], [hfb])
                        pc, pcb = bbank(5, 2)
                        S.add("pe", lambda e, pc=pc, kt_=kt_, vu_=vu_, h=h: e.matmul(
                            pc[:, 0:129], lhsT=kt_[:, h * 128:(h + 1) * 128], rhs=vu_[:], start=True, stop=True), [ktb, vub], [pcb])
                        S.add("dve", lambda e, pc=pc, g=g: e.scalar_tensor_tensor(
                            out=E_st[:, g, :], in0=E_st[:, g, :], scalar=ebp[:, g:g + 1], in1=pc[:, 0:129],
                            op0=ALU.mult, op1=ALU.add), [pcb, EBP, SB_[g]], [SB_[g]])
                        S.add("pool", lambda e, g=g, g1=g1, h=h: e.tensor_scalar(
                            out=C_bf[:, g, :], in0=E_st[:, g, :], scalar1=g1[:, 12 + h:13 + h], scalar2=None, op0=ALU.mult),
                            [SB_[g], g1b], [SB_[g]])
                    S.add("dve", lambda e, g1=g1: e.tensor_copy(out=ebp[:, dr * 4:dr * 4 + 4], in_=g1[:, 12:16]),
                          [g1b, EBP] + SB_[dr * 4:dr * 4 + 4], [EBP])
                    if not need_out:
                        return
                    if dr == 1:
                        S.dma(hb_tm[t0:t0 + 128, :], hf[:], reads=[hfb], writes=[DB["hb"]], q="sp")
                        return
                    hb_, hbb = hbr.next()
                    mo_, mob = mor.next()
                    S.dma(hb_[:], hb_tm[t0:t0 + 128, :], reads=[DB["hb"]], writes=[hbb], q="pool")
                    S.dma(mo_[:], mo_tm[t0:t0 + 128, :], reads=[DB["proj"]], writes=[mob], q="pool")
                    S.add("pool", lambda e, hf=hf, hb_=hb_: e.tensor_tensor(out=hf[:], in0=hf[:], in1=hb_[:], op=ALU.add),
                          [hfb, hbb], [hfb])
                    sm4, sm4b = smallr.next()
                    S.add("dve", lambda e, hb_=hb_, hf=hf: e.tensor_tensor(out=hb_[:], in0=hf[:], in1=hf[:], op=ALU.mult),
                          [hfb, hbb], [hbb])
                    S.add("dve", lambda e, hb_=hb_, sm4=sm4: e.tensor_reduce(
                        out=sm4[:, 0:4], in_=hb_[:].rearrange("p (h e) -> p h e", e=128), axis=AX.X, op=ALU.add), [hbb], [sm4b])
                    S.add("act", lambda e, sm4=sm4: e.activation(out=sm4[:, 0:4], in_=sm4[:, 0:4], func=AF.Ln, bias=eps_sb[:, 0:1],
                                                                 scale=1.0 / 128.0), [sm4b, CB], [sm4b])
                    S.add("act", lambda e, sm4=sm4: e.activation(out=sm4[:, 0:4], in_=sm4[:, 0:4], func=AF.Exp, scale=-0.5),
                          [sm4b], [sm4b])
                    for h in range(4):
                        S.add("dve", lambda e, hf=hf, sm4=sm4, h=h: e.scalar_tensor_tensor(
                            out=hf[:, h * 128:(h + 1) * 128], in0=hf[:, h * 128:(h + 1) * 128], scalar=sm4[:, h:h + 1],
                            in1=gml_sb[:, h * 128:(h + 1) * 128], op0=ALU.mult, op1=ALU.mult), [hfb, sm4b, GB], [hfb])
                    y_, yb_ = ybr.next()
                    S.add("pool", lambda e, hf=hf, mo_=mo_, y_=y_: e.tensor_tensor(out=y_[:], in0=hf[:], in1=mo_[:], op=ALU.mult),
                          [hfb, mob], [yb_])
                    pt, ptb = psum[7], psb[7]
                    ptv = pt[:].bitcast(BF16)
                    for h in range(4):
                        S.add("pe", lambda e, y_=y_, h=h, ptv=ptv: e.transpose(
                            out=ptv[:, h * 128:(h + 1) * 128], in_=y_[:, h * 128:(h + 1) * 128], identity=ident[:]), [yb_, CB], [ptb])
                    yT_, yTb = yTr.next()
                    S.add("act", lambda e, yT_=yT_, ptv=ptv: e.copy(out=yT_[:].rearrange("p a b -> p (a b)"), in_=ptv[:, 0:512]),
                          [ptb], [yTb])
                    S.dma(ya_v[:, :, t0:t0 + 128], yT_[:], reads=[yTb], writes=[DB["ya"]], q="sp")

            for dr_ in (1, 0):
                order = [1, 0] + list(range(NB - 1, 1, -1)) if dr_ == 1 else list(range(NB))
                for c_ in order:
                    b_block(dr_, c_)
            S.barrier()

        with contextlib.ExitStack() as st:
            KT = sb(st, "KT", [128, T], BF16)
            VV = sb(st, "VV", [128, NB, 2, 65], BF16)
            sink_sb = sb(st, "sink_sb", [128, 2, 512], F32)
            KB_ = Buf("KT")
            S.dma(sink_sb[:], W["sink"], writes=[KB_])
            S.add("act", lambda e: e.activation(out=sink_sb[:], in_=sink_sb[:], func=AF.Exp), [KB_], [KB_])
            qr = Ring(nc, st, "c_q", [128, 4, 512], BF16, 2)
            ptr = Ring(nc, st, "c_pt", [128, 1024], BF16, 3)
            ostg = Ring(nc, st, "c_o", [64, 8, 512], BF16, 2)
            rdr = Ring(nc, st, "c_rd", [128, 512], F32, 2)
            bcr = Ring(nc, st, "c_bc", [64, 512], F32, 2)
            ci = [0]

            def cbank(lo, n):
                i = lo + ci[0] % n
                ci[0] += 1
                return psum[i], psb[i]

            for kind in ("win", "glb"):
                kT_src = wkT if kind == "win" else gkT
                vcol = 0 if kind == "win" else 128
                q_src = (wqT if kind == "win" else gqT).rearrange("(j p) t -> p j t", p=128)
                y_dst = ybT if kind == "win" else ycT
                ydb = DB["yb"] if kind == "win" else DB["yc"]
                for (t0, tt) in tiles:
                    S.dma(KT[:, t0:t0 + tt], kT_src[:, t0:t0 + tt], reads=[DB["proj"]], writes=[KB_])
                S.add("pool", lambda e: e.memset(VV[:], 1.0), writes=[KB_])
                for b0 in range(0, NB, 26):
                    b1 = min(NB, b0 + 26)
                    for g in range(2):
                        S.dma(VV[:, b0:b1, g, 0:64],
                              wgv_tm[b0 * 128:b1 * 128, vcol + g * 64:vcol + g * 64 + 64].rearrange("(b p) d -> p b d", p=128),
                              reads=[DB["proj"]], writes=[KB_], q="pool")
                groups = []
                for (t0, tt) in tiles:
                    if t0 == 0 and not ctx_out:
                        continue
                    nblk = tt // 128
                    for blk in range(nblk):
                        c = t0 // 128 + blk
                        if c < 2:
                            kbs = [0, 1]
                        elif kind == "glb":
                            kbs = list(range(NB))
                        else:
                            kbs = ([c - 1] if c - 1 >= 2 else []) + [c] + ([c + 1] if c + 1 < NB else []) + [0, 1]
                        for g in range(2):
                            groups.append(dict(t0=t0, tt=tt, blk=blk, g=g, c=c, kbs=kbs,
                                               tile_last=(blk == nblk - 1 and g == 1)))
                units = []
                for gi, G in enumerate(groups):
                    kbs = G["kbs"]
                    for i in range(0, len(kbs), 2):
                        units.append(dict(gi=gi, kbs=kbs[i:i + 2], first=(i == 0), last=(i + 2 >= len(kbs)), idx=len(units)))
                tile_state = {}

                def get_tile(t0, tt, kind=kind, q_src=q_src, tile_state=tile_state):
                    if t0 not in tile_state:
                        q_, qb = qr.next()
                        S.dma(q_[:, :, 0:tt], q_src[:, :, t0:t0 + tt], reads=[DB["proj"]], writes=[qb])
                        os_, osb = ostg.next()
                        tile_state[t0] = (q_, qb, os_, osb)
                    return tile_state[t0]

                def emit_qk(u, kind=kind, groups=groups):
                    G = groups[u["gi"]]
                    g, blk, c = G["g"], G["blk"], G["c"]
                    q_, qb, os_, osb = get_tile(G["t0"], G["tt"])
                    slot = u["idx"] % 2
                    n = len(u["kbs"])
                    for j, kb_ in enumerate(u["kbs"]):
                        ps_ = psum[2 * slot + j]
                        S.add("pe", lambda e, ps_=ps_, kb_=kb_, g=g, q_=q_, blk=blk: e.matmul(
                            ps_.rearrange("p (r q) -> p r q", q=128), lhsT=KT[g * 64:(g + 1) * 64, kb_ * 128:(kb_ + 1) * 128],
                            rhs=q_[g * 64:(g + 1) * 64, :, blk * 128:(blk + 1) * 128], start=True, stop=True),
                            [KB_, qb], [psb[2 * slot + j]])
                    pt_, ptb_ = ptr.next()
                    S.add("act", lambda e, slot=slot, n=n, pt_=pt_: e.activation(
                        out=pt_[:, 0:512 * n], in_=psum2[slot][:, 0:512 * n], func=AF.Exp, scale=0.125),
                        [psb[2 * slot + j] for j in range(n)], [ptb_])
                    for j, kb_ in enumerate(u["kbs"]):
                        msk = None
                        if kind == "win" and c >= 3 and kb_ == c - 1:
                            msk = m_ge
                        if kind == "win" and c >= 2 and kb_ == c + 1:
                            msk = m_le
                        if msk is not None:
                            S.add("dve", lambda e, pt_=pt_, j=j, msk=msk: e.tensor_tensor(
                                out=pt_[:, j * 512:(j + 1) * 512].rearrange("p (r q) -> p r q", q=128),
                                in0=pt_[:, j * 512:(j + 1) * 512].rearrange("p (r q) -> p r q", q=128),
                                in1=msk[:].unsqueeze(1).to_broadcast([128, 4, 128]), op=ALU.mult), [ptb_, CB], [ptb_])
                    u["pt"] = (pt_, ptb_)

                grp_po = {}

                def emit_pv(u, kind=kind, groups=groups, grp_po=grp_po, y_dst=y_dst, ydb=ydb):
                    G = groups[u["gi"]]
                    g, blk = G["g"], G["blk"]
                    q_, qb, os_, osb = get_tile(G["t0"], G["tt"])
                    if u["first"]:
                        grp_po[u["gi"]] = cbank(4, 2)
                    po, pob = grp_po[u["gi"]]
                    pt_, ptb_ = u["pt"]
                    n = len(u["kbs"])
                    for j, kb_ in enumerate(u["kbs"]):
                        S.add("pe", lambda e, po=po, kb_=kb_, g=g, pt_=pt_, j=j, st_=(u["first"] and j == 0),
                              sp_=(u["last"] and j == n - 1): e.matmul(
                            po[0:65, :], lhsT=VV[:, kb_, g, :], rhs=pt_[:, j * 512:(j + 1) * 512], start=st_, stop=sp_),
                            [KB_, ptb_], [pob])
                    if not u["last"]:
                        return
                    rd, rdb = rdr.next()
                    if kind == "win":
                        S.add("dve", lambda e, po=po, rd=rd, g=g: e.tensor_tensor(
                            out=rd[64:65, :], in0=po[64:65, :], in1=sink_sb[64:65, g, :], op=ALU.add), [pob, KB_], [rdb])
                        S.add("dve", lambda e, rd=rd: e.reciprocal(out=rd[64:65, :], in_=rd[64:65, :]), [rdb], [rdb])
                    else:
                        S.add("dve", lambda e, po=po, rd=rd: e.reciprocal(out=rd[64:65, :], in_=po[64:65, :]), [pob], [rdb])
                    pbc, pbcb = cbank(6, 2)
                    S.add("pe", lambda e, pbc=pbc, rd=rd: e.matmul(
                        pbc[0:64, :], lhsT=ones_f[64:65, 0:64], rhs=rd[64:65, :], start=True, stop=True), [rdb, CB], [pbcb])
                    bc, bcb = bcr.next()
                    S.add("dve", lambda e, pbc=pbc, bc=bc: e.tensor_copy(out=bc[:], in_=pbc[0:64, :]), [pbcb], [bcb])
                    S.add("dve", lambda e, po=po, bc=bc, os_=os_, g=g, blk=blk: e.tensor_tensor(
                        out=os_[:, g * 4:(g + 1) * 4, blk * 128:(blk + 1) * 128],
                        in0=po[0:64, :].rearrange("p (r q) -> p r q", q=128),
                        in1=bc[:].rearrange("p (r q) -> p r q", q=128), op=ALU.mult), [pob, bcb], [osb])
                    if G["tile_last"]:
                        t0, tt = G["t0"], G["tt"]
                        S.dma(y_dst[:, :, t0:t0 + tt], os_[:, :, 0:tt], reads=[osb], writes=[ydb], q="pool")

                LOOK = 2
                for i in range(min(LOOK, len(units))):
                    emit_qk(units[i])
                for i, u in enumerate(units):
                    emit_pv(u)
                    if i + LOOK < len(units):
                        emit_qk(units[i + LOOK])
            S.barrier()

        with contextlib.ExitStack() as st:
            xr = Ring(nc, st, "e_x", [128, KC, 512], F32, 1)
            yar = Ring(nc, st, "e_ya", [128, 4, 512], BF16, 1)
            ybr2 = Ring(nc, st, "e_yb", [64, 8, 512], BF16, 1)
            ycr = Ring(nc, st, "e_yc", [64, 8, 512], BF16, 1)
            bgr = Ring(nc, st, "e_bg", [128, 24, 512], BF16, 1)
            mgr = Ring(nc, st, "e_mg", [128, KC, 512], BF16, 1)
            h2r = Ring(nc, st, "e_h2", [128, KC, 512], BF16, 1)
            sqr = Ring(nc, st, "e_sq", [128, KC, 512], BF16, 1)
            ur = Ring(nc, st, "e_u", [128, NFC, 512], BF16, 1)
            w0r = Ring(nc, st, "e_w0", [128, 4, 128], BF16, 3)
            w12r = Ring(nc, st, "e_w12", [64, 8, 128], BF16, 4)
            wor = Ring(nc, st, "e_wo", [128, KC, 128], BF16, 6)
            w2r = Ring(nc, st, "e_w2", [128, NFC, 128], BF16, 2)
            f32r = Ring(nc, st, "e_f", [128, 512], F32, 6)
            rsr = Ring(nc, st, "e_rs", [128, 512], F32, 2)
            fg_sb = sb(st, "fg_sb", [128, KC], F32)
            FGB = Buf("fing")
            S.dma(fg_sb[:], fing, writes=[FGB])
            ei = [0]

            def ebank(lo, n):
                i = lo + ei[0] % n
                ei[0] += 1
                return psum[i], psb[i]

            def rms(x_, xb, tt):
                sq_, sqb = sqr.next()
                S.add("act", lambda e: e.activation(out=sq_[:, :, 0:tt], in_=x_[:, :, 0:tt], func=AF.Square), [xb], [sqb])
                pm, pmb = psum[0], psb[0]
                for kc in range(KC):
                    S.add("pe", lambda e, kc=kc: e.matmul(pm[:, 0:tt], lhsT=onesm[:], rhs=sq_[:, kc, 0:tt],
                                                          start=(kc == 0), stop=(kc == KC - 1)), [sqb, CB], [pmb])
                rs_, rsb = rsr.next()
                S.add("act", lambda e: e.activation(out=rs_[:, 0:tt], in_=pm[:, 0:tt], func=AF.Ln, bias=eps_sb[:, 0:1], scale=1.0),
                      [pmb, CB], [rsb])
                S.add("act", lambda e: e.activation(out=rs_[:, 0:tt], in_=rs_[:, 0:tt], func=AF.Exp, scale=-0.5), [rsb], [rsb])
                return rs_, rsb

            dbg_mg = dscr(f"dbg_mg{l}", [D, T], BF16) if dbg else None
            dbg_x1 = dscr(f"dbg_x1{l}", [D, T], F32) if dbg else None

            def e_tile(t0, tt):
                wsel = 1 if t0 == 0 else 0
                x_, xb = xr.next()
                S.dma(x_[:, :, 0:tt], xin_v[:, :, t0:t0 + tt], reads=[DB["xs"]], writes=[xb])
                ya_, yab = yar.next()
                yb_, ybb = ybr2.next()
                yc_, ycb = ycr.next()
                bg_, bgb = bgr.next()
                S.dma(ya_[:, :, 0:tt], yaT.rearrange("(h d) t -> d h t", d=128)[:, :, t0:t0 + tt], reads=[DB["ya"]], writes=[yab])
                S.dma(yb_[:, :, 0:tt], ybT[:, :, t0:t0 + tt], reads=[DB["yb"]], writes=[ybb], q="pool")
                S.dma(yc_[:, :, 0:tt], ycT[:, :, t0:t0 + tt], reads=[DB["yc"]], writes=[ycb], q="pool")
                S.dma(bg_[:, :, 0:tt], bgT.rearrange("(c p) t -> p c t", p=128)[:, :, t0:t0 + tt], reads=[DB["proj"]], writes=[bgb])
                mg_, mgb = mgr.next()
                for oc in range(8):
                    wa, wab = w0r.next()
                    wb1, wb1b = w12r.next()
                    wb2, wb2b = w12r.next()
                    S.dma(wa[:].rearrange("p a b -> p (a b)"), b_wbr0[oc], reads=[DB["w"]], writes=[wab], q="pool")
                    S.dma(wb1[:].rearrange("p a b -> p (a b)"), b_wbr1[oc], reads=[DB["w"]], writes=[wb1b], q="pool")
                    S.dma(wb2[:].rearrange("p a b -> p (a b)"), b_wbr2[oc], reads=[DB["w"]], writes=[wb2b], q="pool")
                    p0, p0b = ebank(1, 3)
                    for k in range(4):
                        S.add("pe", lambda e, k=k, p0=p0, wa=wa: e.matmul(p0[:, 0:tt], lhsT=wa[:, k, :], rhs=ya_[:, k, 0:tt],
                                                                          start=(k == 0), stop=(k == 3)), [wab, yab], [p0b])
                    p1, p1b = ebank(1, 3)
                    for k in range(8):
                        S.add("pe", lambda e, k=k, p1=p1, wb1=wb1: e.matmul(p1[:, 0:tt], lhsT=wb1[:, k, :], rhs=yb_[:, k, 0:tt],
                                                                            start=(k == 0), stop=(k == 7)), [wb1b, ybb], [p1b])
                    p2, p2b = ebank(1, 3)
                    for k in range(8):
                        S.add("pe", lambda e, k=k, p2=p2, wb2=wb2: e.matmul(p2[:, 0:tt], lhsT=wb2[:, k, :], rhs=yc_[:, k, 0:tt],
                                                                            start=(k == 0), stop=(k == 7)), [wb2b, ycb], [p2b])
                    a0, a0b = f32r.next()
                    a1, a1b = f32r.next()
                    a2, a2b = f32r.next()
                    S.add("dve", lambda e, p0=p0, a0=a0, oc=oc: e.tensor_tensor(
                        out=a0[:, 0:tt], in0=p0[:, 0:tt], in1=bg_[:, oc, 0:tt], op=ALU.mult), [p0b, bgb], [a0b])
                    S.add("dve", lambda e, p1=p1, a1=a1, oc=oc: e.tensor_tensor(
                        out=a1[:, 0:tt], in0=p1[:, 0:tt], in1=bg_[:, 8 + oc, 0:tt], op=ALU.mult), [p1b, bgb], [a1b])
                    S.add("dve", lambda e, p2=p2, a2=a2, oc=oc: e.tensor_tensor(
                        out=a2[:, 0:tt], in0=p2[:, 0:tt], in1=bg_[:, 16 + oc, 0:tt], op=ALU.mult), [p2b, bgb], [a2b])
                    S.add("pool", lambda e, a0=a0, a1=a1: e.tensor_tensor(out=a0[:, 0:tt], in0=a0[:, 0:tt], in1=a1[:, 0:tt], op=ALU.add),
                          [a0b, a1b], [a0b])
                    S.add("pool", lambda e, a0=a0, a2=a2, oc=oc: e.tensor_tensor(
                        out=mg_[:, oc, 0:tt], in0=a0[:, 0:tt], in1=a2[:, 0:tt], op=ALU.add), [a0b, a2b], [mgb])
                if dbg:
                    S.dma(dbg_mg.rearrange("(k p) t -> p k t", p=128)[:, :, t0:t0 + tt], mg_[:, :, 0:tt], reads=[mgb])
                for oc in range(8):
                    w_, wb_ = wor.next()
                    S.dma(w_[:].rearrange("p a b -> p (a b)"), b_wo[oc], reads=[DB["w"]], writes=[wb_], q="pool")
                    po, pob = ebank(4, 2)
                    for k in range(KC):
                        S.add("pe", lambda e, k=k, po=po, w_=w_: e.matmul(po[:, 0:tt], lhsT=w_[:, k, :], rhs=mg_[:, k, 0:tt],
                                                                          start=(k == 0), stop=(k == KC - 1)), [wb_, mgb], [pob])
                    S.add("dve", lambda e, po=po, oc=oc: e.scalar_tensor_tensor(
                        out=x_[:, oc, 0:tt], in0=po[:, 0:tt], scalar=MOD[:, 16 + oc, wsel:wsel + 1], in1=x_[:, oc, 0:tt],
                        op0=ALU.mult, op1=ALU.add), [pob, MB, xb], [xb])
                if dbg:
                    S.dma(dbg_x1.rearrange("(k p) t -> p k t", p=128)[:, :, t0:t0 + tt], x_[:, :, 0:tt], reads=[xb], writes=[xb])
                rs_, rsb = rms(x_, xb, tt)
                h2, h2b = h2r.next()
                for kc in range(KC):
                    tm_, tmb = f32r.next()
                    S.add("dve" if kc % 2 == 0 else "pool", lambda e, kc=kc, tm_=tm_: e.tensor_tensor(
                        out=tm_[:, 0:tt], in0=x_[:, kc, 0:tt], in1=rs_[:, 0:tt], op=ALU.mult), [xb, rsb], [tmb])
                    S.add("act", lambda e, kc=kc, tm_=tm_: e.activation(
                        out=h2[:, kc, 0:tt], in_=tm_[:, 0:tt], func=AF.Identity,
                        scale=G2[:, kc, wsel:wsel + 1], bias=MOD[:, 24 + kc, wsel:wsel + 1]), [tmb, MB], [h2b])
                u_, ub = ur.next()
                for fc in range(NFC):
                    w1_, w1b = wor.next()
                    w3_, w3b = wor.next()
                    S.dma(w1_[:].rearrange("p a b -> p (a b)"), b_w1[fc], reads=[DB["w"]], writes=[w1b], q="pool")
                    S.dma(w3_[:].rearrange("p a b -> p (a b)"), b_w3[fc], reads=[DB["w"]], writes=[w3b], q="sp")
                    pa, pab = ebank(6, 2)
                    for k in range(KC):
                        S.add("pe", lambda e, k=k, pa=pa, w1_=w1_: e.matmul(pa[:, 0:tt], lhsT=w1_[:, k, :], rhs=h2[:, k, 0:tt],
                                                                            start=(k == 0), stop=(k == KC - 1)), [w1b, h2b], [pab])
                    pb3, pb3b = ebank(6, 2)
                    for k in range(KC):
                        S.add("pe", lambda e, k=k, pb3=pb3, w3_=w3_: e.matmul(pb3[:, 0:tt], lhsT=w3_[:, k, :], rhs=h2[:, k, 0:tt],
                                                                              start=(k == 0), stop=(k == KC - 1)), [w3b, h2b], [pb3b])
                    sl, slb = f32r.next()
                    S.add("act", lambda e, pa=pa, sl=sl: e.activation(out=sl[:, 0:tt], in_=pa[:, 0:tt], func=AF.Silu), [pab], [slb])
                    S.add("dve", lambda e, pb3=pb3, sl=sl, fc=fc: e.tensor_tensor(
                        out=u_[:, fc, 0:tt], in0=pb3[:, 0:tt], in1=sl[:, 0:tt], op=ALU.mult), [pb3b, slb], [ub])
                for oc in range(8):
                    w_, wb_ = w2r.next()
                    S.dma(w_[:].rearrange("p a b -> p (a b)"), b_w2[oc], reads=[DB["w"]], writes=[wb_], q="pool")
                    po, pob = ebank(4, 2)
                    for k in range(NFC):
                        S.add("pe", lambda e, k=k, po=po, w_=w_: e.matmul(po[:, 0:tt], lhsT=w_[:, k, :], rhs=u_[:, k, 0:tt],
                                                                          start=(k == 0), stop=(k == NFC - 1)), [wb_, ub], [pob])
                    S.add("dve", lambda e, po=po, oc=oc: e.scalar_tensor_tensor(
                        out=x_[:, oc, 0:tt], in0=po[:, 0:tt], scalar=MOD[:, 40 + oc, wsel:wsel + 1], in1=x_[:, oc, 0:tt],
                        op0=ALU.mult, op1=ALU.add), [pob, MB, xb], [xb])
                if l < depth - 1:
                    S.dma(xs_v[:, :, t0:t0 + tt], x_[:, :, 0:tt], reads=[xb], writes=[DB["xs"]], q="sp")
                else:
                    rs2_, rsb2 = rms(x_, xb, tt)
                    for kc in range(KC):
                        S.add("dve", lambda e, kc=kc: e.scalar_tensor_tensor(
                            out=x_[:, kc, 0:tt], in0=x_[:, kc, 0:tt], scalar=fg_sb[:, kc:kc + 1], in1=rs2_[:, 0:tt],
                            op0=ALU.mult, op1=ALU.mult), [xb, rsb2, FGB], [xb])
                    S.dma(yT.rearrange("(k p) t -> p k t", p=128)[:, :, t0 - 256:t0 - 256 + tt], x_[:, :, 0:tt], reads=[xb], q="sp")

            for (t0_, tt_) in tiles:
                if t0_ == 0 and not ctx_out:
                    continue
                e_tile(t0_, tt_)
            S.barrier()
        lay.close()

    S.emit()
    return nc


def _swap_d(d):
    r = d % 32
    return d + 16 if r < 16 else d - 16


def _fm_cols():
    OFF = dict(mq=0, mk=512, mv=1024, mo=1536, ig=2048, fg=2056, wq=2064, wk=2576, wv=2704, gq=2832, gk=3344, gv=3472, bg=3600)
    chunks = []
    for h in range(4):
        chunks.append([OFF["mq"] + h * 128 + d for d in range(128)])
    for h in range(4):
        chunks.append([OFF["mk"] + h * 128 + d for d in range(128)])
    for pre in ("w", "g"):
        q0, k0 = OFF[pre + "q"], OFF[pre + "k"]
        for sw in (False, True):
            for j in range(4):
                cols = []
                for p in range(128):
                    head = j if p < 64 else 4 + j
                    d = p % 64
                    dd = _swap_d(d) if sw else d
                    cols.append(q0 + head * 64 + dd)
                chunks.append(cols)
        for sw in (False, True):
            cols = []
            for p in range(128):
                head = p // 64
                d = p % 64
                dd = _swap_d(d) if sw else d
                cols.append(k0 + head * 64 + dd)
            chunks.append(cols)
    for c in range(24):
        chunks.append([OFF["bg"] + c * 128 + p for p in range(128)])
    assert len(chunks) == NFM
    tm = (list(range(512, 1024)) + list(range(1024, 1536)) + list(range(1536, 2048)) + list(range(2704, 2832)) +
          list(range(3472, 3600)) + list(range(2048, 2064)))
    assert len(tm) == TMW
    return np.array(chunks), np.array(tm)


def _pack(w, kp):
    K, N = w.shape
    return np.ascontiguousarray(w.reshape(K // kp, kp, N // 128, 128).transpose(2, 1, 0, 3))


def _rope_tables(nlat):
    T = nlat + CTX
    cos = np.ones((128, T), np.float32)
    sin = np.zeros((128, T), np.float32)
    t = np.arange(nlat)
    row = (t // 64).astype(np.float32)
    col = (t % 64).astype(np.float32)
    half = 32
    inv = (10000.0 ** (-np.arange(0, half, 2, dtype=np.float32) / half)).astype(np.float32)
    for p in range(128):
        d = p % 64
        pos = row if d < 32 else col
        ang = (pos * inv[d % 16]).astype(np.float32)
        sign = -1.0 if (d % 32) < 16 else 1.0
        cos[p, CTX:] = np.cos(ang)
        sin[p, CTX:] = sign * np.sin(ang)
    return cos, sin


def prep_shared(inp, nlat, depth=DEPTH):
    fm, tm = _fm_cols()
    sh = {}
    cos, sin = _rope_tables(nlat)
    sh["cos"], sh["sins"] = cos, sin
    sh["fing"] = np.ascontiguousarray(np.asarray(inp["final_g"]).reshape(KC, 128).T)
    swp = np.array([_swap_d(p % 64) for p in range(128)])
    for l in range(depth):
        w_in = np.asarray(inp["w_in"][l])
        b_in = np.asarray(inp["b_in"][l])
        sh[f"wmod{l}"] = _pack(np.asarray(inp["w_mod"][l]), 128)
        sh[f"bmod{l}"] = np.ascontiguousarray(np.asarray(inp["b_mod"][l]).reshape(48, 128).T)
        sh[f"n1g{l}"] = np.ascontiguousarray(np.asarray(inp["norm1_g"][l]).reshape(KC, 128).T)
        sh[f"n2g{l}"] = np.ascontiguousarray(np.asarray(inp["norm2_g"][l]).reshape(KC, 128).T)
        wf = w_in[:, fm.reshape(-1)]
        sh[f"wfm{l}"] = _pack(wf, 128)
        sh[f"bfm{l}"] = np.ascontiguousarray(b_in[fm].T)
        sh[f"wtm{l}"] = np.ascontiguousarray(w_in[:, tm].reshape(KC, 128, TMW).transpose(1, 0, 2))
        sh[f"btm{l}"] = np.ascontiguousarray(np.broadcast_to(b_in[tm][None, :], (128, TMW)))
        sh[f"gml{l}"] = np.ascontiguousarray(np.broadcast_to(np.asarray(inp["ml_norm_g"][l]).reshape(1, 512), (128, 512)))
        snk = np.repeat(np.asarray(inp["win_sink"][l]), 128).reshape(1, 2, 512)
        sh[f"sink{l}"] = np.ascontiguousarray(np.broadcast_to(snk, (128, 2, 512)))
        for nm, key in (("qng", "qn_g"), ("kng", "kn_g")):
            g = np.asarray(inp[key][l])
            sh[f"{nm}{l}"] = np.ascontiguousarray(np.stack([g[np.arange(128) % 64], g[swp]], axis=1))
        w_br = np.asarray(inp["w_br"][l])
        sh[f"wbr0{l}"] = _pack(w_br[0], 128)
        sh[f"wbr1{l}"] = _pack(w_br[1], 64).transpose(0, 1, 2, 3).copy()
        sh[f"wbr2{l}"] = _pack(w_br[2], 64).copy()
        sh[f"wo{l}"] = _pack(np.asarray(inp["w_o"][l]), 128)
        sh[f"w1{l}"] = _pack(np.asarray(inp["w_ff1"][l]), 128)
        sh[f"w3{l}"] = _pack(np.asarray(inp["w_ff3"][l]), 128)
        sh[f"w2{l}"] = _pack(np.asarray(inp["w_ff2"][l]), 128)
    return {k: np.ascontiguousarray(v, dtype=np.float32) for k, v in sh.items()}


def prep_core(inp, b):
    x = np.asarray(inp["x"][b])
    ctx = np.asarray(inp["ctx"][b])
    xT0 = np.ascontiguousarray(np.concatenate([ctx, x], axis=0).T)
    cc = np.stack([np.asarray(inp["c"][b]), np.asarray(inp["c_ctx"])], axis=1)
    cT = np.ascontiguousarray(cc.reshape(KC, 128, 2).transpose(1, 0, 2))
    return {"xT0": xT0.astype(np.float32), "cT": cT.astype(np.float32)}


_CACHE = {}


def kernel(**inp):
    x = np.asarray(inp["x"])
    B, nlat, _ = x.shape
    depth = np.asarray(inp["w_in"]).shape[0]
    key = (nlat, depth)
    if key not in _CACHE:
        _CACHE[key] = build(nlat // 128, depth)
    nc = _CACHE[key]
    sh = prep_shared(inp, nlat, depth)
    in_maps = []
    for b in range(B):
        m = dict(sh)
        m.update(prep_core(inp, b))
        in_maps.append(m)
    res = run_bass_kernel_spmd(nc, in_maps, core_ids=list(range(B)))
    out = np.stack([np.ascontiguousarray(r["yT"].T) for r in res.results], axis=0)
    return out.astype(np.float32)
```

```python
import contextlib
import numpy as np
import ml_dtypes
import concourse.bass as bass
import concourse.mybir as mybir
from concourse.bass_utils import run_bass_kernel_spmd

F32 = mybir.dt.float32
BF16 = mybir.dt.bfloat16
AF = mybir.ActivationFunctionType
ALU = mybir.AluOpType
AX = mybir.AxisListType

D = 1024
KC = 8
DFF = 2816
NFC = 22
EPS = 1e-6
NFM = 52
TMW = 1808
DEPTH = 2
SEQ = 16384
CTX = 256


_ALLBUFS = []


class Buf:
    __slots__ = ("ws", "rs", "name")

    def __init__(self, name=""):
        self.ws = []
        self.rs = []
        self.name = name
        _ALLBUFS.append(self)


class Op:
    __slots__ = ("eng", "fn", "deps", "dma", "signal", "cnt", "idx")

    def __init__(self, eng, fn, dma):
        self.eng = eng
        self.fn = fn
        self.dma = dma
        self.deps = []
        self.signal = False
        self.cnt = 0
        self.idx = 0


ENGS = ("pe", "act", "dve", "pool", "sp")
EPOCH = 20000
NDMA = 8


class Sched:
    def __init__(self, nc, strict_same=True):
        self.nc = nc
        self.ops = {e: [] for e in ENGS}
        self.strict_same = strict_same
        self.pending_dma = {e: [] for e in ENGS}
        self.last_real = {e: None for e in ENGS}

    def add(self, eng, fn, reads=(), writes=(), dma=False, force=()):
        op = Op(eng, fn, dma)
        deps = []
        for b in reads:
            deps.extend(b.ws)
        acc = []
        for b in writes:
            a = dma and len(b.ws) > 0 and all(x.dma for x in b.ws) and not b.rs
            acc.append(a)
            if not a:
                deps.extend(b.ws)
                deps.extend(b.rs)
        for b in reads:
            b.rs.append(op)
        for b, a in zip(writes, acc):
            if a:
                b.ws.append(op)
            else:
                b.ws = [op]
                b.rs = []
        best = {}
        dmadeps = []
        seen = set()
        for d in list(deps) + list(force):
            if d is op or d is None:
                continue
            if d.dma:
                if id(d) not in seen:
                    seen.add(id(d))
                    dmadeps.append(d)
            else:
                if d.eng == eng and (eng == "pe" or not self.strict_same) and d not in force:
                    continue
                cur = best.get(d.eng)
                if cur is None or d.idx > cur.idx:
                    best[d.eng] = d
        op.deps = list(best.values()) + dmadeps
        for d in op.deps:
            d.signal = True
        op.idx = len(self.ops[eng])
        self.ops[eng].append(op)
        if dma:
            self.pending_dma[eng].append(op)
        elif fn is not None:
            self.last_real[eng] = op
        return op

    def dma(self, out, in_, reads=(), writes=(), q="sp"):
        return self.add(q, lambda e: e.dma_start(out=out, in_=in_), reads, writes, dma=True)

    def barrier(self):
        firsts = []
        for e in ENGS:
            f = [self.last_real[e]] + self.pending_dma[e]
            self.pending_dma[e] = []
            firsts.append(self.add(e, None, force=[x for x in f if x is not None]))
        for e in ENGS:
            self.add(e, None, force=firsts)
        for b in _ALLBUFS:
            b.ws = []
            b.rs = []

    def emit(self):
        nc = self.nc
        sems = {}
        for e in ENGS:
            n = 0
            k = 0
            for op in self.ops[e]:
                if op.dma:
                    op.cnt = k
                    k += 1
                elif op.signal:
                    op.cnt = n
                    n += 1
            nep = (n + EPOCH - 1) // EPOCH
            sems[e] = [nc.alloc_semaphore(name=f"c_{e}_{i}") for i in range(nep)]
            sems[e + "_dma"] = [nc.alloc_semaphore(name=f"d_{e}_{i}") for i in range(min(NDMA, k))]

        def run_engine(e, h):
            waited = {x: -1 for x in ENGS}
            dma_done = set()
            for op in self.ops[e]:
                for d in op.deps:
                    if d.dma:
                        key = (d.eng, d.cnt)
                        if key in dma_done:
                            continue
                        h.wait_ge(sems[d.eng + "_dma"][d.cnt % NDMA], 16 * (d.cnt // NDMA + 1))
                        dma_done.add(key)
                    else:
                        if waited[d.eng] >= d.cnt:
                            continue
                        h.wait_ge(sems[d.eng][d.cnt // EPOCH], d.cnt % EPOCH + 1)
                        waited[d.eng] = d.cnt
                if op.dma:
                    s = sems[e + "_dma"][op.cnt % NDMA]
                    if op.cnt >= NDMA:
                        key = (e, op.cnt - NDMA)
                        if key not in dma_done:
                            h.wait_ge(s, 16 * (op.cnt // NDMA))
                            dma_done.add(key)
                    op.fn(h).then_inc(s, 16)
                elif op.fn is None:
                    if op.signal:
                        h.sem_inc(sems[e][op.cnt // EPOCH], 1)
                else:
                    ins = op.fn(h)
                    if op.signal:
                        ins.then_inc(sems[e][op.cnt // EPOCH], 1)
            k = sum(1 for op in self.ops[e] if op.dma)
            for j in range(min(NDMA, k)):
                last = ((k - 1 - j) // NDMA) * NDMA + j
                if (e, last) not in dma_done:
                    h.wait_ge(sems[e + "_dma"][j], 16 * (last // NDMA + 1))

        with nc.Block() as block:
            @block.sync
            def _(h):
                run_engine("sp", h)

            @block.tensor
            def _(h):
                run_engine("pe", h)

            @block.scalar
            def _(h):
                run_engine("act", h)

            @block.vector
            def _(h):
                run_engine("dve", h)

            @block.gpsimd
            def _(h):
                run_engine("pool", h)


_UID = [0]


class Ring:
    def __init__(self, nc, stack, name, shape, dtype, n):
        _UID[0] += 1
        name = f"{name}u{_UID[0]}"
        self.t = [stack.enter_context(nc.sbuf_tensor(f"{name}_{i}", shape, dtype)) for i in range(n)]
        self.b = [Buf(f"{name}_{i}") for i in range(n)]
        self.i = 0

    def next(self):
        i = self.i
        self.i = (i + 1) % len(self.t)
        return self.t[i], self.b[i]


def build(NLB, depth=DEPTH, dbg=False):
    assert NLB % 4 == 0
    NB = NLB + 2
    T = NB * 128
    tiles = [(0, 256)] + [(256 + 512 * i, 512) for i in range(NLB // 4)]
    nc = bass.Bass("TRN2", target_bir_lowering=False)
    S = Sched(nc)

    def din(name, shape, dt=F32):
        return nc.dram_tensor(name, list(shape), dt, kind="ExternalInput").ap()

    def dscr(name, shape, dt):
        return nc.dram_tensor(name, list(shape), dt, kind=("ExternalOutput" if dbg else "Internal")).ap()

    xT0 = din("xT0", [D, T])
    cT = din("cT", [128, KC, 2])
    cosd = din("cos", [128, T])
    sind = din("sins", [128, T])
    fing = din("fing", [128, KC])
    L = []
    for l in range(depth):
        L.append(dict(
            wmod=din(f"wmod{l}", [48, 128, KC, 128]), bmod=din(f"bmod{l}", [128, 48]),
            n1g=din(f"n1g{l}", [128, KC]), n2g=din(f"n2g{l}", [128, KC]),
            wfm=din(f"wfm{l}", [NFM, 128, KC, 128]), bfm=din(f"bfm{l}", [128, NFM]),
            wtm=din(f"wtm{l}", [128, KC, TMW]), btm=din(f"btm{l}", [128, TMW]),
            gml=din(f"gml{l}", [128, 512]), sink=din(f"sink{l}", [128, 2, 512]),
            qng=din(f"qng{l}", [128, 2]), kng=din(f"kng{l}", [128, 2]),
            wbr0=din(f"wbr0{l}", [8, 128, 4, 128]), wbr1=din(f"wbr1{l}", [8, 64, 8, 128]),
            wbr2=din(f"wbr2{l}", [8, 64, 8, 128]), wo=din(f"wo{l}", [8, 128, KC, 128]),
            w1=din(f"w1{l}", [NFC, 128, KC, 128]), w3=din(f"w3{l}", [NFC, 128, KC, 128]),
            w2=din(f"w2{l}", [8, 128, NFC, 128]),
        ))
    yT = nc.dram_tensor("yT", [D, NLB * 128], F32, kind="ExternalOutput").ap()

    xs = dscr("xs", [D, T], F32)
    b_wmod = dscr("b_wmod", [48, 128, KC * 128], BF16)
    b_wfm = dscr("b_wfm", [NFM, 128, KC * 128], BF16)
    b_wtm = dscr("b_wtm", [128, KC * TMW], BF16)
    b_wbr0 = dscr("b_wbr0", [8, 128, 4 * 128], BF16)
    b_wbr1 = dscr("b_wbr1", [8, 64, 8 * 128], BF16)
    b_wbr2 = dscr("b_wbr2", [8, 64, 8 * 128], BF16)
    b_wo = dscr("b_wo", [8, 128, KC * 128], BF16)
    b_w1 = dscr("b_w1", [NFC, 128, KC * 128], BF16)
    b_w3 = dscr("b_w3", [NFC, 128, KC * 128], BF16)
    b_w2 = dscr("b_w2", [8, 128, NFC * 128], BF16)
    mqT = dscr("mqT", [512, T], BF16)
    mkT = dscr("mkT", [512, T], BF16)
    wqT = dscr("wqT", [512, T], BF16)
    wkT = dscr("wkT", [128, T], BF16)
    gqT = dscr("gqT", [512, T], BF16)
    gkT = dscr("gkT", [128, T], BF16)
    bgT = dscr("bgT", [3072, T], BF16)
    mk_tm = dscr("mk_tm", [T, 512], BF16)
    mv_tm = dscr("mv_tm", [T, 512], BF16)
    mo_tm = dscr("mo_tm", [T, 512], BF16)
    wgv_tm = dscr("wgv_tm", [T, 256], BF16)
    gat_tm = dscr("gat_tm", [T, 16], F32)
    hb_tm = dscr("hb_tm", [T, 512], F32)
    yaT = dscr("yaT", [512, T], BF16)
    ybT = dscr("ybT", [64, 8, T], BF16)
    ycT = dscr("ycT", [64, 8, T], BF16)
    DB = {k: Buf(k) for k in ["xs", "w", "proj", "hb", "ya", "yb", "yc"]}

    stack0 = contextlib.ExitStack()
    psum2 = [nc.alloc_psum_tensor(f"dbank{i}", [128, 1024], F32) for i in range(4)]
    psum = [psum2[i // 2][:, (i % 2) * 512:(i % 2 + 1) * 512] for i in range(8)]
    psb = [Buf(f"bank{i}") for i in range(8)]

    wq_i = [0]

    def wq():
        wq_i[0] += 1
        return "sp" if wq_i[0] % 2 == 0 else "act"

    def sb(stack, name, shape, dt):
        _UID[0] += 1
        return stack.enter_context(nc.sbuf_tensor(f"{name}u{_UID[0]}", shape, dt))

    c_f32 = sb(stack0, "c_f32", [128, 128], F32)
    ones_f = sb(stack0, "ones_f", [128, 128], F32)
    tri_f = sb(stack0, "tri_f", [128, 128], F32)
    tri_b = sb(stack0, "tri_b", [128, 128], F32)
    m_le = sb(stack0, "m_le", [128, 128], BF16)
    m_ge = sb(stack0, "m_ge", [128, 128], BF16)
    onesm = sb(stack0, "onesm", [128, 128], BF16)
    bd64 = sb(stack0, "bd64", [128, 128], BF16)
    ident = sb(stack0, "ident", [128, 128], BF16)
    eps_sb = sb(stack0, "eps_sb", [128, 1], F32)
    CB = Buf("consts")

    def mk_consts():
        S.add("pool", lambda e: e.memset(ones_f[:], 1.0), writes=[CB])
        S.add("pool", lambda e: e.memset(eps_sb[:], EPS), writes=[CB])
        S.add("pool", lambda e: e.memset(onesm[:], 1.0 / 1024.0), writes=[CB])
        S.add("pool", lambda e: e.affine_select(out=tri_f[:], in_=ones_f[:], pattern=[[1, 128]],
                                                compare_op=ALU.is_ge, fill=0.0, base=0, channel_multiplier=-1),
              reads=[CB], writes=[CB])
        S.add("pool", lambda e: e.affine_select(out=tri_b[:], in_=ones_f[:], pattern=[[-1, 128]],
                                                compare_op=ALU.is_ge, fill=0.0, base=0, channel_multiplier=1),
              reads=[CB], writes=[CB])
        S.add("pool", lambda e: e.tensor_copy(out=m_le[:], in_=tri_f[:]), reads=[CB], writes=[CB])
        S.add("pool", lambda e: e.tensor_copy(out=m_ge[:], in_=tri_b[:]), reads=[CB], writes=[CB])
        S.add("pool", lambda e: e.tensor_tensor(out=ident[:], in0=tri_f[:], in1=tri_b[:], op=ALU.mult),
              reads=[CB], writes=[CB])
        S.add("pool", lambda e: e.memset(bd64[:], 0.0), reads=[CB], writes=[CB])
        S.add("pool", lambda e: e.memset(bd64[0:64, 0:64], 1.0 / 64.0), reads=[CB], writes=[CB])
        S.add("pool", lambda e: e.memset(bd64[64:128, 64:128], 1.0 / 64.0), reads=[CB], writes=[CB])

    mk_consts()

    cast_i = [0]

    def cast_pack(stack, src, dst, nchunk, P, F):
        pieces = []
        for c in range(nchunk):
            f0 = 0
            while f0 < F:
                fw = min(2048, F - f0)
                pieces.append((c, f0, fw))
                f0 += fw
        for (c, f0, fw) in pieces:
            a, ab = cast_f.next()
            o, ob = cast_o.next()
            S.dma(a[0:P, 0:fw], srcflat(src, c)[:, f0:f0 + fw], reads=[], writes=[ab], q="sp")
            eng = ("dve", "pool", "act")[cast_i[0] % 3]
            cast_i[0] += 1
            if eng == "act":
                S.add("act", lambda e, a=a, o=o, fw=fw: e.copy(out=o[0:P, 0:fw], in_=a[0:P, 0:fw]), [ab], [ob])
            else:
                S.add(eng, lambda e, a=a, o=o, fw=fw: e.tensor_copy(out=o[0:P, 0:fw], in_=a[0:P, 0:fw]), [ab], [ob])
            S.dma(dst[c][:, f0:f0 + fw], o[0:P, 0:fw], reads=[ob], writes=[DB["w"]], q="pool")

    def srcflat(src, c):
        v = src[c]
        if len(v.shape) == 3:
            return v.rearrange("p a b -> p (a b)")
        return v

    for l in range(depth):
        W = L[l]
        ctx_out = l < depth - 1
        xin = xT0 if l == 0 else xs
        xin_v = xin.rearrange("(k p) t -> p k t", p=128)
        xs_v = xs.rearrange("(k p) t -> p k t", p=128)

        with contextlib.ExitStack() as st:
            cast_f = Ring(nc, st, "cast_f", [128, 2048], F32, 3)
            cast_o = Ring(nc, st, "cast_o", [128, 2048], BF16, 3)
            cast_pack(st, W["wmod"], b_wmod, 48, 128, KC * 128)
            cast_pack(st, W["wfm"], b_wfm, NFM, 128, KC * 128)
            cast_pack(st, W["wtm"].rearrange("p (o a) b -> o p a b", o=1), b_wtm.rearrange("(o p) f -> o p f", o=1), 1, 128, KC * TMW)
            cast_pack(st, W["wbr0"], b_wbr0, 8, 128, 4 * 128)
            cast_pack(st, W["wbr1"], b_wbr1, 8, 64, 8 * 128)
            cast_pack(st, W["wbr2"], b_wbr2, 8, 64, 8 * 128)
            cast_pack(st, W["wo"], b_wo, 8, 128, KC * 128)
            cast_pack(st, W["w1"], b_w1, NFC, 128, KC * 128)
            cast_pack(st, W["w3"], b_w3, NFC, 128, KC * 128)
            cast_pack(st, W["w2"], b_w2, 8, 128, NFC * 128)
            S.barrier()

        lay = contextlib.ExitStack()
        MOD = sb(lay, "MOD", [128, 48, 2], F32)
        G1 = sb(lay, "G1", [128, KC, 2], F32)
        G2 = sb(lay, "G2", [128, KC, 2], F32)
        MB = Buf("MOD")
        with contextlib.ExitStack() as st:
            c_sb = sb(st, "c_sb", [128, KC, 2], F32)
            sc = sb(st, "sc", [128, KC, 2], BF16)
            bm = sb(st, "bm", [128, 48], F32)
            ng = sb(st, "ng", [128, 2, KC], F32)
            wr = Ring(nc, st, "wmodc", [128, KC, 128], BF16, 4)
            tb = Buf("m_tmp")
            S.dma(c_sb[:], cT, writes=[tb])
            S.dma(bm[:], W["bmod"], writes=[tb])
            S.dma(ng[:, 0, :], W["n1g"], writes=[tb])
            S.dma(ng[:, 1, :], W["n2g"], writes=[tb])
            S.add("act", lambda e: e.activation(out=sc[:], in_=c_sb[:], func=AF.Silu), [tb], [tb])
            pm = psum[0]
            for mc in range(48):
                w_, wb = wr.next()
                S.dma(w_[:].rearrange("p a b -> p (a b)"), b_wmod[mc], reads=[DB["w"]], writes=[wb])
                for kc in range(KC):
                    S.add("pe", lambda e, w_=w_, kc=kc, mc=mc: e.matmul(
                        pm[:, 2 * mc:2 * mc + 2], lhsT=w_[:, kc, :], rhs=sc[:, kc, :], start=(kc == 0), stop=(kc == KC - 1)),
                        [wb, tb], [psb[0]])
            S.add("dve", lambda e: e.tensor_tensor(
                out=MOD[:], in0=pm[:, 0:96].rearrange("p (m w) -> p m w", w=2),
                in1=bm[:].unsqueeze(2).to_broadcast([128, 48, 2]), op=ALU.add), [psb[0], tb], [MB])
            S.add("dve", lambda e: e.scalar_tensor_tensor(
                out=G1[:], in0=MOD[:, 8:16, :], scalar=1.0, in1=ng[:, 0, :].unsqueeze(2).to_broadcast([128, KC, 2]),
                op0=ALU.add, op1=ALU.mult), [MB, tb], [MB])
            S.add("dve", lambda e: e.scalar_tensor_tensor(
                out=G2[:], in0=MOD[:, 32:40, :], scalar=1.0, in1=ng[:, 1, :].unsqueeze(2).to_broadcast([128, KC, 2]),
                op0=ALU.add, op1=ALU.mult), [MB, tb], [MB])
            if dbg and l == 0:
                dbg_mod = dscr("dbg_mod", [128, 96], F32)
                dbg_g1 = dscr("dbg_g1", [128, 16], F32)
                S.dma(dbg_mod, MOD[:].rearrange("p a b -> p (a b)"), reads=[MB])
                S.dma(dbg_g1, G1[:].rearrange("p a b -> p (a b)"), reads=[MB])
            S.barrier()

        with contextlib.ExitStack() as st:
            wtm_sb = sb(st, "wtm_sb", [128, KC, TMW], BF16)
            btm_sb = sb(st, "btm_sb", [128, TMW], F32)
            bfm_sb = sb(st, "bfm_sb", [128, NFM], F32)
            qng_sb = sb(st, "qng_sb", [128, 2], F32)
            kng_sb = sb(st, "kng_sb", [128, 2], F32)
            AB = Buf("a_const")
            S.dma(wtm_sb[:].rearrange("p a b -> p (a b)"), b_wtm, reads=[DB["w"]], writes=[AB])
            S.dma(btm_sb[:], W["btm"], writes=[AB])
            S.dma(bfm_sb[:], W["bfm"], writes=[AB])
            S.dma(qng_sb[:], W["qng"], writes=[AB])
            S.dma(kng_sb[:], W["kng"], writes=[AB])
            xr = Ring(nc, st, "a_x", [128, KC, 512], F32, 2)
            sqr = Ring(nc, st, "a_sq", [128, KC, 512], BF16, 1)
            hr = Ring(nc, st, "a_h", [128, KC, 512], BF16, 2)
            rsr = Ring(nc, st, "a_rs", [128, 512], F32, 2)
            tmpr = Ring(nc, st, "a_tmp", [128, 512], F32, 3)
            wcr = Ring(nc, st, "a_wc", [128, KC, 128], BF16, 6)
            csr = Ring(nc, st, "a_cs", [128, 2, 512], F32, 2)
            f32r = Ring(nc, st, "a_f", [128, 512], F32, 8)
            bfr = Ring(nc, st, "a_b", [128, 512], BF16, 6)
            tmo = Ring(nc, st, "a_tmo", [128, 512], BF16, 4)
            gto = Ring(nc, st, "a_gto", [128, 16], F32, 2)
            pi = [0]

            def pbank(lo, n):
                i = lo + pi[0] % n
                pi[0] += 1
                return psum[i], psb[i]

            def proj(c, h_, hb, tt):
                w_, wb = wcr.next()
                S.dma(w_[:].rearrange("p a b -> p (a b)"), b_wfm[c], reads=[DB["w"]], writes=[wb], q=wq())
                p_, pb = pbank(1, 4)
                for kc in range(KC):
                    S.add("pe", lambda e, w_=w_, kc=kc, p_=p_: e.matmul(
                        p_[:, 0:tt], lhsT=w_[:, kc, :], rhs=h_[:, kc, 0:tt], start=(kc == 0), stop=(kc == KC - 1)),
                        [wb, hb], [pb])
                return p_, pb

            def store(dst, row0, t0, tt, o_, ob):
                S.dma(dst[row0:row0 + 128, t0:t0 + tt], o_[:, 0:tt], reads=[ob], writes=[DB["proj"]], q="sp")

            dbg_h = dscr("dbg_h", [D, T], BF16) if (dbg and l == 0) else None

            def a_tile(t0, tt):
                wsel = 1 if t0 == 0 else 0
                x_, xb = xr.next()
                S.dma(x_[:, :, 0:tt], xin_v[:, :, t0:t0 + tt], reads=[DB["xs"]], writes=[xb])
                cs_, csb = csr.next()
                S.dma(cs_[:, 0, 0:tt], cosd[:, t0:t0 + tt], writes=[csb], q="pool")
                S.dma(cs_[:, 1, 0:tt], sind[:, t0:t0 + tt], writes=[csb], q="pool")
                sq_, sqb = sqr.next()
                S.add("act", lambda e, x_=x_, sq_=sq_: e.activation(out=sq_[:, :, 0:tt], in_=x_[:, :, 0:tt], func=AF.Square),
                      [xb], [sqb])
                pm, pmb = psum[0], psb[0]
                for kc in range(KC):
                    S.add("pe", lambda e, kc=kc, sq_=sq_: e.matmul(pm[:, 0:tt], lhsT=onesm[:], rhs=sq_[:, kc, 0:tt],
                                                                   start=(kc == 0), stop=(kc == KC - 1)), [sqb, CB], [pmb])
                rs_, rsb = rsr.next()
                S.add("act", lambda e, rs_=rs_: e.activation(out=rs_[:, 0:tt], in_=pm[:, 0:tt], func=AF.Ln, bias=eps_sb[:, 0:1], scale=1.0),
                      [pmb, CB], [rsb])
                S.add("act", lambda e, rs_=rs_: e.activation(out=rs_[:, 0:tt], in_=rs_[:, 0:tt], func=AF.Exp, scale=-0.5), [rsb], [rsb])
                h_, hb = hr.next()
                for kc in range(KC):
                    tm_, tmb = tmpr.next()
                    S.add("dve" if kc % 2 == 0 else "pool", lambda e, kc=kc, tm_=tm_, x_=x_, rs_=rs_: e.tensor_tensor(
                        out=tm_[:, 0:tt], in0=x_[:, kc, 0:tt], in1=rs_[:, 0:tt], op=ALU.mult), [xb, rsb], [tmb])
                    S.add("act", lambda e, kc=kc, tm_=tm_, h_=h_: e.activation(
                        out=h_[:, kc, 0:tt], in_=tm_[:, 0:tt], func=AF.Identity,
                        scale=G1[:, kc, wsel:wsel + 1], bias=MOD[:, kc, wsel:wsel + 1]), [tmb, MB], [hb])
                if dbg and l == 0:
                    S.dma(dbg_h.rearrange("(k p) t -> p k t", p=128)[:, :, t0:t0 + tt], h_[:, :, 0:tt], reads=[hb])
                for c in range(8):
                    p_, pb = proj(c, h_, hb, tt)
                    o_, ob = bfr.next()
                    S.add("act", lambda e, p_=p_, o_=o_, c=c: e.activation(
                        out=o_[:, 0:tt], in_=p_[:, 0:tt], func=AF.Identity, bias=bfm_sb[:, c:c + 1], scale=1.0), [pb, AB], [ob])
                    store(mqT if c < 4 else mkT, (c % 4) * 128, t0, tt, o_, ob)
                for (ca, cb_, dst, row0, norm) in (
                        [(8 + j, 12 + j, wqT, j * 128, None) for j in range(4)] + [(16, 17, wkT, 0, None)] +
                        [(18 + j, 22 + j, gqT, j * 128, qng_sb) for j in range(4)] + [(26, 27, gkT, 0, kng_sb)]):
                    pa, pab = proj(ca, h_, hb, tt)
                    pb2, pbb = proj(cb_, h_, hb, tt)
                    q1, q1b = f32r.next()
                    q2, q2b = f32r.next()
                    if norm is None:
                        S.add("dve", lambda e, pa=pa, q1=q1, ca=ca, cs_=cs_: e.scalar_tensor_tensor(
                            out=q1[:, 0:tt], in0=pa[:, 0:tt], scalar=bfm_sb[:, ca:ca + 1], in1=cs_[:, 0, 0:tt],
                            op0=ALU.add, op1=ALU.mult), [pab, AB, csb], [q1b])
                        S.add("dve", lambda e, pb2=pb2, q2=q2, cb_=cb_, cs_=cs_: e.scalar_tensor_tensor(
                            out=q2[:, 0:tt], in0=pb2[:, 0:tt], scalar=bfm_sb[:, cb_:cb_ + 1], in1=cs_[:, 1, 0:tt],
                            op0=ALU.add, op1=ALU.mult), [pbb, AB, csb], [q2b])
                        o_, ob = bfr.next()
                        S.add("pool", lambda e, q1=q1, q2=q2, o_=o_: e.tensor_tensor(
                            out=o_[:, 0:tt], in0=q1[:, 0:tt], in1=q2[:, 0:tt], op=ALU.add), [q1b, q2b], [ob])
                    else:
                        qa, qab = f32r.next()
                        qs, qsb = f32r.next()
                        S.add("act", lambda e, pa=pa, qa=qa, ca=ca: e.activation(
                            out=qa[:, 0:tt], in_=pa[:, 0:tt], func=AF.Identity, bias=bfm_sb[:, ca:ca + 1], scale=1.0),
                            [pab, AB], [qab])
                        S.add("act", lambda e, pb2=pb2, qs=qs, cb_=cb_: e.activation(
                            out=qs[:, 0:tt], in_=pb2[:, 0:tt], func=AF.Identity, bias=bfm_sb[:, cb_:cb_ + 1], scale=1.0),
                            [pbb, AB], [qsb])
                        s2, s2b = bfr.next()
                        S.add("pool", lambda e, qa=qa, s2=s2: e.tensor_tensor(
                            out=s2[:, 0:tt], in0=qa[:, 0:tt], in1=qa[:, 0:tt], op=ALU.mult), [qab], [s2b])
                        pn, pnb = pbank(5, 2)
                        S.add("pe", lambda e, pn=pn, s2=s2: e.matmul(pn[:, 0:tt], lhsT=bd64[:], rhs=s2[:, 0:tt],
                                                                    start=True, stop=True), [s2b, CB], [pnb])
                        rn, rnb = f32r.next()
                        S.add("act", lambda e, pn=pn, rn=rn: e.activation(
                            out=rn[:, 0:tt], in_=pn[:, 0:tt], func=AF.Ln, bias=eps_sb[:, 0:1], scale=1.0), [pnb, CB], [rnb])
                        S.add("act", lambda e, rn=rn: e.activation(out=rn[:, 0:tt], in_=rn[:, 0:tt], func=AF.Exp, scale=-0.5),
                              [rnb], [rnb])
                        S.add("dve", lambda e, qa=qa, q1=q1, cs_=cs_, norm=norm: e.scalar_tensor_tensor(
                            out=q1[:, 0:tt], in0=qa[:, 0:tt], scalar=norm[:, 0:1], in1=cs_[:, 0, 0:tt],
                            op0=ALU.mult, op1=ALU.mult), [qab, AB, csb], [q1b])
                        S.add("dve", lambda e, qs=qs, q2=q2, cs_=cs_, norm=norm: e.scalar_tensor_tensor(
                            out=q2[:, 0:tt], in0=qs[:, 0:tt], scalar=norm[:, 1:2], in1=cs_[:, 1, 0:tt],
                            op0=ALU.mult, op1=ALU.mult), [qsb, AB, csb], [q2b])
                        S.add("dve", lambda e, q1=q1, q2=q2: e.tensor_tensor(
                            out=q1[:, 0:tt], in0=q1[:, 0:tt], in1=q2[:, 0:tt], op=ALU.add), [q1b, q2b], [q1b])
                        o_, ob = bfr.next()
                        S.add("pool", lambda e, q1=q1, rn=rn, o_=o_: e.tensor_tensor(
                            out=o_[:, 0:tt], in0=q1[:, 0:tt], in1=rn[:, 0:tt], op=ALU.mult), [q1b, rnb], [ob])
                    store(dst, row0, t0, tt, o_, ob)
                for c in range(28, 52):
                    p_, pb = proj(c, h_, hb, tt)
                    o_, ob = bfr.next()
                    S.add("act", lambda e, p_=p_, o_=o_, c=c: e.activation(
                        out=o_[:, 0:tt], in_=p_[:, 0:tt], func=AF.Sigmoid, bias=bfm_sb[:, c:c + 1], scale=1.0), [pb, AB], [ob])
                    store(bgT, (c - 28) * 128, t0, tt, o_, ob)
                for blk in range(tt // 128):
                    r0 = t0 + blk * 128
                    for gi, (c0, cw, dst, dcol) in enumerate(((0, 512, mk_tm, 0), (512, 512, mv_tm, 0), (1024, 512, mo_tm, 0),
                                                              (1536, 256, wgv_tm, 0), (1792, 16, gat_tm, 0))):
                        p_, pb = pbank(5, 3)
                        for kc in range(KC):
                            S.add("pe", lambda e, p_=p_, kc=kc, c0=c0, cw=cw, blk=blk: e.matmul(
                                p_[:, 0:cw], lhsT=h_[:, kc, blk * 128:(blk + 1) * 128], rhs=wtm_sb[:, kc, c0:c0 + cw],
                                start=(kc == 0), stop=(kc == KC - 1)), [hb, AB], [pb])
                        if gi == 4:
                            o_, ob = gto.next()
                            S.add("dve", lambda e, p_=p_, o_=o_, c0=c0, cw=cw: e.tensor_tensor(
                                out=o_[:, 0:cw], in0=p_[:, 0:cw], in1=btm_sb[:, c0:c0 + cw], op=ALU.add), [pb, AB], [ob])
                        else:
                            o_, ob = tmo.next()
                            S.add("dve", lambda e, p_=p_, o_=o_, c0=c0, cw=cw: e.tensor_tensor(
                                out=o_[:, 0:cw], in0=p_[:, 0:cw], in1=btm_sb[:, c0:c0 + cw], op=ALU.add), [pb, AB], [ob])
                            if gi == 2:
                                S.add("act", lambda e, o_=o_, cw=cw: e.activation(out=o_[:, 0:cw], in_=o_[:, 0:cw], func=AF.Sigmoid),
                                      [ob], [ob])
                        S.dma(dst[r0:r0 + 128, 0:cw], o_[:, 0:cw], reads=[ob], writes=[DB["proj"]], q="sp")

            for (t0_, tt_) in tiles:
                a_tile(t0_, tt_)
            S.barrier()

        with contextlib.ExitStack() as st:
            E_st = sb(st, "E_st", [128, 8, 129], F32)
            C_bf = sb(st, "C_bf", [128, 8, 129], BF16)
            ebp = sb(st, "ebp", [128, 8], F32)
            gml_sb = sb(st, "gml_sb", [128, 512], F32)
            SB_ = [Buf(f"st{g}") for g in range(8)]
            EBP = Buf("ebp")
            GB = Buf("gml")
            S.dma(gml_sb[:], W["gml"], writes=[GB])
            S.add("pool", lambda e: e.memset(E_st[:], 0.0), writes=SB_)
            S.add("pool", lambda e: e.memset(C_bf[:], 0.0), writes=SB_)
            S.add("pool", lambda e: e.memset(ebp[:], 1.0), writes=[EBP])
            qTr = Ring(nc, st, "b_qT", [128, 4, 128], BF16, 3)
            kTr = Ring(nc, st, "b_kT", [128, 4, 128], BF16, 3)
            ktr = Ring(nc, st, "b_kt", [128, 512], BF16, 3)
            vtr = Ring(nc, st, "b_vt", [128, 4, 129], BF16, 3)
            gtr = Ring(nc, st, "b_gt", [128, 16], F32, 3)
            for (v_, vb_) in zip(vtr.t, vtr.b):
                S.add("pool", lambda e, v_=v_: e.memset(v_[:], 1.0), writes=[vb_])
            smr = Ring(nc, st, "b_sm", [128, 128], BF16, 4)
            vur = Ring(nc, st, "b_vu", [128, 129], BF16, 4)
            g1r = Ring(nc, st, "b_g1", [128, 32], F32, 3)
            smallr = Ring(nc, st, "b_sml", [128, 4], F32, 8)
            hfr = Ring(nc, st, "b_hf", [128, 512], F32, 3)
            hbr = Ring(nc, st, "b_hb", [128, 512], F32, 2)
            mor = Ring(nc, st, "b_mo", [128, 512], BF16, 2)
            ybr = Ring(nc, st, "b_y", [128, 512], BF16, 2)
            yTr = Ring(nc, st, "b_yT", [128, 4, 128], BF16, 2)
            mq_v = mqT.rearrange("(h d) t -> d h t", d=128)
            mk_v = mkT.rearrange("(h d) t -> d h t", d=128)
            ya_v = yaT.rearrange("(h d) t -> d h t", d=128)
            scale_ml = 128.0 ** -0.5
            bi = [0]

            def bbank(lo, n):
                i = lo + bi[0] % n
                bi[0] += 1
                return psum[i], psb[i]

            def b_block(dr, c):
                    t0 = c * 128
                    need_out = ctx_out or c >= 2
                    q_, qb = qTr.next()
                    k_, kb = kTr.next()
                    kt_, ktb = ktr.next()
                    vt_, vtb = vtr.next()
                    gt_, gtb = gtr.next()
                    S.dma(q_[:], mq_v[:, :, t0:t0 + 128], reads=[DB["proj"]], writes=[qb])
                    S.dma(k_[:], mk_v[:, :, t0:t0 + 128], reads=[DB["proj"]], writes=[kb])
                    S.dma(kt_[:], mk_tm[t0:t0 + 128, :], reads=[DB["proj"]], writes=[ktb], q="pool")
                    S.dma(vt_[:, :, 0:128], mv_tm[t0:t0 + 128, :].rearrange("t (h e) -> t h e", e=128), reads=[DB["proj"]],
                          writes=[vtb], q="pool")
                    S.dma(gt_[:], gat_tm[t0:t0 + 128, :], reads=[DB["proj"]], writes=[gtb])
                    g1, g1b = g1r.next()
                    S.add("act", lambda e, gt_=gt_, g1=g1: e.activation(
                        out=g1[:, 16:20], in_=gt_[:, 8 + dr * 4:12 + dr * 4], func=AF.Exp, scale=-1.0), [gtb], [g1b])
                    S.add("act", lambda e, g1=g1: e.activation(out=g1[:, 0:4], in_=g1[:, 16:20], func=AF.Ln, bias=1.0, scale=1.0),
                          [g1b], [g1b])
                    S.add("dve", lambda e, g1=g1: e.tensor_scalar(out=g1[:, 0:4], in0=g1[:, 0:4], scalar1=-1.0, scalar2=None,
                                                                  op0=ALU.mult), [g1b], [g1b])
                    pg, pgb = psum[0], psb[0]
                    tri = tri_b if dr == 1 else tri_f
                    S.add("pe", lambda e, g1=g1, tri=tri: e.matmul(pg[:, 0:4], lhsT=tri[:], rhs=g1[:, 0:4], start=True, stop=True),
                          [g1b, CB], [pgb])
                    S.add("pe", lambda e, g1=g1: e.matmul(pg[:, 4:8], lhsT=ones_f[:], rhs=g1[:, 0:4], start=True, stop=True),
                          [g1b, CB], [pgb])
                    S.add("dve", lambda e, g1=g1, gt_=gt_: e.tensor_tensor(
                        out=g1[:, 16:20], in0=gt_[:, dr * 4:dr * 4 + 4], in1=pg[:, 0:4], op=ALU.subtract), [gtb, pgb, g1b], [g1b])
                    S.add("act", lambda e, g1=g1: e.activation(out=g1[:, 4:8], in_=g1[:, 16:20], func=AF.Exp), [g1b], [g1b])
                    S.add("act", lambda e, g1=g1: e.activation(out=g1[:, 8:16], in_=pg[:, 0:8], func=AF.Exp), [pgb, g1b], [g1b])
                    S.add("dve", lambda e, g1=g1: e.tensor_scalar(out=g1[:, 8:12], in0=g1[:, 8:12], scalar1=scale_ml, scalar2=None,
                                                                  op0=ALU.mult), [g1b], [g1b])
                    if need_out:
                        hf, hfb = hfr.next()
                    for h in range(4):
                        g = dr * 4 + h
                        ps_, psb_ = bbank(1, 2)
                        S.add("pe", lambda e, ps_=ps_, k_=k_, q_=q_, h=h: e.matmul(
                            ps_[:, 0:128], lhsT=k_[:, h, :], rhs=q_[:, h, :], start=True, stop=True), [kb, qb], [psb_])
                        sm_, smb = smr.next()
                        msk = m_ge if dr == 1 else m_le
                        S.add("dve", lambda e, ps_=ps_, sm_=sm_, msk=msk: e.tensor_tensor(
                            out=sm_[:], in0=ps_[:, 0:128], in1=msk[:], op=ALU.mult), [psb_, CB], [smb])
                        vu_, vub = vur.next()
                        S.add("act", lambda e, vu_=vu_, vt_=vt_, g1=g1, h=h: e.activation(
                            out=vu_[:], in_=vt_[:, h, :], func=AF.Identity, scale=g1[:, 4 + h:5 + h]), [vtb, g1b], [vub])
                        if need_out:
                            ph, phb = bbank(3, 2)
                            S.add("pe", lambda e, ph=ph, sm_=sm_, vu_=vu_: e.matmul(
                                ph[:, 0:129], lhsT=sm_[:], rhs=vu_[:], start=True, stop=False), [smb, vub], [phb])
                            S.add("pe", lambda e, ph=ph, q_=q_, h=h, g=g: e.matmul(
                                ph[:, 0:129], lhsT=q_[:, h, :], rhs=C_bf[:, g, :], start=False, stop=True), [qb, SB_[g]], [phb])
                            sm4, sm4b = smallr.next()
                            S.add("act", lambda e, ph=ph, sm4=sm4, g1=g1, h=h: e.activation(
                                out=sm4[:, 0:1], in_=ph[:, 128:129], func=AF.Abs, scale=g1[:, 8 + h:9 + h]), [phb, g1b], [sm4b])
                            S.add("dve", lambda e, sm4=sm4: e.tensor_scalar(
                                out=sm4[:, 0:1], in0=sm4[:, 0:1], scalar1=1.0, scalar2=None, op0=ALU.max), [sm4b], [sm4b])
                            S.add("dve", lambda e, sm4=sm4: e.reciprocal(out=sm4[:, 2:3], in_=sm4[:, 0:1]), [sm4b], [sm4b])
                            S.add("dve", lambda e, sm4=sm4, g1=g1, h=h: e.tensor_tensor(
                                out=sm4[:, 1:2], in0=g1[:, 8 + h:9 + h], in1=sm4[:, 2:3], op=ALU.mult), [sm4b, g1b], [sm4b])
                            S.add("act", lambda e, ph=ph, hf=hf, sm4=sm4, h=h: e.activation(
                                out=hf[:, h * 128:(h + 1) * 128], in_=ph[:, 0:128], func=AF.Identity, scale=sm4[:, 1:2]),
                                [phb, sm4b], [hfb])
                        pc, pcb = bbank(5, 2)
                        S.add("pe", lambda e, pc=pc, kt_=kt_, vu_=vu_, h=h: e.matmul(
                            pc[:, 0:129], lhsT=kt_[:, h * 128:(h + 1) * 128], rhs=vu_[:], start=True, stop=True), [ktb, vub], [pcb])
                        S.add("dve", lambda e, pc=pc, g=g: e.scalar_tensor_tensor(
                            out=E_st[:, g, :], in0=E_st[:, g, :], scalar=ebp[:, g:g + 1], in1=pc[:, 0:129],
                            op0=ALU.mult, op1=ALU.add), [pcb, EBP, SB_[g]], [SB_[g]])
                        S.add("pool", lambda e, g=g, g1=g1, h=h: e.tensor_scalar(
                            out=C_bf[:, g, :], in0=E_st[:, g, :], scalar1=g1[:, 12 + h:13 + h], scalar2=None, op0=ALU.mult),
                            [SB_[g], g1b], [SB_[g]])
                    S.add("dve", lambda e, g1=g1: e.tensor_copy(out=ebp[:, dr * 4:dr * 4 + 4], in_=g1[:, 12:16]),
                          [g1b, EBP] + SB_[dr * 4:dr * 4 + 4], [EBP])
                    if not need_out:
                        return
                    if dr == 1:
                        S.dma(hb_tm[t0:t0 + 128, :], hf[:], reads=[hfb], writes=[DB["hb"]], q="sp")
                        return
                    hb_, hbb = hbr.next()
                    mo_, mob = mor.next()
                    S.dma(hb_[:], hb_tm[t0:t0 + 128, :], reads=[DB["hb"]], writes=[hbb], q="pool")
                    S.dma(mo_[:], mo_tm[t0:t0 + 128, :], reads=[DB["proj"]], writes=[mob], q="pool")
                    S.add("pool", lambda e, hf=hf, hb_=hb_: e.tensor_tensor(out=hf[:], in0=hf[:], in1=hb_[:], op=ALU.add),
                          [hfb, hbb], [hfb])
                    sm4, sm4b = smallr.next()
                    S.add("dve", lambda e, hb_=hb_, hf=hf: e.tensor_tensor(out=hb_[:], in0=hf[:], in1=hf[:], op=ALU.mult),
                          [hfb, hbb], [hbb])
                    S.add("dve", lambda e, hb_=hb_, sm4=sm4: e.tensor_reduce(
                        out=sm4[:, 0:4], in_=hb_[:].rearrange("p (h e) -> p h e", e=128), axis=AX.X, op=ALU.add), [hbb], [sm4b])
                    S.add("act", lambda e, sm4=sm4: e.activation(out=sm4[:, 0:4], in_=sm4[:, 0:4], func=AF.Ln, bias=eps_sb[:, 0:1],
                                                                 scale=1.0 / 128.0), [sm4b, CB], [sm4b])
                    S.add("act", lambda e, sm4=sm4: e.activation(out=sm4[:, 0:4], in_=sm4[:, 0:4], func=AF.Exp, scale=-0.5),
                          [sm4b], [sm4b])
                    for h in range(4):
                        S.add("dve", lambda e, hf=hf, sm4=sm4, h=h: e.scalar_tensor_tensor(
                            out=hf[:, h * 128:(h + 1) * 128], in0=hf[:, h * 128:(h + 1) * 128], scalar=sm4[:, h:h + 1],
                            in1=gml_sb[:, h * 128:(h + 1) * 128], op0=ALU.mult, op1=ALU.mult), [hfb, sm4b, GB], [hfb])
                    y_, yb_ = ybr.next()
                    S.add("pool", lambda e, hf=hf, mo_=mo_, y_=y_: e.tensor_tensor(out=y_[:], in0=hf[:], in1=mo_[:], op=ALU.mult),
                          [hfb, mob], [yb_])
                    pt, ptb = psum[7], psb[7]
                    ptv = pt[:].bitcast(BF16)
                    for h in range(4):
                        S.add("pe", lambda e, y_=y_, h=h, ptv=ptv: e.transpose(
                            out=ptv[:, h * 128:(h + 1) * 128], in_=y_[:, h * 128:(h + 1) * 128], identity=ident[:]), [yb_, CB], [ptb])
                    yT_, yTb = yTr.next()
                    S.add("act", lambda e, yT_=yT_, ptv=ptv: e.copy(out=yT_[:].rearrange("p a b -> p (a b)"), in_=ptv[:, 0:512]),
                          [ptb], [yTb])
                    S.dma(ya_v[:, :, t0:t0 + 128], yT_[:], reads=[yTb], writes=[DB["ya"]], q="sp")

            for dr_ in (1, 0):
                order = [1, 0] + list(range(NB - 1, 1, -1)) if dr_ == 1 else list(range(NB))
                for c_ in order:
                    b_block(dr_, c_)
            S.barrier()

        with contextlib.ExitStack() as st:
            KT = sb(st, "KT", [128, T], BF16)
            VV = sb(st, "VV", [128, NB, 2, 65], BF16)
            sink_sb = sb(st, "sink_sb", [128, 2, 512], F32)
            KB_ = Buf("KT")
            S.dma(sink_sb[:], W["sink"], writes=[KB_])
            S.add("act", lambda e: e.activation(out=sink_sb[:], in_=sink_sb[:], func=AF.Exp), [KB_], [KB_])
            qr = Ring(nc, st, "c_q", [128, 4, 512], BF16, 2)
            ptr = Ring(nc, st, "c_pt", [128, 1024], BF16, 3)
            ostg = Ring(nc, st, "c_o", [64, 8, 512], BF16, 2)
            rdr = Ring(nc, st, "c_rd", [128, 512], F32, 2)
            bcr = Ring(nc, st, "c_bc", [64, 512], F32, 2)
            ci = [0]

            def cbank(lo, n):
                i = lo + ci[0] % n
                ci[0] += 1
                return psum[i], psb[i]

            for kind in ("win", "glb"):
                kT_src = wkT if kind == "win" else gkT
                vcol = 0 if kind == "win" else 128
                q_src = (wqT if kind == "win" else gqT).rearrange("(j p) t -> p j t", p=128)
                y_dst = ybT if kind == "win" else ycT
                ydb = DB["yb"] if kind == "win" else DB["yc"]
                for (t0, tt) in tiles:
                    S.dma(KT[:, t0:t0 + tt], kT_src[:, t0:t0 + tt], reads=[DB["proj"]], writes=[KB_])
                S.add("pool", lambda e: e.memset(VV[:], 1.0), writes=[KB_])
                for b0 in range(0, NB, 26):
                    b1 = min(NB, b0 + 26)
                    for g in range(2):
                        S.dma(VV[:, b0:b1, g, 0:64],
                              wgv_tm[b0 * 128:b1 * 128, vcol + g * 64:vcol + g * 64 + 64].rearrange("(b p) d -> p b d", p=128),
                              reads=[DB["proj"]], writes=[KB_], q="pool")
                groups = []
                for (t0, tt) in tiles:
                    if t0 == 0 and not ctx_out:
                        continue
                    nblk = tt // 128
                    for blk in range(nblk):
                        c = t0 // 128 + blk
                        if c < 2:
                            kbs = [0, 1]
                        elif kind == "glb":
                            kbs = list(range(NB))
                        else:
                            kbs = ([c - 1] if c - 1 >= 2 else []) + [c] + ([c + 1] if c + 1 < NB else []) + [0, 1]
                        groups.append(dict(t0=t0, tt=tt, blk=blk, c=c, kbs=kbs, tile_last=(blk == nblk - 1)))
                units = []
                for gi, G in enumerate(groups):
                    kbs = G["kbs"]
                    for i, kb_ in enumerate(kbs):
                        units.append(dict(gi=gi, kb=kb_, first=(i == 0), last=(i == len(kbs) - 1), idx=len(units)))
                tile_state = {}

                def get_tile(t0, tt, kind=kind, q_src=q_src, tile_state=tile_state):
                    if t0 not in tile_state:
                        q_, qb = qr.next()
                        S.dma(q_[:, :, 0:tt], q_src[:, :, t0:t0 + tt], reads=[DB["proj"]], writes=[qb])
                        os_, osb = ostg.next()
                        tile_state[t0] = (q_, qb, os_, osb)
                    return tile_state[t0]

                def emit_qk(u, kind=kind, groups=groups):
                    G = groups[u["gi"]]
                    blk, c, kb_ = G["blk"], G["c"], u["kb"]
                    q_, qb, os_, osb = get_tile(G["t0"], G["tt"])
                    slot = u["idx"] % 2
                    for g in range(2):
                        ps_ = psum[2 * slot + g]
                        S.add("pe", lambda e, ps_=ps_, kb_=kb_, g=g, q_=q_, blk=blk: e.matmul(
                            ps_.rearrange("p (r q) -> p r q", q=128), lhsT=KT[g * 64:(g + 1) * 64, kb_ * 128:(kb_ + 1) * 128],
                            rhs=q_[g * 64:(g + 1) * 64, :, blk * 128:(blk + 1) * 128], start=True, stop=True),
                            [KB_, qb], [psb[2 * slot + g]])
                    pt_, ptb_ = ptr.next()
                    S.add("act", lambda e, slot=slot, pt_=pt_: e.activation(
                        out=pt_[:, 0:1024], in_=psum2[slot][:, 0:1024], func=AF.Exp, scale=0.125),
                        [psb[2 * slot], psb[2 * slot + 1]], [ptb_])
                    msk = None
                    if kind == "win" and c >= 3 and kb_ == c - 1:
                        msk = m_ge
                    if kind == "win" and c >= 2 and kb_ == c + 1:
                        msk = m_le
                    if msk is not None:
                        S.add("dve", lambda e, pt_=pt_, msk=msk: e.tensor_tensor(
                            out=pt_[:, 0:1024].rearrange("p (r q) -> p r q", q=128),
                            in0=pt_[:, 0:1024].rearrange("p (r q) -> p r q", q=128),
                            in1=msk[:].unsqueeze(1).to_broadcast([128, 8, 128]), op=ALU.mult), [ptb_, CB], [ptb_])
                    u["pt"] = (pt_, ptb_)

                def emit_pv(u, kind=kind, groups=groups, y_dst=y_dst, ydb=ydb):
                    G = groups[u["gi"]]
                    blk, kb_ = G["blk"], u["kb"]
                    q_, qb, os_, osb = get_tile(G["t0"], G["tt"])
                    pt_, ptb_ = u["pt"]
                    for g in range(2):
                        po, pob = psum[4 + g], psb[4 + g]
                        S.add("pe", lambda e, po=po, kb_=kb_, g=g, pt_=pt_, st_=u["first"], sp_=u["last"]: e.matmul(
                            po[0:65, :], lhsT=VV[:, kb_, g, :], rhs=pt_[:, g * 512:(g + 1) * 512], start=st_, stop=sp_),
                            [KB_, ptb_], [pob])
                    if not u["last"]:
                        return
                    for g in range(2):
                        po, pob = psum[4 + g], psb[4 + g]
                        rd, rdb = rdr.next()
                        if kind == "win":
                            S.add("dve", lambda e, po=po, rd=rd, g=g: e.tensor_tensor(
                                out=rd[64:65, :], in0=po[64:65, :], in1=sink_sb[64:65, g, :], op=ALU.add), [pob, KB_], [rdb])
                            S.add("dve", lambda e, rd=rd: e.reciprocal(out=rd[64:65, :], in_=rd[64:65, :]), [rdb], [rdb])
                        else:
                            S.add("dve", lambda e, po=po, rd=rd: e.reciprocal(out=rd[64:65, :], in_=po[64:65, :]), [pob], [rdb])
                        pbc, pbcb = psum[6 + g], psb[6 + g]
                        S.add("pe", lambda e, pbc=pbc, rd=rd: e.matmul(
                            pbc[0:64, :], lhsT=ones_f[64:65, 0:64], rhs=rd[64:65, :], start=True, stop=True), [rdb, CB], [pbcb])
                        bc, bcb = bcr.next()
                        S.add("dve", lambda e, pbc=pbc, bc=bc: e.tensor_copy(out=bc[:], in_=pbc[0:64, :]), [pbcb], [bcb])
                        S.add("dve", lambda e, po=po, bc=bc, os_=os_, g=g, blk=blk: e.tensor_tensor(
                            out=os_[:, g * 4:(g + 1) * 4, blk * 128:(blk + 1) * 128],
                            in0=po[0:64, :].rearrange("p (r q) -> p r q", q=128),
                            in1=bc[:].rearrange("p (r q) -> p r q", q=128), op=ALU.mult), [pob, bcb], [osb])
                    if G["tile_last"]:
                        t0, tt = G["t0"], G["tt"]
                        S.dma(y_dst[:, :, t0:t0 + tt], os_[:, :, 0:tt], reads=[osb], writes=[ydb], q="pool")

                LOOK = 2
                for i in range(min(LOOK, len(units))):
                    emit_qk(units[i])
                for i, u in enumerate(units):
                    emit_pv(u)
                    if i + LOOK < len(units):
                        emit_qk(units[i + LOOK])
            S.barrier()

        with contextlib.ExitStack() as st:
            xr = Ring(nc, st, "e_x", [128, KC, 512], F32, 1)
            yar = Ring(nc, st, "e_ya", [128, 4, 512], BF16, 1)
            ybr2 = Ring(nc, st, "e_yb", [64, 8, 512], BF16, 1)
            ycr = Ring(nc, st, "e_yc", [64, 8, 512], BF16, 1)
            bgr = Ring(nc, st, "e_bg", [128, 24, 512], BF16, 1)
            mgr = Ring(nc, st, "e_mg", [128, KC, 512], BF16, 1)
            h2r = Ring(nc, st, "e_h2", [128, KC, 512], BF16, 1)
            sqr = Ring(nc, st, "e_sq", [128, KC, 512], BF16, 1)
            ur = Ring(nc, st, "e_u", [128, NFC, 512], BF16, 1)
            w0r = Ring(nc, st, "e_w0", [128, 4, 128], BF16, 3)
            w12r = Ring(nc, st, "e_w12", [64, 8, 128], BF16, 4)
            wor = Ring(nc, st, "e_wo", [128, KC, 128], BF16, 6)
            w2r = Ring(nc, st, "e_w2", [128, NFC, 128], BF16, 2)
            f32r = Ring(nc, st, "e_f", [128, 512], F32, 6)
            rsr = Ring(nc, st, "e_rs", [128, 512], F32, 2)
            fg_sb = sb(st, "fg_sb", [128, KC], F32)
            FGB = Buf("fing")
            S.dma(fg_sb[:], fing, writes=[FGB])
            ei = [0]

            def ebank(lo, n):
                i = lo + ei[0] % n
                ei[0] += 1
                return psum[i], psb[i]

            def rms(x_, xb, tt):
                sq_, sqb = sqr.next()
                S.add("act", lambda e: e.activation(out=sq_[:, :, 0:tt], in_=x_[:, :, 0:tt], func=AF.Square), [xb], [sqb])
                pm, pmb = psum[0], psb[0]
                for kc in range(KC):
                    S.add("pe", lambda e, kc=kc: e.matmul(pm[:, 0:tt], lhsT=onesm[:], rhs=sq_[:, kc, 0:tt],
                                                          start=(kc == 0), stop=(kc == KC - 1)), [sqb, CB], [pmb])
                rs_, rsb = rsr.next()
                S.add("act", lambda e: e.activation(out=rs_[:, 0:tt], in_=pm[:, 0:tt], func=AF.Ln, bias=eps_sb[:, 0:1], scale=1.0),
                      [pmb, CB], [rsb])
                S.add("act", lambda e: e.activation(out=rs_[:, 0:tt], in_=rs_[:, 0:tt], func=AF.Exp, scale=-0.5), [rsb], [rsb])
                return rs_, rsb

            dbg_mg = dscr(f"dbg_mg{l}", [D, T], BF16) if dbg else None
            dbg_x1 = dscr(f"dbg_x1{l}", [D, T], F32) if dbg else None

            def e_tile(t0, tt):
                wsel = 1 if t0 == 0 else 0
                x_, xb = xr.next()
                S.dma(x_[:, :, 0:tt], xin_v[:, :, t0:t0 + tt], reads=[DB["xs"]], writes=[xb])
                ya_, yab = yar.next()
                yb_, ybb = ybr2.next()
                yc_, ycb = ycr.next()
                bg_, bgb = bgr.next()
                S.dma(ya_[:, :, 0:tt], yaT.rearrange("(h d) t -> d h t", d=128)[:, :, t0:t0 + tt], reads=[DB["ya"]], writes=[yab])
                S.dma(yb_[:, :, 0:tt], ybT[:, :, t0:t0 + tt], reads=[DB["yb"]], writes=[ybb], q="pool")
                S.dma(yc_[:, :, 0:tt], ycT[:, :, t0:t0 + tt], reads=[DB["yc"]], writes=[ycb], q="pool")
                S.dma(bg_[:, :, 0:tt], bgT.rearrange("(c p) t -> p c t", p=128)[:, :, t0:t0 + tt], reads=[DB["proj"]], writes=[bgb])
                mg_, mgb = mgr.next()
                for oc in range(8):
                    wa, wab = w0r.next()
                    wb1, wb1b = w12r.next()
                    wb2, wb2b = w12r.next()
                    S.dma(wa[:].rearrange("p a b -> p (a b)"), b_wbr0[oc], reads=[DB["w"]], writes=[wab], q=wq())
                    S.dma(wb1[:].rearrange("p a b -> p (a b)"), b_wbr1[oc], reads=[DB["w"]], writes=[wb1b], q=wq())
                    S.dma(wb2[:].rearrange("p a b -> p (a b)"), b_wbr2[oc], reads=[DB["w"]], writes=[wb2b], q=wq())
                    p0, p0b = ebank(1, 3)
                    for k in range(4):
                        S.add("pe", lambda e, k=k, p0=p0, wa=wa: e.matmul(p0[:, 0:tt], lhsT=wa[:, k, :], rhs=ya_[:, k, 0:tt],
                                                                          start=(k == 0), stop=(k == 3)), [wab, yab], [p0b])
                    p1, p1b = ebank(1, 3)
                    for k in range(8):
                        S.add("pe", lambda e, k=k, p1=p1, wb1=wb1: e.matmul(p1[:, 0:tt], lhsT=wb1[:, k, :], rhs=yb_[:, k, 0:tt],
                                                                            start=(k == 0), stop=(k == 7)), [wb1b, ybb], [p1b])
                    p2, p2b = ebank(1, 3)
                    for k in range(8):
                        S.add("pe", lambda e, k=k, p2=p2, wb2=wb2: e.matmul(p2[:, 0:tt], lhsT=wb2[:, k, :], rhs=yc_[:, k, 0:tt],
                                                                            start=(k == 0), stop=(k == 7)), [wb2b, ycb], [p2b])
                    a0, a0b = f32r.next()
                    a1, a1b = f32r.next()
                    a2, a2b = f32r.next()
                    S.add("dve", lambda e, p0=p0, a0=a0, oc=oc: e.tensor_tensor(
                        out=a0[:, 0:tt], in0=p0[:, 0:tt], in1=bg_[:, oc, 0:tt], op=ALU.mult), [p0b, bgb], [a0b])
                    S.add("dve", lambda e, p1=p1, a1=a1, oc=oc: e.tensor_tensor(
                        out=a1[:, 0:tt], in0=p1[:, 0:tt], in1=bg_[:, 8 + oc, 0:tt], op=ALU.mult), [p1b, bgb], [a1b])
                    S.add("dve", lambda e, p2=p2, a2=a2, oc=oc: e.tensor_tensor(
                        out=a2[:, 0:tt], in0=p2[:, 0:tt], in1=bg_[:, 16 + oc, 0:tt], op=ALU.mult), [p2b, bgb], [a2b])
                    S.add("pool", lambda e, a0=a0, a1=a1: e.tensor_tensor(out=a0[:, 0:tt], in0=a0[:, 0:tt], in1=a1[:, 0:tt], op=ALU.add),
                          [a0b, a1b], [a0b])
                    S.add("pool", lambda e, a0=a0, a2=a2, oc=oc: e.tensor_tensor(
                        out=mg_[:, oc, 0:tt], in0=a0[:, 0:tt], in1=a2[:, 0:tt], op=ALU.add), [a0b, a2b], [mgb])
                if dbg:
                    S.dma(dbg_mg.rearrange("(k p) t -> p k t", p=128)[:, :, t0:t0 + tt], mg_[:, :, 0:tt], reads=[mgb])
                for oc in range(8):
                    w_, wb_ = wor.next()
                    S.dma(w_[:].rearrange("p a b -> p (a b)"), b_wo[oc], reads=[DB["w"]], writes=[wb_], q=wq())
                    po, pob = ebank(4, 2)
                    for k in range(KC):
                        S.add("pe", lambda e, k=k, po=po, w_=w_: e.matmul(po[:, 0:tt], lhsT=w_[:, k, :], rhs=mg_[:, k, 0:tt],
                                                                          start=(k == 0), stop=(k == KC - 1)), [wb_, mgb], [pob])
                    S.add("dve", lambda e, po=po, oc=oc: e.scalar_tensor_tensor(
                        out=x_[:, oc, 0:tt], in0=po[:, 0:tt], scalar=MOD[:, 16 + oc, wsel:wsel + 1], in1=x_[:, oc, 0:tt],
                        op0=ALU.mult, op1=ALU.add), [pob, MB, xb], [xb])
                if dbg:
                    S.dma(dbg_x1.rearrange("(k p) t -> p k t", p=128)[:, :, t0:t0 + tt], x_[:, :, 0:tt], reads=[xb], writes=[xb])
                rs_, rsb = rms(x_, xb, tt)
                h2, h2b = h2r.next()
                for kc in range(KC):
                    tm_, tmb = f32r.next()
                    S.add("dve" if kc % 2 == 0 else "pool", lambda e, kc=kc, tm_=tm_: e.tensor_tensor(
                        out=tm_[:, 0:tt], in0=x_[:, kc, 0:tt], in1=rs_[:, 0:tt], op=ALU.mult), [xb, rsb], [tmb])
                    S.add("act", lambda e, kc=kc, tm_=tm_: e.activation(
                        out=h2[:, kc, 0:tt], in_=tm_[:, 0:tt], func=AF.Identity,
                        scale=G2[:, kc, wsel:wsel + 1], bias=MOD[:, 24 + kc, wsel:wsel + 1]), [tmb, MB], [h2b])
                u_, ub = ur.next()
                for fc in range(NFC):
                    w1_, w1b = wor.next()
                    w3_, w3b = wor.next()
                    S.dma(w1_[:].rearrange("p a b -> p (a b)"), b_w1[fc], reads=[DB["w"]], writes=[w1b], q="act")
                    S.dma(w3_[:].rearrange("p a b -> p (a b)"), b_w3[fc], reads=[DB["w"]], writes=[w3b], q="sp")
                    pa, pab = ebank(6, 2)
                    for k in range(KC):
                        S.add("pe", lambda e, k=k, pa=pa, w1_=w1_: e.matmul(pa[:, 0:tt], lhsT=w1_[:, k, :], rhs=h2[:, k, 0:tt],
                                                                            start=(k == 0), stop=(k == KC - 1)), [w1b, h2b], [pab])
                    pb3, pb3b = ebank(6, 2)
                    for k in range(KC):
                        S.add("pe", lambda e, k=k, pb3=pb3, w3_=w3_: e.matmul(pb3[:, 0:tt], lhsT=w3_[:, k, :], rhs=h2[:, k, 0:tt],
                                                                              start=(k == 0), stop=(k == KC - 1)), [w3b, h2b], [pb3b])
                    sl, slb = f32r.next()
                    S.add("act", lambda e, pa=pa, sl=sl: e.activation(out=sl[:, 0:tt], in_=pa[:, 0:tt], func=AF.Silu), [pab], [slb])
                    S.add("dve", lambda e, pb3=pb3, sl=sl, fc=fc: e.tensor_tensor(
                        out=u_[:, fc, 0:tt], in0=pb3[:, 0:tt], in1=sl[:, 0:tt], op=ALU.mult), [pb3b, slb], [ub])
                for oc in range(8):
                    w_, wb_ = w2r.next()
                    S.dma(w_[:].rearrange("p a b -> p (a b)"), b_w2[oc], reads=[DB["w"]], writes=[wb_], q=wq())
                    po, pob = ebank(4, 2)
                    for k in range(NFC):
                        S.add("pe", lambda e, k=k, po=po, w_=w_: e.matmul(po[:, 0:tt], lhsT=w_[:, k, :], rhs=u_[:, k, 0:tt],
                                                                          start=(k == 0), stop=(k == NFC - 1)), [wb_, ub], [pob])
                    S.add("dve", lambda e, po=po, oc=oc: e.scalar_tensor_tensor(
                        out=x_[:, oc, 0:tt], in0=po[:, 0:tt], scalar=MOD[:, 40 + oc, wsel:wsel + 1], in1=x_[:, oc, 0:tt],
                        op0=ALU.mult, op1=ALU.add), [pob, MB, xb], [xb])
                if l < depth - 1:
                    S.dma(xs_v[:, :, t0:t0 + tt], x_[:, :, 0:tt], reads=[xb], writes=[DB["xs"]], q="sp")
                else:
                    rs2_, rsb2 = rms(x_, xb, tt)
                    for kc in range(KC):
                        S.add("dve", lambda e, kc=kc: e.scalar_tensor_tensor(
                            out=x_[:, kc, 0:tt], in0=x_[:, kc, 0:tt], scalar=fg_sb[:, kc:kc + 1], in1=rs2_[:, 0:tt],
                            op0=ALU.mult, op1=ALU.mult), [xb, rsb2, FGB], [xb])
                    S.dma(yT.rearrange("(k p) t -> p k t", p=128)[:, :, t0 - 256:t0 - 256 + tt], x_[:, :, 0:tt], reads=[xb], q="sp")

            for (t0_, tt_) in tiles:
                if t0_ == 0 and not ctx_out:
                    continue
                e_tile(t0_, tt_)
            S.barrier()
        lay.close()

    S.emit()
    return nc


def _swap_d(d):
    r = d % 32
    return d + 16 if r < 16 else d - 16


def _fm_cols():
    OFF = dict(mq=0, mk=512, mv=1024, mo=1536, ig=2048, fg=2056, wq=2064, wk=2576, wv=2704, gq=2832, gk=3344, gv=3472, bg=3600)
    chunks = []
    for h in range(4):
        chunks.append([OFF["mq"] + h * 128 + d for d in range(128)])
    for h in range(4):
        chunks.append([OFF["mk"] + h * 128 + d for d in range(128)])
    for pre in ("w", "g"):
        q0, k0 = OFF[pre + "q"], OFF[pre + "k"]
        for sw in (False, True):
            for j in range(4):
                cols = []
                for p in range(128):
                    head = j if p < 64 else 4 + j
                    d = p % 64
                    dd = _swap_d(d) if sw else d
                    cols.append(q0 + head * 64 + dd)
                chunks.append(cols)
        for sw in (False, True):
            cols = []
            for p in range(128):
                head = p // 64
                d = p % 64
                dd = _swap_d(d) if sw else d
                cols.append(k0 + head * 64 + dd)
            chunks.append(cols)
    for c in range(24):
        chunks.append([OFF["bg"] + c * 128 + p for p in range(128)])
    assert len(chunks) == NFM
    tm = (list(range(512, 1024)) + list(range(1024, 1536)) + list(range(1536, 2048)) + list(range(2704, 2832)) +
          list(range(3472, 3600)) + list(range(2048, 2064)))
    assert len(tm) == TMW
    return np.array(chunks), np.array(tm)


def _pack(w, kp):
    K, N = w.shape
    return np.ascontiguousarray(w.reshape(K // kp, kp, N // 128, 128).transpose(2, 1, 0, 3))


def _rope_tables(nlat):
    T = nlat + CTX
    cos = np.ones((128, T), np.float32)
    sin = np.zeros((128, T), np.float32)
    t = np.arange(nlat)
    row = (t // 64).astype(np.float32)
    col = (t % 64).astype(np.float32)
    half = 32
    inv = (10000.0 ** (-np.arange(0, half, 2, dtype=np.float32) / half)).astype(np.float32)
    for p in range(128):
        d = p % 64
        pos = row if d < 32 else col
        ang = (pos * inv[d % 16]).astype(np.float32)
        sign = -1.0 if (d % 32) < 16 else 1.0
        cos[p, CTX:] = np.cos(ang)
        sin[p, CTX:] = sign * np.sin(ang)
    return cos, sin


def prep_shared(inp, nlat, depth=DEPTH):
    fm, tm = _fm_cols()
    sh = {}
    cos, sin = _rope_tables(nlat)
    sh["cos"], sh["sins"] = cos, sin
    sh["fing"] = np.ascontiguousarray(np.asarray(inp["final_g"]).reshape(KC, 128).T)
    swp = np.array([_swap_d(p % 64) for p in range(128)])
    for l in range(depth):
        w_in = np.asarray(inp["w_in"][l])
        b_in = np.asarray(inp["b_in"][l])
        sh[f"wmod{l}"] = _pack(np.asarray(inp["w_mod"][l]), 128)
        sh[f"bmod{l}"] = np.ascontiguousarray(np.asarray(inp["b_mod"][l]).reshape(48, 128).T)
        sh[f"n1g{l}"] = np.ascontiguousarray(np.asarray(inp["norm1_g"][l]).reshape(KC, 128).T)
        sh[f"n2g{l}"] = np.ascontiguousarray(np.asarray(inp["norm2_g"][l]).reshape(KC, 128).T)
        wf = w_in[:, fm.reshape(-1)]
        sh[f"wfm{l}"] = _pack(wf, 128)
        sh[f"bfm{l}"] = np.ascontiguousarray(b_in[fm].T)
        sh[f"wtm{l}"] = np.ascontiguousarray(w_in[:, tm].reshape(KC, 128, TMW).transpose(1, 0, 2))
        sh[f"btm{l}"] = np.ascontiguousarray(np.broadcast_to(b_in[tm][None, :], (128, TMW)))
        sh[f"gml{l}"] = np.ascontiguousarray(np.broadcast_to(np.asarray(inp["ml_norm_g"][l]).reshape(1, 512), (128, 512)))
        snk = np.repeat(np.asarray(inp["win_sink"][l]), 128).reshape(1, 2, 512)
        sh[f"sink{l}"] = np.ascontiguousarray(np.broadcast_to(snk, (128, 2, 512)))
        for nm, key in (("qng", "qn_g"), ("kng", "kn_g")):
            g = np.asarray(inp[key][l])
            sh[f"{nm}{l}"] = np.ascontiguousarray(np.stack([g[np.arange(128) % 64], g[swp]], axis=1))
        w_br = np.asarray(inp["w_br"][l])
        sh[f"wbr0{l}"] = _pack(w_br[0], 128)
        sh[f"wbr1{l}"] = _pack(w_br[1], 64).transpose(0, 1, 2, 3).copy()
        sh[f"wbr2{l}"] = _pack(w_br[2], 64).copy()
        sh[f"wo{l}"] = _pack(np.asarray(inp["w_o"][l]), 128)
        sh[f"w1{l}"] = _pack(np.asarray(inp["w_ff1"][l]), 128)
        sh[f"w3{l}"] = _pack(np.asarray(inp["w_ff3"][l]), 128)
        sh[f"w2{l}"] = _pack(np.asarray(inp["w_ff2"][l]), 128)
    return {k: np.ascontiguousarray(v, dtype=np.float32) for k, v in sh.items()}


def prep_core(inp, b):
    x = np.asarray(inp["x"][b])
    ctx = np.asarray(inp["ctx"][b])
    xT0 = np.ascontiguousarray(np.concatenate([ctx, x], axis=0).T)
    cc = np.stack([np.asarray(inp["c"][b]), np.asarray(inp["c_ctx"])], axis=1)
    cT = np.ascontiguousarray(cc.reshape(KC, 128, 2).transpose(1, 0, 2))
    return {"xT0": xT0.astype(np.float32), "cT": cT.astype(np.float32)}


_CACHE = {}


def kernel(**inp):
    x = np.asarray(inp["x"])
    B, nlat, _ = x.shape
    depth = np.asarray(inp["w_in"]).shape[0]
    key = (nlat, depth)
    if key not in _CACHE:
        _CACHE[key] = build(nlat // 128, depth)
    nc = _CACHE[key]
    sh = prep_shared(inp, nlat, depth)
    in_maps = []
    for b in range(B):
        m = dict(sh)
        m.update(prep_core(inp, b))
        in_maps.append(m)
    res = run_bass_kernel_spmd(nc, in_maps, core_ids=list(range(B)))
    out = np.stack([np.ascontiguousarray(r["yT"].T) for r in res.results], axis=0)
    return out.astype(np.float32)
```

```python
import contextlib
import numpy as np
import ml_dtypes
import concourse.bass as bass
import concourse.mybir as mybir
from concourse.bass_utils import run_bass_kernel_spmd

F32 = mybir.dt.float32
BF16 = mybir.dt.bfloat16
AF = mybir.ActivationFunctionType
ALU = mybir.AluOpType
AX = mybir.AxisListType

D = 1024
KC = 8
DFF = 2816
NFC = 22
EPS = 1e-6
NFM = 52
TMW = 1808
DEPTH = 2
SEQ = 16384
CTX = 256


_ALLBUFS = []


class Buf:
    __slots__ = ("ws", "rs", "name")

    def __init__(self, name=""):
        self.ws = []
        self.rs = []
        self.name = name
        _ALLBUFS.append(self)


class Op:
    __slots__ = ("eng", "fn", "deps", "dma", "signal", "cnt", "idx")

    def __init__(self, eng, fn, dma):
        self.eng = eng
        self.fn = fn
        self.dma = dma
        self.deps = []
        self.signal = False
        self.cnt = 0
        self.idx = 0


ENGS = ("pe", "act", "dve", "pool", "sp")
EPOCH = 20000
NDMA = 8


class Sched:
    def __init__(self, nc, strict_same=True):
        self.nc = nc
        self.ops = {e: [] for e in ENGS}
        self.strict_same = strict_same
        self.pending_dma = {e: [] for e in ENGS}
        self.last_real = {e: None for e in ENGS}

    def add(self, eng, fn, reads=(), writes=(), dma=False, force=()):
        op = Op(eng, fn, dma)
        deps = []
        for b in reads:
            deps.extend(b.ws)
        acc = []
        for b in writes:
            a = dma and len(b.ws) > 0 and all(x.dma for x in b.ws) and not b.rs
            acc.append(a)
            if not a:
                deps.extend(b.ws)
                deps.extend(b.rs)
        for b in reads:
            b.rs.append(op)
        for b, a in zip(writes, acc):
            if a:
                b.ws.append(op)
            else:
                b.ws = [op]
                b.rs = []
        best = {}
        dmadeps = []
        seen = set()
        for d in list(deps) + list(force):
            if d is op or d is None:
                continue
            if d.dma:
                if id(d) not in seen:
                    seen.add(id(d))
                    dmadeps.append(d)
            else:
                if d.eng == eng and (eng == "pe" or not self.strict_same) and d not in force:
                    continue
                cur = best.get(d.eng)
                if cur is None or d.idx > cur.idx:
                    best[d.eng] = d
        op.deps = list(best.values()) + dmadeps
        for d in op.deps:
            d.signal = True
        op.idx = len(self.ops[eng])
        self.ops[eng].append(op)
        if dma:
            self.pending_dma[eng].append(op)
        elif fn is not None:
            self.last_real[eng] = op
        return op

    def dma(self, out, in_, reads=(), writes=(), q="sp"):
        return self.add(q, lambda e: e.dma_start(out=out, in_=in_), reads, writes, dma=True)

    def barrier(self):
        firsts = []
        for e in ENGS:
            f = [self.last_real[e]] + self.pending_dma[e]
            self.pending_dma[e] = []
            firsts.append(self.add(e, None, force=[x for x in f if x is not None]))
        for e in ENGS:
            self.add(e, None, force=firsts)
        for b in _ALLBUFS:
            b.ws = []
            b.rs = []

    def emit(self):
        nc = self.nc
        sems = {}
        for e in ENGS:
            n = 0
            k = 0
            for op in self.ops[e]:
                if op.dma:
                    op.cnt = k
                    k += 1
                elif op.signal:
                    op.cnt = n
                    n += 1
            nep = (n + EPOCH - 1) // EPOCH
            sems[e] = [nc.alloc_semaphore(name=f"c_{e}_{i}") for i in range(nep)]
            sems[e + "_dma"] = [nc.alloc_semaphore(name=f"d_{e}_{i}") for i in range(min(NDMA, k))]

        def run_engine(e, h):
            waited = {x: -1 for x in ENGS}
            dma_done = set()
            for op in self.ops[e]:
                for d in op.deps:
                    if d.dma:
                        key = (d.eng, d.cnt)
                        if key in dma_done:
                            continue
                        h.wait_ge(sems[d.eng + "_dma"][d.cnt % NDMA], 16 * (d.cnt // NDMA + 1))
                        dma_done.add(key)
                    else:
                        if waited[d.eng] >= d.cnt:
                            continue
                        h.wait_ge(sems[d.eng][d.cnt // EPOCH], d.cnt % EPOCH + 1)
                        waited[d.eng] = d.cnt
                if op.dma:
                    s = sems[e + "_dma"][op.cnt % NDMA]
                    if op.cnt >= NDMA:
                        key = (e, op.cnt - NDMA)
                        if key not in dma_done:
                            h.wait_ge(s, 16 * (op.cnt // NDMA))
                            dma_done.add(key)
                    op.fn(h).then_inc(s, 16)
                elif op.fn is None:
                    if op.signal:
                        h.sem_inc(sems[e][op.cnt // EPOCH], 1)
                else:
                    ins = op.fn(h)
                    if op.signal:
                        ins.then_inc(sems[e][op.cnt // EPOCH], 1)
            k = sum(1 for op in self.ops[e] if op.dma)
            for j in range(min(NDMA, k)):
                last = ((k - 1 - j) // NDMA) * NDMA + j
                if (e, last) not in dma_done:
                    h.wait_ge(sems[e + "_dma"][j], 16 * (last // NDMA + 1))

        with nc.Block() as block:
            @block.sync
            def _(h):
                run_engine("sp", h)

            @block.tensor
            def _(h):
                run_engine("pe", h)

            @block.scalar
            def _(h):
                run_engine("act", h)

            @block.vector
            def _(h):
                run_engine("dve", h)

            @block.gpsimd
            def _(h):
                run_engine("pool", h)


_UID = [0]


class Ring:
    def __init__(self, nc, stack, name, shape, dtype, n):
        _UID[0] += 1
        name = f"{name}u{_UID[0]}"
        self.t = [stack.enter_context(nc.sbuf_tensor(f"{name}_{i}", shape, dtype)) for i in range(n)]
        self.b = [Buf(f"{name}_{i}") for i in range(n)]
        self.i = 0

    def next(self):
        i = self.i
        self.i = (i + 1) % len(self.t)
        return self.t[i], self.b[i]


def build(NLB, depth=DEPTH, dbg=False):
    assert NLB % 4 == 0
    NB = NLB + 2
    T = NB * 128
    tiles = [(0, 256)] + [(256 + 512 * i, 512) for i in range(NLB // 4)]
    nc = bass.Bass("TRN2", target_bir_lowering=False)
    S = Sched(nc)

    def din(name, shape, dt=F32):
        return nc.dram_tensor(name, list(shape), dt, kind="ExternalInput").ap()

    def dscr(name, shape, dt):
        return nc.dram_tensor(name, list(shape), dt, kind=("ExternalOutput" if dbg else "Internal")).ap()

    xT0 = din("xT0", [D, T])
    cT = din("cT", [128, KC, 2])
    cosd = din("cos", [128, T])
    sind = din("sins", [128, T])
    fing = din("fing", [128, KC])
    L = []
    for l in range(depth):
        L.append(dict(
            wmod=din(f"wmod{l}", [48, 128, KC, 128]), bmod=din(f"bmod{l}", [128, 48]),
            n1g=din(f"n1g{l}", [128, KC]), n2g=din(f"n2g{l}", [128, KC]),
            wfm=din(f"wfm{l}", [NFM, 128, KC, 128]), bfm=din(f"bfm{l}", [128, NFM]),
            wtm=din(f"wtm{l}", [128, KC, TMW]), btm=din(f"btm{l}", [128, TMW]),
            gml=din(f"gml{l}", [128, 512]), sink=din(f"sink{l}", [128, 2, 512]),
            qng=din(f"qng{l}", [128, 2]), kng=din(f"kng{l}", [128, 2]),
            wbr0=din(f"wbr0{l}", [8, 128, 4, 128]), wbr1=din(f"wbr1{l}", [8, 64, 8, 128]),
            wbr2=din(f"wbr2{l}", [8, 64, 8, 128]), wo=din(f"wo{l}", [8, 128, KC, 128]),
            w1=din(f"w1{l}", [NFC, 128, KC, 128]), w3=din(f"w3{l}", [NFC, 128, KC, 128]),
            w2=din(f"w2{l}", [8, 128, NFC, 128]),
        ))
    yT = nc.dram_tensor("yT", [D, NLB * 128], F32, kind="ExternalOutput").ap()

    xs = dscr("xs", [D, T], F32)
    b_wmod = dscr("b_wmod", [48, 128, KC * 128], BF16)
    b_wfm = dscr("b_wfm", [NFM, 128, KC * 128], BF16)
    b_wtm = dscr("b_wtm", [128, KC * TMW], BF16)
    b_wbr0 = dscr("b_wbr0", [8, 128, 4 * 128], BF16)
    b_wbr1 = dscr("b_wbr1", [8, 64, 8 * 128], BF16)
    b_wbr2 = dscr("b_wbr2", [8, 64, 8 * 128], BF16)
    b_wo = dscr("b_wo", [8, 128, KC * 128], BF16)
    b_w1 = dscr("b_w1", [NFC, 128, KC * 128], BF16)
    b_w3 = dscr("b_w3", [NFC, 128, KC * 128], BF16)
    b_w2 = dscr("b_w2", [8, 128, NFC * 128], BF16)
    mqT = dscr("mqT", [512, T], BF16)
    mkT = dscr("mkT", [512, T], BF16)
    wqT = dscr("wqT", [512, T], BF16)
    wkT = dscr("wkT", [128, T], BF16)
    gqT = dscr("gqT", [512, T], BF16)
    gkT = dscr("gkT", [128, T], BF16)
    bgT = dscr("bgT", [3072, T], BF16)
    mk_tm = dscr("mk_tm", [T, 512], BF16)
    mv_tm = dscr("mv_tm", [T, 512], BF16)
    mo_tm = dscr("mo_tm", [T, 512], BF16)
    wgv_tm = dscr("wgv_tm", [T, 256], BF16)
    gat_tm = dscr("gat_tm", [T, 16], F32)
    hb_tm = dscr("hb_tm", [T, 512], F32)
    yaT = dscr("yaT", [512, T], BF16)
    ybT = dscr("ybT", [64, 8, T], BF16)
    ycT = dscr("ycT", [64, 8, T], BF16)
    DB = {k: Buf(k) for k in ["xs", "w", "proj", "hb", "ya", "yb", "yc"]}

    stack0 = contextlib.ExitStack()
    psum2 = [nc.alloc_psum_tensor(f"dbank{i}", [128, 1024], F32) for i in range(4)]
    psum = [psum2[i // 2][:, (i % 2) * 512:(i % 2 + 1) * 512] for i in range(8)]
    psb = [Buf(f"bank{i}") for i in range(8)]

    wq_i = [0]

    def wq():
        return "sp"

    def sb(stack, name, shape, dt):
        _UID[0] += 1
        return stack.enter_context(nc.sbuf_tensor(f"{name}u{_UID[0]}", shape, dt))

    c_f32 = sb(stack0, "c_f32", [128, 128], F32)
    ones_f = sb(stack0, "ones_f", [128, 128], F32)
    tri_f = sb(stack0, "tri_f", [128, 128], F32)
    tri_b = sb(stack0, "tri_b", [128, 128], F32)
    m_le = sb(stack0, "m_le", [128, 128], BF16)
    m_ge = sb(stack0, "m_ge", [128, 128], BF16)
    onesm = sb(stack0, "onesm", [128, 128], BF16)
    bd64 = sb(stack0, "bd64", [128, 128], BF16)
    ident = sb(stack0, "ident", [128, 128], BF16)
    eps_sb = sb(stack0, "eps_sb", [128, 1], F32)
    CB = Buf("consts")

    def mk_consts():
        S.add("pool", lambda e: e.memset(ones_f[:], 1.0), writes=[CB])
        S.add("pool", lambda e: e.memset(eps_sb[:], EPS), writes=[CB])
        S.add("pool", lambda e: e.memset(onesm[:], 1.0 / 1024.0), writes=[CB])
        S.add("pool", lambda e: e.affine_select(out=tri_f[:], in_=ones_f[:], pattern=[[1, 128]],
                                                compare_op=ALU.is_ge, fill=0.0, base=0, channel_multiplier=-1),
              reads=[CB], writes=[CB])
        S.add("pool", lambda e: e.affine_select(out=tri_b[:], in_=ones_f[:], pattern=[[-1, 128]],
                                                compare_op=ALU.is_ge, fill=0.0, base=0, channel_multiplier=1),
              reads=[CB], writes=[CB])
        S.add("pool", lambda e: e.tensor_copy(out=m_le[:], in_=tri_f[:]), reads=[CB], writes=[CB])
        S.add("pool", lambda e: e.tensor_copy(out=m_ge[:], in_=tri_b[:]), reads=[CB], writes=[CB])
        S.add("pool", lambda e: e.tensor_tensor(out=ident[:], in0=tri_f[:], in1=tri_b[:], op=ALU.mult),
              reads=[CB], writes=[CB])
        S.add("pool", lambda e: e.memset(bd64[:], 0.0), reads=[CB], writes=[CB])
        S.add("pool", lambda e: e.memset(bd64[0:64, 0:64], 1.0 / 64.0), reads=[CB], writes=[CB])
        S.add("pool", lambda e: e.memset(bd64[64:128, 64:128], 1.0 / 64.0), reads=[CB], writes=[CB])

    mk_consts()

    cast_i = [0]

    def cast_pack(stack, src, dst, nchunk, P, F):
        pieces = []
        for c in range(nchunk):
            f0 = 0
            while f0 < F:
                fw = min(2048, F - f0)
                pieces.append((c, f0, fw))
                f0 += fw
        for (c, f0, fw) in pieces:
            a, ab = cast_f.next()
            o, ob = cast_o.next()
            S.dma(a[0:P, 0:fw], srcflat(src, c)[:, f0:f0 + fw], reads=[], writes=[ab], q="sp")
            eng = ("dve", "pool", "act")[cast_i[0] % 3]
            cast_i[0] += 1
            if eng == "act":
                S.add("act", lambda e, a=a, o=o, fw=fw: e.copy(out=o[0:P, 0:fw], in_=a[0:P, 0:fw]), [ab], [ob])
            else:
                S.add(eng, lambda e, a=a, o=o, fw=fw: e.tensor_copy(out=o[0:P, 0:fw], in_=a[0:P, 0:fw]), [ab], [ob])
            S.dma(dst[c][:, f0:f0 + fw], o[0:P, 0:fw], reads=[ob], writes=[DB["w"]], q="pool")

    def srcflat(src, c):
        v = src[c]
        if len(v.shape) == 3:
            return v.rearrange("p a b -> p (a b)")
        return v

    for l in range(depth):
        W = L[l]
        ctx_out = l < depth - 1
        xin = xT0 if l == 0 else xs
        xin_v = xin.rearrange("(k p) t -> p k t", p=128)
        xs_v = xs.rearrange("(k p) t -> p k t", p=128)

        with contextlib.ExitStack() as st:
            cast_f = Ring(nc, st, "cast_f", [128, 2048], F32, 3)
            cast_o = Ring(nc, st, "cast_o", [128, 2048], BF16, 3)
            cast_pack(st, W["wmod"], b_wmod, 48, 128, KC * 128)
            cast_pack(st, W["wfm"], b_wfm, NFM, 128, KC * 128)
            cast_pack(st, W["wtm"].rearrange("p (o a) b -> o p a b", o=1), b_wtm.rearrange("(o p) f -> o p f", o=1), 1, 128, KC * TMW)
            cast_pack(st, W["wbr0"], b_wbr0, 8, 128, 4 * 128)
            cast_pack(st, W["wbr1"], b_wbr1, 8, 64, 8 * 128)
            cast_pack(st, W["wbr2"], b_wbr2, 8, 64, 8 * 128)
            cast_pack(st, W["wo"], b_wo, 8, 128, KC * 128)
            cast_pack(st, W["w1"], b_w1, NFC, 128, KC * 128)
            cast_pack(st, W["w3"], b_w3, NFC, 128, KC * 128)
            cast_pack(st, W["w2"], b_w2, 8, 128, NFC * 128)
            S.barrier()

        lay = contextlib.ExitStack()
        MOD = sb(lay, "MOD", [128, 48, 2], F32)
        G1 = sb(lay, "G1", [128, KC, 2], F32)
        G2 = sb(lay, "G2", [128, KC, 2], F32)
        MB = Buf("MOD")
        with contextlib.ExitStack() as st:
            c_sb = sb(st, "c_sb", [128, KC, 2], F32)
            sc = sb(st, "sc", [128, KC, 2], BF16)
            bm = sb(st, "bm", [128, 48], F32)
            ng = sb(st, "ng", [128, 2, KC], F32)
            wr = Ring(nc, st, "wmodc", [128, KC, 128], BF16, 4)
            tb = Buf("m_tmp")
            S.dma(c_sb[:], cT, writes=[tb])
            S.dma(bm[:], W["bmod"], writes=[tb])
            S.dma(ng[:, 0, :], W["n1g"], writes=[tb])
            S.dma(ng[:, 1, :], W["n2g"], writes=[tb])
            S.add("act", lambda e: e.activation(out=sc[:], in_=c_sb[:], func=AF.Silu), [tb], [tb])
            pm = psum[0]
            for mc in range(48):
                w_, wb = wr.next()
                S.dma(w_[:].rearrange("p a b -> p (a b)"), b_wmod[mc], reads=[DB["w"]], writes=[wb])
                for kc in range(KC):
                    S.add("pe", lambda e, w_=w_, kc=kc, mc=mc: e.matmul(
                        pm[:, 2 * mc:2 * mc + 2], lhsT=w_[:, kc, :], rhs=sc[:, kc, :], start=(kc == 0), stop=(kc == KC - 1)),
                        [wb, tb], [psb[0]])
            S.add("dve", lambda e: e.tensor_tensor(
                out=MOD[:], in0=pm[:, 0:96].rearrange("p (m w) -> p m w", w=2),
                in1=bm[:].unsqueeze(2).to_broadcast([128, 48, 2]), op=ALU.add), [psb[0], tb], [MB])
            S.add("dve", lambda e: e.scalar_tensor_tensor(
                out=G1[:], in0=MOD[:, 8:16, :], scalar=1.0, in1=ng[:, 0, :].unsqueeze(2).to_broadcast([128, KC, 2]),
                op0=ALU.add, op1=ALU.mult), [MB, tb], [MB])
            S.add("dve", lambda e: e.scalar_tensor_tensor(
                out=G2[:], in0=MOD[:, 32:40, :], scalar=1.0, in1=ng[:, 1, :].unsqueeze(2).to_broadcast([128, KC, 2]),
                op0=ALU.add, op1=ALU.mult), [MB, tb], [MB])
            if dbg and l == 0:
                dbg_mod = dscr("dbg_mod", [128, 96], F32)
                dbg_g1 = dscr("dbg_g1", [128, 16], F32)
                S.dma(dbg_mod, MOD[:].rearrange("p a b -> p (a b)"), reads=[MB])
                S.dma(dbg_g1, G1[:].rearrange("p a b -> p (a b)"), reads=[MB])
            S.barrier()

        with contextlib.ExitStack() as st:
            wtm_sb = sb(st, "wtm_sb", [128, KC, TMW], BF16)
            btm_sb = sb(st, "btm_sb", [128, TMW], F32)
            bfm_sb = sb(st, "bfm_sb", [128, NFM], F32)
            qng_sb = sb(st, "qng_sb", [128, 2], F32)
            kng_sb = sb(st, "kng_sb", [128, 2], F32)
            AB = Buf("a_const")
            S.dma(wtm_sb[:].rearrange("p a b -> p (a b)"), b_wtm, reads=[DB["w"]], writes=[AB])
            S.dma(btm_sb[:], W["btm"], writes=[AB])
            S.dma(bfm_sb[:], W["bfm"], writes=[AB])
            S.dma(qng_sb[:], W["qng"], writes=[AB])
            S.dma(kng_sb[:], W["kng"], writes=[AB])
            xr = Ring(nc, st, "a_x", [128, KC, 512], F32, 2)
            sqr = Ring(nc, st, "a_sq", [128, KC, 512], BF16, 1)
            hr = Ring(nc, st, "a_h", [128, KC, 512], BF16, 2)
            rsr = Ring(nc, st, "a_rs", [128, 512], F32, 2)
            tmpr = Ring(nc, st, "a_tmp", [128, 512], F32, 3)
            wcr = Ring(nc, st, "a_wc", [128, KC, 128], BF16, 6)
            csr = Ring(nc, st, "a_cs", [128, 2, 512], F32, 2)
            f32r = Ring(nc, st, "a_f", [128, 512], F32, 8)
            bfr = Ring(nc, st, "a_b", [128, 512], BF16, 6)
            tmo = Ring(nc, st, "a_tmo", [128, 512], BF16, 4)
            gto = Ring(nc, st, "a_gto", [128, 16], F32, 2)
            pi = [0]

            def pbank(lo, n):
                i = lo + pi[0] % n
                pi[0] += 1
                return psum[i], psb[i]

            def proj(c, h_, hb, tt):
                w_, wb = wcr.next()
                S.dma(w_[:].rearrange("p a b -> p (a b)"), b_wfm[c], reads=[DB["w"]], writes=[wb], q=wq())
                p_, pb = pbank(1, 4)
                for kc in range(KC):
                    S.add("pe", lambda e, w_=w_, kc=kc, p_=p_: e.matmul(
                        p_[:, 0:tt], lhsT=w_[:, kc, :], rhs=h_[:, kc, 0:tt], start=(kc == 0), stop=(kc == KC - 1)),
                        [wb, hb], [pb])
                return p_, pb

            def store(dst, row0, t0, tt, o_, ob):
                S.dma(dst[row0:row0 + 128, t0:t0 + tt], o_[:, 0:tt], reads=[ob], writes=[DB["proj"]], q="pool")

            dbg_h = dscr("dbg_h", [D, T], BF16) if (dbg and l == 0) else None

            def a_tile(t0, tt):
                wsel = 1 if t0 == 0 else 0
                x_, xb = xr.next()
                S.dma(x_[:, :, 0:tt], xin_v[:, :, t0:t0 + tt], reads=[DB["xs"]], writes=[xb])
                cs_, csb = csr.next()
                S.dma(cs_[:, 0, 0:tt], cosd[:, t0:t0 + tt], writes=[csb], q="pool")
                S.dma(cs_[:, 1, 0:tt], sind[:, t0:t0 + tt], writes=[csb], q="pool")
                sq_, sqb = sqr.next()
                S.add("act", lambda e, x_=x_, sq_=sq_: e.activation(out=sq_[:, :, 0:tt], in_=x_[:, :, 0:tt], func=AF.Square),
                      [xb], [sqb])
                pm, pmb = psum[0], psb[0]
                for kc in range(KC):
                    S.add("pe", lambda e, kc=kc, sq_=sq_: e.matmul(pm[:, 0:tt], lhsT=onesm[:], rhs=sq_[:, kc, 0:tt],
                                                                   start=(kc == 0), stop=(kc == KC - 1)), [sqb, CB], [pmb])
                rs_, rsb = rsr.next()
                S.add("act", lambda e, rs_=rs_: e.activation(out=rs_[:, 0:tt], in_=pm[:, 0:tt], func=AF.Ln, bias=eps_sb[:, 0:1], scale=1.0),
                      [pmb, CB], [rsb])
                S.add("act", lambda e, rs_=rs_: e.activation(out=rs_[:, 0:tt], in_=rs_[:, 0:tt], func=AF.Exp, scale=-0.5), [rsb], [rsb])
                h_, hb = hr.next()
                for kc in range(KC):
                    tm_, tmb = tmpr.next()
                    S.add("dve" if kc % 2 == 0 else "pool", lambda e, kc=kc, tm_=tm_, x_=x_, rs_=rs_: e.tensor_tensor(
                        out=tm_[:, 0:tt], in0=x_[:, kc, 0:tt], in1=rs_[:, 0:tt], op=ALU.mult), [xb, rsb], [tmb])
                    S.add("act", lambda e, kc=kc, tm_=tm_, h_=h_: e.activation(
                        out=h_[:, kc, 0:tt], in_=tm_[:, 0:tt], func=AF.Identity,
                        scale=G1[:, kc, wsel:wsel + 1], bias=MOD[:, kc, wsel:wsel + 1]), [tmb, MB], [hb])
                if dbg and l == 0:
                    S.dma(dbg_h.rearrange("(k p) t -> p k t", p=128)[:, :, t0:t0 + tt], h_[:, :, 0:tt], reads=[hb])
                for c in range(8):
                    p_, pb = proj(c, h_, hb, tt)
                    o_, ob = bfr.next()
                    S.add("act", lambda e, p_=p_, o_=o_, c=c: e.activation(
                        out=o_[:, 0:tt], in_=p_[:, 0:tt], func=AF.Identity, bias=bfm_sb[:, c:c + 1], scale=1.0), [pb, AB], [ob])
                    store(mqT if c < 4 else mkT, (c % 4) * 128, t0, tt, o_, ob)
                for (ca, cb_, dst, row0, norm) in (
                        [(8 + j, 12 + j, wqT, j * 128, None) for j in range(4)] + [(16, 17, wkT, 0, None)] +
                        [(18 + j, 22 + j, gqT, j * 128, qng_sb) for j in range(4)] + [(26, 27, gkT, 0, kng_sb)]):
                    pa, pab = proj(ca, h_, hb, tt)
                    pb2, pbb = proj(cb_, h_, hb, tt)
                    q1, q1b = f32r.next()
                    q2, q2b = f32r.next()
                    if norm is None:
                        S.add("dve", lambda e, pa=pa, q1=q1, ca=ca, cs_=cs_: e.scalar_tensor_tensor(
                            out=q1[:, 0:tt], in0=pa[:, 0:tt], scalar=bfm_sb[:, ca:ca + 1], in1=cs_[:, 0, 0:tt],
                            op0=ALU.add, op1=ALU.mult), [pab, AB, csb], [q1b])
                        S.add("dve", lambda e, pb2=pb2, q2=q2, cb_=cb_, cs_=cs_: e.scalar_tensor_tensor(
                            out=q2[:, 0:tt], in0=pb2[:, 0:tt], scalar=bfm_sb[:, cb_:cb_ + 1], in1=cs_[:, 1, 0:tt],
                            op0=ALU.add, op1=ALU.mult), [pbb, AB, csb], [q2b])
                        o_, ob = bfr.next()
                        S.add("pool", lambda e, q1=q1, q2=q2, o_=o_: e.tensor_tensor(
                            out=o_[:, 0:tt], in0=q1[:, 0:tt], in1=q2[:, 0:tt], op=ALU.add), [q1b, q2b], [ob])
                    else:
                        qa, qab = f32r.next()
                        qs, qsb = f32r.next()
                        S.add("act", lambda e, pa=pa, qa=qa, ca=ca: e.activation(
                            out=qa[:, 0:tt], in_=pa[:, 0:tt], func=AF.Identity, bias=bfm_sb[:, ca:ca + 1], scale=1.0),
                            [pab, AB], [qab])
                        S.add("act", lambda e, pb2=pb2, qs=qs, cb_=cb_: e.activation(
                            out=qs[:, 0:tt], in_=pb2[:, 0:tt], func=AF.Identity, bias=bfm_sb[:, cb_:cb_ + 1], scale=1.0),
                            [pbb, AB], [qsb])
                        s2, s2b = bfr.next()
                        S.add("pool", lambda e, qa=qa, s2=s2: e.tensor_tensor(
                            out=s2[:, 0:tt], in0=qa[:, 0:tt], in1=qa[:, 0:tt], op=ALU.mult), [qab], [s2b])
                        pn, pnb = pbank(5, 2)
                        S.add("pe", lambda e, pn=pn, s2=s2: e.matmul(pn[:, 0:tt], lhsT=bd64[:], rhs=s2[:, 0:tt],
                                                                    start=True, stop=True), [s2b, CB], [pnb])
                        rn, rnb = f32r.next()
                        S.add("act", lambda e, pn=pn, rn=rn: e.activation(
                            out=rn[:, 0:tt], in_=pn[:, 0:tt], func=AF.Ln, bias=eps_sb[:, 0:1], scale=1.0), [pnb, CB], [rnb])
                        S.add("act", lambda e, rn=rn: e.activation(out=rn[:, 0:tt], in_=rn[:, 0:tt], func=AF.Exp, scale=-0.5),
                              [rnb], [rnb])
                        S.add("dve", lambda e, qa=qa, q1=q1, cs_=cs_, norm=norm: e.scalar_tensor_tensor(
                            out=q1[:, 0:tt], in0=qa[:, 0:tt], scalar=norm[:, 0:1], in1=cs_[:, 0, 0:tt],
                            op0=ALU.mult, op1=ALU.mult), [qab, AB, csb], [q1b])
                        S.add("dve", lambda e, qs=qs, q2=q2, cs_=cs_, norm=norm: e.scalar_tensor_tensor(
                            out=q2[:, 0:tt], in0=qs[:, 0:tt], scalar=norm[:, 1:2], in1=cs_[:, 1, 0:tt],
                            op0=ALU.mult, op1=ALU.mult), [qsb, AB, csb], [q2b])
                        S.add("dve", lambda e, q1=q1, q2=q2: e.tensor_tensor(
                            out=q1[:, 0:tt], in0=q1[:, 0:tt], in1=q2[:, 0:tt], op=ALU.add), [q1b, q2b], [q1b])
                        o_, ob = bfr.next()
                        S.add("pool", lambda e, q1=q1, rn=rn, o_=o_: e.tensor_tensor(
                            out=o_[:, 0:tt], in0=q1[:, 0:tt], in1=rn[:, 0:tt], op=ALU.mult), [q1b, rnb], [ob])
                    store(dst, row0, t0, tt, o_, ob)
                for c in range(28, 52):
                    p_, pb = proj(c, h_, hb, tt)
                    o_, ob = bfr.next()
                    S.add("act", lambda e, p_=p_, o_=o_, c=c: e.activation(
                        out=o_[:, 0:tt], in_=p_[:, 0:tt], func=AF.Sigmoid, bias=bfm_sb[:, c:c + 1], scale=1.0), [pb, AB], [ob])
                    store(bgT, (c - 28) * 128, t0, tt, o_, ob)
                for blk in range(tt // 128):
                    r0 = t0 + blk * 128
                    for gi, (c0, cw, dst, dcol) in enumerate(((0, 512, mk_tm, 0), (512, 512, mv_tm, 0), (1024, 512, mo_tm, 0),
                                                              (1536, 256, wgv_tm, 0), (1792, 16, gat_tm, 0))):
                        p_, pb = pbank(5, 3)
                        for kc in range(KC):
                            S.add("pe", lambda e, p_=p_, kc=kc, c0=c0, cw=cw, blk=blk: e.matmul(
                                p_[:, 0:cw], lhsT=h_[:, kc, blk * 128:(blk + 1) * 128], rhs=wtm_sb[:, kc, c0:c0 + cw],
                                start=(kc == 0), stop=(kc == KC - 1)), [hb, AB], [pb])
                        if gi == 4:
                            o_, ob = gto.next()
                            S.add("dve", lambda e, p_=p_, o_=o_, c0=c0, cw=cw: e.tensor_tensor(
                                out=o_[:, 0:cw], in0=p_[:, 0:cw], in1=btm_sb[:, c0:c0 + cw], op=ALU.add), [pb, AB], [ob])
                        else:
                            o_, ob = tmo.next()
                            S.add("dve", lambda e, p_=p_, o_=o_, c0=c0, cw=cw: e.tensor_tensor(
                                out=o_[:, 0:cw], in0=p_[:, 0:cw], in1=btm_sb[:, c0:c0 + cw], op=ALU.add), [pb, AB], [ob])
                            if gi == 2:
                                S.add("act", lambda e, o_=o_, cw=cw: e.activation(out=o_[:, 0:cw], in_=o_[:, 0:cw], func=AF.Sigmoid),
                                      [ob], [ob])
                        S.dma(dst[r0:r0 + 128, 0:cw], o_[:, 0:cw], reads=[ob], writes=[DB["proj"]], q="pool")

            for (t0_, tt_) in tiles:
                a_tile(t0_, tt_)
            S.barrier()

        with contextlib.ExitStack() as st:
            E_st = sb(st, "E_st", [128, 8, 129], F32)
            C_bf = sb(st, "C_bf", [128, 8, 129], BF16)
            ebp = sb(st, "ebp", [128, 8], F32)
            gml_sb = sb(st, "gml_sb", [128, 512], F32)
            SB_ = [Buf(f"st{g}") for g in range(8)]
            EBP = Buf("ebp")
            GB = Buf("gml")
            S.dma(gml_sb[:], W["gml"], writes=[GB])
            S.add("pool", lambda e: e.memset(E_st[:], 0.0), writes=SB_)
            S.add("pool", lambda e: e.memset(C_bf[:], 0.0), writes=SB_)
            S.add("pool", lambda e: e.memset(ebp[:], 1.0), writes=[EBP])
            qTr = Ring(nc, st, "b_qT", [128, 4, 128], BF16, 3)
            kTr = Ring(nc, st, "b_kT", [128, 4, 128], BF16, 3)
            ktr = Ring(nc, st, "b_kt", [128, 512], BF16, 3)
            vtr = Ring(nc, st, "b_vt", [128, 4, 129], BF16, 3)
            gtr = Ring(nc, st, "b_gt", [128, 16], F32, 3)
            for (v_, vb_) in zip(vtr.t, vtr.b):
                S.add("pool", lambda e, v_=v_: e.memset(v_[:], 1.0), writes=[vb_])
            smr = Ring(nc, st, "b_sm", [128, 128], BF16, 4)
            vur = Ring(nc, st, "b_vu", [128, 129], BF16, 4)
            g1r = Ring(nc, st, "b_g1", [128, 32], F32, 3)
            smallr = Ring(nc, st, "b_sml", [128, 4], F32, 8)
            hfr = Ring(nc, st, "b_hf", [128, 512], F32, 3)
            hbr = Ring(nc, st, "b_hb", [128, 512], F32, 2)
            mor = Ring(nc, st, "b_mo", [128, 512], BF16, 2)
            ybr = Ring(nc, st, "b_y", [128, 512], BF16, 2)
            yTr = Ring(nc, st, "b_yT", [128, 4, 128], BF16, 2)
            mq_v = mqT.rearrange("(h d) t -> d h t", d=128)
            mk_v = mkT.rearrange("(h d) t -> d h t", d=128)
            ya_v = yaT.rearrange("(h d) t -> d h t", d=128)
            scale_ml = 128.0 ** -0.5
            bi = [0]

            def bbank(lo, n):
                i = lo + bi[0] % n
                bi[0] += 1
                return psum[i], psb[i]

            def b_block(dr, c):
                    t0 = c * 128
                    need_out = ctx_out or c >= 2
                    q_, qb = qTr.next()
                    k_, kb = kTr.next()
                    kt_, ktb = ktr.next()
                    vt_, vtb = vtr.next()
                    gt_, gtb = gtr.next()
                    S.dma(q_[:], mq_v[:, :, t0:t0 + 128], reads=[DB["proj"]], writes=[qb])
                    S.dma(k_[:], mk_v[:, :, t0:t0 + 128], reads=[DB["proj"]], writes=[kb])
                    S.dma(kt_[:], mk_tm[t0:t0 + 128, :], reads=[DB["proj"]], writes=[ktb], q="pool")
                    S.dma(vt_[:, :, 0:128], mv_tm[t0:t0 + 128, :].rearrange("t (h e) -> t h e", e=128), reads=[DB["proj"]],
                          writes=[vtb], q="pool")
                    S.dma(gt_[:], gat_tm[t0:t0 + 128, :], reads=[DB["proj"]], writes=[gtb])
                    g1, g1b = g1r.next()
                    S.add("act", lambda e, gt_=gt_, g1=g1: e.activation(
                        out=g1[:, 16:20], in_=gt_[:, 8 + dr * 4:12 + dr * 4], func=AF.Exp, scale=-1.0), [gtb], [g1b])
                    S.add("act", lambda e, g1=g1: e.activation(out=g1[:, 0:4], in_=g1[:, 16:20], func=AF.Ln, bias=1.0, scale=1.0),
                          [g1b], [g1b])
                    S.add("dve", lambda e, g1=g1: e.tensor_scalar(out=g1[:, 0:4], in0=g1[:, 0:4], scalar1=-1.0, scalar2=None,
                                                                  op0=ALU.mult), [g1b], [g1b])
                    pg, pgb = psum[0], psb[0]
                    tri = tri_b if dr == 1 else tri_f
                    S.add("pe", lambda e, g1=g1, tri=tri: e.matmul(pg[:, 0:4], lhsT=tri[:], rhs=g1[:, 0:4], start=True, stop=True),
                          [g1b, CB], [pgb])
                    S.add("pe", lambda e, g1=g1: e.matmul(pg[:, 4:8], lhsT=ones_f[:], rhs=g1[:, 0:4], start=True, stop=True),
                          [g1b, CB], [pgb])
                    S.add("dve", lambda e, g1=g1, gt_=gt_: e.tensor_tensor(
                        out=g1[:, 16:20], in0=gt_[:, dr * 4:dr * 4 + 4], in1=pg[:, 0:4], op=ALU.subtract), [gtb, pgb, g1b], [g1b])
                    S.add("act", lambda e, g1=g1: e.activation(out=g1[:, 4:8], in_=g1[:, 16:20], func=AF.Exp), [g1b], [g1b])
                    S.add("act", lambda e, g1=g1: e.activation(out=g1[:, 8:16], in_=pg[:, 0:8], func=AF.Exp), [pgb, g1b], [g1b])
                    S.add("dve", lambda e, g1=g1: e.tensor_scalar(out=g1[:, 8:12], in0=g1[:, 8:12], scalar1=scale_ml, scalar2=None,
                                                                  op0=ALU.mult), [g1b], [g1b])
                    if need_out:
                        hf, hfb = hfr.next()
                    for h in range(4):
                        g = dr * 4 + h
                        ps_, psb_ = bbank(1, 2)
                        S.add("pe", lambda e, ps_=ps_, k_=k_, q_=q_, h=h: e.matmul(
                            ps_[:, 0:128], lhsT=k_[:, h, :], rhs=q_[:, h, :], start=True, stop=True), [kb, qb], [psb_])
                        sm_, smb = smr.next()
                        msk = m_ge if dr == 1 else m_le
                        S.add("dve", lambda e, ps_=ps_, sm_=sm_, msk=msk: e.tensor_tensor(
                            out=sm_[:], in0=ps_[:, 0:128], in1=msk[:], op=ALU.mult), [psb_, CB], [smb])
                        vu_, vub = vur.next()
                        S.add("act", lambda e, vu_=vu_, vt_=vt_, g1=g1, h=h: e.activation(
                            out=vu_[:], in_=vt_[:, h, :], func=AF.Identity, scale=g1[:, 4 + h:5 + h]), [vtb, g1b], [vub])
                        if need_out:
                            ph, phb = bbank(3, 2)
                            S.add("pe", lambda e, ph=ph, sm_=sm_, vu_=vu_: e.matmul(
                                ph[:, 0:129], lhsT=sm_[:], rhs=vu_[:], start=True, stop=False), [smb, vub], [phb])
                            S.add("pe", lambda e, ph=ph, q_=q_, h=h, g=g: e.matmul(
                                ph[:, 0:129], lhsT=q_[:, h, :], rhs=C_bf[:, g, :], start=False, stop=True), [qb, SB_[g]], [phb])
                            sm4, sm4b = smallr.next()
                            S.add("act", lambda e, ph=ph, sm4=sm4, g1=g1, h=h: e.activation(
                                out=sm4[:, 0:1], in_=ph[:, 128:129], func=AF.Abs, scale=g1[:, 8 + h:9 + h]), [phb, g1b], [sm4b])
                            S.add("dve", lambda e, sm4=sm4: e.tensor_scalar(
                                out=sm4[:, 0:1], in0=sm4[:, 0:1], scalar1=1.0, scalar2=None, op0=ALU.max), [sm4b], [sm4b])
                            S.add("dve", lambda e, sm4=sm4: e.reciprocal(out=sm4[:, 2:3], in_=sm4[:, 0:1]), [sm4b], [sm4b])
                            S.add("dve", lambda e, sm4=sm4, g1=g1, h=h: e.tensor_tensor(
                                out=sm4[:, 1:2], in0=g1[:, 8 + h:9 + h], in1=sm4[:, 2:3], op=ALU.mult), [sm4b, g1b], [sm4b])
                            S.add("act", lambda e, ph=ph, hf=hf, sm4=sm4, h=h: e.activation(
                                out=hf[:, h * 128:(h + 1) * 128], in_=ph[:, 0:128], func=AF.Identity, scale=sm4[:, 1:2]),
                                [phb, sm4b], [hfb])
                        pc, pcb = bbank(5, 2)
                        S.add("pe", lambda e, pc=pc, kt_=kt_, vu_=vu_, h=h: e.matmul(
                            pc[:, 0:129], lhsT=kt_[:, h * 128:(h + 1) * 128], rhs=vu_[:], start=True, stop=True), [ktb, vub], [pcb])
                        S.add("dve", lambda e, pc=pc, g=g: e.scalar_tensor_tensor(
                            out=E_st[:, g, :], in0=E_st[:, g, :], scalar=ebp[:, g:g + 1], in1=pc[:, 0:129],
                            op0=ALU.mult, op1=ALU.add), [pcb, EBP, SB_[g]], [SB_[g]])
                        S.add("pool", lambda e, g=g, g1=g1, h=h: e.tensor_scalar(
                            out=C_bf[:, g, :], in0=E_st[:, g, :], scalar1=g1[:, 12 + h:13 + h], scalar2=None, op0=ALU.mult),
                            [SB_[g], g1b], [SB_[g]])
                    S.add("dve", lambda e, g1=g1: e.tensor_copy(out=ebp[:, dr * 4:dr * 4 + 4], in_=g1[:, 12:16]),
                          [g1b, EBP] + SB_[dr * 4:dr * 4 + 4], [EBP])
                    if not need_out:
                        return
                    if dr == 1:
                        S.dma(hb_tm[t0:t0 + 128, :], hf[:], reads=[hfb], writes=[DB["hb"]], q="sp")
                        return
                    hb_, hbb = hbr.next()
                    mo_, mob = mor.next()
                    S.dma(hb_[:], hb_tm[t0:t0 + 128, :], reads=[DB["hb"]], writes=[hbb], q="pool")
                    S.dma(mo_[:], mo_tm[t0:t0 + 128, :], reads=[DB["proj"]], writes=[mob], q="pool")
                    S.add("pool", lambda e, hf=hf, hb_=hb_: e.tensor_tensor(out=hf[:], in0=hf[:], in1=hb_[:], op=ALU.add),
                          [hfb, hbb], [hfb])
                    sm4, sm4b = smallr.next()
                    S.add("dve", lambda e, hb_=hb_, hf=hf: e.tensor_tensor(out=hb_[:], in0=hf[:], in1=hf[:], op=ALU.mult),
                          [hfb, hbb], [hbb])
                    S.add("dve", lambda e, hb_=hb_, sm4=sm4: e.tensor_reduce(
                        out=sm4[:, 0:4], in_=hb_[:].rearrange("p (h e) -> p h e", e=128), axis=AX.X, op=ALU.add), [hbb], [sm4b])
                    S.add("act", lambda e, sm4=sm4: e.activation(out=sm4[:, 0:4], in_=sm4[:, 0:4], func=AF.Ln, bias=eps_sb[:, 0:1],
                                                                 scale=1.0 / 128.0), [sm4b, CB], [sm4b])
                    S.add("act", lambda e, sm4=sm4: e.activation(out=sm4[:, 0:4], in_=sm4[:, 0:4], func=AF.Exp, scale=-0.5),
                          [sm4b], [sm4b])
                    for h in range(4):
                        S.add("dve", lambda e, hf=hf, sm4=sm4, h=h: e.scalar_tensor_tensor(
                            out=hf[:, h * 128:(h + 1) * 128], in0=hf[:, h * 128:(h + 1) * 128], scalar=sm4[:, h:h + 1],
                            in1=gml_sb[:, h * 128:(h + 1) * 128], op0=ALU.mult, op1=ALU.mult), [hfb, sm4b, GB], [hfb])
                    y_, yb_ = ybr.next()
                    S.add("pool", lambda e, hf=hf, mo_=mo_, y_=y_: e.tensor_tensor(out=y_[:], in0=hf[:], in1=mo_[:], op=ALU.mult),
                          [hfb, mob], [yb_])
                    pt, ptb = psum[7], psb[7]
                    ptv = pt[:].bitcast(BF16)
                    for h in range(4):
                        S.add("pe", lambda e, y_=y_, h=h, ptv=ptv: e.transpose(
                            out=ptv[:, h * 128:(h + 1) * 128], in_=y_[:, h * 128:(h + 1) * 128], identity=ident[:]), [yb_, CB], [ptb])
                    yT_, yTb = yTr.next()
                    S.add("act", lambda e, yT_=yT_, ptv=ptv: e.copy(out=yT_[:].rearrange("p a b -> p (a b)"), in_=ptv[:, 0:512]),
                          [ptb], [yTb])
                    S.dma(ya_v[:, :, t0:t0 + 128], yT_[:], reads=[yTb], writes=[DB["ya"]], q="sp")

            for dr_ in (1, 0):
                order = [1, 0] + list(range(NB - 1, 1, -1)) if dr_ == 1 else list(range(NB))
                for c_ in order:
                    b_block(dr_, c_)
            S.barrier()

        with contextlib.ExitStack() as st:
            KT = sb(st, "KT", [128, T], BF16)
            VV = sb(st, "VV", [128, NB, 2, 65], BF16)
            sink_sb = sb(st, "sink_sb", [128, 2, 512], F32)
            KB_ = Buf("KT")
            S.dma(sink_sb[:], W["sink"], writes=[KB_])
            S.add("act", lambda e: e.activation(out=sink_sb[:], in_=sink_sb[:], func=AF.Exp), [KB_], [KB_])
            qr = Ring(nc, st, "c_q", [128, 4, 512], BF16, 2)
            ptr = Ring(nc, st, "c_pt", [128, 1024], BF16, 3)
            ostg = Ring(nc, st, "c_o", [64, 8, 512], BF16, 2)
            rdr = Ring(nc, st, "c_rd", [128, 512], F32, 2)
            bcr = Ring(nc, st, "c_bc", [64, 512], F32, 2)
            ci = [0]

            def cbank(lo, n):
                i = lo + ci[0] % n
                ci[0] += 1
                return psum[i], psb[i]

            for kind in ("win", "glb"):
                kT_src = wkT if kind == "win" else gkT
                vcol = 0 if kind == "win" else 128
                q_src = (wqT if kind == "win" else gqT).rearrange("(j p) t -> p j t", p=128)
                y_dst = ybT if kind == "win" else ycT
                ydb = DB["yb"] if kind == "win" else DB["yc"]
                for (t0, tt) in tiles:
                    S.dma(KT[:, t0:t0 + tt], kT_src[:, t0:t0 + tt], reads=[DB["proj"]], writes=[KB_])
                S.add("pool", lambda e: e.memset(VV[:], 1.0), writes=[KB_])
                for b0 in range(0, NB, 26):
                    b1 = min(NB, b0 + 26)
                    for g in range(2):
                        S.dma(VV[:, b0:b1, g, 0:64],
                              wgv_tm[b0 * 128:b1 * 128, vcol + g * 64:vcol + g * 64 + 64].rearrange("(b p) d -> p b d", p=128),
                              reads=[DB["proj"]], writes=[KB_], q="pool")
                groups = []
                for (t0, tt) in tiles:
                    if t0 == 0 and not ctx_out:
                        continue
                    nblk = tt // 128
                    for blk in range(nblk):
                        c = t0 // 128 + blk
                        if c < 2:
                            kbs = [0, 1]
                        elif kind == "glb":
                            kbs = list(range(NB))
                        else:
                            kbs = ([c - 1] if c - 1 >= 2 else []) + [c] + ([c + 1] if c + 1 < NB else []) + [0, 1]
                        groups.append(dict(t0=t0, tt=tt, blk=blk, c=c, kbs=kbs, tile_last=(blk == nblk - 1)))
                units = []
                for gi, G in enumerate(groups):
                    kbs = G["kbs"]
                    for i, kb_ in enumerate(kbs):
                        units.append(dict(gi=gi, kb=kb_, first=(i == 0), last=(i == len(kbs) - 1), idx=len(units)))
                tile_state = {}

                def get_tile(t0, tt, kind=kind, q_src=q_src, tile_state=tile_state):
                    if t0 not in tile_state:
                        q_, qb = qr.next()
                        S.dma(q_[:, :, 0:tt], q_src[:, :, t0:t0 + tt], reads=[DB["proj"]], writes=[qb])
                        os_, osb = ostg.next()
                        tile_state[t0] = (q_, qb, os_, osb)
                    return tile_state[t0]

                def emit_qk(u, kind=kind, groups=groups):
                    G = groups[u["gi"]]
                    blk, c, kb_ = G["blk"], G["c"], u["kb"]
                    q_, qb, os_, osb = get_tile(G["t0"], G["tt"])
                    slot = u["idx"] % 2
                    for g in range(2):
                        ps_ = psum[2 * slot + g]
                        S.add("pe", lambda e, ps_=ps_, kb_=kb_, g=g, q_=q_, blk=blk: e.matmul(
                            ps_.rearrange("p (r q) -> p r q", q=128), lhsT=KT[g * 64:(g + 1) * 64, kb_ * 128:(kb_ + 1) * 128],
                            rhs=q_[g * 64:(g + 1) * 64, :, blk * 128:(blk + 1) * 128], start=True, stop=True),
                            [KB_, qb], [psb[2 * slot + g]])
                    pt_, ptb_ = ptr.next()
                    S.add("act", lambda e, slot=slot, pt_=pt_: e.activation(
                        out=pt_[:, 0:1024], in_=psum2[slot][:, 0:1024], func=AF.Exp, scale=0.125),
                        [psb[2 * slot], psb[2 * slot + 1]], [ptb_])
                    msk = None
                    if kind == "win" and c >= 3 and kb_ == c - 1:
                        msk = m_ge
                    if kind == "win" and c >= 2 and kb_ == c + 1:
                        msk = m_le
                    if msk is not None:
                        S.add("dve", lambda e, pt_=pt_, msk=msk: e.tensor_tensor(
                            out=pt_[:, 0:1024].rearrange("p (r q) -> p r q", q=128),
                            in0=pt_[:, 0:1024].rearrange("p (r q) -> p r q", q=128),
                            in1=msk[:].unsqueeze(1).to_broadcast([128, 8, 128]), op=ALU.mult), [ptb_, CB], [ptb_])
                    u["pt"] = (pt_, ptb_)

                def emit_pv(u, kind=kind, groups=groups, y_dst=y_dst, ydb=ydb):
                    G = groups[u["gi"]]
                    blk, kb_ = G["blk"], u["kb"]
                    q_, qb, os_, osb = get_tile(G["t0"], G["tt"])
                    pt_, ptb_ = u["pt"]
                    for g in range(2):
                        po, pob = psum[4 + g], psb[4 + g]
                        S.add("pe", lambda e, po=po, kb_=kb_, g=g, pt_=pt_, st_=u["first"], sp_=u["last"]: e.matmul(
                            po[0:65, :], lhsT=VV[:, kb_, g, :], rhs=pt_[:, g * 512:(g + 1) * 512], start=st_, stop=sp_),
                            [KB_, ptb_], [pob])
                    if not u["last"]:
                        return
                    for g in range(2):
                        po, pob = psum[4 + g], psb[4 + g]
                        rd, rdb = rdr.next()
                        if kind == "win":
                            S.add("dve", lambda e, po=po, rd=rd, g=g: e.tensor_tensor(
                                out=rd[64:65, :], in0=po[64:65, :], in1=sink_sb[64:65, g, :], op=ALU.add), [pob, KB_], [rdb])
                            S.add("dve", lambda e, rd=rd: e.reciprocal(out=rd[64:65, :], in_=rd[64:65, :]), [rdb], [rdb])
                        else:
                            S.add("dve", lambda e, po=po, rd=rd: e.reciprocal(out=rd[64:65, :], in_=po[64:65, :]), [pob], [rdb])
                        pbc, pbcb = psum[6 + g], psb[6 + g]
                        S.add("pe", lambda e, pbc=pbc, rd=rd: e.matmul(
                            pbc[0:64, :], lhsT=ones_f[64:65, 0:64], rhs=rd[64:65, :], start=True, stop=True), [rdb, CB], [pbcb])
                        bc, bcb = bcr.next()
                        S.add("dve", lambda e, pbc=pbc, bc=bc: e.tensor_copy(out=bc[:], in_=pbc[0:64, :]), [pbcb], [bcb])
                        S.add("dve", lambda e, po=po, bc=bc, os_=os_, g=g, blk=blk: e.tensor_tensor(
                            out=os_[:, g * 4:(g + 1) * 4, blk * 128:(blk + 1) * 128],
                            in0=po[0:64, :].rearrange("p (r q) -> p r q", q=128),
                            in1=bc[:].rearrange("p (r q) -> p r q", q=128), op=ALU.mult), [pob, bcb], [osb])
                    if G["tile_last"]:
                        t0, tt = G["t0"], G["tt"]
                        S.dma(y_dst[:, :, t0:t0 + tt], os_[:, :, 0:tt], reads=[osb], writes=[ydb], q="pool")

                LOOK = 2
                for i in range(min(LOOK, len(units))):
                    emit_qk(units[i])
                for i, u in enumerate(units):
                    emit_pv(u)
                    if i + LOOK < len(units):
                        emit_qk(units[i + LOOK])
            S.barrier()

        with contextlib.ExitStack() as st:
            xr = Ring(nc, st, "e_x", [128, KC, 512], F32, 1)
            yar = Ring(nc, st, "e_ya", [128, 4, 512], BF16, 1)
            ybr2 = Ring(nc, st, "e_yb", [64, 8, 512], BF16, 1)
            ycr = Ring(nc, st, "e_yc", [64, 8, 512], BF16, 1)
            bgr = Ring(nc, st, "e_bg", [128, 24, 512], BF16, 1)
            mgr = Ring(nc, st, "e_mg", [128, KC, 512], BF16, 1)
            h2r = Ring(nc, st, "e_h2", [128, KC, 512], BF16, 1)
            sqr = Ring(nc, st, "e_sq", [128, KC, 512], BF16, 1)
            ur = Ring(nc, st, "e_u", [128, NFC, 512], BF16, 1)
            w0r = Ring(nc, st, "e_w0", [128, 4, 128], BF16, 3)
            w12r = Ring(nc, st, "e_w12", [64, 8, 128], BF16, 4)
            wor = Ring(nc, st, "e_wo", [128, KC, 128], BF16, 6)
            w2r = Ring(nc, st, "e_w2", [128, NFC, 128], BF16, 2)
            f32r = Ring(nc, st, "e_f", [128, 512], F32, 6)
            rsr = Ring(nc, st, "e_rs", [128, 512], F32, 2)
            fg_sb = sb(st, "fg_sb", [128, KC], F32)
            FGB = Buf("fing")
            S.dma(fg_sb[:], fing, writes=[FGB])
            ei = [0]

            def ebank(lo, n):
                i = lo + ei[0] % n
                ei[0] += 1
                return psum[i], psb[i]

            def rms(x_, xb, tt):
                sq_, sqb = sqr.next()
                S.add("act", lambda e: e.activation(out=sq_[:, :, 0:tt], in_=x_[:, :, 0:tt], func=AF.Square), [xb], [sqb])
                pm, pmb = psum[0], psb[0]
                for kc in range(KC):
                    S.add("pe", lambda e, kc=kc: e.matmul(pm[:, 0:tt], lhsT=onesm[:], rhs=sq_[:, kc, 0:tt],
                                                          start=(kc == 0), stop=(kc == KC - 1)), [sqb, CB], [pmb])
                rs_, rsb = rsr.next()
                S.add("act", lambda e: e.activation(out=rs_[:, 0:tt], in_=pm[:, 0:tt], func=AF.Ln, bias=eps_sb[:, 0:1], scale=1.0),
                      [pmb, CB], [rsb])
                S.add("act", lambda e: e.activation(out=rs_[:, 0:tt], in_=rs_[:, 0:tt], func=AF.Exp, scale=-0.5), [rsb], [rsb])
                return rs_, rsb

            dbg_mg = dscr(f"dbg_mg{l}", [D, T], BF16) if dbg else None
            dbg_x1 = dscr(f"dbg_x1{l}", [D, T], F32) if dbg else None

            def e_tile(t0, tt):
                wsel = 1 if t0 == 0 else 0
                x_, xb = xr.next()
                S.dma(x_[:, :, 0:tt], xin_v[:, :, t0:t0 + tt], reads=[DB["xs"]], writes=[xb])
                ya_, yab = yar.next()
                yb_, ybb = ybr2.next()
                yc_, ycb = ycr.next()
                bg_, bgb = bgr.next()
                S.dma(ya_[:, :, 0:tt], yaT.rearrange("(h d) t -> d h t", d=128)[:, :, t0:t0 + tt], reads=[DB["ya"]], writes=[yab], q="act")
                S.dma(yb_[:, :, 0:tt], ybT[:, :, t0:t0 + tt], reads=[DB["yb"]], writes=[ybb], q="pool")
                S.dma(yc_[:, :, 0:tt], ycT[:, :, t0:t0 + tt], reads=[DB["yc"]], writes=[ycb], q="pool")
                S.dma(bg_[:, :, 0:tt], bgT.rearrange("(c p) t -> p c t", p=128)[:, :, t0:t0 + tt], reads=[DB["proj"]], writes=[bgb], q="act")
                mg_, mgb = mgr.next()
                for oc in range(8):
                    wa, wab = w0r.next()
                    wb1, wb1b = w12r.next()
                    wb2, wb2b = w12r.next()
                    S.dma(wa[:].rearrange("p a b -> p (a b)"), b_wbr0[oc], reads=[DB["w"]], writes=[wab], q=wq())
                    S.dma(wb1[:].rearrange("p a b -> p (a b)"), b_wbr1[oc], reads=[DB["w"]], writes=[wb1b], q=wq())
                    S.dma(wb2[:].rearrange("p a b -> p (a b)"), b_wbr2[oc], reads=[DB["w"]], writes=[wb2b], q=wq())
                    p0, p0b = ebank(1, 3)
                    for k in range(4):
                        S.add("pe", lambda e, k=k, p0=p0, wa=wa: e.matmul(p0[:, 0:tt], lhsT=wa[:, k, :], rhs=ya_[:, k, 0:tt],
                                                                          start=(k == 0), stop=(k == 3)), [wab, yab], [p0b])
                    p1, p1b = ebank(1, 3)
                    for k in range(8):
                        S.add("pe", lambda e, k=k, p1=p1, wb1=wb1: e.matmul(p1[:, 0:tt], lhsT=wb1[:, k, :], rhs=yb_[:, k, 0:tt],
                                                                            start=(k == 0), stop=(k == 7)), [wb1b, ybb], [p1b])
                    p2, p2b = ebank(1, 3)
                    for k in range(8):
                        S.add("pe", lambda e, k=k, p2=p2, wb2=wb2: e.matmul(p2[:, 0:tt], lhsT=wb2[:, k, :], rhs=yc_[:, k, 0:tt],
                                                                            start=(k == 0), stop=(k == 7)), [wb2b, ycb], [p2b])
                    a0, a0b = f32r.next()
                    a1, a1b = f32r.next()
                    a2, a2b = f32r.next()
                    S.add("dve", lambda e, p0=p0, a0=a0, oc=oc: e.tensor_tensor(
                        out=a0[:, 0:tt], in0=p0[:, 0:tt], in1=bg_[:, oc, 0:tt], op=ALU.mult), [p0b, bgb], [a0b])
                    S.add("dve", lambda e, p1=p1, a1=a1, oc=oc: e.tensor_tensor(
                        out=a1[:, 0:tt], in0=p1[:, 0:tt], in1=bg_[:, 8 + oc, 0:tt], op=ALU.mult), [p1b, bgb], [a1b])
                    S.add("dve", lambda e, p2=p2, a2=a2, oc=oc: e.tensor_tensor(
                        out=a2[:, 0:tt], in0=p2[:, 0:tt], in1=bg_[:, 16 + oc, 0:tt], op=ALU.mult), [p2b, bgb], [a2b])
                    S.add("pool", lambda e, a0=a0, a1=a1: e.tensor_tensor(out=a0[:, 0:tt], in0=a0[:, 0:tt], in1=a1[:, 0:tt], op=ALU.add),
                          [a0b, a1b], [a0b])
                    S.add("pool", lambda e, a0=a0, a2=a2, oc=oc: e.tensor_tensor(
                        out=mg_[:, oc, 0:tt], in0=a0[:, 0:tt], in1=a2[:, 0:tt], op=ALU.add), [a0b, a2b], [mgb])
                if dbg:
                    S.dma(dbg_mg.rearrange("(k p) t -> p k t", p=128)[:, :, t0:t0 + tt], mg_[:, :, 0:tt], reads=[mgb])
                for oc in range(8):
                    w_, wb_ = wor.next()
                    S.dma(w_[:].rearrange("p a b -> p (a b)"), b_wo[oc], reads=[DB["w"]], writes=[wb_], q=wq())
                    po, pob = ebank(4, 2)
                    for k in range(KC):
                        S.add("pe", lambda e, k=k, po=po, w_=w_: e.matmul(po[:, 0:tt], lhsT=w_[:, k, :], rhs=mg_[:, k, 0:tt],
                                                                          start=(k == 0), stop=(k == KC - 1)), [wb_, mgb], [pob])
                    S.add("dve", lambda e, po=po, oc=oc: e.scalar_tensor_tensor(
                        out=x_[:, oc, 0:tt], in0=po[:, 0:tt], scalar=MOD[:, 16 + oc, wsel:wsel + 1], in1=x_[:, oc, 0:tt],
                        op0=ALU.mult, op1=ALU.add), [pob, MB, xb], [xb])
                if dbg:
                    S.dma(dbg_x1.rearrange("(k p) t -> p k t", p=128)[:, :, t0:t0 + tt], x_[:, :, 0:tt], reads=[xb], writes=[xb])
                rs_, rsb = rms(x_, xb, tt)
                h2, h2b = h2r.next()
                for kc in range(KC):
                    tm_, tmb = f32r.next()
                    S.add("dve" if kc % 2 == 0 else "pool", lambda e, kc=kc, tm_=tm_: e.tensor_tensor(
                        out=tm_[:, 0:tt], in0=x_[:, kc, 0:tt], in1=rs_[:, 0:tt], op=ALU.mult), [xb, rsb], [tmb])
                    S.add("act", lambda e, kc=kc, tm_=tm_: e.activation(
                        out=h2[:, kc, 0:tt], in_=tm_[:, 0:tt], func=AF.Identity,
                        scale=G2[:, kc, wsel:wsel + 1], bias=MOD[:, 24 + kc, wsel:wsel + 1]), [tmb, MB], [h2b])
                u_, ub = ur.next()
                for fc in range(NFC):
                    w1_, w1b = wor.next()
                    w3_, w3b = wor.next()
                    S.dma(w1_[:].rearrange("p a b -> p (a b)"), b_w1[fc], reads=[DB["w"]], writes=[w1b], q="sp")
                    S.dma(w3_[:].rearrange("p a b -> p (a b)"), b_w3[fc], reads=[DB["w"]], writes=[w3b], q="sp")
                    pa, pab = ebank(6, 2)
                    for k in range(KC):
                        S.add("pe", lambda e, k=k, pa=pa, w1_=w1_: e.matmul(pa[:, 0:tt], lhsT=w1_[:, k, :], rhs=h2[:, k, 0:tt],
                                                                            start=(k == 0), stop=(k == KC - 1)), [w1b, h2b], [pab])
                    pb3, pb3b = ebank(6, 2)
                    for k in range(KC):
                        S.add("pe", lambda e, k=k, pb3=pb3, w3_=w3_: e.matmul(pb3[:, 0:tt], lhsT=w3_[:, k, :], rhs=h2[:, k, 0:tt],
                                                                              start=(k == 0), stop=(k == KC - 1)), [w3b, h2b], [pb3b])
                    sl, slb = f32r.next()
                    S.add("act", lambda e, pa=pa, sl=sl: e.activation(out=sl[:, 0:tt], in_=pa[:, 0:tt], func=AF.Silu), [pab], [slb])
                    S.add("dve", lambda e, pb3=pb3, sl=sl, fc=fc: e.tensor_tensor(
                        out=u_[:, fc, 0:tt], in0=pb3[:, 0:tt], in1=sl[:, 0:tt], op=ALU.mult), [pb3b, slb], [ub])
                for oc in range(8):
                    w_, wb_ = w2r.next()
                    S.dma(w_[:].rearrange("p a b -> p (a b)"), b_w2[oc], reads=[DB["w"]], writes=[wb_], q=wq())
                    po, pob = ebank(4, 2)
                    for k in range(NFC):
                        S.add("pe", lambda e, k=k, po=po, w_=w_: e.matmul(po[:, 0:tt], lhsT=w_[:, k, :], rhs=u_[:, k, 0:tt],
                                                                          start=(k == 0), stop=(k == NFC - 1)), [wb_, ub], [pob])
                    S.add("dve", lambda e, po=po, oc=oc: e.scalar_tensor_tensor(
                        out=x_[:, oc, 0:tt], in0=po[:, 0:tt], scalar=MOD[:, 40 + oc, wsel:wsel + 1], in1=x_[:, oc, 0:tt],
                        op0=ALU.mult, op1=ALU.add), [pob, MB, xb], [xb])
                if l < depth - 1:
                    S.dma(xs_v[:, :, t0:t0 + tt], x_[:, :, 0:tt], reads=[xb], writes=[DB["xs"]], q="pool")
                else:
                    rs2_, rsb2 = rms(x_, xb, tt)
                    for kc in range(KC):
                        S.add("dve", lambda e, kc=kc: e.scalar_tensor_tensor(
                            out=x_[:, kc, 0:tt], in0=x_[:, kc, 0:tt], scalar=fg_sb[:, kc:kc + 1], in1=rs2_[:, 0:tt],
                            op0=ALU.mult, op1=ALU.mult), [xb, rsb2, FGB], [xb])
                    S.dma(yT.rearrange("(k p) t -> p k t", p=128)[:, :, t0 - 256:t0 - 256 + tt], x_[:, :, 0:tt], reads=[xb], q="pool")

            for (t0_, tt_) in tiles:
                if t0_ == 0 and not ctx_out:
                    continue
                e_tile(t0_, tt_)
            S.barrier()
        lay.close()

    S.emit()
    return nc


def _swap_d(d):
    r = d % 32
    return d + 16 if r < 16 else d - 16


def _fm_cols():
    OFF = dict(mq=0, mk=512, mv=1024, mo=1536, ig=2048, fg=2056, wq=2064, wk=2576, wv=2704, gq=2832, gk=3344, gv=3472, bg=3600)
    chunks = []
    for h in range(4):
        chunks.append([OFF["mq"] + h * 128 + d for d in range(128)])
    for h in range(4):
        chunks.append([OFF["mk"] + h * 128 + d for d in range(128)])
    for pre in ("w", "g"):
        q0, k0 = OFF[pre + "q"], OFF[pre + "k"]
        for sw in (False, True):
            for j in range(4):
                cols = []
                for p in range(128):
                    head = j if p < 64 else 4 + j
                    d = p % 64
                    dd = _swap_d(d) if sw else d
                    cols.append(q0 + head * 64 + dd)
                chunks.append(cols)
        for sw in (False, True):
            cols = []
            for p in range(128):
                head = p // 64
                d = p % 64
                dd = _swap_d(d) if sw else d
                cols.append(k0 + head * 64 + dd)
            chunks.append(cols)
    for c in range(24):
        chunks.append([OFF["bg"] + c * 128 + p for p in range(128)])
    assert len(chunks) == NFM
    tm = (list(range(512, 1024)) + list(range(1024, 1536)) + list(range(1536, 2048)) + list(range(2704, 2832)) +
          list(range(3472, 3600)) + list(range(2048, 2064)))
    assert len(tm) == TMW
    return np.array(chunks), np.array(tm)


def _pack(w, kp):
    K, N = w.shape
    return np.ascontiguousarray(w.reshape(K // kp, kp, N // 128, 128).transpose(2, 1, 0, 3))


def _rope_tables(nlat):
    T = nlat + CTX
    cos = np.ones((128, T), np.float32)
    sin = np.zeros((128, T), np.float32)
    t = np.arange(nlat)
    row = (t // 64).astype(np.float32)
    col = (t % 64).astype(np.float32)
    half = 32
    inv = (10000.0 ** (-np.arange(0, half, 2, dtype=np.float32) / half)).astype(np.float32)
    for p in range(128):
        d = p % 64
        pos = row if d < 32 else col
        ang = (pos * inv[d % 16]).astype(np.float32)
        sign = -1.0 if (d % 32) < 16 else 1.0
        cos[p, CTX:] = np.cos(ang)
        sin[p, CTX:] = sign * np.sin(ang)
    return cos, sin


def prep_shared(inp, nlat, depth=DEPTH):
    fm, tm = _fm_cols()
    sh = {}
    cos, sin = _rope_tables(nlat)
    sh["cos"], sh["sins"] = cos, sin
    sh["fing"] = np.ascontiguousarray(np.asarray(inp["final_g"]).reshape(KC, 128).T)
    swp = np.array([_swap_d(p % 64) for p in range(128)])
    for l in range(depth):
        w_in = np.asarray(inp["w_in"][l])
        b_in = np.asarray(inp["b_in"][l])
        sh[f"wmod{l}"] = _pack(np.asarray(inp["w_mod"][l]), 128)
        sh[f"bmod{l}"] = np.ascontiguousarray(np.asarray(inp["b_mod"][l]).reshape(48, 128).T)
        sh[f"n1g{l}"] = np.ascontiguousarray(np.asarray(inp["norm1_g"][l]).reshape(KC, 128).T)
        sh[f"n2g{l}"] = np.ascontiguousarray(np.asarray(inp["norm2_g"][l]).reshape(KC, 128).T)
        wf = w_in[:, fm.reshape(-1)]
        sh[f"wfm{l}"] = _pack(wf, 128)
        sh[f"bfm{l}"] = np.ascontiguousarray(b_in[fm].T)
        sh[f"wtm{l}"] = np.ascontiguousarray(w_in[:, tm].reshape(KC, 128, TMW).transpose(1, 0, 2))
        sh[f"btm{l}"] = np.ascontiguousarray(np.broadcast_to(b_in[tm][None, :], (128, TMW)))
        sh[f"gml{l}"] = np.ascontiguousarray(np.broadcast_to(np.asarray(inp["ml_norm_g"][l]).reshape(1, 512), (128, 512)))
        snk = np.repeat(np.asarray(inp["win_sink"][l]), 128).reshape(1, 2, 512)
        sh[f"sink{l}"] = np.ascontiguousarray(np.broadcast_to(snk, (128, 2, 512)))
        for nm, key in (("qng", "qn_g"), ("kng", "kn_g")):
            g = np.asarray(inp[key][l])
            sh[f"{nm}{l}"] = np.ascontiguousarray(np.stack([g[np.arange(128) % 64], g[swp]], axis=1))
        w_br = np.asarray(inp["w_br"][l])
        sh[f"wbr0{l}"] = _pack(w_br[0], 128)
        sh[f"wbr1{l}"] = _pack(w_br[1], 64).transpose(0, 1, 2, 3).copy()
        sh[f"wbr2{l}"] = _pack(w_br[2], 64).copy()
        sh[f"wo{l}"] = _pack(np.asarray(inp["w_o"][l]), 128)
        sh[f"w1{l}"] = _pack(np.asarray(inp["w_ff1"][l]), 128)
        sh[f"w3{l}"] = _pack(np.asarray(inp["w_ff3"][l]), 128)
        sh[f"w2{l}"] = _pack(np.asarray(inp["w_ff2"][l]), 128)
    return {k: np.ascontiguousarray(v, dtype=np.float32) for k, v in sh.items()}


def prep_core(inp, b):
    x = np.asarray(inp["x"][b])
    ctx = np.asarray(inp["ctx"][b])
    xT0 = np.ascontiguousarray(np.concatenate([ctx, x], axis=0).T)
    cc = np.stack([np.asarray(inp["c"][b]), np.asarray(inp["c_ctx"])], axis=1)
    cT = np.ascontiguousarray(cc.reshape(KC, 128, 2).transpose(1, 0, 2))
    return {"xT0": xT0.astype(np.float32), "cT": cT.astype(np.float32)}


_CACHE = {}


def kernel(**inp):
    x = np.asarray(inp["x"])
    B, nlat, _ = x.shape
    depth = np.asarray(inp["w_in"]).shape[0]
    key = (nlat, depth)
    if key not in _CACHE:
        _CACHE[key] = build(nlat // 128, depth)
    nc = _CACHE[key]
    sh = prep_shared(inp, nlat, depth)
    in_maps = []
    for b in range(B):
        m = dict(sh)
        m.update(prep_core(inp, b))
        in_maps.append(m)
    res = run_bass_kernel_spmd(nc, in_maps, core_ids=list(range(B)))
    out = np.stack([np.ascontiguousarray(r["yT"].T) for r in res.results], axis=0)
    return out.astype(np.float32)
```

```python
import contextlib
import numpy as np
import ml_dtypes
import concourse.bass as bass
import concourse.mybir as mybir
from concourse.bass_utils import run_bass_kernel_spmd

F32 = mybir.dt.float32
BF16 = mybir.dt.bfloat16
AF = mybir.ActivationFunctionType
ALU = mybir.AluOpType
AX = mybir.AxisListType

D = 1024
KC = 8
DFF = 2816
NFC = 22
EPS = 1e-6
NFM = 52
TMW = 1808
DEPTH = 2
SEQ = 16384
CTX = 256


_ALLBUFS = []


class Buf:
    __slots__ = ("ws", "rs", "name")

    def __init__(self, name=""):
        self.ws = []
        self.rs = []
        self.name = name
        _ALLBUFS.append(self)


class Op:
    __slots__ = ("eng", "fn", "deps", "dma", "signal", "cnt", "idx")

    def __init__(self, eng, fn, dma):
        self.eng = eng
        self.fn = fn
        self.dma = dma
        self.deps = []
        self.signal = False
        self.cnt = 0
        self.idx = 0


ENGS = ("pe", "act", "dve", "pool", "sp")
EPOCH = 20000
NDMA = 8


class Sched:
    def __init__(self, nc, strict_same=True):
        self.nc = nc
        self.ops = {e: [] for e in ENGS}
        self.strict_same = strict_same
        self.pending_dma = {e: [] for e in ENGS}
        self.last_real = {e: None for e in ENGS}

    def add(self, eng, fn, reads=(), writes=(), dma=False, force=()):
        op = Op(eng, fn, dma)
        deps = []
        for b in reads:
            deps.extend(b.ws)
        acc = []
        for b in writes:
            a = dma and len(b.ws) > 0 and all(x.dma for x in b.ws) and not b.rs
            acc.append(a)
            if not a:
                deps.extend(b.ws)
                deps.extend(b.rs)
        for b in reads:
            b.rs.append(op)
        for b, a in zip(writes, acc):
            if a:
                b.ws.append(op)
            else:
                b.ws = [op]
                b.rs = []
        best = {}
        dmadeps = []
        seen = set()
        for d in list(deps) + list(force):
            if d is op or d is None:
                continue
            if d.dma:
                if id(d) not in seen:
                    seen.add(id(d))
                    dmadeps.append(d)
            else:
                if d.eng == eng and (eng == "pe" or not self.strict_same) and d not in force:
                    continue
                cur = best.get(d.eng)
                if cur is None or d.idx > cur.idx:
                    best[d.eng] = d
        op.deps = list(best.values()) + dmadeps
        for d in op.deps:
            d.signal = True
        op.idx = len(self.ops[eng])
        self.ops[eng].append(op)
        if dma:
            self.pending_dma[eng].append(op)
        elif fn is not None:
            self.last_real[eng] = op
        return op

    def dma(self, out, in_, reads=(), writes=(), q="sp"):
        return self.add(q, lambda e: e.dma_start(out=out, in_=in_), reads, writes, dma=True)

    def barrier(self):
        firsts = []
        for e in ENGS:
            f = [self.last_real[e]] + self.pending_dma[e]
            self.pending_dma[e] = []
            firsts.append(self.add(e, None, force=[x for x in f if x is not None]))
        for e in ENGS:
            self.add(e, None, force=firsts)
        for b in _ALLBUFS:
            b.ws = []
            b.rs = []

    def emit(self):
        nc = self.nc
        sems = {}
        for e in ENGS:
            n = 0
            k = 0
            for op in self.ops[e]:
                if op.dma:
                    op.cnt = k
                    k += 1
                elif op.signal:
                    op.cnt = n
                    n += 1
            nep = (n + EPOCH - 1) // EPOCH
            sems[e] = [nc.alloc_semaphore(name=f"c_{e}_{i}") for i in range(nep)]
            sems[e + "_dma"] = [nc.alloc_semaphore(name=f"d_{e}_{i}") for i in range(min(NDMA, k))]

        def run_engine(e, h):
            waited = {x: -1 for x in ENGS}
            dma_done = set()
            for op in self.ops[e]:
                for d in op.deps:
                    if d.dma:
                        key = (d.eng, d.cnt)
                        if key in dma_done:
                            continue
                        h.wait_ge(sems[d.eng + "_dma"][d.cnt % NDMA], 16 * (d.cnt // NDMA + 1))
                        dma_done.add(key)
                    else:
                        if waited[d.eng] >= d.cnt:
                            continue
                        h.wait_ge(sems[d.eng][d.cnt // EPOCH], d.cnt % EPOCH + 1)
                        waited[d.eng] = d.cnt
                if op.dma:
                    s = sems[e + "_dma"][op.cnt % NDMA]
                    if op.cnt >= NDMA:
                        key = (e, op.cnt - NDMA)
                        if key not in dma_done:
                            h.wait_ge(s, 16 * (op.cnt // NDMA))
                            dma_done.add(key)
                    op.fn(h).then_inc(s, 16)
                elif op.fn is None:
                    if op.signal:
                        h.sem_inc(sems[e][op.cnt // EPOCH], 1)
                else:
                    ins = op.fn(h)
                    if op.signal:
                        ins.then_inc(sems[e][op.cnt // EPOCH], 1)
            k = sum(1 for op in self.ops[e] if op.dma)
            for j in range(min(NDMA, k)):
                last = ((k - 1 - j) // NDMA) * NDMA + j
                if (e, last) not in dma_done:
                    h.wait_ge(sems[e + "_dma"][j], 16 * (last // NDMA + 1))

        with nc.Block() as block:
            @block.sync
            def _(h):
                run_engine("sp", h)

            @block.tensor
            def _(h):
                run_engine("pe", h)

            @block.scalar
            def _(h):
                run_engine("act", h)

            @block.vector
            def _(h):
                run_engine("dve", h)

            @block.gpsimd
            def _(h):
                run_engine("pool", h)


_UID = [0]


class Ring:
    def __init__(self, nc, stack, name, shape, dtype, n):
        _UID[0] += 1
        name = f"{name}u{_UID[0]}"
        self.t = [stack.enter_context(nc.sbuf_tensor(f"{name}_{i}", shape, dtype)) for i in range(n)]
        self.b = [Buf(f"{name}_{i}") for i in range(n)]
        self.i = 0

    def next(self):
        i = self.i
        self.i = (i + 1) % len(self.t)
        return self.t[i], self.b[i]


def build(NLB, depth=DEPTH, dbg=False):
    assert NLB % 4 == 0
    NB = NLB + 2
    T = NB * 128
    tiles = [(0, 256)] + [(256 + 512 * i, 512) for i in range(NLB // 4)]
    nc = bass.Bass("TRN2", target_bir_lowering=False)
    S = Sched(nc)

    def din(name, shape, dt=F32):
        return nc.dram_tensor(name, list(shape), dt, kind="ExternalInput").ap()

    def dscr(name, shape, dt):
        return nc.dram_tensor(name, list(shape), dt, kind=("ExternalOutput" if dbg else "Internal")).ap()

    xT0 = din("xT0", [D, T])
    cT = din("cT", [128, KC, 2])
    cosd = din("cos", [128, T])
    sind = din("sins", [128, T])
    fing = din("fing", [128, KC])
    L = []
    for l in range(depth):
        L.append(dict(
            wmod=din(f"wmod{l}", [48, 128, KC, 128]), bmod=din(f"bmod{l}", [128, 48]),
            n1g=din(f"n1g{l}", [128, KC]), n2g=din(f"n2g{l}", [128, KC]),
            wfm=din(f"wfm{l}", [NFM, 128, KC, 128]), bfm=din(f"bfm{l}", [128, NFM]),
            wtm=din(f"wtm{l}", [128, KC, TMW]), btm=din(f"btm{l}", [128, TMW]),
            gml=din(f"gml{l}", [128, 512]), sink=din(f"sink{l}", [128, 2, 512]),
            qng=din(f"qng{l}", [128, 2]), kng=din(f"kng{l}", [128, 2]),
            wbr0=din(f"wbr0{l}", [8, 128, 4, 128]), wbr1=din(f"wbr1{l}", [8, 64, 8, 128]),
            wbr2=din(f"wbr2{l}", [8, 64, 8, 128]), wo=din(f"wo{l}", [8, 128, KC, 128]),
            w1=din(f"w1{l}", [NFC, 128, KC, 128]), w3=din(f"w3{l}", [NFC, 128, KC, 128]),
            w2=din(f"w2{l}", [8, 128, NFC, 128]),
        ))
    yT = nc.dram_tensor("yT", [D, NLB * 128], F32, kind="ExternalOutput").ap()

    xs = dscr("xs", [D, T], F32)
    b_wmod = dscr("b_wmod", [48, 128, KC * 128], BF16)
    b_wfm = dscr("b_wfm", [NFM, 128, KC * 128], BF16)
    b_wtm = dscr("b_wtm", [128, KC * TMW], BF16)
    b_wbr0 = dscr("b_wbr0", [8, 128, 4 * 128], BF16)
    b_wbr1 = dscr("b_wbr1", [8, 64, 8 * 128], BF16)
    b_wbr2 = dscr("b_wbr2", [8, 64, 8 * 128], BF16)
    b_wo = dscr("b_wo", [8, 128, KC * 128], BF16)
    b_w1 = dscr("b_w1", [NFC, 128, KC * 128], BF16)
    b_w3 = dscr("b_w3", [NFC, 128, KC * 128], BF16)
    b_w2 = dscr("b_w2", [8, 128, NFC * 128], BF16)
    mqT = dscr("mqT", [512, T], BF16)
    mkT = dscr("mkT", [512, T], BF16)
    wqT = dscr("wqT", [512, T], BF16)
    wkT = dscr("wkT", [128, T], BF16)
    gqT = dscr("gqT", [512, T], BF16)
    gkT = dscr("gkT", [128, T], BF16)
    bgT = dscr("bgT", [3072, T], BF16)
    mk_tm = dscr("mk_tm", [T, 512], BF16)
    mv_tm = dscr("mv_tm", [T, 512], BF16)
    mo_tm = dscr("mo_tm", [T, 512], BF16)
    wgv_tm = dscr("wgv_tm", [T, 256], BF16)
    gat_tm = dscr("gat_tm", [T, 16], F32)
    hb_tm = dscr("hb_tm", [T, 512], F32)
    yaT = dscr("yaT", [512, T], BF16)
    ybT = dscr("ybT", [64, 8, T], BF16)
    ycT = dscr("ycT", [64, 8, T], BF16)
    DB = {k: Buf(k) for k in ["xs", "w", "proj", "hb", "ya", "yb", "yc"]}

    stack0 = contextlib.ExitStack()
    psum2 = [nc.alloc_psum_tensor(f"dbank{i}", [128, 1024], F32) for i in range(4)]
    psum = [psum2[i // 2][:, (i % 2) * 512:(i % 2 + 1) * 512] for i in range(8)]
    psb = [Buf(f"bank{i}") for i in range(8)]

    wq_i = [0]

    def wq():
        return "sp"

    def sb(stack, name, shape, dt):
        _UID[0] += 1
        return stack.enter_context(nc.sbuf_tensor(f"{name}u{_UID[0]}", shape, dt))

    c_f32 = sb(stack0, "c_f32", [128, 128], F32)
    ones_f = sb(stack0, "ones_f", [128, 128], F32)
    tri_f = sb(stack0, "tri_f", [128, 128], F32)
    tri_b = sb(stack0, "tri_b", [128, 128], F32)
    m_le = sb(stack0, "m_le", [128, 128], BF16)
    m_ge = sb(stack0, "m_ge", [128, 128], BF16)
    onesm = sb(stack0, "onesm", [128, 128], BF16)
    bd64 = sb(stack0, "bd64", [128, 128], BF16)
    ident = sb(stack0, "ident", [128, 128], BF16)
    eps_sb = sb(stack0, "eps_sb", [128, 1], F32)
    CB = Buf("consts")

    def mk_consts():
        S.add("pool", lambda e: e.memset(ones_f[:], 1.0), writes=[CB])
        S.add("pool", lambda e: e.memset(eps_sb[:], EPS), writes=[CB])
        S.add("pool", lambda e: e.memset(onesm[:], 1.0 / 1024.0), writes=[CB])
        S.add("pool", lambda e: e.affine_select(out=tri_f[:], in_=ones_f[:], pattern=[[1, 128]],
                                                compare_op=ALU.is_ge, fill=0.0, base=0, channel_multiplier=-1),
              reads=[CB], writes=[CB])
        S.add("pool", lambda e: e.affine_select(out=tri_b[:], in_=ones_f[:], pattern=[[-1, 128]],
                                                compare_op=ALU.is_ge, fill=0.0, base=0, channel_multiplier=1),
              reads=[CB], writes=[CB])
        S.add("pool", lambda e: e.tensor_copy(out=m_le[:], in_=tri_f[:]), reads=[CB], writes=[CB])
        S.add("pool", lambda e: e.tensor_copy(out=m_ge[:], in_=tri_b[:]), reads=[CB], writes=[CB])
        S.add("pool", lambda e: e.tensor_tensor(out=ident[:], in0=tri_f[:], in1=tri_b[:], op=ALU.mult),
              reads=[CB], writes=[CB])
        S.add("pool", lambda e: e.memset(bd64[:], 0.0), reads=[CB], writes=[CB])
        S.add("pool", lambda e: e.memset(bd64[0:64, 0:64], 1.0 / 64.0), reads=[CB], writes=[CB])
        S.add("pool", lambda e: e.memset(bd64[64:128, 64:128], 1.0 / 64.0), reads=[CB], writes=[CB])

    mk_consts()

    cast_i = [0]

    def cast_pack(stack, src, dst, nchunk, P, F):
        pieces = []
        for c in range(nchunk):
            f0 = 0
            while f0 < F:
                fw = min(2048, F - f0)
                pieces.append((c, f0, fw))
                f0 += fw
        for (c, f0, fw) in pieces:
            a, ab = cast_f.next()
            o, ob = cast_o.next()
            S.dma(a[0:P, 0:fw], srcflat(src, c)[:, f0:f0 + fw], reads=[], writes=[ab], q="sp")
            eng = ("dve", "pool", "act")[cast_i[0] % 3]
            cast_i[0] += 1
            if eng == "act":
                S.add("act", lambda e, a=a, o=o, fw=fw: e.copy(out=o[0:P, 0:fw], in_=a[0:P, 0:fw]), [ab], [ob])
            else:
                S.add(eng, lambda e, a=a, o=o, fw=fw: e.tensor_copy(out=o[0:P, 0:fw], in_=a[0:P, 0:fw]), [ab], [ob])
            S.dma(dst[c][:, f0:f0 + fw], o[0:P, 0:fw], reads=[ob], writes=[DB["w"]], q="pool")

    def srcflat(src, c):
        v = src[c]
        if len(v.shape) == 3:
            return v.rearrange("p a b -> p (a b)")
        return v

    for l in range(depth):
        W = L[l]
        ctx_out = l < depth - 1
        xin = xT0 if l == 0 else xs
        xin_v = xin.rearrange("(k p) t -> p k t", p=128)
        xs_v = xs.rearrange("(k p) t -> p k t", p=128)

        with contextlib.ExitStack() as st:
            cast_f = Ring(nc, st, "cast_f", [128, 2048], F32, 3)
            cast_o = Ring(nc, st, "cast_o", [128, 2048], BF16, 3)
            cast_pack(st, W["wmod"], b_wmod, 48, 128, KC * 128)
            cast_pack(st, W["wfm"], b_wfm, NFM, 128, KC * 128)
            cast_pack(st, W["wtm"].rearrange("p (o a) b -> o p a b", o=1), b_wtm.rearrange("(o p) f -> o p f", o=1), 1, 128, KC * TMW)
            cast_pack(st, W["wbr0"], b_wbr0, 8, 128, 4 * 128)
            cast_pack(st, W["wbr1"], b_wbr1, 8, 64, 8 * 128)
            cast_pack(st, W["wbr2"], b_wbr2, 8, 64, 8 * 128)
            cast_pack(st, W["wo"], b_wo, 8, 128, KC * 128)
            cast_pack(st, W["w1"], b_w1, NFC, 128, KC * 128)
            cast_pack(st, W["w3"], b_w3, NFC, 128, KC * 128)
            cast_pack(st, W["w2"], b_w2, 8, 128, NFC * 128)
            S.barrier()

        lay = contextlib.ExitStack()
        MOD = sb(lay, "MOD", [128, 48, 2], F32)
        G1 = sb(lay, "G1", [128, KC, 2], F32)
        G2 = sb(lay, "G2", [128, KC, 2], F32)
        MB = Buf("MOD")
        with contextlib.ExitStack() as st:
            c_sb = sb(st, "c_sb", [128, KC, 2], F32)
            sc = sb(st, "sc", [128, KC, 2], BF16)
            bm = sb(st, "bm", [128, 48], F32)
            ng = sb(st, "ng", [128, 2, KC], F32)
            wr = Ring(nc, st, "wmodc", [128, KC, 128], BF16, 4)
            tb = Buf("m_tmp")
            S.dma(c_sb[:], cT, writes=[tb])
            S.dma(bm[:], W["bmod"], writes=[tb])
            S.dma(ng[:, 0, :], W["n1g"], writes=[tb])
            S.dma(ng[:, 1, :], W["n2g"], writes=[tb])
            S.add("act", lambda e: e.activation(out=sc[:], in_=c_sb[:], func=AF.Silu), [tb], [tb])
            pm = psum[0]
            for mc in range(48):
                w_, wb = wr.next()
                S.dma(w_[:].rearrange("p a b -> p (a b)"), b_wmod[mc], reads=[DB["w"]], writes=[wb])
                for kc in range(KC):
                    S.add("pe", lambda e, w_=w_, kc=kc, mc=mc: e.matmul(
                        pm[:, 2 * mc:2 * mc + 2], lhsT=w_[:, kc, :], rhs=sc[:, kc, :], start=(kc == 0), stop=(kc == KC - 1)),
                        [wb, tb], [psb[0]])
            S.add("dve", lambda e: e.tensor_tensor(
                out=MOD[:], in0=pm[:, 0:96].rearrange("p (m w) -> p m w", w=2),
                in1=bm[:].unsqueeze(2).to_broadcast([128, 48, 2]), op=ALU.add), [psb[0], tb], [MB])
            S.add("dve", lambda e: e.scalar_tensor_tensor(
                out=G1[:], in0=MOD[:, 8:16, :], scalar=1.0, in1=ng[:, 0, :].unsqueeze(2).to_broadcast([128, KC, 2]),
                op0=ALU.add, op1=ALU.mult), [MB, tb], [MB])
            S.add("dve", lambda e: e.scalar_tensor_tensor(
                out=G2[:], in0=MOD[:, 32:40, :], scalar=1.0, in1=ng[:, 1, :].unsqueeze(2).to_broadcast([128, KC, 2]),
                op0=ALU.add, op1=ALU.mult), [MB, tb], [MB])
            if dbg and l == 0:
                dbg_mod = dscr("dbg_mod", [128, 96], F32)
                dbg_g1 = dscr("dbg_g1", [128, 16], F32)
                S.dma(dbg_mod, MOD[:].rearrange("p a b -> p (a b)"), reads=[MB])
                S.dma(dbg_g1, G1[:].rearrange("p a b -> p (a b)"), reads=[MB])
            S.barrier()

        with contextlib.ExitStack() as st:
            wtm_sb = sb(st, "wtm_sb", [128, KC, TMW], BF16)
            btm_sb = sb(st, "btm_sb", [128, TMW], F32)
            bfm_sb = sb(st, "bfm_sb", [128, NFM], F32)
            qng_sb = sb(st, "qng_sb", [128, 2], F32)
            kng_sb = sb(st, "kng_sb", [128, 2], F32)
            AB = Buf("a_const")
            S.dma(wtm_sb[:].rearrange("p a b -> p (a b)"), b_wtm, reads=[DB["w"]], writes=[AB])
            S.dma(btm_sb[:], W["btm"], writes=[AB])
            S.dma(bfm_sb[:], W["bfm"], writes=[AB])
            S.dma(qng_sb[:], W["qng"], writes=[AB])
            S.dma(kng_sb[:], W["kng"], writes=[AB])
            xr = Ring(nc, st, "a_x", [128, KC, 512], F32, 2)
            sqr = Ring(nc, st, "a_sq", [128, KC, 512], BF16, 1)
            hr = Ring(nc, st, "a_h", [128, KC, 512], BF16, 2)
            rsr = Ring(nc, st, "a_rs", [128, 512], F32, 2)
            tmpr = Ring(nc, st, "a_tmp", [128, 512], F32, 3)
            wcr = Ring(nc, st, "a_wc", [128, KC, 128], BF16, 6)
            csr = Ring(nc, st, "a_cs", [128, 2, 512], F32, 2)
            f32r = Ring(nc, st, "a_f", [128, 512], F32, 8)
            bfr = Ring(nc, st, "a_b", [128, 512], BF16, 6)
            tmo = Ring(nc, st, "a_tmo", [128, 512], BF16, 4)
            gto = Ring(nc, st, "a_gto", [128, 16], F32, 2)
            pi = [0]

            def pbank(lo, n):
                i = lo + pi[0] % n
                pi[0] += 1
                return psum[i], psb[i]

            def proj(c, h_, hb, tt):
                w_, wb = wcr.next()
                S.dma(w_[:].rearrange("p a b -> p (a b)"), b_wfm[c], reads=[DB["w"]], writes=[wb], q=wq())
                p_, pb = pbank(1, 4)
                for kc in range(KC):
                    S.add("pe", lambda e, w_=w_, kc=kc, p_=p_: e.matmul(
                        p_[:, 0:tt], lhsT=w_[:, kc, :], rhs=h_[:, kc, 0:tt], start=(kc == 0), stop=(kc == KC - 1)),
                        [wb, hb], [pb])
                return p_, pb

            def store(dst, row0, t0, tt, o_, ob):
                S.dma(dst[row0:row0 + 128, t0:t0 + tt], o_[:, 0:tt], reads=[ob], writes=[DB["proj"]], q="pool")

            dbg_h = dscr("dbg_h", [D, T], BF16) if (dbg and l == 0) else None

            def a_tile(t0, tt):
                wsel = 1 if t0 == 0 else 0
                x_, xb = xr.next()
                S.dma(x_[:, :, 0:tt], xin_v[:, :, t0:t0 + tt], reads=[DB["xs"]], writes=[xb])
                cs_, csb = csr.next()
                S.dma(cs_[:, 0, 0:tt], cosd[:, t0:t0 + tt], writes=[csb], q="pool")
                S.dma(cs_[:, 1, 0:tt], sind[:, t0:t0 + tt], writes=[csb], q="pool")
                sq_, sqb = sqr.next()
                S.add("act", lambda e, x_=x_, sq_=sq_: e.activation(out=sq_[:, :, 0:tt], in_=x_[:, :, 0:tt], func=AF.Square),
                      [xb], [sqb])
                pm, pmb = psum[0], psb[0]
                for kc in range(KC):
                    S.add("pe", lambda e, kc=kc, sq_=sq_: e.matmul(pm[:, 0:tt], lhsT=onesm[:], rhs=sq_[:, kc, 0:tt],
                                                                   start=(kc == 0), stop=(kc == KC - 1)), [sqb, CB], [pmb])
                rs_, rsb = rsr.next()
                S.add("act", lambda e, rs_=rs_: e.activation(out=rs_[:, 0:tt], in_=pm[:, 0:tt], func=AF.Ln, bias=eps_sb[:, 0:1], scale=1.0),
                      [pmb, CB], [rsb])
                S.add("act", lambda e, rs_=rs_: e.activation(out=rs_[:, 0:tt], in_=rs_[:, 0:tt], func=AF.Exp, scale=-0.5), [rsb], [rsb])
                h_, hb = hr.next()
                for kc in range(KC):
                    tm_, tmb = tmpr.next()
                    S.add("dve" if kc % 2 == 0 else "pool", lambda e, kc=kc, tm_=tm_, x_=x_, rs_=rs_: e.tensor_tensor(
                        out=tm_[:, 0:tt], in0=x_[:, kc, 0:tt], in1=rs_[:, 0:tt], op=ALU.mult), [xb, rsb], [tmb])
                    S.add("act", lambda e, kc=kc, tm_=tm_, h_=h_: e.activation(
                        out=h_[:, kc, 0:tt], in_=tm_[:, 0:tt], func=AF.Identity,
                        scale=G1[:, kc, wsel:wsel + 1], bias=MOD[:, kc, wsel:wsel + 1]), [tmb, MB], [hb])
                if dbg and l == 0:
                    S.dma(dbg_h.rearrange("(k p) t -> p k t", p=128)[:, :, t0:t0 + tt], h_[:, :, 0:tt], reads=[hb])
                for c in range(8):
                    p_, pb = proj(c, h_, hb, tt)
                    o_, ob = bfr.next()
                    S.add("act", lambda e, p_=p_, o_=o_, c=c: e.activation(
                        out=o_[:, 0:tt], in_=p_[:, 0:tt], func=AF.Identity, bias=bfm_sb[:, c:c + 1], scale=1.0), [pb, AB], [ob])
                    store(mqT if c < 4 else mkT, (c % 4) * 128, t0, tt, o_, ob)
                for (ca, cb_, dst, row0, norm) in (
                        [(8 + j, 12 + j, wqT, j * 128, None) for j in range(4)] + [(16, 17, wkT, 0, None)] +
                        [(18 + j, 22 + j, gqT, j * 128, qng_sb) for j in range(4)] + [(26, 27, gkT, 0, kng_sb)]):
                    pa, pab = proj(ca, h_, hb, tt)
                    pb2, pbb = proj(cb_, h_, hb, tt)
                    q1, q1b = f32r.next()
                    q2, q2b = f32r.next()
                    if norm is None:
                        S.add("dve", lambda e, pa=pa, q1=q1, ca=ca, cs_=cs_: e.scalar_tensor_tensor(
                            out=q1[:, 0:tt], in0=pa[:, 0:tt], scalar=bfm_sb[:, ca:ca + 1], in1=cs_[:, 0, 0:tt],
                            op0=ALU.add, op1=ALU.mult), [pab, AB, csb], [q1b])
                        S.add("dve", lambda e, pb2=pb2, q2=q2, cb_=cb_, cs_=cs_: e.scalar_tensor_tensor(
                            out=q2[:, 0:tt], in0=pb2[:, 0:tt], scalar=bfm_sb[:, cb_:cb_ + 1], in1=cs_[:, 1, 0:tt],
                            op0=ALU.add, op1=ALU.mult), [pbb, AB, csb], [q2b])
                        o_, ob = bfr.next()
                        S.add("pool", lambda e, q1=q1, q2=q2, o_=o_: e.tensor_tensor(
                            out=o_[:, 0:tt], in0=q1[:, 0:tt], in1=q2[:, 0:tt], op=ALU.add), [q1b, q2b], [ob])
                    else:
                        qa, qab = f32r.next()
                        qs, qsb = f32r.next()
                        S.add("act", lambda e, pa=pa, qa=qa, ca=ca: e.activation(
                            out=qa[:, 0:tt], in_=pa[:, 0:tt], func=AF.Identity, bias=bfm_sb[:, ca:ca + 1], scale=1.0),
                            [pab, AB], [qab])
                        S.add("act", lambda e, pb2=pb2, qs=qs, cb_=cb_: e.activation(
                            out=qs[:, 0:tt], in_=pb2[:, 0:tt], func=AF.Identity, bias=bfm_sb[:, cb_:cb_ + 1], scale=1.0),
                            [pbb, AB], [qsb])
                        s2, s2b = bfr.next()
                        S.add("pool", lambda e, qa=qa, s2=s2: e.tensor_tensor(
                            out=s2[:, 0:tt], in0=qa[:, 0:tt], in1=qa[:, 0:tt], op=ALU.mult), [qab], [s2b])
                        pn, pnb = pbank(5, 2)
                        S.add("pe", lambda e, pn=pn, s2=s2: e.matmul(pn[:, 0:tt], lhsT=bd64[:], rhs=s2[:, 0:tt],
                                                                    start=True, stop=True), [s2b, CB], [pnb])
                        rn, rnb = f32r.next()
                        S.add("act", lambda e, pn=pn, rn=rn: e.activation(
                            out=rn[:, 0:tt], in_=pn[:, 0:tt], func=AF.Ln, bias=eps_sb[:, 0:1], scale=1.0), [pnb, CB], [rnb])
                        S.add("act", lambda e, rn=rn: e.activation(out=rn[:, 0:tt], in_=rn[:, 0:tt], func=AF.Exp, scale=-0.5),
                              [rnb], [rnb])
                        S.add("dve", lambda e, qa=qa, q1=q1, cs_=cs_, norm=norm: e.scalar_tensor_tensor(
                            out=q1[:, 0:tt], in0=qa[:, 0:tt], scalar=norm[:, 0:1], in1=cs_[:, 0, 0:tt],
                            op0=ALU.mult, op1=ALU.mult), [qab, AB, csb], [q1b])
                        S.add("dve", lambda e, qs=qs, q2=q2, cs_=cs_, norm=norm: e.scalar_tensor_tensor(
                            out=q2[:, 0:tt], in0=qs[:, 0:tt], scalar=norm[:, 1:2], in1=cs_[:, 1, 0:tt],
                            op0=ALU.mult, op1=ALU.mult), [qsb, AB, csb], [q2b])
                        S.add("dve", lambda e, q1=q1, q2=q2: e.tensor_tensor(
                            out=q1[:, 0:tt], in0=q1[:, 0:tt], in1=q2[:, 0:tt], op=ALU.add), [q1b, q2b], [q1b])
                        o_, ob = bfr.next()
                        S.add("pool", lambda e, q1=q1, rn=rn, o_=o_: e.tensor_tensor(
                            out=o_[:, 0:tt], in0=q1[:, 0:tt], in1=rn[:, 0:tt], op=ALU.mult), [q1b, rnb], [ob])
                    store(dst, row0, t0, tt, o_, ob)
                for c in range(28, 52):
                    p_, pb = proj(c, h_, hb, tt)
                    o_, ob = bfr.next()
                    S.add("act", lambda e, p_=p_, o_=o_, c=c: e.activation(
                        out=o_[:, 0:tt], in_=p_[:, 0:tt], func=AF.Sigmoid, bias=bfm_sb[:, c:c + 1], scale=1.0), [pb, AB], [ob])
                    store(bgT, (c - 28) * 128, t0, tt, o_, ob)
                for blk in range(tt // 128):
                    r0 = t0 + blk * 128
                    for gi, (c0, cw, dst, dcol) in enumerate(((0, 512, mk_tm, 0), (512, 512, mv_tm, 0), (1024, 512, mo_tm, 0),
                                                              (1536, 256, wgv_tm, 0), (1792, 16, gat_tm, 0))):
                        p_, pb = pbank(5, 3)
                        for kc in range(KC):
                            S.add("pe", lambda e, p_=p_, kc=kc, c0=c0, cw=cw, blk=blk: e.matmul(
                                p_[:, 0:cw], lhsT=h_[:, kc, blk * 128:(blk + 1) * 128], rhs=wtm_sb[:, kc, c0:c0 + cw],
                                start=(kc == 0), stop=(kc == KC - 1)), [hb, AB], [pb])
                        if gi == 4:
                            o_, ob = gto.next()
                            S.add("dve", lambda e, p_=p_, o_=o_, c0=c0, cw=cw: e.tensor_tensor(
                                out=o_[:, 0:cw], in0=p_[:, 0:cw], in1=btm_sb[:, c0:c0 + cw], op=ALU.add), [pb, AB], [ob])
                        else:
                            o_, ob = tmo.next()
                            S.add("dve", lambda e, p_=p_, o_=o_, c0=c0, cw=cw: e.tensor_tensor(
                                out=o_[:, 0:cw], in0=p_[:, 0:cw], in1=btm_sb[:, c0:c0 + cw], op=ALU.add), [pb, AB], [ob])
                            if gi == 2:
                                S.add("act", lambda e, o_=o_, cw=cw: e.activation(out=o_[:, 0:cw], in_=o_[:, 0:cw], func=AF.Sigmoid),
                                      [ob], [ob])
                        S.dma(dst[r0:r0 + 128, 0:cw], o_[:, 0:cw], reads=[ob], writes=[DB["proj"]], q="pool")

            for (t0_, tt_) in tiles:
                a_tile(t0_, tt_)
            S.barrier()

        with contextlib.ExitStack() as st:
            E_st = sb(st, "E_st", [128, 8, 129], F32)
            C_bf = sb(st, "C_bf", [128, 8, 129], BF16)
            ebp = sb(st, "ebp", [128, 8], F32)
            gml_sb = sb(st, "gml_sb", [128, 512], F32)
            SB_ = [Buf(f"st{g}") for g in range(8)]
            EBP = Buf("ebp")
            GB = Buf("gml")
            S.dma(gml_sb[:], W["gml"], writes=[GB])
            S.add("pool", lambda e: e.memset(E_st[:], 0.0), writes=SB_)
            S.add("pool", lambda e: e.memset(C_bf[:], 0.0), writes=SB_)
            S.add("pool", lambda e: e.memset(ebp[:], 1.0), writes=[EBP])
            qTr = Ring(nc, st, "b_qT", [128, 4, 128], BF16, 3)
            kTr = Ring(nc, st, "b_kT", [128, 4, 128], BF16, 3)
            ktr = Ring(nc, st, "b_kt", [128, 512], BF16, 3)
            vtr = Ring(nc, st, "b_vt", [128, 4, 129], BF16, 3)
            gtr = Ring(nc, st, "b_gt", [128, 16], F32, 3)
            for (v_, vb_) in zip(vtr.t, vtr.b):
                S.add("pool", lambda e, v_=v_: e.memset(v_[:], 1.0), writes=[vb_])
            smr = Ring(nc, st, "b_sm", [128, 128], BF16, 4)
            vur = Ring(nc, st, "b_vu", [128, 129], BF16, 4)
            g1r = Ring(nc, st, "b_g1", [128, 32], F32, 3)
            smallr = Ring(nc, st, "b_sml", [128, 4], F32, 8)
            hfr = Ring(nc, st, "b_hf", [128, 512], F32, 3)
            hbr = Ring(nc, st, "b_hb", [128, 512], F32, 2)
            mor = Ring(nc, st, "b_mo", [128, 512], BF16, 2)
            ybr = Ring(nc, st, "b_y", [128, 512], BF16, 2)
            yTr = Ring(nc, st, "b_yT", [128, 4, 128], BF16, 2)
            mq_v = mqT.rearrange("(h d) t -> d h t", d=128)
            mk_v = mkT.rearrange("(h d) t -> d h t", d=128)
            ya_v = yaT.rearrange("(h d) t -> d h t", d=128)
            scale_ml = 128.0 ** -0.5
            bi = [0]

            def bbank(lo, n):
                i = lo + bi[0] % n
                bi[0] += 1
                return psum[i], psb[i]

            def b_block(dr, c):
                    t0 = c * 128
                    need_out = ctx_out or c >= 2
                    q_, qb = qTr.next()
                    k_, kb = kTr.next()
                    kt_, ktb = ktr.next()
                    vt_, vtb = vtr.next()
                    gt_, gtb = gtr.next()
                    S.dma(q_[:], mq_v[:, :, t0:t0 + 128], reads=[DB["proj"]], writes=[qb])
                    S.dma(k_[:], mk_v[:, :, t0:t0 + 128], reads=[DB["proj"]], writes=[kb])
                    S.dma(kt_[:], mk_tm[t0:t0 + 128, :], reads=[DB["proj"]], writes=[ktb], q="sp")
                    S.dma(vt_[:, :, 0:128], mv_tm[t0:t0 + 128, :].rearrange("t (h e) -> t h e", e=128), reads=[DB["proj"]],
                          writes=[vtb], q="sp")
                    S.dma(gt_[:], gat_tm[t0:t0 + 128, :], reads=[DB["proj"]], writes=[gtb])
                    g1, g1b = g1r.next()
                    S.add("act", lambda e, gt_=gt_, g1=g1: e.activation(
                        out=g1[:, 16:20], in_=gt_[:, 8 + dr * 4:12 + dr * 4], func=AF.Exp, scale=-1.0), [gtb], [g1b])
                    S.add("act", lambda e, g1=g1: e.activation(out=g1[:, 0:4], in_=g1[:, 16:20], func=AF.Ln, bias=1.0, scale=1.0),
                          [g1b], [g1b])
                    S.add("dve", lambda e, g1=g1: e.tensor_scalar(out=g1[:, 0:4], in0=g1[:, 0:4], scalar1=-1.0, scalar2=None,
                                                                  op0=ALU.mult), [g1b], [g1b])
                    pg, pgb = psum[0], psb[0]
                    tri = tri_b if dr == 1 else tri_f
                    S.add("pe", lambda e, g1=g1, tri=tri: e.matmul(pg[:, 0:4], lhsT=tri[:], rhs=g1[:, 0:4], start=True, stop=True),
                          [g1b, CB], [pgb])
                    S.add("pe", lambda e, g1=g1: e.matmul(pg[:, 4:8], lhsT=ones_f[:], rhs=g1[:, 0:4], start=True, stop=True),
                          [g1b, CB], [pgb])
                    S.add("dve", lambda e, g1=g1, gt_=gt_: e.tensor_tensor(
                        out=g1[:, 16:20], in0=gt_[:, dr * 4:dr * 4 + 4], in1=pg[:, 0:4], op=ALU.subtract), [gtb, pgb, g1b], [g1b])
                    S.add("act", lambda e, g1=g1: e.activation(out=g1[:, 4:8], in_=g1[:, 16:20], func=AF.Exp), [g1b], [g1b])
                    S.add("act", lambda e, g1=g1: e.activation(out=g1[:, 8:16], in_=pg[:, 0:8], func=AF.Exp), [pgb, g1b], [g1b])
                    S.add("dve", lambda e, g1=g1: e.tensor_scalar(out=g1[:, 8:12], in0=g1[:, 8:12], scalar1=scale_ml, scalar2=None,
                                                                  op0=ALU.mult), [g1b], [g1b])
                    if need_out:
                        hf, hfb = hfr.next()
                    for h in range(4):
                        g = dr * 4 + h
                        ps_, psb_ = bbank(1, 2)
                        S.add("pe", lambda e, ps_=ps_, k_=k_, q_=q_, h=h: e.matmul(
                            ps_[:, 0:128], lhsT=k_[:, h, :], rhs=q_[:, h, :], start=True, stop=True), [kb, qb], [psb_])
                        sm_, smb = smr.next()
                        msk = m_ge if dr == 1 else m_le
                        S.add("dve", lambda e, ps_=ps_, sm_=sm_, msk=msk: e.tensor_tensor(
                            out=sm_[:], in0=ps_[:, 0:128], in1=msk[:], op=ALU.mult), [psb_, CB], [smb])
                        vu_, vub = vur.next()
                        S.add("act", lambda e, vu_=vu_, vt_=vt_, g1=g1, h=h: e.activation(
                            out=vu_[:], in_=vt_[:, h, :], func=AF.Identity, scale=g1[:, 4 + h:5 + h]), [vtb, g1b], [vub])
                        if need_out:
                            ph, phb = bbank(3, 2)
                            S.add("pe", lambda e, ph=ph, sm_=sm_, vu_=vu_: e.matmul(
                                ph[:, 0:129], lhsT=sm_[:], rhs=vu_[:], start=True, stop=False), [smb, vub], [phb])
                            S.add("pe", lambda e, ph=ph, q_=q_, h=h, g=g: e.matmul(
                                ph[:, 0:129], lhsT=q_[:, h, :], rhs=C_bf[:, g, :], start=False, stop=True), [qb, SB_[g]], [phb])
                            sm4, sm4b = smallr.next()
                            S.add("act", lambda e, ph=ph, sm4=sm4, g1=g1, h=h: e.activation(
                                out=sm4[:, 0:1], in_=ph[:, 128:129], func=AF.Abs, scale=g1[:, 8 + h:9 + h]), [phb, g1b], [sm4b])
                            S.add("dve", lambda e, sm4=sm4: e.tensor_scalar(
                                out=sm4[:, 0:1], in0=sm4[:, 0:1], scalar1=1.0, scalar2=None, op0=ALU.max), [sm4b], [sm4b])
                            S.add("dve", lambda e, sm4=sm4: e.reciprocal(out=sm4[:, 2:3], in_=sm4[:, 0:1]), [sm4b], [sm4b])
                            S.add("dve", lambda e, sm4=sm4, g1=g1, h=h: e.tensor_tensor(
                                out=sm4[:, 1:2], in0=g1[:, 8 + h:9 + h], in1=sm4[:, 2:3], op=ALU.mult), [sm4b, g1b], [sm4b])
                            S.add("act", lambda e, ph=ph, hf=hf, sm4=sm4, h=h: e.activation(
                                out=hf[:, h * 128:(h + 1) * 128], in_=ph[:, 0:128], func=AF.Identity, scale=sm4[:, 1:2]),
                                [phb, sm4b], [hfb])
                        pc, pcb = bbank(5, 2)
                        S.add("pe", lambda e, pc=pc, kt_=kt_, vu_=vu_, h=h: e.matmul(
                            pc[:, 0:129], lhsT=kt_[:, h * 128:(h + 1) * 128], rhs=vu_[:], start=True, stop=True), [ktb, vub], [pcb])
                        S.add("dve", lambda e, pc=pc, g=g: e.scalar_tensor_tensor(
                            out=E_st[:, g, :], in0=E_st[:, g, :], scalar=ebp[:, g:g + 1], in1=pc[:, 0:129],
                            op0=ALU.mult, op1=ALU.add), [pcb, EBP, SB_[g]], [SB_[g]])
                        S.add("pool", lambda e, g=g, g1=g1, h=h: e.tensor_scalar(
                            out=C_bf[:, g, :], in0=E_st[:, g, :], scalar1=g1[:, 12 + h:13 + h], scalar2=None, op0=ALU.mult),
                            [SB_[g], g1b], [SB_[g]])
                    S.add("dve", lambda e, g1=g1: e.tensor_copy(out=ebp[:, dr * 4:dr * 4 + 4], in_=g1[:, 12:16]),
                          [g1b, EBP] + SB_[dr * 4:dr * 4 + 4], [EBP])
                    if not need_out:
                        return
                    if dr == 1:
                        S.dma(hb_tm[t0:t0 + 128, :], hf[:], reads=[hfb], writes=[DB["hb"]], q="act")
                        return
                    hb_, hbb = hbr.next()
                    mo_, mob = mor.next()
                    S.dma(hb_[:], hb_tm[t0:t0 + 128, :], reads=[DB["hb"]], writes=[hbb], q="sp")
                    S.dma(mo_[:], mo_tm[t0:t0 + 128, :], reads=[DB["proj"]], writes=[mob], q="sp")
                    S.add("pool", lambda e, hf=hf, hb_=hb_: e.tensor_tensor(out=hf[:], in0=hf[:], in1=hb_[:], op=ALU.add),
                          [hfb, hbb], [hfb])
                    sm4, sm4b = smallr.next()
                    S.add("dve", lambda e, hb_=hb_, hf=hf: e.tensor_tensor(out=hb_[:], in0=hf[:], in1=hf[:], op=ALU.mult),
                          [hfb, hbb], [hbb])
                    S.add("dve", lambda e, hb_=hb_, sm4=sm4: e.tensor_reduce(
                        out=sm4[:, 0:4], in_=hb_[:].rearrange("p (h e) -> p h e", e=128), axis=AX.X, op=ALU.add), [hbb], [sm4b])
                    S.add("act", lambda e, sm4=sm4: e.activation(out=sm4[:, 0:4], in_=sm4[:, 0:4], func=AF.Ln, bias=eps_sb[:, 0:1],
                                                                 scale=1.0 / 128.0), [sm4b, CB], [sm4b])
                    S.add("act", lambda e, sm4=sm4: e.activation(out=sm4[:, 0:4], in_=sm4[:, 0:4], func=AF.Exp, scale=-0.5),
                          [sm4b], [sm4b])
                    for h in range(4):
                        S.add("dve", lambda e, hf=hf, sm4=sm4, h=h: e.scalar_tensor_tensor(
                            out=hf[:, h * 128:(h + 1) * 128], in0=hf[:, h * 128:(h + 1) * 128], scalar=sm4[:, h:h + 1],
                            in1=gml_sb[:, h * 128:(h + 1) * 128], op0=ALU.mult, op1=ALU.mult), [hfb, sm4b, GB], [hfb])
                    y_, yb_ = ybr.next()
                    S.add("pool", lambda e, hf=hf, mo_=mo_, y_=y_: e.tensor_tensor(out=y_[:], in0=hf[:], in1=mo_[:], op=ALU.mult),
                          [hfb, mob], [yb_])
                    pt, ptb = psum[7], psb[7]
                    ptv = pt[:].bitcast(BF16)
                    for h in range(4):
                        S.add("pe", lambda e, y_=y_, h=h, ptv=ptv: e.transpose(
                            out=ptv[:, h * 128:(h + 1) * 128], in_=y_[:, h * 128:(h + 1) * 128], identity=ident[:]), [yb_, CB], [ptb])
                    yT_, yTb = yTr.next()
                    S.add("act", lambda e, yT_=yT_, ptv=ptv: e.copy(out=yT_[:].rearrange("p a b -> p (a b)"), in_=ptv[:, 0:512]),
                          [ptb], [yTb])
                    S.dma(ya_v[:, :, t0:t0 + 128], yT_[:], reads=[yTb], writes=[DB["ya"]], q="act")

            for dr_ in (1, 0):
                order = [1, 0] + list(range(NB - 1, 1, -1)) if dr_ == 1 else list(range(NB))
                for c_ in order:
                    b_block(dr_, c_)
            S.barrier()

        with contextlib.ExitStack() as st:
            KT = sb(st, "KT", [128, T], BF16)
            VV = sb(st, "VV", [128, NB, 2, 65], BF16)
            sink_sb = sb(st, "sink_sb", [128, 2, 512], F32)
            KB_ = Buf("KT")
            S.dma(sink_sb[:], W["sink"], writes=[KB_])
            S.add("act", lambda e: e.activation(out=sink_sb[:], in_=sink_sb[:], func=AF.Exp), [KB_], [KB_])
            qr = Ring(nc, st, "c_q", [128, 4, 512], BF16, 2)
            ptr = Ring(nc, st, "c_pt", [128, 1024], BF16, 3)
            ostg = Ring(nc, st, "c_o", [64, 8, 512], BF16, 2)
            rdr = Ring(nc, st, "c_rd", [128, 512], F32, 2)
            bcr = Ring(nc, st, "c_bc", [64, 512], F32, 2)
            ci = [0]

            def cbank(lo, n):
                i = lo + ci[0] % n
                ci[0] += 1
                return psum[i], psb[i]

            for kind in ("win", "glb"):
                kT_src = wkT if kind == "win" else gkT
                vcol = 0 if kind == "win" else 128
                q_src = (wqT if kind == "win" else gqT).rearrange("(j p) t -> p j t", p=128)
                y_dst = ybT if kind == "win" else ycT
                ydb = DB["yb"] if kind == "win" else DB["yc"]
                for (t0, tt) in tiles:
                    S.dma(KT[:, t0:t0 + tt], kT_src[:, t0:t0 + tt], reads=[DB["proj"]], writes=[KB_])
                S.add("pool", lambda e: e.memset(VV[:], 1.0), writes=[KB_])
                for b0 in range(0, NB, 26):
                    b1 = min(NB, b0 + 26)
                    for g in range(2):
                        S.dma(VV[:, b0:b1, g, 0:64],
                              wgv_tm[b0 * 128:b1 * 128, vcol + g * 64:vcol + g * 64 + 64].rearrange("(b p) d -> p b d", p=128),
                              reads=[DB["proj"]], writes=[KB_], q="pool")
                groups = []
                for (t0, tt) in tiles:
                    if t0 == 0 and not ctx_out:
                        continue
                    nblk = tt // 128
                    for blk in range(nblk):
                        c = t0 // 128 + blk
                        if c < 2:
                            kbs = [0, 1]
                        elif kind == "glb":
                            kbs = list(range(NB))
                        else:
                            kbs = ([c - 1] if c - 1 >= 2 else []) + [c] + ([c + 1] if c + 1 < NB else []) + [0, 1]
                        groups.append(dict(t0=t0, tt=tt, blk=blk, c=c, kbs=kbs, tile_last=(blk == nblk - 1)))
                units = []
                for gi, G in enumerate(groups):
                    kbs = G["kbs"]
                    for i, kb_ in enumerate(kbs):
                        units.append(dict(gi=gi, kb=kb_, first=(i == 0), last=(i == len(kbs) - 1), idx=len(units)))
                tile_state = {}

                def get_tile(t0, tt, kind=kind, q_src=q_src, tile_state=tile_state):
                    if t0 not in tile_state:
                        q_, qb = qr.next()
                        S.dma(q_[:, :, 0:tt], q_src[:, :, t0:t0 + tt], reads=[DB["proj"]], writes=[qb])
                        os_, osb = ostg.next()
                        tile_state[t0] = (q_, qb, os_, osb)
                    return tile_state[t0]

                def emit_qk(u, kind=kind, groups=groups):
                    G = groups[u["gi"]]
                    blk, c, kb_ = G["blk"], G["c"], u["kb"]
                    q_, qb, os_, osb = get_tile(G["t0"], G["tt"])
                    slot = u["idx"] % 2
                    for g in range(2):
                        ps_ = psum[2 * slot + g]
                        S.add("pe", lambda e, ps_=ps_, kb_=kb_, g=g, q_=q_, blk=blk: e.matmul(
                            ps_.rearrange("p (r q) -> p r q", q=128), lhsT=KT[g * 64:(g + 1) * 64, kb_ * 128:(kb_ + 1) * 128],
                            rhs=q_[g * 64:(g + 1) * 64, :, blk * 128:(blk + 1) * 128], start=True, stop=True),
                            [KB_, qb], [psb[2 * slot + g]])
                    pt_, ptb_ = ptr.next()
                    S.add("act", lambda e, slot=slot, pt_=pt_: e.activation(
                        out=pt_[:, 0:1024], in_=psum2[slot][:, 0:1024], func=AF.Exp, scale=0.125),
                        [psb[2 * slot], psb[2 * slot + 1]], [ptb_])
                    msk = None
                    if kind == "win" and c >= 3 and kb_ == c - 1:
                        msk = m_ge
                    if kind == "win" and c >= 2 and kb_ == c + 1:
                        msk = m_le
                    if msk is not None:
                        S.add("dve", lambda e, pt_=pt_, msk=msk: e.tensor_tensor(
                            out=pt_[:, 0:1024].rearrange("p (r q) -> p r q", q=128),
                            in0=pt_[:, 0:1024].rearrange("p (r q) -> p r q", q=128),
                            in1=msk[:].unsqueeze(1).to_broadcast([128, 8, 128]), op=ALU.mult), [ptb_, CB], [ptb_])
                    u["pt"] = (pt_, ptb_)

                def emit_pv(u, kind=kind, groups=groups, y_dst=y_dst, ydb=ydb):
                    G = groups[u["gi"]]
                    blk, kb_ = G["blk"], u["kb"]
                    q_, qb, os_, osb = get_tile(G["t0"], G["tt"])
                    pt_, ptb_ = u["pt"]
                    for g in range(2):
                        po, pob = psum[4 + g], psb[4 + g]
                        S.add("pe", lambda e, po=po, kb_=kb_, g=g, pt_=pt_, st_=u["first"], sp_=u["last"]: e.matmul(
                            po[0:65, :], lhsT=VV[:, kb_, g, :], rhs=pt_[:, g * 512:(g + 1) * 512], start=st_, stop=sp_),
                            [KB_, ptb_], [pob])
                    if not u["last"]:
                        return
                    for g in range(2):
                        po, pob = psum[4 + g], psb[4 + g]
                        rd, rdb = rdr.next()
                        if kind == "win":
                            S.add("dve", lambda e, po=po, rd=rd, g=g: e.tensor_tensor(
                                out=rd[64:65, :], in0=po[64:65, :], in1=sink_sb[64:65, g, :], op=ALU.add), [pob, KB_], [rdb])
                            S.add("dve", lambda e, rd=rd: e.reciprocal(out=rd[64:65, :], in_=rd[64:65, :]), [rdb], [rdb])
                        else:
                            S.add("dve", lambda e, po=po, rd=rd: e.reciprocal(out=rd[64:65, :], in_=po[64:65, :]), [pob], [rdb])
                        pbc, pbcb = psum[6 + g], psb[6 + g]
                        S.add("pe", lambda e, pbc=pbc, rd=rd: e.matmul(
                            pbc[0:64, :], lhsT=ones_f[64:65, 0:64], rhs=rd[64:65, :], start=True, stop=True), [rdb, CB], [pbcb])
                        bc, bcb = bcr.next()
                        S.add("dve", lambda e, pbc=pbc, bc=bc: e.tensor_copy(out=bc[:], in_=pbc[0:64, :]), [pbcb], [bcb])
                        S.add("dve", lambda e, po=po, bc=bc, os_=os_, g=g, blk=blk: e.tensor_tensor(
                            out=os_[:, g * 4:(g + 1) * 4, blk * 128:(blk + 1) * 128],
                            in0=po[0:64, :].rearrange("p (r q) -> p r q", q=128),
                            in1=bc[:].rearrange("p (r q) -> p r q", q=128), op=ALU.mult), [pob, bcb], [osb])
                    if G["tile_last"]:
                        t0, tt = G["t0"], G["tt"]
                        S.dma(y_dst[:, :, t0:t0 + tt], os_[:, :, 0:tt], reads=[osb], writes=[ydb], q="pool")

                LOOK = 2
                for i in range(min(LOOK, len(units))):
                    emit_qk(units[i])
                for i, u in enumerate(units):
                    emit_pv(u)
                    if i + LOOK < len(units):
                        emit_qk(units[i + LOOK])
            S.barrier()

        with contextlib.ExitStack() as st:
            xr = Ring(nc, st, "e_x", [128, KC, 512], F32, 1)
            yar = Ring(nc, st, "e_ya", [128, 4, 512], BF16, 1)
            ybr2 = Ring(nc, st, "e_yb", [64, 8, 512], BF16, 1)
            ycr = Ring(nc, st, "e_yc", [64, 8, 512], BF16, 1)
            bgr = Ring(nc, st, "e_bg", [128, 24, 512], BF16, 1)
            mgr = Ring(nc, st, "e_mg", [128, KC, 512], BF16, 1)
            h2r = Ring(nc, st, "e_h2", [128, KC, 512], BF16, 1)
            sqr = Ring(nc, st, "e_sq", [128, KC, 512], BF16, 1)
            ur = Ring(nc, st, "e_u", [128, NFC, 512], BF16, 1)
            w0r = Ring(nc, st, "e_w0", [128, 4, 128], BF16, 3)
            w12r = Ring(nc, st, "e_w12", [64, 8, 128], BF16, 4)
            wor = Ring(nc, st, "e_wo", [128, KC, 128], BF16, 6)
            w2r = Ring(nc, st, "e_w2", [128, NFC, 128], BF16, 2)
            f32r = Ring(nc, st, "e_f", [128, 512], F32, 6)
            rsr = Ring(nc, st, "e_rs", [128, 512], F32, 2)
            fg_sb = sb(st, "fg_sb", [128, KC], F32)
            FGB = Buf("fing")
            S.dma(fg_sb[:], fing, writes=[FGB])
            ei = [0]

            def ebank(lo, n):
                i = lo + ei[0] % n
                ei[0] += 1
                return psum[i], psb[i]

            def rms(x_, xb, tt):
                sq_, sqb = sqr.next()
                S.add("act", lambda e: e.activation(out=sq_[:, :, 0:tt], in_=x_[:, :, 0:tt], func=AF.Square), [xb], [sqb])
                pm, pmb = psum[0], psb[0]
                for kc in range(KC):
                    S.add("pe", lambda e, kc=kc: e.matmul(pm[:, 0:tt], lhsT=onesm[:], rhs=sq_[:, kc, 0:tt],
                                                          start=(kc == 0), stop=(kc == KC - 1)), [sqb, CB], [pmb])
                rs_, rsb = rsr.next()
                S.add("act", lambda e: e.activation(out=rs_[:, 0:tt], in_=pm[:, 0:tt], func=AF.Ln, bias=eps_sb[:, 0:1], scale=1.0),
                      [pmb, CB], [rsb])
                S.add("act", lambda e: e.activation(out=rs_[:, 0:tt], in_=rs_[:, 0:tt], func=AF.Exp, scale=-0.5), [rsb], [rsb])
                return rs_, rsb

            dbg_mg = dscr(f"dbg_mg{l}", [D, T], BF16) if dbg else None
            dbg_x1 = dscr(f"dbg_x1{l}", [D, T], F32) if dbg else None

            def e_tile(t0, tt):
                wsel = 1 if t0 == 0 else 0
                x_, xb = xr.next()
                S.dma(x_[:, :, 0:tt], xin_v[:, :, t0:t0 + tt], reads=[DB["xs"]], writes=[xb])
                ya_, yab = yar.next()
                yb_, ybb = ybr2.next()
                yc_, ycb = ycr.next()
                bg_, bgb = bgr.next()
                S.dma(ya_[:, :, 0:tt], yaT.rearrange("(h d) t -> d h t", d=128)[:, :, t0:t0 + tt], reads=[DB["ya"]], writes=[yab], q="act")
                S.dma(yb_[:, :, 0:tt], ybT[:, :, t0:t0 + tt], reads=[DB["yb"]], writes=[ybb], q="pool")
                S.dma(yc_[:, :, 0:tt], ycT[:, :, t0:t0 + tt], reads=[DB["yc"]], writes=[ycb], q="pool")
                S.dma(bg_[:, :, 0:tt], bgT.rearrange("(c p) t -> p c t", p=128)[:, :, t0:t0 + tt], reads=[DB["proj"]], writes=[bgb], q="act")
                mg_, mgb = mgr.next()
                for oc in range(8):
                    wa, wab = w0r.next()
                    wb1, wb1b = w12r.next()
                    wb2, wb2b = w12r.next()
                    S.dma(wa[:].rearrange("p a b -> p (a b)"), b_wbr0[oc], reads=[DB["w"]], writes=[wab], q=wq())
                    S.dma(wb1[:].rearrange("p a b -> p (a b)"), b_wbr1[oc], reads=[DB["w"]], writes=[wb1b], q=wq())
                    S.dma(wb2[:].rearrange("p a b -> p (a b)"), b_wbr2[oc], reads=[DB["w"]], writes=[wb2b], q=wq())
                    p0, p0b = ebank(1, 3)
                    for k in range(4):
                        S.add("pe", lambda e, k=k, p0=p0, wa=wa: e.matmul(p0[:, 0:tt], lhsT=wa[:, k, :], rhs=ya_[:, k, 0:tt],
                                                                          start=(k == 0), stop=(k == 3)), [wab, yab], [p0b])
                    p1, p1b = ebank(1, 3)
                    for k in range(8):
                        S.add("pe", lambda e, k=k, p1=p1, wb1=wb1: e.matmul(p1[:, 0:tt], lhsT=wb1[:, k, :], rhs=yb_[:, k, 0:tt],
                                                                            start=(k == 0), stop=(k == 7)), [wb1b, ybb], [p1b])
                    p2, p2b = ebank(1, 3)
                    for k in range(8):
                        S.add("pe", lambda e, k=k, p2=p2, wb2=wb2: e.matmul(p2[:, 0:tt], lhsT=wb2[:, k, :], rhs=yc_[:, k, 0:tt],
                                                                            start=(k == 0), stop=(k == 7)), [wb2b, ycb], [p2b])
                    a0, a0b = f32r.next()
                    a1, a1b = f32r.next()
                    a2, a2b = f32r.next()
                    S.add("dve", lambda e, p0=p0, a0=a0, oc=oc: e.tensor_tensor(
                        out=a0[:, 0:tt], in0=p0[:, 0:tt], in1=bg_[:, oc, 0:tt], op=ALU.mult), [p0b, bgb], [a0b])
                    S.add("dve", lambda e, p1=p1, a1=a1, oc=oc: e.tensor_tensor(
                        out=a1[:, 0:tt], in0=p1[:, 0:tt], in1=bg_[:, 8 + oc, 0:tt], op=ALU.mult), [p1b, bgb], [a1b])
                    S.add("dve", lambda e, p2=p2, a2=a2, oc=oc: e.tensor_tensor(
                        out=a2[:, 0:tt], in0=p2[:, 0:tt], in1=bg_[:, 16 + oc, 0:tt], op=ALU.mult), [p2b, bgb], [a2b])
                    S.add("pool", lambda e, a0=a0, a1=a1: e.tensor_tensor(out=a0[:, 0:tt], in0=a0[:, 0:tt], in1=a1[:, 0:tt], op=ALU.add),
                          [a0b, a1b], [a0b])
                    S.add("pool", lambda e, a0=a0, a2=a2, oc=oc: e.tensor_tensor(
                        out=mg_[:, oc, 0:tt], in0=a0[:, 0:tt], in1=a2[:, 0:tt], op=ALU.add), [a0b, a2b], [mgb])
                if dbg:
                    S.dma(dbg_mg.rearrange("(k p) t -> p k t", p=128)[:, :, t0:t0 + tt], mg_[:, :, 0:tt], reads=[mgb])
                for oc in range(8):
                    w_, wb_ = wor.next()
                    S.dma(w_[:].rearrange("p a b -> p (a b)"), b_wo[oc], reads=[DB["w"]], writes=[wb_], q=wq())
                    po, pob = ebank(4, 2)
                    for k in range(KC):
                        S.add("pe", lambda e, k=k, po=po, w_=w_: e.matmul(po[:, 0:tt], lhsT=w_[:, k, :], rhs=mg_[:, k, 0:tt],
                                                                          start=(k == 0), stop=(k == KC - 1)), [wb_, mgb], [pob])
                    S.add("dve", lambda e, po=po, oc=oc: e.scalar_tensor_tensor(
                        out=x_[:, oc, 0:tt], in0=po[:, 0:tt], scalar=MOD[:, 16 + oc, wsel:wsel + 1], in1=x_[:, oc, 0:tt],
                        op0=ALU.mult, op1=ALU.add), [pob, MB, xb], [xb])
                if dbg:
                    S.dma(dbg_x1.rearrange("(k p) t -> p k t", p=128)[:, :, t0:t0 + tt], x_[:, :, 0:tt], reads=[xb], writes=[xb])
                rs_, rsb = rms(x_, xb, tt)
                h2, h2b = h2r.next()
                for kc in range(KC):
                    tm_, tmb = f32r.next()
                    S.add("dve" if kc % 2 == 0 else "pool", lambda e, kc=kc, tm_=tm_: e.tensor_tensor(
                        out=tm_[:, 0:tt], in0=x_[:, kc, 0:tt], in1=rs_[:, 0:tt], op=ALU.mult), [xb, rsb], [tmb])
                    S.add("act", lambda e, kc=kc, tm_=tm_: e.activation(
                        out=h2[:, kc, 0:tt], in_=tm_[:, 0:tt], func=AF.Identity,
                        scale=G2[:, kc, wsel:wsel + 1], bias=MOD[:, 24 + kc, wsel:wsel + 1]), [tmb, MB], [h2b])
                u_, ub = ur.next()
                for fc in range(NFC):
                    w1_, w1b = wor.next()
                    w3_, w3b = wor.next()
                    S.dma(w1_[:].rearrange("p a b -> p (a b)"), b_w1[fc], reads=[DB["w"]], writes=[w1b], q="sp")
                    S.dma(w3_[:].rearrange("p a b -> p (a b)"), b_w3[fc], reads=[DB["w"]], writes=[w3b], q="sp")
                    pa, pab = ebank(6, 2)
                    for k in range(KC):
                        S.add("pe", lambda e, k=k, pa=pa, w1_=w1_: e.matmul(pa[:, 0:tt], lhsT=w1_[:, k, :], rhs=h2[:, k, 0:tt],
                                                                            start=(k == 0), stop=(k == KC - 1)), [w1b, h2b], [pab])
                    pb3, pb3b = ebank(6, 2)
                    for k in range(KC):
                        S.add("pe", lambda e, k=k, pb3=pb3, w3_=w3_: e.matmul(pb3[:, 0:tt], lhsT=w3_[:, k, :], rhs=h2[:, k, 0:tt],
                                                                              start=(k == 0), stop=(k == KC - 1)), [w3b, h2b], [pb3b])
                    sl, slb = f32r.next()
                    S.add("act", lambda e, pa=pa, sl=sl: e.activation(out=sl[:, 0:tt], in_=pa[:, 0:tt], func=AF.Silu), [pab], [slb])
                    S.add("dve", lambda e, pb3=pb3, sl=sl, fc=fc: e.tensor_tensor(
                        out=u_[:, fc, 0:tt], in0=pb3[:, 0:tt], in1=sl[:, 0:tt], op=ALU.mult), [pb3b, slb], [ub])
                for oc in range(8):
                    w_, wb_ = w2r.next()
                    S.dma(w_[:].rearrange("p a b -> p (a b)"), b_w2[oc], reads=[DB["w"]], writes=[wb_], q=wq())
                    po, pob = ebank(4, 2)
                    for k in range(NFC):
                        S.add("pe", lambda e, k=k, po=po, w_=w_: e.matmul(po[:, 0:tt], lhsT=w_[:, k, :], rhs=u_[:, k, 0:tt],
                                                                          start=(k == 0), stop=(k == NFC - 1)), [wb_, ub], [pob])
                    S.add("dve", lambda e, po=po, oc=oc: e.scalar_tensor_tensor(
                        out=x_[:, oc, 0:tt], in0=po[:, 0:tt], scalar=MOD[:, 40 + oc, wsel:wsel + 1], in1=x_[:, oc, 0:tt],
                        op0=ALU.mult, op1=ALU.add), [pob, MB, xb], [xb])
                if l < depth - 1:
                    S.dma(xs_v[:, :, t0:t0 + tt], x_[:, :, 0:tt], reads=[xb], writes=[DB["xs"]], q="pool")
                else:
                    rs2_, rsb2 = rms(x_, xb, tt)
                    for kc in range(KC):
                        S.add("dve", lambda e, kc=kc: e.scalar_tensor_tensor(
                            out=x_[:, kc, 0:tt], in0=x_[:, kc, 0:tt], scalar=fg_sb[:, kc:kc + 1], in1=rs2_[:, 0:tt],
                            op0=ALU.mult, op1=ALU.mult), [xb, rsb2, FGB], [xb])
                    S.dma(yT.rearrange("(k p) t -> p k t", p=128)[:, :, t0 - 256:t0 - 256 + tt], x_[:, :, 0:tt], reads=[xb], q="pool")

            for (t0_, tt_) in tiles:
                if t0_ == 0 and not ctx_out:
                    continue
                e_tile(t0_, tt_)
            S.barrier()
        lay.close()

    S.emit()
    return nc


def _swap_d(d):
    r = d % 32
    return d + 16 if r < 16 else d - 16


def _fm_cols():
    OFF = dict(mq=0, mk=512, mv=1024, mo=1536, ig=2048, fg=2056, wq=2064, wk=2576, wv=2704, gq=2832, gk=3344, gv=3472, bg=3600)
    chunks = []
    for h in range(4):
        chunks.append([OFF["mq"] + h * 128 + d for d in range(128)])
    for h in range(4):
        chunks.append([OFF["mk"] + h * 128 + d for d in range(128)])
    for pre in ("w", "g"):
        q0, k0 = OFF[pre + "q"], OFF[pre + "k"]
        for sw in (False, True):
            for j in range(4):
                cols = []
                for p in range(128):
                    head = j if p < 64 else 4 + j
                    d = p % 64
                    dd = _swap_d(d) if sw else d
                    cols.append(q0 + head * 64 + dd)
                chunks.append(cols)
        for sw in (False, True):
            cols = []
            for p in range(128):
                head = p // 64
                d = p % 64
                dd = _swap_d(d) if sw else d
                cols.append(k0 + head * 64 + dd)
            chunks.append(cols)
    for c in range(24):
        chunks.append([OFF["bg"] + c * 128 + p for p in range(128)])
    assert len(chunks) == NFM
    tm = (list(range(512, 1024)) + list(range(1024, 1536)) + list(range(1536, 2048)) + list(range(2704, 2832)) +
          list(range(3472, 3600)) + list(range(2048, 2064)))
    assert len(tm) == TMW
    return np.array(chunks), np.array(tm)


def _pack(w, kp):
    K, N = w.shape
    return np.ascontiguousarray(w.reshape(K // kp, kp, N // 128, 128).transpose(2, 1, 0, 3))


def _rope_tables(nlat):
    T = nlat + CTX
    cos = np.ones((128, T), np.float32)
    sin = np.zeros((128, T), np.float32)
    t = np.arange(nlat)
    row = (t // 64).astype(np.float32)
    col = (t % 64).astype(np.float32)
    half = 32
    inv = (10000.0 ** (-np.arange(0, half, 2, dtype=np.float32) / half)).astype(np.float32)
    for p in range(128):
        d = p % 64
        pos = row if d < 32 else col
        ang = (pos * inv[d % 16]).astype(np.float32)
        sign = -1.0 if (d % 32) < 16 else 1.0
        cos[p, CTX:] = np.cos(ang)
        sin[p, CTX:] = sign * np.sin(ang)
    return cos, sin


def prep_shared(inp, nlat, depth=DEPTH):
    fm, tm = _fm_cols()
    sh = {}
    cos, sin = _rope_tables(nlat)
    sh["cos"], sh["sins"] = cos, sin
    sh["fing"] = np.ascontiguousarray(np.asarray(inp["final_g"]).reshape(KC, 128).T)
    swp = np.array([_swap_d(p % 64) for p in range(128)])
    for l in range(depth):
        w_in = np.asarray(inp["w_in"][l])
        b_in = np.asarray(inp["b_in"][l])
        sh[f"wmod{l}"] = _pack(np.asarray(inp["w_mod"][l]), 128)
        sh[f"bmod{l}"] = np.ascontiguousarray(np.asarray(inp["b_mod"][l]).reshape(48, 128).T)
        sh[f"n1g{l}"] = np.ascontiguousarray(np.asarray(inp["norm1_g"][l]).reshape(KC, 128).T)
        sh[f"n2g{l}"] = np.ascontiguousarray(np.asarray(inp["norm2_g"][l]).reshape(KC, 128).T)
        wf = w_in[:, fm.reshape(-1)]
        sh[f"wfm{l}"] = _pack(wf, 128)
        sh[f"bfm{l}"] = np.ascontiguousarray(b_in[fm].T)
        sh[f"wtm{l}"] = np.ascontiguousarray(w_in[:, tm].reshape(KC, 128, TMW).transpose(1, 0, 2))
        sh[f"btm{l}"] = np.ascontiguousarray(np.broadcast_to(b_in[tm][None, :], (128, TMW)))
        sh[f"gml{l}"] = np.ascontiguousarray(np.broadcast_to(np.asarray(inp["ml_norm_g"][l]).reshape(1, 512), (128, 512)))
        snk = np.repeat(np.asarray(inp["win_sink"][l]), 128).reshape(1, 2, 512)
        sh[f"sink{l}"] = np.ascontiguousarray(np.broadcast_to(snk, (128, 2, 512)))
        for nm, key in (("qng", "qn_g"), ("kng", "kn_g")):
            g = np.asarray(inp[key][l])
            sh[f"{nm}{l}"] = np.ascontiguousarray(np.stack([g[np.arange(128) % 64], g[swp]], axis=1))
        w_br = np.asarray(inp["w_br"][l])
        sh[f"wbr0{l}"] = _pack(w_br[0], 128)
        sh[f"wbr1{l}"] = _pack(w_br[1], 64).transpose(0, 1, 2, 3).copy()
        sh[f"wbr2{l}"] = _pack(w_br[2], 64).copy()
        sh[f"wo{l}"] = _pack(np.asarray(inp["w_o"][l]), 128)
        sh[f"w1{l}"] = _pack(np.asarray(inp["w_ff1"][l]), 128)
        sh[f"w3{l}"] = _pack(np.asarray(inp["w_ff3"][l]), 128)
        sh[f"w2{l}"] = _pack(np.asarray(inp["w_ff2"][l]), 128)
    return {k: np.ascontiguousarray(v, dtype=np.float32) for k, v in sh.items()}


def prep_core(inp, b):
    x = np.asarray(inp["x"][b])
    ctx = np.asarray(inp["ctx"][b])
    xT0 = np.ascontiguousarray(np.concatenate([ctx, x], axis=0).T)
    cc = np.stack([np.asarray(inp["c"][b]), np.asarray(inp["c_ctx"])], axis=1)
    cT = np.ascontiguousarray(cc.reshape(KC, 128, 2).transpose(1, 0, 2))
    return {"xT0": xT0.astype(np.float32), "cT": cT.astype(np.float32)}


_CACHE = {}


def kernel(**inp):
    x = np.asarray(inp["x"])
    B, nlat, _ = x.shape
    depth = np.asarray(inp["w_in"]).shape[0]
    key = (nlat, depth)
    if key not in _CACHE:
        _CACHE[key] = build(nlat // 128, depth)
    nc = _CACHE[key]
    sh = prep_shared(inp, nlat, depth)
    in_maps = []
    for b in range(B):
        m = dict(sh)
        m.update(prep_core(inp, b))
        in_maps.append(m)
    res = run_bass_kernel_spmd(nc, in_maps, core_ids=list(range(B)))
    out = np.stack([np.ascontiguousarray(r["yT"].T) for r in res.results], axis=0)
    return out.astype(np.float32)
```

```python
import contextlib
import numpy as np
import ml_dtypes
import concourse.bass as bass
import concourse.mybir as mybir
from concourse.bass_utils import run_bass_kernel_spmd

F32 = mybir.dt.float32
BF16 = mybir.dt.bfloat16
AF = mybir.ActivationFunctionType
ALU = mybir.AluOpType
AX = mybir.AxisListType

D = 1024
KC = 8
DFF = 2816
NFC = 22
EPS = 1e-6
NFM = 52
TMW = 1808
DEPTH = 2
SEQ = 16384
CTX = 256


_ALLBUFS = []


class Buf:
    __slots__ = ("ws", "rs", "name")

    def __init__(self, name=""):
        self.ws = []
        self.rs = []
        self.name = name
        _ALLBUFS.append(self)


class Op:
    __slots__ = ("eng", "fn", "deps", "dma", "signal", "cnt", "idx")

    def __init__(self, eng, fn, dma):
        self.eng = eng
        self.fn = fn
        self.dma = dma
        self.deps = []
        self.signal = False
        self.cnt = 0
        self.idx = 0


ENGS = ("pe", "act", "dve", "pool", "sp")
EPOCH = 20000
NDMA = 8


class Sched:
    def __init__(self, nc, strict_same=True):
        self.nc = nc
        self.ops = {e: [] for e in ENGS}
        self.strict_same = strict_same
        self.pending_dma = {e: [] for e in ENGS}
        self.last_real = {e: None for e in ENGS}

    def add(self, eng, fn, reads=(), writes=(), dma=False, force=()):
        op = Op(eng, fn, dma)
        deps = []
        for b in reads:
            deps.extend(b.ws)
        acc = []
        for b in writes:
            a = dma and len(b.ws) > 0 and all(x.dma for x in b.ws) and not b.rs
            acc.append(a)
            if not a:
                deps.extend(b.ws)
                deps.extend(b.rs)
        for b in reads:
            b.rs.append(op)
        for b, a in zip(writes, acc):
            if a:
                b.ws.append(op)
            else:
                b.ws = [op]
                b.rs = []
        best = {}
        dmadeps = []
        seen = set()
        for d in list(deps) + list(force):
            if d is op or d is None:
                continue
            if d.dma:
                if id(d) not in seen:
                    seen.add(id(d))
                    dmadeps.append(d)
            else:
                if d.eng == eng and (eng == "pe" or not self.strict_same) and d not in force:
                    continue
                cur = best.get(d.eng)
                if cur is None or d.idx > cur.idx:
                    best[d.eng] = d
        op.deps = list(best.values()) + dmadeps
        for d in op.deps:
            d.signal = True
        op.idx = len(self.ops[eng])
        self.ops[eng].append(op)
        if dma:
            self.pending_dma[eng].append(op)
        elif fn is not None:
            self.last_real[eng] = op
        return op

    def dma(self, out, in_, reads=(), writes=(), q="sp"):
        return self.add(q, lambda e: e.dma_start(out=out, in_=in_), reads, writes, dma=True)

    def barrier(self):
        firsts = []
        for e in ENGS:
            f = [self.last_real[e]] + self.pending_dma[e]
            self.pending_dma[e] = []
            firsts.append(self.add(e, None, force=[x for x in f if x is not None]))
        for e in ENGS:
            self.add(e, None, force=firsts)
        for b in _ALLBUFS:
            b.ws = []
            b.rs = []

    def emit(self):
        nc = self.nc
        sems = {}
        for e in ENGS:
            n = 0
            k = 0
            for op in self.ops[e]:
                if op.dma:
                    op.cnt = k
                    k += 1
                elif op.signal:
                    op.cnt = n
                    n += 1
            nep = (n + EPOCH - 1) // EPOCH
            sems[e] = [nc.alloc_semaphore(name=f"c_{e}_{i}") for i in range(nep)]
            sems[e + "_dma"] = [nc.alloc_semaphore(name=f"d_{e}_{i}") for i in range(min(NDMA, k))]

        def run_engine(e, h):
            waited = {x: -1 for x in ENGS}
            dma_done = set()
            for op in self.ops[e]:
                for d in op.deps:
                    if d.dma:
                        key = (d.eng, d.cnt)
                        if key in dma_done:
                            continue
                        h.wait_ge(sems[d.eng + "_dma"][d.cnt % NDMA], 16 * (d.cnt // NDMA + 1))
                        dma_done.add(key)
                    else:
                        if waited[d.eng] >= d.cnt:
                            continue
                        h.wait_ge(sems[d.eng][d.cnt // EPOCH], d.cnt % EPOCH + 1)
                        waited[d.eng] = d.cnt
                if op.dma:
                    s = sems[e + "_dma"][op.cnt % NDMA]
                    if op.cnt >= NDMA:
                        key = (e, op.cnt - NDMA)
                        if key not in dma_done:
                            h.wait_ge(s, 16 * (op.cnt // NDMA))
                            dma_done.add(key)
                    op.fn(h).then_inc(s, 16)
                elif op.fn is None:
                    if op.signal:
                        h.sem_inc(sems[e][op.cnt // EPOCH], 1)
                else:
                    ins = op.fn(h)
                    if op.signal:
                        ins.then_inc(sems[e][op.cnt // EPOCH], 1)
            k = sum(1 for op in self.ops[e] if op.dma)
            for j in range(min(NDMA, k)):
                last = ((k - 1 - j) // NDMA) * NDMA + j
                if (e, last) not in dma_done:
                    h.wait_ge(sems[e + "_dma"][j], 16 * (last // NDMA + 1))

        with nc.Block() as block:
            @block.sync
            def _(h):
                run_engine("sp", h)

            @block.tensor
            def _(h):
                run_engine("pe", h)

            @block.scalar
            def _(h):
                run_engine("act", h)

            @block.vector
            def _(h):
                run_engine("dve", h)

            @block.gpsimd
            def _(h):
                run_engine("pool", h)


_UID = [0]


class Ring:
    def __init__(self, nc, stack, name, shape, dtype, n):
        _UID[0] += 1
        name = f"{name}u{_UID[0]}"
        self.t = [stack.enter_context(nc.sbuf_tensor(f"{name}_{i}", shape, dtype)) for i in range(n)]
        self.b = [Buf(f"{name}_{i}") for i in range(n)]
        self.i = 0

    def next(self):
        i = self.i
        self.i = (i + 1) % len(self.t)
        return self.t[i], self.b[i]


def build(NLB, depth=DEPTH, dbg=False):
    assert NLB % 4 == 0
    NB = NLB + 2
    T = NB * 128
    tiles = [(0, 256)] + [(256 + 512 * i, 512) for i in range(NLB // 4)]
    nc = bass.Bass("TRN2", target_bir_lowering=False)
    S = Sched(nc)

    def din(name, shape, dt=F32):
        return nc.dram_tensor(name, list(shape), dt, kind="ExternalInput").ap()

    def dscr(name, shape, dt):
        return nc.dram_tensor(name, list(shape), dt, kind=("ExternalOutput" if dbg else "Internal")).ap()

    xT0 = din("xT0", [D, T])
    cT = din("cT", [128, KC, 2])
    cosd = din("cos", [128, T])
    sind = din("sins", [128, T])
    fing = din("fing", [128, KC])
    L = []
    for l in range(depth):
        L.append(dict(
            wmod=din(f"wmod{l}", [48, 128, KC, 128]), bmod=din(f"bmod{l}", [128, 48]),
            n1g=din(f"n1g{l}", [128, KC]), n2g=din(f"n2g{l}", [128, KC]),
            wfm=din(f"wfm{l}", [NFM, 128, KC, 128]), bfm=din(f"bfm{l}", [128, NFM]),
            wtm=din(f"wtm{l}", [128, KC, TMW]), btm=din(f"btm{l}", [128, TMW]),
            gml=din(f"gml{l}", [128, 512]), sink=din(f"sink{l}", [128, 2, 512]),
            qng=din(f"qng{l}", [128, 2]), kng=din(f"kng{l}", [128, 2]),
            wbr0=din(f"wbr0{l}", [8, 128, 4, 128]), wbr1=din(f"wbr1{l}", [8, 64, 8, 128]),
            wbr2=din(f"wbr2{l}", [8, 64, 8, 128]), wo=din(f"wo{l}", [8, 128, KC, 128]),
            w1=din(f"w1{l}", [NFC, 128, KC, 128]), w3=din(f"w3{l}", [NFC, 128, KC, 128]),
            w2=din(f"w2{l}", [8, 128, NFC, 128]),
        ))
    yT = nc.dram_tensor("yT", [D, NLB * 128], F32, kind="ExternalOutput").ap()

    xs = dscr("xs", [D, T], F32)
    b_wmod = dscr("b_wmod", [48, 128, KC * 128], BF16)
    b_wfm = dscr("b_wfm", [NFM, 128, KC * 128], BF16)
    b_wtm = dscr("b_wtm", [128, KC * TMW], BF16)
    b_wbr0 = dscr("b_wbr0", [8, 128, 4 * 128], BF16)
    b_wbr1 = dscr("b_wbr1", [8, 64, 8 * 128], BF16)
    b_wbr2 = dscr("b_wbr2", [8, 64, 8 * 128], BF16)
    b_wo = dscr("b_wo", [8, 128, KC * 128], BF16)
    b_w1 = dscr("b_w1", [NFC, 128, KC * 128], BF16)
    b_w3 = dscr("b_w3", [NFC, 128, KC * 128], BF16)
    b_w2 = dscr("b_w2", [8, 128, NFC * 128], BF16)
    mqT = dscr("mqT", [512, T], BF16)
    mkT = dscr("mkT", [512, T], BF16)
    wqT = dscr("wqT", [512, T], BF16)
    wkT = dscr("wkT", [128, T], BF16)
    gqT = dscr("gqT", [512, T], BF16)
    gkT = dscr("gkT", [128, T], BF16)
    bgT = dscr("bgT", [3072, T], BF16)
    mk_tm = dscr("mk_tm", [T, 512], BF16)
    mv_tm = dscr("mv_tm", [T, 512], BF16)
    mo_tm = dscr("mo_tm", [T, 512], BF16)
    wgv_tm = dscr("wgv_tm", [T, 256], BF16)
    gat_tm = dscr("gat_tm", [T, 16], F32)
    hb_tm = dscr("hb_tm", [T, 512], F32)
    yaT = dscr("yaT", [512, T], BF16)
    ybT = dscr("ybT", [64, 8, T], BF16)
    ycT = dscr("ycT", [64, 8, T], BF16)
    DB = {k: Buf(k) for k in ["xs", "w", "proj", "hb", "ya", "yb", "yc"]}

    stack0 = contextlib.ExitStack()
    psum2 = [nc.alloc_psum_tensor(f"dbank{i}", [128, 1024], F32) for i in range(4)]
    psum = [psum2[i // 2][:, (i % 2) * 512:(i % 2 + 1) * 512] for i in range(8)]
    psb = [Buf(f"bank{i}") for i in range(8)]

    wq_i = [0]

    def wq():
        return "sp"

    def sb(stack, name, shape, dt):
        _UID[0] += 1
        return stack.enter_context(nc.sbuf_tensor(f"{name}u{_UID[0]}", shape, dt))

    c_f32 = sb(stack0, "c_f32", [128, 128], F32)
    ones_f = sb(stack0, "ones_f", [128, 128], F32)
    tri_f = sb(stack0, "tri_f", [128, 128], F32)
    tri_b = sb(stack0, "tri_b", [128, 128], F32)
    m_le = sb(stack0, "m_le", [128, 128], BF16)
    m_ge = sb(stack0, "m_ge", [128, 128], BF16)
    onesm = sb(stack0, "onesm", [128, 128], BF16)
    bd64 = sb(stack0, "bd64", [128, 128], BF16)
    ident = sb(stack0, "ident", [128, 128], BF16)
    eps_sb = sb(stack0, "eps_sb", [128, 1], F32)
    CB = Buf("consts")

    def mk_consts():
        S.add("pool", lambda e: e.memset(ones_f[:], 1.0), writes=[CB])
        S.add("pool", lambda e: e.memset(eps_sb[:], EPS), writes=[CB])
        S.add("pool", lambda e: e.memset(onesm[:], 1.0 / 1024.0), writes=[CB])
        S.add("pool", lambda e: e.affine_select(out=tri_f[:], in_=ones_f[:], pattern=[[1, 128]],
                                                compare_op=ALU.is_ge, fill=0.0, base=0, channel_multiplier=-1),
              reads=[CB], writes=[CB])
        S.add("pool", lambda e: e.affine_select(out=tri_b[:], in_=ones_f[:], pattern=[[-1, 128]],
                                                compare_op=ALU.is_ge, fill=0.0, base=0, channel_multiplier=1),
              reads=[CB], writes=[CB])
        S.add("pool", lambda e: e.tensor_copy(out=m_le[:], in_=tri_f[:]), reads=[CB], writes=[CB])
        S.add("pool", lambda e: e.tensor_copy(out=m_ge[:], in_=tri_b[:]), reads=[CB], writes=[CB])
        S.add("pool", lambda e: e.tensor_tensor(out=ident[:], in0=tri_f[:], in1=tri_b[:], op=ALU.mult),
              reads=[CB], writes=[CB])
        S.add("pool", lambda e: e.memset(bd64[:], 0.0), reads=[CB], writes=[CB])
        S.add("pool", lambda e: e.memset(bd64[0:64, 0:64], 1.0 / 64.0), reads=[CB], writes=[CB])
        S.add("pool", lambda e: e.memset(bd64[64:128, 64:128], 1.0 / 64.0), reads=[CB], writes=[CB])

    mk_consts()

    cast_i = [0]

    def cast_pack(stack, src, dst, nchunk, P, F):
        pieces = []
        for c in range(nchunk):
            f0 = 0
            while f0 < F:
                fw = min(2048, F - f0)
                pieces.append((c, f0, fw))
                f0 += fw
        for (c, f0, fw) in pieces:
            a, ab = cast_f.next()
            o, ob = cast_o.next()
            S.dma(a[0:P, 0:fw], srcflat(src, c)[:, f0:f0 + fw], reads=[], writes=[ab], q="sp")
            eng = ("dve", "pool", "act")[cast_i[0] % 3]
            cast_i[0] += 1
            if eng == "act":
                S.add("act", lambda e, a=a, o=o, fw=fw: e.copy(out=o[0:P, 0:fw], in_=a[0:P, 0:fw]), [ab], [ob])
            else:
                S.add(eng, lambda e, a=a, o=o, fw=fw: e.tensor_copy(out=o[0:P, 0:fw], in_=a[0:P, 0:fw]), [ab], [ob])
            S.dma(dst[c][:, f0:f0 + fw], o[0:P, 0:fw], reads=[ob], writes=[DB["w"]], q="pool")

    def srcflat(src, c):
        v = src[c]
        if len(v.shape) == 3:
            return v.rearrange("p a b -> p (a b)")
        return v

    for l in range(depth):
        W = L[l]
        ctx_out = l < depth - 1
        xin = xT0 if l == 0 else xs
        xin_v = xin.rearrange("(k p) t -> p k t", p=128)
        xs_v = xs.rearrange("(k p) t -> p k t", p=128)

        with contextlib.ExitStack() as st:
            cast_f = Ring(nc, st, "cast_f", [128, 2048], F32, 3)
            cast_o = Ring(nc, st, "cast_o", [128, 2048], BF16, 3)
            cast_pack(st, W["wmod"], b_wmod, 48, 128, KC * 128)
            cast_pack(st, W["wfm"], b_wfm, NFM, 128, KC * 128)
            cast_pack(st, W["wtm"].rearrange("p (o a) b -> o p a b", o=1), b_wtm.rearrange("(o p) f -> o p f", o=1), 1, 128, KC * TMW)
            cast_pack(st, W["wbr0"], b_wbr0, 8, 128, 4 * 128)
            cast_pack(st, W["wbr1"], b_wbr1, 8, 64, 8 * 128)
            cast_pack(st, W["wbr2"], b_wbr2, 8, 64, 8 * 128)
            cast_pack(st, W["wo"], b_wo, 8, 128, KC * 128)
            cast_pack(st, W["w1"], b_w1, NFC, 128, KC * 128)
            cast_pack(st, W["w3"], b_w3, NFC, 128, KC * 128)
            cast_pack(st, W["w2"], b_w2, 8, 128, NFC * 128)
            S.barrier()

        lay = contextlib.ExitStack()
        MOD = sb(lay, "MOD", [128, 48, 2], F32)
        G1 = sb(lay, "G1", [128, KC, 2], F32)
        G2 = sb(lay, "G2", [128, KC, 2], F32)
        MB = Buf("MOD")
        with contextlib.ExitStack() as st:
            c_sb = sb(st, "c_sb", [128, KC, 2], F32)
            sc = sb(st, "sc", [128, KC, 2], BF16)
            bm = sb(st, "bm", [128, 48], F32)
            ng = sb(st, "ng", [128, 2, KC], F32)
            wr = Ring(nc, st, "wmodc", [128, KC, 128], BF16, 4)
            tb = Buf("m_tmp")
            S.dma(c_sb[:], cT, writes=[tb])
            S.dma(bm[:], W["bmod"], writes=[tb])
            S.dma(ng[:, 0, :], W["n1g"], writes=[tb])
            S.dma(ng[:, 1, :], W["n2g"], writes=[tb])
            S.add("act", lambda e: e.activation(out=sc[:], in_=c_sb[:], func=AF.Silu), [tb], [tb])
            pm = psum[0]
            for mc in range(48):
                w_, wb = wr.next()
                S.dma(w_[:].rearrange("p a b -> p (a b)"), b_wmod[mc], reads=[DB["w"]], writes=[wb])
                for kc in range(KC):
                    S.add("pe", lambda e, w_=w_, kc=kc, mc=mc: e.matmul(
                        pm[:, 2 * mc:2 * mc + 2], lhsT=w_[:, kc, :], rhs=sc[:, kc, :], start=(kc == 0), stop=(kc == KC - 1)),
                        [wb, tb], [psb[0]])
            S.add("dve", lambda e: e.tensor_tensor(
                out=MOD[:], in0=pm[:, 0:96].rearrange("p (m w) -> p m w", w=2),
                in1=bm[:].unsqueeze(2).to_broadcast([128, 48, 2]), op=ALU.add), [psb[0], tb], [MB])
            S.add("dve", lambda e: e.scalar_tensor_tensor(
                out=G1[:], in0=MOD[:, 8:16, :], scalar=1.0, in1=ng[:, 0, :].unsqueeze(2).to_broadcast([128, KC, 2]),
                op0=ALU.add, op1=ALU.mult), [MB, tb], [MB])
            S.add("dve", lambda e: e.scalar_tensor_tensor(
                out=G2[:], in0=MOD[:, 32:40, :], scalar=1.0, in1=ng[:, 1, :].unsqueeze(2).to_broadcast([128, KC, 2]),
                op0=ALU.add, op1=ALU.mult), [MB, tb], [MB])
            if dbg and l == 0:
                dbg_mod = dscr("dbg_mod", [128, 96], F32)
                dbg_g1 = dscr("dbg_g1", [128, 16], F32)
                S.dma(dbg_mod, MOD[:].rearrange("p a b -> p (a b)"), reads=[MB])
                S.dma(dbg_g1, G1[:].rearrange("p a b -> p (a b)"), reads=[MB])
            S.barrier()

        with contextlib.ExitStack() as st:
            wtm_sb = sb(st, "wtm_sb", [128, KC, TMW], BF16)
            btm_sb = sb(st, "btm_sb", [128, TMW], F32)
            bfm_sb = sb(st, "bfm_sb", [128, NFM], F32)
            qng_sb = sb(st, "qng_sb", [128, 2], F32)
            kng_sb = sb(st, "kng_sb", [128, 2], F32)
            AB = Buf("a_const")
            S.dma(wtm_sb[:].rearrange("p a b -> p (a b)"), b_wtm, reads=[DB["w"]], writes=[AB])
            S.dma(btm_sb[:], W["btm"], writes=[AB])
            S.dma(bfm_sb[:], W["bfm"], writes=[AB])
            S.dma(qng_sb[:], W["qng"], writes=[AB])
            S.dma(kng_sb[:], W["kng"], writes=[AB])
            xr = Ring(nc, st, "a_x", [128, KC, 512], F32, 2)
            sqr = Ring(nc, st, "a_sq", [128, KC, 512], BF16, 1)
            hr = Ring(nc, st, "a_h", [128, KC, 512], BF16, 2)
            rsr = Ring(nc, st, "a_rs", [128, 512], F32, 2)
            tmpr = Ring(nc, st, "a_tmp", [128, 512], F32, 3)
            wcr = Ring(nc, st, "a_wc", [128, KC, 128], BF16, 6)
            csr = Ring(nc, st, "a_cs", [128, 2, 512], F32, 2)
            f32r = Ring(nc, st, "a_f", [128, 512], F32, 8)
            bfr = Ring(nc, st, "a_b", [128, 512], BF16, 6)
            tmo = Ring(nc, st, "a_tmo", [128, 512], BF16, 4)
            gto = Ring(nc, st, "a_gto", [128, 16], F32, 2)
            pi = [0]

            def pbank(lo, n):
                i = lo + pi[0] % n
                pi[0] += 1
                return psum[i], psb[i]

            def proj(c, h_, hb, tt):
                w_, wb = wcr.next()
                S.dma(w_[:].rearrange("p a b -> p (a b)"), b_wfm[c], reads=[DB["w"]], writes=[wb], q=wq())
                p_, pb = pbank(1, 4)
                for kc in range(KC):
                    S.add("pe", lambda e, w_=w_, kc=kc, p_=p_: e.matmul(
                        p_[:, 0:tt], lhsT=w_[:, kc, :], rhs=h_[:, kc, 0:tt], start=(kc == 0), stop=(kc == KC - 1)),
                        [wb, hb], [pb])
                return p_, pb

            def store(dst, row0, t0, tt, o_, ob):
                S.dma(dst[row0:row0 + 128, t0:t0 + tt], o_[:, 0:tt], reads=[ob], writes=[DB["proj"]], q="pool")

            dbg_h = dscr("dbg_h", [D, T], BF16) if (dbg and l == 0) else None

            def a_tile(t0, tt):
                wsel = 1 if t0 == 0 else 0
                x_, xb = xr.next()
                S.dma(x_[:, :, 0:tt], xin_v[:, :, t0:t0 + tt], reads=[DB["xs"]], writes=[xb])
                cs_, csb = csr.next()
                S.dma(cs_[:, 0, 0:tt], cosd[:, t0:t0 + tt], writes=[csb], q="pool")
                S.dma(cs_[:, 1, 0:tt], sind[:, t0:t0 + tt], writes=[csb], q="pool")
                sq_, sqb = sqr.next()
                S.add("act", lambda e, x_=x_, sq_=sq_: e.activation(out=sq_[:, :, 0:tt], in_=x_[:, :, 0:tt], func=AF.Square),
                      [xb], [sqb])
                pm, pmb = psum[0], psb[0]
                for kc in range(KC):
                    S.add("pe", lambda e, kc=kc, sq_=sq_: e.matmul(pm[:, 0:tt], lhsT=onesm[:], rhs=sq_[:, kc, 0:tt],
                                                                   start=(kc == 0), stop=(kc == KC - 1)), [sqb, CB], [pmb])
                rs_, rsb = rsr.next()
                S.add("act", lambda e, rs_=rs_: e.activation(out=rs_[:, 0:tt], in_=pm[:, 0:tt], func=AF.Ln, bias=eps_sb[:, 0:1], scale=1.0),
                      [pmb, CB], [rsb])
                S.add("act", lambda e, rs_=rs_: e.activation(out=rs_[:, 0:tt], in_=rs_[:, 0:tt], func=AF.Exp, scale=-0.5), [rsb], [rsb])
                h_, hb = hr.next()
                for kc in range(KC):
                    tm_, tmb = tmpr.next()
                    S.add("dve" if kc % 2 == 0 else "pool", lambda e, kc=kc, tm_=tm_, x_=x_, rs_=rs_: e.tensor_tensor(
                        out=tm_[:, 0:tt], in0=x_[:, kc, 0:tt], in1=rs_[:, 0:tt], op=ALU.mult), [xb, rsb], [tmb])
                    S.add("act", lambda e, kc=kc, tm_=tm_, h_=h_: e.activation(
                        out=h_[:, kc, 0:tt], in_=tm_[:, 0:tt], func=AF.Identity,
                        scale=G1[:, kc, wsel:wsel + 1], bias=MOD[:, kc, wsel:wsel + 1]), [tmb, MB], [hb])
                if dbg and l == 0:
                    S.dma(dbg_h.rearrange("(k p) t -> p k t", p=128)[:, :, t0:t0 + tt], h_[:, :, 0:tt], reads=[hb])
                for c in range(8):
                    p_, pb = proj(c, h_, hb, tt)
                    o_, ob = bfr.next()
                    S.add("act", lambda e, p_=p_, o_=o_, c=c: e.activation(
                        out=o_[:, 0:tt], in_=p_[:, 0:tt], func=AF.Identity, bias=bfm_sb[:, c:c + 1], scale=1.0), [pb, AB], [ob])
                    store(mqT if c < 4 else mkT, (c % 4) * 128, t0, tt, o_, ob)
                for (ca, cb_, dst, row0, norm) in (
                        [(8 + j, 12 + j, wqT, j * 128, None) for j in range(4)] + [(16, 17, wkT, 0, None)] +
                        [(18 + j, 22 + j, gqT, j * 128, qng_sb) for j in range(4)] + [(26, 27, gkT, 0, kng_sb)]):
                    pa, pab = proj(ca, h_, hb, tt)
                    pb2, pbb = proj(cb_, h_, hb, tt)
                    q1, q1b = f32r.next()
                    q2, q2b = f32r.next()
                    if norm is None:
                        S.add("dve", lambda e, pa=pa, q1=q1, ca=ca, cs_=cs_: e.scalar_tensor_tensor(
                            out=q1[:, 0:tt], in0=pa[:, 0:tt], scalar=bfm_sb[:, ca:ca + 1], in1=cs_[:, 0, 0:tt],
                            op0=ALU.add, op1=ALU.mult), [pab, AB, csb], [q1b])
                        S.add("dve", lambda e, pb2=pb2, q2=q2, cb_=cb_, cs_=cs_: e.scalar_tensor_tensor(
                            out=q2[:, 0:tt], in0=pb2[:, 0:tt], scalar=bfm_sb[:, cb_:cb_ + 1], in1=cs_[:, 1, 0:tt],
                            op0=ALU.add, op1=ALU.mult), [pbb, AB, csb], [q2b])
                        o_, ob = bfr.next()
                        S.add("pool", lambda e, q1=q1, q2=q2, o_=o_: e.tensor_tensor(
                            out=o_[:, 0:tt], in0=q1[:, 0:tt], in1=q2[:, 0:tt], op=ALU.add), [q1b, q2b], [ob])
                    else:
                        qa, qab = f32r.next()
                        qs, qsb = f32r.next()
                        S.add("act", lambda e, pa=pa, qa=qa, ca=ca: e.activation(
                            out=qa[:, 0:tt], in_=pa[:, 0:tt], func=AF.Identity, bias=bfm_sb[:, ca:ca + 1], scale=1.0),
                            [pab, AB], [qab])
                        S.add("act", lambda e, pb2=pb2, qs=qs, cb_=cb_: e.activation(
                            out=qs[:, 0:tt], in_=pb2[:, 0:tt], func=AF.Identity, bias=bfm_sb[:, cb_:cb_ + 1], scale=1.0),
                            [pbb, AB], [qsb])
                        s2, s2b = bfr.next()
                        S.add("pool", lambda e, qa=qa, s2=s2: e.tensor_tensor(
                            out=s2[:, 0:tt], in0=qa[:, 0:tt], in1=qa[:, 0:tt], op=ALU.mult), [qab], [s2b])
                        pn, pnb = pbank(5, 2)
                        S.add("pe", lambda e, pn=pn, s2=s2: e.matmul(pn[:, 0:tt], lhsT=bd64[:], rhs=s2[:, 0:tt],
                                                                    start=True, stop=True), [s2b, CB], [pnb])
                        rn, rnb = f32r.next()
                        S.add("act", lambda e, pn=pn, rn=rn: e.activation(
                            out=rn[:, 0:tt], in_=pn[:, 0:tt], func=AF.Ln, bias=eps_sb[:, 0:1], scale=1.0), [pnb, CB], [rnb])
                        S.add("act", lambda e, rn=rn: e.activation(out=rn[:, 0:tt], in_=rn[:, 0:tt], func=AF.Exp, scale=-0.5),
                              [rnb], [rnb])
                        S.add("dve", lambda e, qa=qa, q1=q1, cs_=cs_, norm=norm: e.scalar_tensor_tensor(
                            out=q1[:, 0:tt], in0=qa[:, 0:tt], scalar=norm[:, 0:1], in1=cs_[:, 0, 0:tt],
                            op0=ALU.mult, op1=ALU.mult), [qab, AB, csb], [q1b])
                        S.add("dve", lambda e, qs=qs, q2=q2, cs_=cs_, norm=norm: e.scalar_tensor_tensor(
                            out=q2[:, 0:tt], in0=qs[:, 0:tt], scalar=norm[:, 1:2], in1=cs_[:, 1, 0:tt],
                            op0=ALU.mult, op1=ALU.mult), [qsb, AB, csb], [q2b])
                        S.add("dve", lambda e, q1=q1, q2=q2: e.tensor_tensor(
                            out=q1[:, 0:tt], in0=q1[:, 0:tt], in1=q2[:, 0:tt], op=ALU.add), [q1b, q2b], [q1b])
                        o_, ob = bfr.next()
                        S.add("pool", lambda e, q1=q1, rn=rn, o_=o_: e.tensor_tensor(
                            out=o_[:, 0:tt], in0=q1[:, 0:tt], in1=rn[:, 0:tt], op=ALU.mult), [q1b, rnb], [ob])
                    store(dst, row0, t0, tt, o_, ob)
                for c in range(28, 52):
                    p_, pb = proj(c, h_, hb, tt)
                    o_, ob = bfr.next()
                    S.add("act", lambda e, p_=p_, o_=o_, c=c: e.activation(
                        out=o_[:, 0:tt], in_=p_[:, 0:tt], func=AF.Sigmoid, bias=bfm_sb[:, c:c + 1], scale=1.0), [pb, AB], [ob])
                    store(bgT, (c - 28) * 128, t0, tt, o_, ob)
                for blk in range(tt // 128):
                    r0 = t0 + blk * 128
                    for gi, (c0, cw, dst, dcol) in enumerate(((0, 512, mk_tm, 0), (512, 512, mv_tm, 0), (1024, 512, mo_tm, 0),
                                                              (1536, 256, wgv_tm, 0), (1792, 16, gat_tm, 0))):
                        p_, pb = pbank(5, 3)
                        for kc in range(KC):
                            S.add("pe", lambda e, p_=p_, kc=kc, c0=c0, cw=cw, blk=blk: e.matmul(
                                p_[:, 0:cw], lhsT=h_[:, kc, blk * 128:(blk + 1) * 128], rhs=wtm_sb[:, kc, c0:c0 + cw],
                                start=(kc == 0), stop=(kc == KC - 1)), [hb, AB], [pb])
                        if gi == 4:
                            o_, ob = gto.next()
                            S.add("dve", lambda e, p_=p_, o_=o_, c0=c0, cw=cw: e.tensor_tensor(
                                out=o_[:, 0:cw], in0=p_[:, 0:cw], in1=btm_sb[:, c0:c0 + cw], op=ALU.add), [pb, AB], [ob])
                        else:
                            o_, ob = tmo.next()
                            S.add("dve", lambda e, p_=p_, o_=o_, c0=c0, cw=cw: e.tensor_tensor(
                                out=o_[:, 0:cw], in0=p_[:, 0:cw], in1=btm_sb[:, c0:c0 + cw], op=ALU.add), [pb, AB], [ob])
                            if gi == 2:
                                S.add("act", lambda e, o_=o_, cw=cw: e.activation(out=o_[:, 0:cw], in_=o_[:, 0:cw], func=AF.Sigmoid),
                                      [ob], [ob])
                        S.dma(dst[r0:r0 + 128, 0:cw], o_[:, 0:cw], reads=[ob], writes=[DB["proj"]], q="pool")

            for (t0_, tt_) in tiles:
                a_tile(t0_, tt_)
            S.barrier()

        with contextlib.ExitStack() as st:
            E_st = sb(st, "E_st", [128, 8, 129], F32)
            C_bf = sb(st, "C_bf", [128, 8, 129], BF16)
            ebp = sb(st, "ebp", [128, 8], F32)
            gml_sb = sb(st, "gml_sb", [128, 512], F32)
            SB_ = [Buf(f"st{g}") for g in range(8)]
            EBP = Buf("ebp")
            GB = Buf("gml")
            S.dma(gml_sb[:], W["gml"], writes=[GB])
            S.add("pool", lambda e: e.memset(E_st[:], 0.0), writes=SB_)
            S.add("pool", lambda e: e.memset(C_bf[:], 0.0), writes=SB_)
            S.add("pool", lambda e: e.memset(ebp[:], 1.0), writes=[EBP])
            qTr = Ring(nc, st, "b_qT", [128, 4, 128], BF16, 3)
            kTr = Ring(nc, st, "b_kT", [128, 4, 128], BF16, 3)
            ktr = Ring(nc, st, "b_kt", [128, 512], BF16, 3)
            vtr = Ring(nc, st, "b_vt", [128, 4, 129], BF16, 3)
            gtr = Ring(nc, st, "b_gt", [128, 16], F32, 3)
            for (v_, vb_) in zip(vtr.t, vtr.b):
                S.add("pool", lambda e, v_=v_: e.memset(v_[:], 1.0), writes=[vb_])
            smr = Ring(nc, st, "b_sm", [128, 128], BF16, 4)
            vur = Ring(nc, st, "b_vu", [128, 129], BF16, 4)
            g1r = Ring(nc, st, "b_g1", [128, 32], F32, 3)
            smallr = Ring(nc, st, "b_sml", [128, 4], F32, 8)
            hfr = Ring(nc, st, "b_hf", [128, 512], F32, 3)
            hbr = Ring(nc, st, "b_hb", [128, 512], F32, 2)
            mor = Ring(nc, st, "b_mo", [128, 512], BF16, 2)
            ybr = Ring(nc, st, "b_y", [128, 512], BF16, 2)
            yTr = Ring(nc, st, "b_yT", [128, 4, 128], BF16, 2)
            mq_v = mqT.rearrange("(h d) t -> d h t", d=128)
            mk_v = mkT.rearrange("(h d) t -> d h t", d=128)
            ya_v = yaT.rearrange("(h d) t -> d h t", d=128)
            scale_ml = 128.0 ** -0.5
            bi = [0]

            def bbank(lo, n):
                i = lo + bi[0] % n
                bi[0] += 1
                return psum[i], psb[i]

            def b_block(dr, c):
                    t0 = c * 128
                    need_out = ctx_out or c >= 2
                    q_, qb = qTr.next()
                    k_, kb = kTr.next()
                    kt_, ktb = ktr.next()
                    vt_, vtb = vtr.next()
                    gt_, gtb = gtr.next()
                    S.dma(q_[:], mq_v[:, :, t0:t0 + 128], reads=[DB["proj"]], writes=[qb])
                    S.dma(k_[:], mk_v[:, :, t0:t0 + 128], reads=[DB["proj"]], writes=[kb])
                    S.dma(kt_[:], mk_tm[t0:t0 + 128, :], reads=[DB["proj"]], writes=[ktb], q="sp")
                    S.dma(vt_[:, :, 0:128], mv_tm[t0:t0 + 128, :].rearrange("t (h e) -> t h e", e=128), reads=[DB["proj"]],
                          writes=[vtb], q="sp")
                    S.dma(gt_[:], gat_tm[t0:t0 + 128, :], reads=[DB["proj"]], writes=[gtb])
                    g1, g1b = g1r.next()
                    S.add("act", lambda e, gt_=gt_, g1=g1: e.activation(
                        out=g1[:, 16:20], in_=gt_[:, 8 + dr * 4:12 + dr * 4], func=AF.Exp, scale=-1.0), [gtb], [g1b])
                    S.add("act", lambda e, g1=g1: e.activation(out=g1[:, 0:4], in_=g1[:, 16:20], func=AF.Ln, bias=1.0, scale=1.0),
                          [g1b], [g1b])
                    S.add("dve", lambda e, g1=g1: e.tensor_scalar(out=g1[:, 0:4], in0=g1[:, 0:4], scalar1=-1.0, scalar2=None,
                                                                  op0=ALU.mult), [g1b], [g1b])
                    pg, pgb = psum[0], psb[0]
                    tri = tri_b if dr == 1 else tri_f
                    S.add("pe", lambda e, g1=g1, tri=tri: e.matmul(pg[:, 0:4], lhsT=tri[:], rhs=g1[:, 0:4], start=True, stop=True),
                          [g1b, CB], [pgb])
                    S.add("pe", lambda e, g1=g1: e.matmul(pg[:, 4:8], lhsT=ones_f[:], rhs=g1[:, 0:4], start=True, stop=True),
                          [g1b, CB], [pgb])
                    S.add("dve", lambda e, g1=g1, gt_=gt_: e.tensor_tensor(
                        out=g1[:, 16:20], in0=gt_[:, dr * 4:dr * 4 + 4], in1=pg[:, 0:4], op=ALU.subtract), [gtb, pgb, g1b], [g1b])
                    S.add("act", lambda e, g1=g1: e.activation(out=g1[:, 4:8], in_=g1[:, 16:20], func=AF.Exp), [g1b], [g1b])
                    S.add("act", lambda e, g1=g1: e.activation(out=g1[:, 8:16], in_=pg[:, 0:8], func=AF.Exp), [pgb, g1b], [g1b])
                    S.add("dve", lambda e, g1=g1: e.tensor_scalar(out=g1[:, 8:12], in0=g1[:, 8:12], scalar1=scale_ml, scalar2=None,
                                                                  op0=ALU.mult), [g1b], [g1b])
                    if need_out:
                        hf, hfb = hfr.next()
                    for h in range(4):
                        g = dr * 4 + h
                        ps_, psb_ = bbank(1, 2)
                        S.add("pe", lambda e, ps_=ps_, k_=k_, q_=q_, h=h: e.matmul(
                            ps_[:, 0:128], lhsT=k_[:, h, :], rhs=q_[:, h, :], start=True, stop=True), [kb, qb], [psb_])
                        sm_, smb = smr.next()
                        msk = m_ge if dr == 1 else m_le
                        S.add("dve", lambda e, ps_=ps_, sm_=sm_, msk=msk: e.tensor_tensor(
                            out=sm_[:], in0=ps_[:, 0:128], in1=msk[:], op=ALU.mult), [psb_, CB], [smb])
                        vu_, vub = vur.next()
                        S.add("act", lambda e, vu_=vu_, vt_=vt_, g1=g1, h=h: e.activation(
                            out=vu_[:], in_=vt_[:, h, :], func=AF.Identity, scale=g1[:, 4 + h:5 + h]), [vtb, g1b], [vub])
                        if need_out:
                            ph, phb = bbank(3, 2)
                            S.add("pe", lambda e, ph=ph, sm_=sm_, vu_=vu_: e.matmul(
                                ph[:, 0:129], lhsT=sm_[:], rhs=vu_[:], start=True, stop=False), [smb, vub], [phb])
                            S.add("pe", lambda e, ph=ph, q_=q_, h=h, g=g: e.matmul(
                                ph[:, 0:129], lhsT=q_[:, h, :], rhs=C_bf[:, g, :], start=False, stop=True), [qb, SB_[g]], [phb])
                            sm4, sm4b = smallr.next()
                            S.add("act", lambda e, ph=ph, sm4=sm4, g1=g1, h=h: e.activation(
                                out=sm4[:, 0:1], in_=ph[:, 128:129], func=AF.Abs, scale=g1[:, 8 + h:9 + h]), [phb, g1b], [sm4b])
                            S.add("dve", lambda e, sm4=sm4: e.tensor_scalar(
                                out=sm4[:, 0:1], in0=sm4[:, 0:1], scalar1=1.0, scalar2=None, op0=ALU.max), [sm4b], [sm4b])
                            S.add("dve", lambda e, sm4=sm4: e.reciprocal(out=sm4[:, 2:3], in_=sm4[:, 0:1]), [sm4b], [sm4b])
                            S.add("dve", lambda e, sm4=sm4, g1=g1, h=h: e.tensor_tensor(
                                out=sm4[:, 1:2], in0=g1[:, 8 + h:9 + h], in1=sm4[:, 2:3], op=ALU.mult), [sm4b, g1b], [sm4b])
                            S.add("act", lambda e, ph=ph, hf=hf, sm4=sm4, h=h: e.activation(
                                out=hf[:, h * 128:(h + 1) * 128], in_=ph[:, 0:128], func=AF.Identity, scale=sm4[:, 1:2]),
                                [phb, sm4b], [hfb])
                        pc, pcb = bbank(5, 2)
                        S.add("pe", lambda e, pc=pc, kt_=kt_, vu_=vu_, h=h: e.matmul(
                            pc[:, 0:129], lhsT=kt_[:, h * 128:(h + 1) * 128], rhs=vu_[:], start=True, stop=True), [ktb, vub], [pcb])
                        S.add("dve", lambda e, pc=pc, g=g: e.scalar_tensor_tensor(
                            out=E_st[:, g, :], in0=E_st[:, g, :], scalar=ebp[:, g:g + 1], in1=pc[:, 0:129],
                            op0=ALU.mult, op1=ALU.add), [pcb, EBP, SB_[g]], [SB_[g]])
                        S.add("act", lambda e, g=g, g1=g1, h=h: e.activation(
                            out=C_bf[:, g, :], in_=E_st[:, g, :], func=AF.Identity, scale=g1[:, 12 + h:13 + h]),
                            [SB_[g], g1b], [SB_[g]])
                    S.add("dve", lambda e, g1=g1: e.tensor_copy(out=ebp[:, dr * 4:dr * 4 + 4], in_=g1[:, 12:16]),
                          [g1b, EBP] + SB_[dr * 4:dr * 4 + 4], [EBP])
                    if not need_out:
                        return
                    if dr == 1:
                        S.dma(hb_tm[t0:t0 + 128, :], hf[:], reads=[hfb], writes=[DB["hb"]], q="act")
                        return
                    hb_, hbb = hbr.next()
                    mo_, mob = mor.next()
                    S.dma(hb_[:], hb_tm[t0:t0 + 128, :], reads=[DB["hb"]], writes=[hbb], q="sp")
                    S.dma(mo_[:], mo_tm[t0:t0 + 128, :], reads=[DB["proj"]], writes=[mob], q="sp")
                    S.add("dve", lambda e, hf=hf, hb_=hb_: e.tensor_tensor(out=hf[:], in0=hf[:], in1=hb_[:], op=ALU.add),
                          [hfb, hbb], [hfb])
                    sm4, sm4b = smallr.next()
                    S.add("dve", lambda e, hb_=hb_, hf=hf: e.tensor_tensor(out=hb_[:], in0=hf[:], in1=hf[:], op=ALU.mult),
                          [hfb, hbb], [hbb])
                    S.add("dve", lambda e, hb_=hb_, sm4=sm4: e.tensor_reduce(
                        out=sm4[:, 0:4], in_=hb_[:].rearrange("p (h e) -> p h e", e=128), axis=AX.X, op=ALU.add), [hbb], [sm4b])
                    S.add("act", lambda e, sm4=sm4: e.activation(out=sm4[:, 0:4], in_=sm4[:, 0:4], func=AF.Ln, bias=eps_sb[:, 0:1],
                                                                 scale=1.0 / 128.0), [sm4b, CB], [sm4b])
                    S.add("act", lambda e, sm4=sm4: e.activation(out=sm4[:, 0:4], in_=sm4[:, 0:4], func=AF.Exp, scale=-0.5),
                          [sm4b], [sm4b])
                    for h in range(4):
                        S.add("dve", lambda e, hf=hf, sm4=sm4, h=h: e.scalar_tensor_tensor(
                            out=hf[:, h * 128:(h + 1) * 128], in0=hf[:, h * 128:(h + 1) * 128], scalar=sm4[:, h:h + 1],
                            in1=gml_sb[:, h * 128:(h + 1) * 128], op0=ALU.mult, op1=ALU.mult), [hfb, sm4b, GB], [hfb])
                    y_, yb_ = ybr.next()
                    S.add("dve", lambda e, hf=hf, mo_=mo_, y_=y_: e.tensor_tensor(out=y_[:], in0=hf[:], in1=mo_[:], op=ALU.mult),
                          [hfb, mob], [yb_])
                    pt, ptb = psum[7], psb[7]
                    ptv = pt[:].bitcast(BF16)
                    for h in range(4):
                        S.add("pe", lambda e, y_=y_, h=h, ptv=ptv: e.transpose(
                            out=ptv[:, h * 128:(h + 1) * 128], in_=y_[:, h * 128:(h + 1) * 128], identity=ident[:]), [yb_, CB], [ptb])
                    yT_, yTb = yTr.next()
                    S.add("act", lambda e, yT_=yT_, ptv=ptv: e.copy(out=yT_[:].rearrange("p a b -> p (a b)"), in_=ptv[:, 0:512]),
                          [ptb], [yTb])
                    S.dma(ya_v[:, :, t0:t0 + 128], yT_[:], reads=[yTb], writes=[DB["ya"]], q="act")

            for dr_ in (1, 0):
                order = [1, 0] + list(range(NB - 1, 1, -1)) if dr_ == 1 else list(range(NB))
                for c_ in order:
                    b_block(dr_, c_)
            S.barrier()

        with contextlib.ExitStack() as st:
            KT = sb(st, "KT", [128, T], BF16)
            VV = sb(st, "VV", [128, NB, 2, 65], BF16)
            sink_sb = sb(st, "sink_sb", [128, 2, 512], F32)
            KB_ = Buf("KT")
            S.dma(sink_sb[:], W["sink"], writes=[KB_])
            S.add("act", lambda e: e.activation(out=sink_sb[:], in_=sink_sb[:], func=AF.Exp), [KB_], [KB_])
            qr = Ring(nc, st, "c_q", [128, 4, 512], BF16, 2)
            ptr = Ring(nc, st, "c_pt", [128, 1024], BF16, 3)
            ostg = Ring(nc, st, "c_o", [64, 8, 512], BF16, 2)
            rdr = Ring(nc, st, "c_rd", [128, 512], F32, 2)
            bcr = Ring(nc, st, "c_bc", [64, 512], F32, 2)
            ci = [0]

            def cbank(lo, n):
                i = lo + ci[0] % n
                ci[0] += 1
                return psum[i], psb[i]

            for kind in ("win", "glb"):
                kT_src = wkT if kind == "win" else gkT
                vcol = 0 if kind == "win" else 128
                q_src = (wqT if kind == "win" else gqT).rearrange("(j p) t -> p j t", p=128)
                y_dst = ybT if kind == "win" else ycT
                ydb = DB["yb"] if kind == "win" else DB["yc"]
                for (t0, tt) in tiles:
                    S.dma(KT[:, t0:t0 + tt], kT_src[:, t0:t0 + tt], reads=[DB["proj"]], writes=[KB_])
                S.add("pool", lambda e: e.memset(VV[:], 1.0), writes=[KB_])
                for b0 in range(0, NB, 26):
                    b1 = min(NB, b0 + 26)
                    for g in range(2):
                        S.dma(VV[:, b0:b1, g, 0:64],
                              wgv_tm[b0 * 128:b1 * 128, vcol + g * 64:vcol + g * 64 + 64].rearrange("(b p) d -> p b d", p=128),
                              reads=[DB["proj"]], writes=[KB_], q="pool")
                groups = []
                for (t0, tt) in tiles:
                    if t0 == 0 and not ctx_out:
                        continue
                    nblk = tt // 128
                    for blk in range(nblk):
                        c = t0 // 128 + blk
                        if c < 2:
                            kbs = [0, 1]
                        elif kind == "glb":
                            kbs = list(range(NB))
                        else:
                            kbs = ([c - 1] if c - 1 >= 2 else []) + [c] + ([c + 1] if c + 1 < NB else []) + [0, 1]
                        groups.append(dict(t0=t0, tt=tt, blk=blk, c=c, kbs=kbs, tile_last=(blk == nblk - 1)))
                units = []
                for gi, G in enumerate(groups):
                    kbs = G["kbs"]
                    for i, kb_ in enumerate(kbs):
                        units.append(dict(gi=gi, kb=kb_, first=(i == 0), last=(i == len(kbs) - 1), idx=len(units)))
                tile_state = {}

                def get_tile(t0, tt, kind=kind, q_src=q_src, tile_state=tile_state):
                    if t0 not in tile_state:
                        q_, qb = qr.next()
                        S.dma(q_[:, :, 0:tt], q_src[:, :, t0:t0 + tt], reads=[DB["proj"]], writes=[qb])
                        os_, osb = ostg.next()
                        tile_state[t0] = (q_, qb, os_, osb)
                    return tile_state[t0]

                def emit_qk(u, kind=kind, groups=groups):
                    G = groups[u["gi"]]
                    blk, c, kb_ = G["blk"], G["c"], u["kb"]
                    q_, qb, os_, osb = get_tile(G["t0"], G["tt"])
                    slot = u["idx"] % 2
                    for g in range(2):
                        ps_ = psum[2 * slot + g]
                        S.add("pe", lambda e, ps_=ps_, kb_=kb_, g=g, q_=q_, blk=blk: e.matmul(
                            ps_.rearrange("p (r q) -> p r q", q=128), lhsT=KT[g * 64:(g + 1) * 64, kb_ * 128:(kb_ + 1) * 128],
                            rhs=q_[g * 64:(g + 1) * 64, :, blk * 128:(blk + 1) * 128], start=True, stop=True),
                            [KB_, qb], [psb[2 * slot + g]])
                    pt_, ptb_ = ptr.next()
                    S.add("act", lambda e, slot=slot, pt_=pt_: e.activation(
                        out=pt_[:, 0:1024], in_=psum2[slot][:, 0:1024], func=AF.Exp, scale=0.125),
                        [psb[2 * slot], psb[2 * slot + 1]], [ptb_])
                    msk = None
                    if kind == "win" and c >= 3 and kb_ == c - 1:
                        msk = m_ge
                    if kind == "win" and c >= 2 and kb_ == c + 1:
                        msk = m_le
                    if msk is not None:
                        S.add("dve", lambda e, pt_=pt_, msk=msk: e.tensor_tensor(
                            out=pt_[:, 0:1024].rearrange("p (r q) -> p r q", q=128),
                            in0=pt_[:, 0:1024].rearrange("p (r q) -> p r q", q=128),
                            in1=msk[:].unsqueeze(1).to_broadcast([128, 8, 128]), op=ALU.mult), [ptb_, CB], [ptb_])
                    u["pt"] = (pt_, ptb_)

                def emit_pv(u, kind=kind, groups=groups, y_dst=y_dst, ydb=ydb):
                    G = groups[u["gi"]]
                    blk, kb_ = G["blk"], u["kb"]
                    q_, qb, os_, osb = get_tile(G["t0"], G["tt"])
                    pt_, ptb_ = u["pt"]
                    for g in range(2):
                        po, pob = psum[4 + g], psb[4 + g]
                        S.add("pe", lambda e, po=po, kb_=kb_, g=g, pt_=pt_, st_=u["first"], sp_=u["last"]: e.matmul(
                            po[0:65, :], lhsT=VV[:, kb_, g, :], rhs=pt_[:, g * 512:(g + 1) * 512], start=st_, stop=sp_),
                            [KB_, ptb_], [pob])
                    if not u["last"]:
                        return
                    for g in range(2):
                        po, pob = psum[4 + g], psb[4 + g]
                        rd, rdb = rdr.next()
                        if kind == "win":
                            S.add("dve", lambda e, po=po, rd=rd, g=g: e.tensor_tensor(
                                out=rd[64:65, :], in0=po[64:65, :], in1=sink_sb[64:65, g, :], op=ALU.add), [pob, KB_], [rdb])
                            S.add("dve", lambda e, rd=rd: e.reciprocal(out=rd[64:65, :], in_=rd[64:65, :]), [rdb], [rdb])
                        else:
                            S.add("dve", lambda e, po=po, rd=rd: e.reciprocal(out=rd[64:65, :], in_=po[64:65, :]), [pob], [rdb])
                        pbc, pbcb = psum[6 + g], psb[6 + g]
                        S.add("pe", lambda e, pbc=pbc, rd=rd: e.matmul(
                            pbc[0:64, :], lhsT=ones_f[64:65, 0:64], rhs=rd[64:65, :], start=True, stop=True), [rdb, CB], [pbcb])
                        bc, bcb = bcr.next()
                        S.add("dve", lambda e, pbc=pbc, bc=bc: e.tensor_copy(out=bc[:], in_=pbc[0:64, :]), [pbcb], [bcb])
                        S.add("dve", lambda e, po=po, bc=bc, os_=os_, g=g, blk=blk: e.tensor_tensor(
                            out=os_[:, g * 4:(g + 1) * 4, blk * 128:(blk + 1) * 128],
                            in0=po[0:64, :].rearrange("p (r q) -> p r q", q=128),
                            in1=bc[:].rearrange("p (r q) -> p r q", q=128), op=ALU.mult), [pob, bcb], [osb])
                    if G["tile_last"]:
                        t0, tt = G["t0"], G["tt"]
                        S.dma(y_dst[:, :, t0:t0 + tt], os_[:, :, 0:tt], reads=[osb], writes=[ydb], q="pool")

                LOOK = 2
                for i in range(min(LOOK, len(units))):
                    emit_qk(units[i])
                for i, u in enumerate(units):
                    emit_pv(u)
                    if i + LOOK < len(units):
                        emit_qk(units[i + LOOK])
            S.barrier()

        with contextlib.ExitStack() as st:
            xr = Ring(nc, st, "e_x", [128, KC, 512], F32, 1)
            yar = Ring(nc, st, "e_ya", [128, 4, 512], BF16, 1)
            ybr2 = Ring(nc, st, "e_yb", [64, 8, 512], BF16, 1)
            ycr = Ring(nc, st, "e_yc", [64, 8, 512], BF16, 1)
            bgr = Ring(nc, st, "e_bg", [128, 24, 512], BF16, 1)
            mgr = Ring(nc, st, "e_mg", [128, KC, 512], BF16, 1)
            h2r = Ring(nc, st, "e_h2", [128, KC, 512], BF16, 1)
            sqr = Ring(nc, st, "e_sq", [128, KC, 512], BF16, 1)
            ur = Ring(nc, st, "e_u", [128, NFC, 512], BF16, 1)
            w0r = Ring(nc, st, "e_w0", [128, 4, 128], BF16, 3)
            w12r = Ring(nc, st, "e_w12", [64, 8, 128], BF16, 4)
            wor = Ring(nc, st, "e_wo", [128, KC, 128], BF16, 6)
            w2r = Ring(nc, st, "e_w2", [128, NFC, 128], BF16, 2)
            f32r = Ring(nc, st, "e_f", [128, 512], F32, 6)
            rsr = Ring(nc, st, "e_rs", [128, 512], F32, 2)
            fg_sb = sb(st, "fg_sb", [128, KC], F32)
            FGB = Buf("fing")
            S.dma(fg_sb[:], fing, writes=[FGB])
            ei = [0]

            def ebank(lo, n):
                i = lo + ei[0] % n
                ei[0] += 1
                return psum[i], psb[i]

            def rms(x_, xb, tt):
                sq_, sqb = sqr.next()
                S.add("act", lambda e: e.activation(out=sq_[:, :, 0:tt], in_=x_[:, :, 0:tt], func=AF.Square), [xb], [sqb])
                pm, pmb = psum[0], psb[0]
                for kc in range(KC):
                    S.add("pe", lambda e, kc=kc: e.matmul(pm[:, 0:tt], lhsT=onesm[:], rhs=sq_[:, kc, 0:tt],
                                                          start=(kc == 0), stop=(kc == KC - 1)), [sqb, CB], [pmb])
                rs_, rsb = rsr.next()
                S.add("act", lambda e: e.activation(out=rs_[:, 0:tt], in_=pm[:, 0:tt], func=AF.Ln, bias=eps_sb[:, 0:1], scale=1.0),
                      [pmb, CB], [rsb])
                S.add("act", lambda e: e.activation(out=rs_[:, 0:tt], in_=rs_[:, 0:tt], func=AF.Exp, scale=-0.5), [rsb], [rsb])
                return rs_, rsb

            dbg_mg = dscr(f"dbg_mg{l}", [D, T], BF16) if dbg else None
            dbg_x1 = dscr(f"dbg_x1{l}", [D, T], F32) if dbg else None

            def e_tile(t0, tt):
                wsel = 1 if t0 == 0 else 0
                x_, xb = xr.next()
                S.dma(x_[:, :, 0:tt], xin_v[:, :, t0:t0 + tt], reads=[DB["xs"]], writes=[xb])
                ya_, yab = yar.next()
                yb_, ybb = ybr2.next()
                yc_, ycb = ycr.next()
                bg_, bgb = bgr.next()
                S.dma(ya_[:, :, 0:tt], yaT.rearrange("(h d) t -> d h t", d=128)[:, :, t0:t0 + tt], reads=[DB["ya"]], writes=[yab], q="act")
                S.dma(yb_[:, :, 0:tt], ybT[:, :, t0:t0 + tt], reads=[DB["yb"]], writes=[ybb], q="pool")
                S.dma(yc_[:, :, 0:tt], ycT[:, :, t0:t0 + tt], reads=[DB["yc"]], writes=[ycb], q="pool")
                S.dma(bg_[:, :, 0:tt], bgT.rearrange("(c p) t -> p c t", p=128)[:, :, t0:t0 + tt], reads=[DB["proj"]], writes=[bgb], q="act")
                mg_, mgb = mgr.next()
                for oc in range(8):
                    wa, wab = w0r.next()
                    wb1, wb1b = w12r.next()
                    wb2, wb2b = w12r.next()
                    S.dma(wa[:].rearrange("p a b -> p (a b)"), b_wbr0[oc], reads=[DB["w"]], writes=[wab], q=wq())
                    S.dma(wb1[:].rearrange("p a b -> p (a b)"), b_wbr1[oc], reads=[DB["w"]], writes=[wb1b], q=wq())
                    S.dma(wb2[:].rearrange("p a b -> p (a b)"), b_wbr2[oc], reads=[DB["w"]], writes=[wb2b], q=wq())
                    p0, p0b = ebank(1, 3)
                    for k in range(4):
                        S.add("pe", lambda e, k=k, p0=p0, wa=wa: e.matmul(p0[:, 0:tt], lhsT=wa[:, k, :], rhs=ya_[:, k, 0:tt],
                                                                          start=(k == 0), stop=(k == 3)), [wab, yab], [p0b])
                    p1, p1b = ebank(1, 3)
                    for k in range(8):
                        S.add("pe", lambda e, k=k, p1=p1, wb1=wb1: e.matmul(p1[:, 0:tt], lhsT=wb1[:, k, :], rhs=yb_[:, k, 0:tt],
                                                                            start=(k == 0), stop=(k == 7)), [wb1b, ybb], [p1b])
                    p2, p2b = ebank(1, 3)
                    for k in range(8):
                        S.add("pe", lambda e, k=k, p2=p2, wb2=wb2: e.matmul(p2[:, 0:tt], lhsT=wb2[:, k, :], rhs=yc_[:, k, 0:tt],
                                                                            start=(k == 0), stop=(k == 7)), [wb2b, ycb], [p2b])
                    a0, a0b = f32r.next()
                    a1, a1b = f32r.next()
                    a2, a2b = f32r.next()
                    S.add("dve", lambda e, p0=p0, a0=a0, oc=oc: e.tensor_tensor(
                        out=a0[:, 0:tt], in0=p0[:, 0:tt], in1=bg_[:, oc, 0:tt], op=ALU.mult), [p0b, bgb], [a0b])
                    S.add("dve", lambda e, p1=p1, a1=a1, oc=oc: e.tensor_tensor(
                        out=a1[:, 0:tt], in0=p1[:, 0:tt], in1=bg_[:, 8 + oc, 0:tt], op=ALU.mult), [p1b, bgb], [a1b])
                    S.add("dve", lambda e, p2=p2, a2=a2, oc=oc: e.tensor_tensor(
                        out=a2[:, 0:tt], in0=p2[:, 0:tt], in1=bg_[:, 16 + oc, 0:tt], op=ALU.mult), [p2b, bgb], [a2b])
                    S.add("pool", lambda e, a0=a0, a1=a1: e.tensor_tensor(out=a0[:, 0:tt], in0=a0[:, 0:tt], in1=a1[:, 0:tt], op=ALU.add),
                          [a0b, a1b], [a0b])
                    S.add("pool", lambda e, a0=a0, a2=a2, oc=oc: e.tensor_tensor(
                        out=mg_[:, oc, 0:tt], in0=a0[:, 0:tt], in1=a2[:, 0:tt], op=ALU.add), [a0b, a2b], [mgb])
                if dbg:
                    S.dma(dbg_mg.rearrange("(k p) t -> p k t", p=128)[:, :, t0:t0 + tt], mg_[:, :, 0:tt], reads=[mgb])
                for oc in range(8):
                    w_, wb_ = wor.next()
                    S.dma(w_[:].rearrange("p a b -> p (a b)"), b_wo[oc], reads=[DB["w"]], writes=[wb_], q=wq())
                    po, pob = ebank(4, 2)
                    for k in range(KC):
                        S.add("pe", lambda e, k=k, po=po, w_=w_: e.matmul(po[:, 0:tt], lhsT=w_[:, k, :], rhs=mg_[:, k, 0:tt],
                                                                          start=(k == 0), stop=(k == KC - 1)), [wb_, mgb], [pob])
                    S.add("dve", lambda e, po=po, oc=oc: e.scalar_tensor_tensor(
                        out=x_[:, oc, 0:tt], in0=po[:, 0:tt], scalar=MOD[:, 16 + oc, wsel:wsel + 1], in1=x_[:, oc, 0:tt],
                        op0=ALU.mult, op1=ALU.add), [pob, MB, xb], [xb])
                if dbg:
                    S.dma(dbg_x1.rearrange("(k p) t -> p k t", p=128)[:, :, t0:t0 + tt], x_[:, :, 0:tt], reads=[xb], writes=[xb])
                rs_, rsb = rms(x_, xb, tt)
                h2, h2b = h2r.next()
                for kc in range(KC):
                    tm_, tmb = f32r.next()
                    S.add("dve" if kc % 2 == 0 else "pool", lambda e, kc=kc, tm_=tm_: e.tensor_tensor(
                        out=tm_[:, 0:tt], in0=x_[:, kc, 0:tt], in1=rs_[:, 0:tt], op=ALU.mult), [xb, rsb], [tmb])
                    S.add("act", lambda e, kc=kc, tm_=tm_: e.activation(
                        out=h2[:, kc, 0:tt], in_=tm_[:, 0:tt], func=AF.Identity,
                        scale=G2[:, kc, wsel:wsel + 1], bias=MOD[:, 24 + kc, wsel:wsel + 1]), [tmb, MB], [h2b])
                u_, ub = ur.next()
                for fc in range(NFC):
                    w1_, w1b = wor.next()
                    w3_, w3b = wor.next()
                    S.dma(w1_[:].rearrange("p a b -> p (a b)"), b_w1[fc], reads=[DB["w"]], writes=[w1b], q="sp")
                    S.dma(w3_[:].rearrange("p a b -> p (a b)"), b_w3[fc], reads=[DB["w"]], writes=[w3b], q="sp")
                    pa, pab = ebank(6, 2)
                    for k in range(KC):
                        S.add("pe", lambda e, k=k, pa=pa, w1_=w1_: e.matmul(pa[:, 0:tt], lhsT=w1_[:, k, :], rhs=h2[:, k, 0:tt],
                                                                            start=(k == 0), stop=(k == KC - 1)), [w1b, h2b], [pab])
                    pb3, pb3b = ebank(6, 2)
                    for k in range(KC):
                        S.add("pe", lambda e, k=k, pb3=pb3, w3_=w3_: e.matmul(pb3[:, 0:tt], lhsT=w3_[:, k, :], rhs=h2[:, k, 0:tt],
                                                                              start=(k == 0), stop=(k == KC - 1)), [w3b, h2b], [pb3b])
                    sl, slb = f32r.next()
                    S.add("act", lambda e, pa=pa, sl=sl: e.activation(out=sl[:, 0:tt], in_=pa[:, 0:tt], func=AF.Silu), [pab], [slb])
                    S.add("dve", lambda e, pb3=pb3, sl=sl, fc=fc: e.tensor_tensor(
                        out=u_[:, fc, 0:tt], in0=pb3[:, 0:tt], in1=sl[:, 0:tt], op=ALU.mult), [pb3b, slb], [ub])
                for oc in range(8):
                    w_, wb_ = w2r.next()
                    S.dma(w_[:].rearrange("p a b -> p (a b)"), b_w2[oc], reads=[DB["w"]], writes=[wb_], q=wq())
                    po, pob = ebank(4, 2)
                    for k in range(NFC):
                        S.add("pe", lambda e, k=k, po=po, w_=w_: e.matmul(po[:, 0:tt], lhsT=w_[:, k, :], rhs=u_[:, k, 0:tt],
                                                                          start=(k == 0), stop=(k == NFC - 1)), [wb_, ub], [pob])
                    S.add("dve", lambda e, po=po, oc=oc: e.scalar_tensor_tensor(
                        out=x_[:, oc, 0:tt], in0=po[:, 0:tt], scalar=MOD[:, 40 + oc, wsel:wsel + 1], in1=x_[:, oc, 0:tt],
                        op0=ALU.mult, op1=ALU.add), [pob, MB, xb], [xb])
                if l < depth - 1:
                    S.dma(xs_v[:, :, t0:t0 + tt], x_[:, :, 0:tt], reads=[xb], writes=[DB["xs"]], q="pool")
                else:
                    rs2_, rsb2 = rms(x_, xb, tt)
                    for kc in range(KC):
                        S.add("dve", lambda e, kc=kc: e.scalar_tensor_tensor(
                            out=x_[:, kc, 0:tt], in0=x_[:, kc, 0:tt], scalar=fg_sb[:, kc:kc + 1], in1=rs2_[:, 0:tt],
                            op0=ALU.mult, op1=ALU.mult), [xb, rsb2, FGB], [xb])
                    S.dma(yT.rearrange("(k p) t -> p k t", p=128)[:, :, t0 - 256:t0 - 256 + tt], x_[:, :, 0:tt], reads=[xb], q="pool")

            for (t0_, tt_) in tiles:
                if t0_ == 0 and not ctx_out:
                    continue
                e_tile(t0_, tt_)
            S.barrier()
        lay.close()

    S.emit()
    return nc


def _swap_d(d):
    r = d % 32
    return d + 16 if r < 16 else d - 16


def _fm_cols():
    OFF = dict(mq=0, mk=512, mv=1024, mo=1536, ig=2048, fg=2056, wq=2064, wk=2576, wv=2704, gq=2832, gk=3344, gv=3472, bg=3600)
    chunks = []
    for h in range(4):
        chunks.append([OFF["mq"] + h * 128 + d for d in range(128)])
    for h in range(4):
        chunks.append([OFF["mk"] + h * 128 + d for d in range(128)])
    for pre in ("w", "g"):
        q0, k0 = OFF[pre + "q"], OFF[pre + "k"]
        for sw in (False, True):
            for j in range(4):
                cols = []
                for p in range(128):
                    head = j if p < 64 else 4 + j
                    d = p % 64
                    dd = _swap_d(d) if sw else d
                    cols.append(q0 + head * 64 + dd)
                chunks.append(cols)
        for sw in (False, True):
            cols = []
            for p in range(128):
                head = p // 64
                d = p % 64
                dd = _swap_d(d) if sw else d
                cols.append(k0 + head * 64 + dd)
            chunks.append(cols)
    for c in range(24):
        chunks.append([OFF["bg"] + c * 128 + p for p in range(128)])
    assert len(chunks) == NFM
    tm = (list(range(512, 1024)) + list(range(1024, 1536)) + list(range(1536, 2048)) + list(range(2704, 2832)) +
          list(range(3472, 3600)) + list(range(2048, 2064)))
    assert len(tm) == TMW
    return np.array(chunks), np.array(tm)


def _pack(w, kp):
    K, N = w.shape
    return np.ascontiguousarray(w.reshape(K // kp, kp, N // 128, 128).transpose(2, 1, 0, 3))


def _rope_tables(nlat):
    T = nlat + CTX
    cos = np.ones((128, T), np.float32)
    sin = np.zeros((128, T), np.float32)
    t = np.arange(nlat)
    row = (t // 64).astype(np.float32)
    col = (t % 64).astype(np.float32)
    half = 32
    inv = (10000.0 ** (-np.arange(0, half, 2, dtype=np.float32) / half)).astype(np.float32)
    for p in range(128):
        d = p % 64
        pos = row if d < 32 else col
        ang = (pos * inv[d % 16]).astype(np.float32)
        sign = -1.0 if (d % 32) < 16 else 1.0
        cos[p, CTX:] = np.cos(ang)
        sin[p, CTX:] = sign * np.sin(ang)
    return cos, sin


def prep_shared(inp, nlat, depth=DEPTH):
    fm, tm = _fm_cols()
    sh = {}
    cos, sin = _rope_tables(nlat)
    sh["cos"], sh["sins"] = cos, sin
    sh["fing"] = np.ascontiguousarray(np.asarray(inp["final_g"]).reshape(KC, 128).T)
    swp = np.array([_swap_d(p % 64) for p in range(128)])
    for l in range(depth):
        w_in = np.asarray(inp["w_in"][l])
        b_in = np.asarray(inp["b_in"][l])
        sh[f"wmod{l}"] = _pack(np.asarray(inp["w_mod"][l]), 128)
        sh[f"bmod{l}"] = np.ascontiguousarray(np.asarray(inp["b_mod"][l]).reshape(48, 128).T)
        sh[f"n1g{l}"] = np.ascontiguousarray(np.asarray(inp["norm1_g"][l]).reshape(KC, 128).T)
        sh[f"n2g{l}"] = np.ascontiguousarray(np.asarray(inp["norm2_g"][l]).reshape(KC, 128).T)
        wf = w_in[:, fm.reshape(-1)]
        sh[f"wfm{l}"] = _pack(wf, 128)
        sh[f"bfm{l}"] = np.ascontiguousarray(b_in[fm].T)
        sh[f"wtm{l}"] = np.ascontiguousarray(w_in[:, tm].reshape(KC, 128, TMW).transpose(1, 0, 2))
        sh[f"btm{l}"] = np.ascontiguousarray(np.broadcast_to(b_in[tm][None, :], (128, TMW)))
        sh[f"gml{l}"] = np.ascontiguousarray(np.broadcast_to(np.asarray(inp["ml_norm_g"][l]).reshape(1, 512), (128, 512)))
        snk = np.repeat(np.asarray(inp["win_sink"][l]), 128).reshape(1, 2, 512)
        sh[f"sink{l}"] = np.ascontiguousarray(np.broadcast_to(snk, (128, 2, 512)))
        for nm, key in (("qng", "qn_g"), ("kng", "kn_g")):
            g = np.asarray(inp[key][l])
            sh[f"{nm}{l}"] = np.ascontiguousarray(np.stack([g[np.arange(128) % 64], g[swp]], axis=1))
        w_br = np.asarray(inp["w_br"][l])
        sh[f"wbr0{l}"] = _pack(w_br[0], 128)
        sh[f"wbr1{l}"] = _pack(w_br[1], 64).transpose(0, 1, 2, 3).copy()
        sh[f"wbr2{l}"] = _pack(w_br[2], 64).copy()
        sh[f"wo{l}"] = _pack(np.asarray(inp["w_o"][l]), 128)
        sh[f"w1{l}"] = _pack(np.asarray(inp["w_ff1"][l]), 128)
        sh[f"w3{l}"] = _pack(np.asarray(inp["w_ff3"][l]), 128)
        sh[f"w2{l}"] = _pack(np.asarray(inp["w_ff2"][l]), 128)
    return {k: np.ascontiguousarray(v, dtype=np.float32) for k, v in sh.items()}


def prep_core(inp, b):
    x = np.asarray(inp["x"][b])
    ctx = np.asarray(inp["ctx"][b])
    xT0 = np.ascontiguousarray(np.concatenate([ctx, x], axis=0).T)
    cc = np.stack([np.asarray(inp["c"][b]), np.asarray(inp["c_ctx"])], axis=1)
    cT = np.ascontiguousarray(cc.reshape(KC, 128, 2).transpose(1, 0, 2))
    return {"xT0": xT0.astype(np.float32), "cT": cT.astype(np.float32)}


_CACHE = {}


def kernel(**inp):
    x = np.asarray(inp["x"])
    B, nlat, _ = x.shape
    depth = np.asarray(inp["w_in"]).shape[0]
    key = (nlat, depth)
    if key not in _CACHE:
        _CACHE[key] = build(nlat // 128, depth)
    nc = _CACHE[key]
    sh = prep_shared(inp, nlat, depth)
    in_maps = []
    for b in range(B):
        m = dict(sh)
        m.update(prep_core(inp, b))
        in_maps.append(m)
    res = run_bass_kernel_spmd(nc, in_maps, core_ids=list(range(B)))
    out = np.stack([np.ascontiguousarray(r["yT"].T) for r in res.results], axis=0)
    return out.astype(np.float32)
```

```python
import contextlib
import numpy as np
import ml_dtypes
import concourse.bass as bass
import concourse.mybir as mybir
from concourse.bass_utils import run_bass_kernel_spmd

F32 = mybir.dt.float32
BF16 = mybir.dt.bfloat16
AF = mybir.ActivationFunctionType
ALU = mybir.AluOpType
AX = mybir.AxisListType

D = 1024
KC = 8
DFF = 2816
NFC = 22
EPS = 1e-6
NFM = 52
TMW = 1808
DEPTH = 2
SEQ = 16384
CTX = 256


_ALLBUFS = []


class Buf:
    __slots__ = ("ws", "rs", "name")

    def __init__(self, name=""):
        self.ws = []
        self.rs = []
        self.name = name
        _ALLBUFS.append(self)


class Op:
    __slots__ = ("eng", "fn", "deps", "dma", "signal", "cnt", "idx")

    def __init__(self, eng, fn, dma):
        self.eng = eng
        self.fn = fn
        self.dma = dma
        self.deps = []
        self.signal = False
        self.cnt = 0
        self.idx = 0


ENGS = ("pe", "act", "dve", "pool", "sp")
EPOCH = 20000
NDMA = 8


class Sched:
    def __init__(self, nc, strict_same=True):
        self.nc = nc
        self.ops = {e: [] for e in ENGS}
        self.strict_same = strict_same
        self.pending_dma = {e: [] for e in ENGS}
        self.last_real = {e: None for e in ENGS}

    def add(self, eng, fn, reads=(), writes=(), dma=False, force=()):
        op = Op(eng, fn, dma)
        deps = []
        for b in reads:
            deps.extend(b.ws)
        acc = []
        for b in writes:
            a = dma and len(b.ws) > 0 and all(x.dma for x in b.ws) and not b.rs
            acc.append(a)
            if not a:
                deps.extend(b.ws)
                deps.extend(b.rs)
        for b in reads:
            b.rs.append(op)
        for b, a in zip(writes, acc):
            if a:
                b.ws.append(op)
            else:
                b.ws = [op]
                b.rs = []
        best = {}
        dmadeps = []
        seen = set()
        for d in list(deps) + list(force):
            if d is op or d is None:
                continue
            if d.dma:
                if id(d) not in seen:
                    seen.add(id(d))
                    dmadeps.append(d)
            else:
                if d.eng == eng and (eng == "pe" or not self.strict_same) and d not in force:
                    continue
                cur = best.get(d.eng)
                if cur is None or d.idx > cur.idx:
                    best[d.eng] = d
        op.deps = list(best.values()) + dmadeps
        for d in op.deps:
            d.signal = True
        op.idx = len(self.ops[eng])
        self.ops[eng].append(op)
        if dma:
            self.pending_dma[eng].append(op)
        elif fn is not None:
            self.last_real[eng] = op
        return op

    def dma(self, out, in_, reads=(), writes=(), q="sp"):
        return self.add(q, lambda e: e.dma_start(out=out, in_=in_), reads, writes, dma=True)

    def barrier(self):
        firsts = []
        for e in ENGS:
            f = [self.last_real[e]] + self.pending_dma[e]
            self.pending_dma[e] = []
            firsts.append(self.add(e, None, force=[x for x in f if x is not None]))
        for e in ENGS:
            self.add(e, None, force=firsts)
        for b in _ALLBUFS:
            b.ws = []
            b.rs = []

    def emit(self):
        nc = self.nc
        sems = {}
        for e in ENGS:
            n = 0
            k = 0
            for op in self.ops[e]:
                if op.dma:
                    op.cnt = k
                    k += 1
                elif op.signal:
                    op.cnt = n
                    n += 1
            nep = (n + EPOCH - 1) // EPOCH
            sems[e] = [nc.alloc_semaphore(name=f"c_{e}_{i}") for i in range(nep)]
            sems[e + "_dma"] = [nc.alloc_semaphore(name=f"d_{e}_{i}") for i in range(min(NDMA, k))]

        def run_engine(e, h):
            waited = {x: -1 for x in ENGS}
            dma_done = set()
            for op in self.ops[e]:
                for d in op.deps:
                    if d.dma:
                        key = (d.eng, d.cnt)
                        if key in dma_done:
                            continue
                        h.wait_ge(sems[d.eng + "_dma"][d.cnt % NDMA], 16 * (d.cnt // NDMA + 1))
                        dma_done.add(key)
                    else:
                        if waited[d.eng] >= d.cnt:
                            continue
                        h.wait_ge(sems[d.eng][d.cnt // EPOCH], d.cnt % EPOCH + 1)
                        waited[d.eng] = d.cnt
                if op.dma:
                    s = sems[e + "_dma"][op.cnt % NDMA]
                    if op.cnt >= NDMA:
                        key = (e, op.cnt - NDMA)
                        if key not in dma_done:
                            h.wait_ge(s, 16 * (op.cnt // NDMA))
                            dma_done.add(key)
                    op.fn(h).then_inc(s, 16)
                elif op.fn is None:
                    if op.signal:
                        h.sem_inc(sems[e][op.cnt // EPOCH], 1)
                else:
                    ins = op.fn(h)
                    if op.signal:
                        ins.then_inc(sems[e][op.cnt // EPOCH], 1)
            k = sum(1 for op in self.ops[e] if op.dma)
            for j in range(min(NDMA, k)):
                last = ((k - 1 - j) // NDMA) * NDMA + j
                if (e, last) not in dma_done:
                    h.wait_ge(sems[e + "_dma"][j], 16 * (last // NDMA + 1))

        with nc.Block() as block:
            @block.sync
            def _(h):
                run_engine("sp", h)

            @block.tensor
            def _(h):
                run_engine("pe", h)

            @block.scalar
            def _(h):
                run_engine("act", h)

            @block.vector
            def _(h):
                run_engine("dve", h)

            @block.gpsimd
            def _(h):
                run_engine("pool", h)


_UID = [0]


class Ring:
    def __init__(self, nc, stack, name, shape, dtype, n):
        _UID[0] += 1
        name = f"{name}u{_UID[0]}"
        self.t = [stack.enter_context(nc.sbuf_tensor(f"{name}_{i}", shape, dtype)) for i in range(n)]
        self.b = [Buf(f"{name}_{i}") for i in range(n)]
        self.i = 0

    def next(self):
        i = self.i
        self.i = (i + 1) % len(self.t)
        return self.t[i], self.b[i]


def build(NLB, depth=DEPTH, dbg=False):
    assert NLB % 4 == 0
    NB = NLB + 2
    T = NB * 128
    tiles = [(0, 256)] + [(256 + 512 * i, 512) for i in range(NLB // 4)]
    nc = bass.Bass("TRN2", target_bir_lowering=False)
    S = Sched(nc)

    def din(name, shape, dt=F32):
        return nc.dram_tensor(name, list(shape), dt, kind="ExternalInput").ap()

    def dscr(name, shape, dt):
        return nc.dram_tensor(name, list(shape), dt, kind=("ExternalOutput" if dbg else "Internal")).ap()

    xT0 = din("xT0", [D, T])
    cT = din("cT", [128, KC, 2])
    cosd = din("cos", [128, T])
    sind = din("sins", [128, T])
    fing = din("fing", [128, KC])
    L = []
    for l in range(depth):
        L.append(dict(
            wmod=din(f"wmod{l}", [48, 128, KC, 128]), bmod=din(f"bmod{l}", [128, 48]),
            n1g=din(f"n1g{l}", [128, KC]), n2g=din(f"n2g{l}", [128, KC]),
            wfm=din(f"wfm{l}", [NFM, 128, KC, 128]), bfm=din(f"bfm{l}", [128, NFM]),
            wtm=din(f"wtm{l}", [128, KC, TMW]), btm=din(f"btm{l}", [128, TMW]),
            gml=din(f"gml{l}", [128, 512]), sink=din(f"sink{l}", [128, 2, 512]),
            qng=din(f"qng{l}", [128, 2]), kng=din(f"kng{l}", [128, 2]),
            wbr0=din(f"wbr0{l}", [8, 128, 4, 128]), wbr1=din(f"wbr1{l}", [8, 64, 8, 128]),
            wbr2=din(f"wbr2{l}", [8, 64, 8, 128]), wo=din(f"wo{l}", [8, 128, KC, 128]),
            w1=din(f"w1{l}", [NFC, 128, KC, 128]), w3=din(f"w3{l}", [NFC, 128, KC, 128]),
            w2=din(f"w2{l}", [8, 128, NFC, 128]),
        ))
    yT = nc.dram_tensor("yT", [D, NLB * 128], F32, kind="ExternalOutput").ap()

    xs = dscr("xs", [D, T], F32)
    b_wmod = dscr("b_wmod", [48, 128, KC * 128], BF16)
    b_wfm = dscr("b_wfm", [NFM, 128, KC * 128], BF16)
    b_wtm = dscr("b_wtm", [128, KC * TMW], BF16)
    b_wbr0 = dscr("b_wbr0", [8, 128, 4 * 128], BF16)
    b_wbr1 = dscr("b_wbr1", [8, 64, 8 * 128], BF16)
    b_wbr2 = dscr("b_wbr2", [8, 64, 8 * 128], BF16)
    b_wo = dscr("b_wo", [8, 128, KC * 128], BF16)
    b_w1 = dscr("b_w1", [NFC, 128, KC * 128], BF16)
    b_w3 = dscr("b_w3", [NFC, 128, KC * 128], BF16)
    b_w2 = dscr("b_w2", [8, 128, NFC * 128], BF16)
    mqT = dscr("mqT", [512, T], BF16)
    mkT = dscr("mkT", [512, T], BF16)
    wqT = dscr("wqT", [512, T], BF16)
    wkT = dscr("wkT", [128, T], BF16)
    gqT = dscr("gqT", [512, T], BF16)
    gkT = dscr("gkT", [128, T], BF16)
    bgT = dscr("bgT", [3072, T], BF16)
    mk_tm = dscr("mk_tm", [T, 512], BF16)
    mv_tm = dscr("mv_tm", [T, 512], BF16)
    mo_tm = dscr("mo_tm", [T, 512], BF16)
    wgv_tm = dscr("wgv_tm", [T, 256], BF16)
    gat_tm = dscr("gat_tm", [T, 16], F32)
    hb_tm = dscr("hb_tm", [T, 512], F32)
    yaT = dscr("yaT", [512, T], BF16)
    ybT = dscr("ybT", [64, 8, T], BF16)
    ycT = dscr("ycT", [64, 8, T], BF16)
    DB = {k: Buf(k) for k in ["xs", "w", "proj", "hb", "ya", "yb", "yc"]}

    stack0 = contextlib.ExitStack()
    psum2 = [nc.alloc_psum_tensor(f"dbank{i}", [128, 1024], F32) for i in range(4)]
    psum = [psum2[i // 2][:, (i % 2) * 512:(i % 2 + 1) * 512] for i in range(8)]
    psb = [Buf(f"bank{i}") for i in range(8)]

    wq_i = [0]

    def wq():
        return "sp"

    def sb(stack, name, shape, dt):
        _UID[0] += 1
        return stack.enter_context(nc.sbuf_tensor(f"{name}u{_UID[0]}", shape, dt))

    c_f32 = sb(stack0, "c_f32", [128, 128], F32)
    ones_f = sb(stack0, "ones_f", [128, 128], F32)
    tri_f = sb(stack0, "tri_f", [128, 128], F32)
    tri_b = sb(stack0, "tri_b", [128, 128], F32)
    m_le = sb(stack0, "m_le", [128, 128], BF16)
    m_ge = sb(stack0, "m_ge", [128, 128], BF16)
    onesm = sb(stack0, "onesm", [128, 128], BF16)
    bd64 = sb(stack0, "bd64", [128, 128], BF16)
    ident = sb(stack0, "ident", [128, 128], BF16)
    eps_sb = sb(stack0, "eps_sb", [128, 1], F32)
    CB = Buf("consts")

    def mk_consts():
        S.add("pool", lambda e: e.memset(ones_f[:], 1.0), writes=[CB])
        S.add("pool", lambda e: e.memset(eps_sb[:], EPS), writes=[CB])
        S.add("pool", lambda e: e.memset(onesm[:], 1.0 / 1024.0), writes=[CB])
        S.add("pool", lambda e: e.affine_select(out=tri_f[:], in_=ones_f[:], pattern=[[1, 128]],
                                                compare_op=ALU.is_ge, fill=0.0, base=0, channel_multiplier=-1),
              reads=[CB], writes=[CB])
        S.add("pool", lambda e: e.affine_select(out=tri_b[:], in_=ones_f[:], pattern=[[-1, 128]],
                                                compare_op=ALU.is_ge, fill=0.0, base=0, channel_multiplier=1),
              reads=[CB], writes=[CB])
        S.add("pool", lambda e: e.tensor_copy(out=m_le[:], in_=tri_f[:]), reads=[CB], writes=[CB])
        S.add("pool", lambda e: e.tensor_copy(out=m_ge[:], in_=tri_b[:]), reads=[CB], writes=[CB])
        S.add("pool", lambda e: e.tensor_tensor(out=ident[:], in0=tri_f[:], in1=tri_b[:], op=ALU.mult),
              reads=[CB], writes=[CB])
        S.add("pool", lambda e: e.memset(bd64[:], 0.0), reads=[CB], writes=[CB])
        S.add("pool", lambda e: e.memset(bd64[0:64, 0:64], 1.0 / 64.0), reads=[CB], writes=[CB])
        S.add("pool", lambda e: e.memset(bd64[64:128, 64:128], 1.0 / 64.0), reads=[CB], writes=[CB])

    mk_consts()

    cast_i = [0]

    def cast_pack(stack, src, dst, nchunk, P, F):
        pieces = []
        for c in range(nchunk):
            f0 = 0
            while f0 < F:
                fw = min(2048, F - f0)
                pieces.append((c, f0, fw))
                f0 += fw
        for (c, f0, fw) in pieces:
            a, ab = cast_f.next()
            o, ob = cast_o.next()
            S.dma(a[0:P, 0:fw], srcflat(src, c)[:, f0:f0 + fw], reads=[], writes=[ab], q="sp")
            eng = ("dve", "pool", "act")[cast_i[0] % 3]
            cast_i[0] += 1
            if eng == "act":
                S.add("act", lambda e, a=a, o=o, fw=fw: e.copy(out=o[0:P, 0:fw], in_=a[0:P, 0:fw]), [ab], [ob])
            else:
                S.add(eng, lambda e, a=a, o=o, fw=fw: e.tensor_copy(out=o[0:P, 0:fw], in_=a[0:P, 0:fw]), [ab], [ob])
            S.dma(dst[c][:, f0:f0 + fw], o[0:P, 0:fw], reads=[ob], writes=[DB["w"]], q="pool")

    def srcflat(src, c):
        v = src[c]
        if len(v.shape) == 3:
            return v.rearrange("p a b -> p (a b)")
        return v

    for l in range(depth):
        W = L[l]
        ctx_out = l < depth - 1
        xin = xT0 if l == 0 else xs
        xin_v = xin.rearrange("(k p) t -> p k t", p=128)
        xs_v = xs.rearrange("(k p) t -> p k t", p=128)

        with contextlib.ExitStack() as st:
            cast_f = Ring(nc, st, "cast_f", [128, 2048], F32, 3)
            cast_o = Ring(nc, st, "cast_o", [128, 2048], BF16, 3)
            cast_pack(st, W["wmod"], b_wmod, 48, 128, KC * 128)
            cast_pack(st, W["wfm"], b_wfm, NFM, 128, KC * 128)
            cast_pack(st, W["wtm"].rearrange("p (o a) b -> o p a b", o=1), b_wtm.rearrange("(o p) f -> o p f", o=1), 1, 128, KC * TMW)
            cast_pack(st, W["wbr0"], b_wbr0, 8, 128, 4 * 128)
            cast_pack(st, W["wbr1"], b_wbr1, 8, 64, 8 * 128)
            cast_pack(st, W["wbr2"], b_wbr2, 8, 64, 8 * 128)
            cast_pack(st, W["wo"], b_wo, 8, 128, KC * 128)
            cast_pack(st, W["w1"], b_w1, NFC, 128, KC * 128)
            cast_pack(st, W["w3"], b_w3, NFC, 128, KC * 128)
            cast_pack(st, W["w2"], b_w2, 8, 128, NFC * 128)
            S.barrier()

        lay = contextlib.ExitStack()
        MOD = sb(lay, "MOD", [128, 48, 2], F32)
        G1 = sb(lay, "G1", [128, KC, 2], F32)
        G2 = sb(lay, "G2", [128, KC, 2], F32)
        MB = Buf("MOD")
        with contextlib.ExitStack() as st:
            c_sb = sb(st, "c_sb", [128, KC, 2], F32)
            sc = sb(st, "sc", [128, KC, 2], BF16)
            bm = sb(st, "bm", [128, 48], F32)
            ng = sb(st, "ng", [128, 2, KC], F32)
            wr = Ring(nc, st, "wmodc", [128, KC, 128], BF16, 4)
            tb = Buf("m_tmp")
            S.dma(c_sb[:], cT, writes=[tb])
            S.dma(bm[:], W["bmod"], writes=[tb])
            S.dma(ng[:, 0, :], W["n1g"], writes=[tb])
            S.dma(ng[:, 1, :], W["n2g"], writes=[tb])
            S.add("act", lambda e: e.activation(out=sc[:], in_=c_sb[:], func=AF.Silu), [tb], [tb])
            pm = psum[0]
            for mc in range(48):
                w_, wb = wr.next()
                S.dma(w_[:].rearrange("p a b -> p (a b)"), b_wmod[mc], reads=[DB["w"]], writes=[wb])
                for kc in range(KC):
                    S.add("pe", lambda e, w_=w_, kc=kc, mc=mc: e.matmul(
                        pm[:, 2 * mc:2 * mc + 2], lhsT=w_[:, kc, :], rhs=sc[:, kc, :], start=(kc == 0), stop=(kc == KC - 1)),
                        [wb, tb], [psb[0]])
            S.add("dve", lambda e: e.tensor_tensor(
                out=MOD[:], in0=pm[:, 0:96].rearrange("p (m w) -> p m w", w=2),
                in1=bm[:].unsqueeze(2).to_broadcast([128, 48, 2]), op=ALU.add), [psb[0], tb], [MB])
            S.add("dve", lambda e: e.scalar_tensor_tensor(
                out=G1[:], in0=MOD[:, 8:16, :], scalar=1.0, in1=ng[:, 0, :].unsqueeze(2).to_broadcast([128, KC, 2]),
                op0=ALU.add, op1=ALU.mult), [MB, tb], [MB])
            S.add("dve", lambda e: e.scalar_tensor_tensor(
                out=G2[:], in0=MOD[:, 32:40, :], scalar=1.0, in1=ng[:, 1, :].unsqueeze(2).to_broadcast([128, KC, 2]),
                op0=ALU.add, op1=ALU.mult), [MB, tb], [MB])
            if dbg and l == 0:
                dbg_mod = dscr("dbg_mod", [128, 96], F32)
                dbg_g1 = dscr("dbg_g1", [128, 16], F32)
                S.dma(dbg_mod, MOD[:].rearrange("p a b -> p (a b)"), reads=[MB])
                S.dma(dbg_g1, G1[:].rearrange("p a b -> p (a b)"), reads=[MB])
            S.barrier()

        with contextlib.ExitStack() as st:
            wtm_sb = sb(st, "wtm_sb", [128, KC, TMW], BF16)
            btm_sb = sb(st, "btm_sb", [128, TMW], F32)
            bfm_sb = sb(st, "bfm_sb", [128, NFM], F32)
            qng_sb = sb(st, "qng_sb", [128, 2], F32)
            kng_sb = sb(st, "kng_sb", [128, 2], F32)
            AB = Buf("a_const")
            S.dma(wtm_sb[:].rearrange("p a b -> p (a b)"), b_wtm, reads=[DB["w"]], writes=[AB])
            S.dma(btm_sb[:], W["btm"], writes=[AB])
            S.dma(bfm_sb[:], W["bfm"], writes=[AB])
            S.dma(qng_sb[:], W["qng"], writes=[AB])
            S.dma(kng_sb[:], W["kng"], writes=[AB])
            xr = Ring(nc, st, "a_x", [128, KC, 512], F32, 2)
            sqr = Ring(nc, st, "a_sq", [128, KC, 512], BF16, 1)
            hr = Ring(nc, st, "a_h", [128, KC, 512], BF16, 2)
            rsr = Ring(nc, st, "a_rs", [128, 512], F32, 2)
            tmpr = Ring(nc, st, "a_tmp", [128, 512], F32, 3)
            wcr = Ring(nc, st, "a_wc", [128, KC, 128], BF16, 6)
            csr = Ring(nc, st, "a_cs", [128, 2, 512], F32, 2)
            f32r = Ring(nc, st, "a_f", [128, 512], F32, 8)
            bfr = Ring(nc, st, "a_b", [128, 512], BF16, 6)
            tmo = Ring(nc, st, "a_tmo", [128, 512], BF16, 4)
            gto = Ring(nc, st, "a_gto", [128, 16], F32, 2)
            pi = [0]

            def pbank(lo, n):
                i = lo + pi[0] % n
                pi[0] += 1
                return psum[i], psb[i]

            def proj(c, h_, hb, tt):
                w_, wb = wcr.next()
                S.dma(w_[:].rearrange("p a b -> p (a b)"), b_wfm[c], reads=[DB["w"]], writes=[wb], q=wq())
                p_, pb = pbank(1, 4)
                for kc in range(KC):
                    S.add("pe", lambda e, w_=w_, kc=kc, p_=p_: e.matmul(
                        p_[:, 0:tt], lhsT=w_[:, kc, :], rhs=h_[:, kc, 0:tt], start=(kc == 0), stop=(kc == KC - 1)),
                        [wb, hb], [pb])
                return p_, pb

            def store(dst, row0, t0, tt, o_, ob):
                S.dma(dst[row0:row0 + 128, t0:t0 + tt], o_[:, 0:tt], reads=[ob], writes=[DB["proj"]], q="pool")

            dbg_h = dscr("dbg_h", [D, T], BF16) if (dbg and l == 0) else None

            def a_tile(t0, tt):
                wsel = 1 if t0 == 0 else 0
                x_, xb = xr.next()
                S.dma(x_[:, :, 0:tt], xin_v[:, :, t0:t0 + tt], reads=[DB["xs"]], writes=[xb])
                cs_, csb = csr.next()
                S.dma(cs_[:, 0, 0:tt], cosd[:, t0:t0 + tt], writes=[csb], q="pool")
                S.dma(cs_[:, 1, 0:tt], sind[:, t0:t0 + tt], writes=[csb], q="pool")
                sq_, sqb = sqr.next()
                S.add("act", lambda e, x_=x_, sq_=sq_: e.activation(out=sq_[:, :, 0:tt], in_=x_[:, :, 0:tt], func=AF.Square),
                      [xb], [sqb])
                pm, pmb = psum[0], psb[0]
                for kc in range(KC):
                    S.add("pe", lambda e, kc=kc, sq_=sq_: e.matmul(pm[:, 0:tt], lhsT=onesm[:], rhs=sq_[:, kc, 0:tt],
                                                                   start=(kc == 0), stop=(kc == KC - 1)), [sqb, CB], [pmb])
                rs_, rsb = rsr.next()
                S.add("act", lambda e, rs_=rs_: e.activation(out=rs_[:, 0:tt], in_=pm[:, 0:tt], func=AF.Ln, bias=eps_sb[:, 0:1], scale=1.0),
                      [pmb, CB], [rsb])
                S.add("act", lambda e, rs_=rs_: e.activation(out=rs_[:, 0:tt], in_=rs_[:, 0:tt], func=AF.Exp, scale=-0.5), [rsb], [rsb])
                h_, hb = hr.next()
                for kc in range(KC):
                    tm_, tmb = tmpr.next()
                    S.add("dve" if kc % 2 == 0 else "pool", lambda e, kc=kc, tm_=tm_, x_=x_, rs_=rs_: e.tensor_tensor(
                        out=tm_[:, 0:tt], in0=x_[:, kc, 0:tt], in1=rs_[:, 0:tt], op=ALU.mult), [xb, rsb], [tmb])
                    S.add("act", lambda e, kc=kc, tm_=tm_, h_=h_: e.activation(
                        out=h_[:, kc, 0:tt], in_=tm_[:, 0:tt], func=AF.Identity,
                        scale=G1[:, kc, wsel:wsel + 1], bias=MOD[:, kc, wsel:wsel + 1]), [tmb, MB], [hb])
                if dbg and l == 0:
                    S.dma(dbg_h.rearrange("(k p) t -> p k t", p=128)[:, :, t0:t0 + tt], h_[:, :, 0:tt], reads=[hb])
                for c in range(8):
                    p_, pb = proj(c, h_, hb, tt)
                    o_, ob = bfr.next()
                    S.add("act", lambda e, p_=p_, o_=o_, c=c: e.activation(
                        out=o_[:, 0:tt], in_=p_[:, 0:tt], func=AF.Identity, bias=bfm_sb[:, c:c + 1], scale=1.0), [pb, AB], [ob])
                    store(mqT if c < 4 else mkT, (c % 4) * 128, t0, tt, o_, ob)
                for (ca, cb_, dst, row0, norm) in (
                        [(8 + j, 12 + j, wqT, j * 128, None) for j in range(4)] + [(16, 17, wkT, 0, None)] +
                        [(18 + j, 22 + j, gqT, j * 128, qng_sb) for j in range(4)] + [(26, 27, gkT, 0, kng_sb)]):
                    pa, pab = proj(ca, h_, hb, tt)
                    pb2, pbb = proj(cb_, h_, hb, tt)
                    q1, q1b = f32r.next()
                    q2, q2b = f32r.next()
                    if norm is None:
                        S.add("dve", lambda e, pa=pa, q1=q1, ca=ca, cs_=cs_: e.scalar_tensor_tensor(
                            out=q1[:, 0:tt], in0=pa[:, 0:tt], scalar=bfm_sb[:, ca:ca + 1], in1=cs_[:, 0, 0:tt],
                            op0=ALU.add, op1=ALU.mult), [pab, AB, csb], [q1b])
                        S.add("dve", lambda e, pb2=pb2, q2=q2, cb_=cb_, cs_=cs_: e.scalar_tensor_tensor(
                            out=q2[:, 0:tt], in0=pb2[:, 0:tt], scalar=bfm_sb[:, cb_:cb_ + 1], in1=cs_[:, 1, 0:tt],
                            op0=ALU.add, op1=ALU.mult), [pbb, AB, csb], [q2b])
                        o_, ob = bfr.next()
                        S.add("pool", lambda e, q1=q1, q2=q2, o_=o_: e.tensor_tensor(
                            out=o_[:, 0:tt], in0=q1[:, 0:tt], in1=q2[:, 0:tt], op=ALU.add), [q1b, q2b], [ob])
                    else:
                        qa, qab = f32r.next()
                        qs, qsb = f32r.next()
                        S.add("act", lambda e, pa=pa, qa=qa, ca=ca: e.activation(
                            out=qa[:, 0:tt], in_=pa[:, 0:tt], func=AF.Identity, bias=bfm_sb[:, ca:ca + 1], scale=1.0),
                            [pab, AB], [qab])
                        S.add("act", lambda e, pb2=pb2, qs=qs, cb_=cb_: e.activation(
                            out=qs[:, 0:tt], in_=pb2[:, 0:tt], func=AF.Identity, bias=bfm_sb[:, cb_:cb_ + 1], scale=1.0),
                            [pbb, AB], [qsb])
                        s2, s2b = bfr.next()
                        S.add("pool", lambda e, qa=qa, s2=s2: e.tensor_tensor(
                            out=s2[:, 0:tt], in0=qa[:, 0:tt], in1=qa[:, 0:tt], op=ALU.mult), [qab], [s2b])
                        pn, pnb = pbank(5, 2)
                        S.add("pe", lambda e, pn=pn, s2=s2: e.matmul(pn[:, 0:tt], lhsT=bd64[:], rhs=s2[:, 0:tt],
                                                                    start=True, stop=True), [s2b, CB], [pnb])
                        rn, rnb = f32r.next()
                        S.add("act", lambda e, pn=pn, rn=rn: e.activation(
                            out=rn[:, 0:tt], in_=pn[:, 0:tt], func=AF.Ln, bias=eps_sb[:, 0:1], scale=1.0), [pnb, CB], [rnb])
                        S.add("act", lambda e, rn=rn: e.activation(out=rn[:, 0:tt], in_=rn[:, 0:tt], func=AF.Exp, scale=-0.5),
                              [rnb], [rnb])
                        S.add("dve", lambda e, qa=qa, q1=q1, cs_=cs_, norm=norm: e.scalar_tensor_tensor(
                            out=q1[:, 0:tt], in0=qa[:, 0:tt], scalar=norm[:, 0:1], in1=cs_[:, 0, 0:tt],
                            op0=ALU.mult, op1=ALU.mult), [qab, AB, csb], [q1b])
                        S.add("dve", lambda e, qs=qs, q2=q2, cs_=cs_, norm=norm: e.scalar_tensor_tensor(
                            out=q2[:, 0:tt], in0=qs[:, 0:tt], scalar=norm[:, 1:2], in1=cs_[:, 1, 0:tt],
                            op0=ALU.mult, op1=ALU.mult), [qsb, AB, csb], [q2b])
                        S.add("dve", lambda e, q1=q1, q2=q2: e.tensor_tensor(
                            out=q1[:, 0:tt], in0=q1[:, 0:tt], in1=q2[:, 0:tt], op=ALU.add), [q1b, q2b], [q1b])
                        o_, ob = bfr.next()
                        S.add("pool", lambda e, q1=q1, rn=rn, o_=o_: e.tensor_tensor(
                            out=o_[:, 0:tt], in0=q1[:, 0:tt], in1=rn[:, 0:tt], op=ALU.mult), [q1b, rnb], [ob])
                    store(dst, row0, t0, tt, o_, ob)
                for c in range(28, 52):
                    p_, pb = proj(c, h_, hb, tt)
                    o_, ob = bfr.next()
                    S.add("act", lambda e, p_=p_, o_=o_, c=c: e.activation(
                        out=o_[:, 0:tt], in_=p_[:, 0:tt], func=AF.Sigmoid, bias=bfm_sb[:, c:c + 1], scale=1.0), [pb, AB], [ob])
                    store(bgT, (c - 28) * 128, t0, tt, o_, ob)
                for blk in range(tt // 128):
                    r0 = t0 + blk * 128
                    for gi, (c0, cw, dst, dcol) in enumerate(((0, 512, mk_tm, 0), (512, 512, mv_tm, 0), (1024, 512, mo_tm, 0),
                                                              (1536, 256, wgv_tm, 0), (1792, 16, gat_tm, 0))):
                        p_, pb = pbank(5, 3)
                        for kc in range(KC):
                            S.add("pe", lambda e, p_=p_, kc=kc, c0=c0, cw=cw, blk=blk: e.matmul(
                                p_[:, 0:cw], lhsT=h_[:, kc, blk * 128:(blk + 1) * 128], rhs=wtm_sb[:, kc, c0:c0 + cw],
                                start=(kc == 0), stop=(kc == KC - 1)), [hb, AB], [pb])
                        if gi == 4:
                            o_, ob = gto.next()
                            S.add("dve", lambda e, p_=p_, o_=o_, c0=c0, cw=cw: e.tensor_tensor(
                                out=o_[:, 0:cw], in0=p_[:, 0:cw], in1=btm_sb[:, c0:c0 + cw], op=ALU.add), [pb, AB], [ob])
                        else:
                            o_, ob = tmo.next()
                            S.add("dve", lambda e, p_=p_, o_=o_, c0=c0, cw=cw: e.tensor_tensor(
                                out=o_[:, 0:cw], in0=p_[:, 0:cw], in1=btm_sb[:, c0:c0 + cw], op=ALU.add), [pb, AB], [ob])
                            if gi == 2:
                                S.add("act", lambda e, o_=o_, cw=cw: e.activation(out=o_[:, 0:cw], in_=o_[:, 0:cw], func=AF.Sigmoid),
                                      [ob], [ob])
                        S.dma(dst[r0:r0 + 128, 0:cw], o_[:, 0:cw], reads=[ob], writes=[DB["proj"]], q="pool")

            for (t0_, tt_) in tiles:
                a_tile(t0_, tt_)
            S.barrier()

        with contextlib.ExitStack() as st:
            E_st = sb(st, "E_st", [128, 8, 129], F32)
            C_bf = sb(st, "C_bf", [128, 8, 129], BF16)
            ebp = sb(st, "ebp", [128, 8], F32)
            gml_sb = sb(st, "gml_sb", [128, 512], F32)
            SB_ = [Buf(f"st{g}") for g in range(8)]
            EBP = Buf("ebp")
            GB = Buf("gml")
            S.dma(gml_sb[:], W["gml"], writes=[GB])
            S.add("pool", lambda e: e.memset(E_st[:], 0.0), writes=SB_)
            S.add("pool", lambda e: e.memset(C_bf[:], 0.0), writes=SB_)
            S.add("pool", lambda e: e.memset(ebp[:], 1.0), writes=[EBP])
            qTr = Ring(nc, st, "b_qT", [128, 4, 128], BF16, 3)
            kTr = Ring(nc, st, "b_kT", [128, 4, 128], BF16, 3)
            ktr = Ring(nc, st, "b_kt", [128, 512], BF16, 3)
            vtr = Ring(nc, st, "b_vt", [128, 4, 129], BF16, 3)
            gtr = Ring(nc, st, "b_gt", [128, 16], F32, 3)
            for (v_, vb_) in zip(vtr.t, vtr.b):
                S.add("pool", lambda e, v_=v_: e.memset(v_[:], 1.0), writes=[vb_])
            smr = Ring(nc, st, "b_sm", [128, 128], BF16, 4)
            vur = Ring(nc, st, "b_vu", [128, 129], BF16, 4)
            g1r = Ring(nc, st, "b_g1", [128, 32], F32, 3)
            smallr = Ring(nc, st, "b_sml", [128, 4], F32, 8)
            hfr = Ring(nc, st, "b_hf", [128, 512], F32, 3)
            hbr = Ring(nc, st, "b_hb", [128, 512], F32, 2)
            mor = Ring(nc, st, "b_mo", [128, 512], BF16, 2)
            ybr = Ring(nc, st, "b_y", [128, 512], BF16, 2)
            yTr = Ring(nc, st, "b_yT", [128, 4, 128], BF16, 2)
            mq_v = mqT.rearrange("(h d) t -> d h t", d=128)
            mk_v = mkT.rearrange("(h d) t -> d h t", d=128)
            ya_v = yaT.rearrange("(h d) t -> d h t", d=128)
            scale_ml = 128.0 ** -0.5
            bi = [0]

            def bbank(lo, n):
                i = lo + bi[0] % n
                bi[0] += 1
                return psum[i], psb[i]

            def b_block(dr, c):
                    t0 = c * 128
                    need_out = ctx_out or c >= 2
                    q_, qb = qTr.next()
                    k_, kb = kTr.next()
                    kt_, ktb = ktr.next()
                    vt_, vtb = vtr.next()
                    gt_, gtb = gtr.next()
                    S.dma(q_[:], mq_v[:, :, t0:t0 + 128], reads=[DB["proj"]], writes=[qb])
                    S.dma(k_[:], mk_v[:, :, t0:t0 + 128], reads=[DB["proj"]], writes=[kb])
                    S.dma(kt_[:], mk_tm[t0:t0 + 128, :], reads=[DB["proj"]], writes=[ktb], q="sp")
                    S.dma(vt_[:, :, 0:128], mv_tm[t0:t0 + 128, :].rearrange("t (h e) -> t h e", e=128), reads=[DB["proj"]],
                          writes=[vtb], q="sp")
                    S.dma(gt_[:], gat_tm[t0:t0 + 128, :], reads=[DB["proj"]], writes=[gtb])
                    g1, g1b = g1r.next()
                    S.add("act", lambda e, gt_=gt_, g1=g1: e.activation(
                        out=g1[:, 16:20], in_=gt_[:, 8 + dr * 4:12 + dr * 4], func=AF.Exp, scale=-1.0), [gtb], [g1b])
                    S.add("act", lambda e, g1=g1: e.activation(out=g1[:, 0:4], in_=g1[:, 16:20], func=AF.Ln, bias=1.0, scale=1.0),
                          [g1b], [g1b])
                    S.add("dve", lambda e, g1=g1: e.tensor_scalar(out=g1[:, 0:4], in0=g1[:, 0:4], scalar1=-1.0, scalar2=None,
                                                                  op0=ALU.mult), [g1b], [g1b])
                    pg, pgb = psum[0], psb[0]
                    tri = tri_b if dr == 1 else tri_f
                    S.add("pe", lambda e, g1=g1, tri=tri: e.matmul(pg[:, 0:4], lhsT=tri[:], rhs=g1[:, 0:4], start=True, stop=True),
                          [g1b, CB], [pgb])
                    S.add("pe", lambda e, g1=g1: e.matmul(pg[:, 4:8], lhsT=ones_f[:], rhs=g1[:, 0:4], start=True, stop=True),
                          [g1b, CB], [pgb])
                    S.add("dve", lambda e, g1=g1, gt_=gt_: e.tensor_tensor(
                        out=g1[:, 16:20], in0=gt_[:, dr * 4:dr * 4 + 4], in1=pg[:, 0:4], op=ALU.subtract), [gtb, pgb, g1b], [g1b])
                    S.add("act", lambda e, g1=g1: e.activation(out=g1[:, 4:8], in_=g1[:, 16:20], func=AF.Exp), [g1b], [g1b])
                    S.add("act", lambda e, g1=g1: e.activation(out=g1[:, 8:16], in_=pg[:, 0:8], func=AF.Exp), [pgb, g1b], [g1b])
                    S.add("dve", lambda e, g1=g1: e.tensor_scalar(out=g1[:, 8:12], in0=g1[:, 8:12], scalar1=scale_ml, scalar2=None,
                                                                  op0=ALU.mult), [g1b], [g1b])
                    if need_out:
                        hf, hfb = hfr.next()
                    def stage1(h):
                        ps_, psb_ = bbank(1, 2)
                        S.add("pe", lambda e, ps_=ps_, k_=k_, q_=q_, h=h: e.matmul(
                            ps_[:, 0:128], lhsT=k_[:, h, :], rhs=q_[:, h, :], start=True, stop=True), [kb, qb], [psb_])
                        sm_, smb = smr.next()
                        msk = m_ge if dr == 1 else m_le
                        S.add("dve", lambda e, ps_=ps_, sm_=sm_, msk=msk: e.tensor_tensor(
                            out=sm_[:], in0=ps_[:, 0:128], in1=msk[:], op=ALU.mult), [psb_, CB], [smb])
                        vu_, vub = vur.next()
                        S.add("act", lambda e, vu_=vu_, vt_=vt_, g1=g1, h=h: e.activation(
                            out=vu_[:], in_=vt_[:, h, :], func=AF.Identity, scale=g1[:, 4 + h:5 + h]), [vtb, g1b], [vub])
                        return sm_, smb, vu_, vub

                    def stage2(h, sm_, smb, vu_, vub):
                        g = dr * 4 + h
                        if need_out:
                            ph, phb = bbank(3, 2)
                            S.add("pe", lambda e, ph=ph, sm_=sm_, vu_=vu_: e.matmul(
                                ph[:, 0:129], lhsT=sm_[:], rhs=vu_[:], start=True, stop=False), [smb, vub], [phb])
                            S.add("pe", lambda e, ph=ph, q_=q_, h=h, g=g: e.matmul(
                                ph[:, 0:129], lhsT=q_[:, h, :], rhs=C_bf[:, g, :], start=False, stop=True), [qb, SB_[g]], [phb])
                            sm4, sm4b = smallr.next()
                            S.add("act", lambda e, ph=ph, sm4=sm4, g1=g1, h=h: e.activation(
                                out=sm4[:, 0:1], in_=ph[:, 128:129], func=AF.Abs, scale=g1[:, 8 + h:9 + h]), [phb, g1b], [sm4b])
                            S.add("dve", lambda e, sm4=sm4: e.tensor_scalar(
                                out=sm4[:, 0:1], in0=sm4[:, 0:1], scalar1=1.0, scalar2=None, op0=ALU.max), [sm4b], [sm4b])
                            S.add("dve", lambda e, sm4=sm4: e.reciprocal(out=sm4[:, 2:3], in_=sm4[:, 0:1]), [sm4b], [sm4b])
                            S.add("dve", lambda e, sm4=sm4, g1=g1, h=h: e.tensor_tensor(
                                out=sm4[:, 1:2], in0=g1[:, 8 + h:9 + h], in1=sm4[:, 2:3], op=ALU.mult), [sm4b, g1b], [sm4b])
                            S.add("act", lambda e, ph=ph, hf=hf, sm4=sm4, h=h: e.activation(
                                out=hf[:, h * 128:(h + 1) * 128], in_=ph[:, 0:128], func=AF.Identity, scale=sm4[:, 1:2]),
                                [phb, sm4b], [hfb])
                        pc, pcb = bbank(5, 2)
                        S.add("pe", lambda e, pc=pc, kt_=kt_, vu_=vu_, h=h: e.matmul(
                            pc[:, 0:129], lhsT=kt_[:, h * 128:(h + 1) * 128], rhs=vu_[:], start=True, stop=True), [ktb, vub], [pcb])
                        S.add("dve", lambda e, pc=pc, g=g: e.scalar_tensor_tensor(
                            out=E_st[:, g, :], in0=E_st[:, g, :], scalar=ebp[:, g:g + 1], in1=pc[:, 0:129],
                            op0=ALU.mult, op1=ALU.add), [pcb, EBP, SB_[g]], [SB_[g]])
                        S.add("act", lambda e, g=g, g1=g1, h=h: e.activation(
                            out=C_bf[:, g, :], in_=E_st[:, g, :], func=AF.Identity, scale=g1[:, 12 + h:13 + h]),
                            [SB_[g], g1b], [SB_[g]])
                    st1 = {0: stage1(0), 1: stage1(1)}
                    for h in range(4):
                        stage2(h, *st1[h])
                        if h + 2 < 4:
                            st1[h + 2] = stage1(h + 2)
                    S.add("dve", lambda e, g1=g1: e.tensor_copy(out=ebp[:, dr * 4:dr * 4 + 4], in_=g1[:, 12:16]),
                          [g1b, EBP] + SB_[dr * 4:dr * 4 + 4], [EBP])
                    if not need_out:
                        return
                    if dr == 1:
                        S.dma(hb_tm[t0:t0 + 128, :], hf[:], reads=[hfb], writes=[DB["hb"]], q="act")
                        return
                    hb_, hbb = hbr.next()
                    mo_, mob = mor.next()
                    S.dma(hb_[:], hb_tm[t0:t0 + 128, :], reads=[DB["hb"]], writes=[hbb], q="sp")
                    S.dma(mo_[:], mo_tm[t0:t0 + 128, :], reads=[DB["proj"]], writes=[mob], q="sp")
                    S.add("dve", lambda e, hf=hf, hb_=hb_: e.tensor_tensor(out=hf[:], in0=hf[:], in1=hb_[:], op=ALU.add),
                          [hfb, hbb], [hfb])
                    sm4, sm4b = smallr.next()
                    S.add("dve", lambda e, hb_=hb_, hf=hf: e.tensor_tensor(out=hb_[:], in0=hf[:], in1=hf[:], op=ALU.mult),
                          [hfb, hbb], [hbb])
                    S.add("dve", lambda e, hb_=hb_, sm4=sm4: e.tensor_reduce(
                        out=sm4[:, 0:4], in_=hb_[:].rearrange("p (h e) -> p h e", e=128), axis=AX.X, op=ALU.add), [hbb], [sm4b])
                    S.add("act", lambda e, sm4=sm4: e.activation(out=sm4[:, 0:4], in_=sm4[:, 0:4], func=AF.Ln, bias=eps_sb[:, 0:1],
                                                                 scale=1.0 / 128.0), [sm4b, CB], [sm4b])
                    S.add("act", lambda e, sm4=sm4: e.activation(out=sm4[:, 0:4], in_=sm4[:, 0:4], func=AF.Exp, scale=-0.5),
                          [sm4b], [sm4b])
                    for h in range(4):
                        S.add("dve", lambda e, hf=hf, sm4=sm4, h=h: e.scalar_tensor_tensor(
                            out=hf[:, h * 128:(h + 1) * 128], in0=hf[:, h * 128:(h + 1) * 128], scalar=sm4[:, h:h + 1],
                            in1=gml_sb[:, h * 128:(h + 1) * 128], op0=ALU.mult, op1=ALU.mult), [hfb, sm4b, GB], [hfb])
                    y_, yb_ = ybr.next()
                    S.add("dve", lambda e, hf=hf, mo_=mo_, y_=y_: e.tensor_tensor(out=y_[:], in0=hf[:], in1=mo_[:], op=ALU.mult),
                          [hfb, mob], [yb_])
                    pt, ptb = psum[7], psb[7]
                    ptv = pt[:].bitcast(BF16)
                    for h in range(4):
                        S.add("pe", lambda e, y_=y_, h=h, ptv=ptv: e.transpose(
                            out=ptv[:, h * 128:(h + 1) * 128], in_=y_[:, h * 128:(h + 1) * 128], identity=ident[:]), [yb_, CB], [ptb])
                    yT_, yTb = yTr.next()
                    S.add("act", lambda e, yT_=yT_, ptv=ptv: e.copy(out=yT_[:].rearrange("p a b -> p (a b)"), in_=ptv[:, 0:512]),
                          [ptb], [yTb])
                    S.dma(ya_v[:, :, t0:t0 + 128], yT_[:], reads=[yTb], writes=[DB["ya"]], q="act")

            for dr_ in (1, 0):
                order = [1, 0] + list(range(NB - 1, 1, -1)) if dr_ == 1 else list(range(NB))
                for c_ in order:
                    b_block(dr_, c_)
            S.barrier()

        with contextlib.ExitStack() as st:
            KT = sb(st, "KT", [128, T], BF16)
            VV = sb(st, "VV", [128, NB, 2, 65], BF16)
            sink_sb = sb(st, "sink_sb", [128, 2, 512], F32)
            KB_ = Buf("KT")
            S.dma(sink_sb[:], W["sink"], writes=[KB_])
            S.add("act", lambda e: e.activation(out=sink_sb[:], in_=sink_sb[:], func=AF.Exp), [KB_], [KB_])
            qr = Ring(nc, st, "c_q", [128, 4, 512], BF16, 2)
            ptr = Ring(nc, st, "c_pt", [128, 1024], BF16, 3)
            ostg = Ring(nc, st, "c_o", [64, 8, 512], BF16, 2)
            rdr = Ring(nc, st, "c_rd", [128, 512], F32, 2)
            bcr = Ring(nc, st, "c_bc", [64, 512], F32, 2)
            ci = [0]

            def cbank(lo, n):
                i = lo + ci[0] % n
                ci[0] += 1
                return psum[i], psb[i]

            for kind in ("win", "glb"):
                kT_src = wkT if kind == "win" else gkT
                vcol = 0 if kind == "win" else 128
                q_src = (wqT if kind == "win" else gqT).rearrange("(j p) t -> p j t", p=128)
                y_dst = ybT if kind == "win" else ycT
                ydb = DB["yb"] if kind == "win" else DB["yc"]
                for (t0, tt) in tiles:
                    S.dma(KT[:, t0:t0 + tt], kT_src[:, t0:t0 + tt], reads=[DB["proj"]], writes=[KB_])
                S.add("pool", lambda e: e.memset(VV[:], 1.0), writes=[KB_])
                for b0 in range(0, NB, 26):
                    b1 = min(NB, b0 + 26)
                    for g in range(2):
                        S.dma(VV[:, b0:b1, g, 0:64],
                              wgv_tm[b0 * 128:b1 * 128, vcol + g * 64:vcol + g * 64 + 64].rearrange("(b p) d -> p b d", p=128),
                              reads=[DB["proj"]], writes=[KB_], q="pool")
                groups = []
                for (t0, tt) in tiles:
                    if t0 == 0 and not ctx_out:
                        continue
                    nblk = tt // 128
                    for blk in range(nblk):
                        c = t0 // 128 + blk
                        if c < 2:
                            kbs = [0, 1]
                        elif kind == "glb":
                            kbs = list(range(NB))
                        else:
                            kbs = ([c - 1] if c - 1 >= 2 else []) + [c] + ([c + 1] if c + 1 < NB else []) + [0, 1]
                        groups.append(dict(t0=t0, tt=tt, blk=blk, c=c, kbs=kbs, tile_last=(blk == nblk - 1)))
                units = []
                for gi, G in enumerate(groups):
                    kbs = G["kbs"]
                    for i, kb_ in enumerate(kbs):
                        units.append(dict(gi=gi, kb=kb_, first=(i == 0), last=(i == len(kbs) - 1), idx=len(units)))
                tile_state = {}

                def get_tile(t0, tt, kind=kind, q_src=q_src, tile_state=tile_state):
                    if t0 not in tile_state:
                        q_, qb = qr.next()
                        S.dma(q_[:, :, 0:tt], q_src[:, :, t0:t0 + tt], reads=[DB["proj"]], writes=[qb])
                        os_, osb = ostg.next()
                        tile_state[t0] = (q_, qb, os_, osb)
                    return tile_state[t0]

                def emit_qk(u, kind=kind, groups=groups):
                    G = groups[u["gi"]]
                    blk, c, kb_ = G["blk"], G["c"], u["kb"]
                    q_, qb, os_, osb = get_tile(G["t0"], G["tt"])
                    slot = u["idx"] % 2
                    for g in range(2):
                        ps_ = psum[2 * slot + g]
                        S.add("pe", lambda e, ps_=ps_, kb_=kb_, g=g, q_=q_, blk=blk: e.matmul(
                            ps_.rearrange("p (r q) -> p r q", q=128), lhsT=KT[g * 64:(g + 1) * 64, kb_ * 128:(kb_ + 1) * 128],
                            rhs=q_[g * 64:(g + 1) * 64, :, blk * 128:(blk + 1) * 128], start=True, stop=True),
                            [KB_, qb], [psb[2 * slot + g]])
                    pt_, ptb_ = ptr.next()
                    S.add("act", lambda e, slot=slot, pt_=pt_: e.activation(
                        out=pt_[:, 0:1024], in_=psum2[slot][:, 0:1024], func=AF.Exp, scale=0.125),
                        [psb[2 * slot], psb[2 * slot + 1]], [ptb_])
                    msk = None
                    if kind == "win" and c >= 3 and kb_ == c - 1:
                        msk = m_ge
                    if kind == "win" and c >= 2 and kb_ == c + 1:
                        msk = m_le
                    if msk is not None:
                        S.add("dve", lambda e, pt_=pt_, msk=msk: e.tensor_tensor(
                            out=pt_[:, 0:1024].rearrange("p (r q) -> p r q", q=128),
                            in0=pt_[:, 0:1024].rearrange("p (r q) -> p r q", q=128),
                            in1=msk[:].unsqueeze(1).to_broadcast([128, 8, 128]), op=ALU.mult), [ptb_, CB], [ptb_])
                    u["pt"] = (pt_, ptb_)

                def emit_pv(u, kind=kind, groups=groups, y_dst=y_dst, ydb=ydb):
                    G = groups[u["gi"]]
                    blk, kb_ = G["blk"], u["kb"]
                    q_, qb, os_, osb = get_tile(G["t0"], G["tt"])
                    pt_, ptb_ = u["pt"]
                    for g in range(2):
                        po, pob = psum[4 + g], psb[4 + g]
                        S.add("pe", lambda e, po=po, kb_=kb_, g=g, pt_=pt_, st_=u["first"], sp_=u["last"]: e.matmul(
                            po[0:65, :], lhsT=VV[:, kb_, g, :], rhs=pt_[:, g * 512:(g + 1) * 512], start=st_, stop=sp_),
                            [KB_, ptb_], [pob])
                    if not u["last"]:
                        return
                    for g in range(2):
                        po, pob = psum[4 + g], psb[4 + g]
                        rd, rdb = rdr.next()
                        if kind == "win":
                            S.add("dve", lambda e, po=po, rd=rd, g=g: e.tensor_tensor(
                                out=rd[64:65, :], in0=po[64:65, :], in1=sink_sb[64:65, g, :], op=ALU.add), [pob, KB_], [rdb])
                            S.add("dve", lambda e, rd=rd: e.reciprocal(out=rd[64:65, :], in_=rd[64:65, :]), [rdb], [rdb])
                        else:
                            S.add("dve", lambda e, po=po, rd=rd: e.reciprocal(out=rd[64:65, :], in_=po[64:65, :]), [pob], [rdb])
                        pbc, pbcb = psum[6 + g], psb[6 + g]
                        S.add("pe", lambda e, pbc=pbc, rd=rd: e.matmul(
                            pbc[0:64, :], lhsT=ones_f[64:65, 0:64], rhs=rd[64:65, :], start=True, stop=True), [rdb, CB], [pbcb])
                        bc, bcb = bcr.next()
                        S.add("dve", lambda e, pbc=pbc, bc=bc: e.tensor_copy(out=bc[:], in_=pbc[0:64, :]), [pbcb], [bcb])
                        S.add("dve", lambda e, po=po, bc=bc, os_=os_, g=g, blk=blk: e.tensor_tensor(
                            out=os_[:, g * 4:(g + 1) * 4, blk * 128:(blk + 1) * 128],
                            in0=po[0:64, :].rearrange("p (r q) -> p r q", q=128),
                            in1=bc[:].rearrange("p (r q) -> p r q", q=128), op=ALU.mult), [pob, bcb], [osb])
                    if G["tile_last"]:
                        t0, tt = G["t0"], G["tt"]
                        S.dma(y_dst[:, :, t0:t0 + tt], os_[:, :, 0:tt], reads=[osb], writes=[ydb], q="pool")

                LOOK = 2
                for i in range(min(LOOK, len(units))):
                    emit_qk(units[i])
                for i, u in enumerate(units):
                    emit_pv(u)
                    if i + LOOK < len(units):
                        emit_qk(units[i + LOOK])
            S.barrier()

        with contextlib.ExitStack() as st:
            xr = Ring(nc, st, "e_x", [128, KC, 512], F32, 1)
            yar = Ring(nc, st, "e_ya", [128, 4, 512], BF16, 1)
            ybr2 = Ring(nc, st, "e_yb", [64, 8, 512], BF16, 1)
            ycr = Ring(nc, st, "e_yc", [64, 8, 512], BF16, 1)
            bgr = Ring(nc, st, "e_bg", [128, 24, 512], BF16, 1)
            mgr = Ring(nc, st, "e_mg", [128, KC, 512], BF16, 1)
            h2r = Ring(nc, st, "e_h2", [128, KC, 512], BF16, 1)
            sqr = Ring(nc, st, "e_sq", [128, KC, 512], BF16, 1)
            ur = Ring(nc, st, "e_u", [128, NFC, 512], BF16, 1)
            w0r = Ring(nc, st, "e_w0", [128, 4, 128], BF16, 3)
            w12r = Ring(nc, st, "e_w12", [64, 8, 128], BF16, 4)
            wor = Ring(nc, st, "e_wo", [128, KC, 128], BF16, 6)
            w2r = Ring(nc, st, "e_w2", [128, NFC, 128], BF16, 2)
            f32r = Ring(nc, st, "e_f", [128, 512], F32, 6)
            rsr = Ring(nc, st, "e_rs", [128, 512], F32, 2)
            fg_sb = sb(st, "fg_sb", [128, KC], F32)
            FGB = Buf("fing")
            S.dma(fg_sb[:], fing, writes=[FGB])
            ei = [0]

            def ebank(lo, n):
                i = lo + ei[0] % n
                ei[0] += 1
                return psum[i], psb[i]

            def rms(x_, xb, tt):
                sq_, sqb = sqr.next()
                S.add("act", lambda e: e.activation(out=sq_[:, :, 0:tt], in_=x_[:, :, 0:tt], func=AF.Square), [xb], [sqb])
                pm, pmb = psum[0], psb[0]
                for kc in range(KC):
                    S.add("pe", lambda e, kc=kc: e.matmul(pm[:, 0:tt], lhsT=onesm[:], rhs=sq_[:, kc, 0:tt],
                                                          start=(kc == 0), stop=(kc == KC - 1)), [sqb, CB], [pmb])
                rs_, rsb = rsr.next()
                S.add("act", lambda e: e.activation(out=rs_[:, 0:tt], in_=pm[:, 0:tt], func=AF.Ln, bias=eps_sb[:, 0:1], scale=1.0),
                      [pmb, CB], [rsb])
                S.add("act", lambda e: e.activation(out=rs_[:, 0:tt], in_=rs_[:, 0:tt], func=AF.Exp, scale=-0.5), [rsb], [rsb])
                return rs_, rsb

            dbg_mg = dscr(f"dbg_mg{l}", [D, T], BF16) if dbg else None
            dbg_x1 = dscr(f"dbg_x1{l}", [D, T], F32) if dbg else None

            def e_tile(t0, tt):
                wsel = 1 if t0 == 0 else 0
                x_, xb = xr.next()
                S.dma(x_[:, :, 0:tt], xin_v[:, :, t0:t0 + tt], reads=[DB["xs"]], writes=[xb])
                ya_, yab = yar.next()
                yb_, ybb = ybr2.next()
                yc_, ycb = ycr.next()
                bg_, bgb = bgr.next()
                S.dma(ya_[:, :, 0:tt], yaT.rearrange("(h d) t -> d h t", d=128)[:, :, t0:t0 + tt], reads=[DB["ya"]], writes=[yab], q="act")
                S.dma(yb_[:, :, 0:tt], ybT[:, :, t0:t0 + tt], reads=[DB["yb"]], writes=[ybb], q="pool")
                S.dma(yc_[:, :, 0:tt], ycT[:, :, t0:t0 + tt], reads=[DB["yc"]], writes=[ycb], q="pool")
                S.dma(bg_[:, :, 0:tt], bgT.rearrange("(c p) t -> p c t", p=128)[:, :, t0:t0 + tt], reads=[DB["proj"]], writes=[bgb], q="act")
                mg_, mgb = mgr.next()
                for oc in range(8):
                    wa, wab = w0r.next()
                    wb1, wb1b = w12r.next()
                    wb2, wb2b = w12r.next()
                    S.dma(wa[:].rearrange("p a b -> p (a b)"), b_wbr0[oc], reads=[DB["w"]], writes=[wab], q=wq())
                    S.dma(wb1[:].rearrange("p a b -> p (a b)"), b_wbr1[oc], reads=[DB["w"]], writes=[wb1b], q=wq())
                    S.dma(wb2[:].rearrange("p a b -> p (a b)"), b_wbr2[oc], reads=[DB["w"]], writes=[wb2b], q=wq())
                    p0, p0b = ebank(1, 3)
                    for k in range(4):
                        S.add("pe", lambda e, k=k, p0=p0, wa=wa: e.matmul(p0[:, 0:tt], lhsT=wa[:, k, :], rhs=ya_[:, k, 0:tt],
                                                                          start=(k == 0), stop=(k == 3)), [wab, yab], [p0b])
                    p1, p1b = ebank(1, 3)
                    for k in range(8):
                        S.add("pe", lambda e, k=k, p1=p1, wb1=wb1: e.matmul(p1[:, 0:tt], lhsT=wb1[:, k, :], rhs=yb_[:, k, 0:tt],
                                                                            start=(k == 0), stop=(k == 7)), [wb1b, ybb], [p1b])
                    p2, p2b = ebank(1, 3)
                    for k in range(8):
                        S.add("pe", lambda e, k=k, p2=p2, wb2=wb2: e.matmul(p2[:, 0:tt], lhsT=wb2[:, k, :], rhs=yc_[:, k, 0:tt],
                                                                            start=(k == 0), stop=(k == 7)), [wb2b, ycb], [p2b])
                    a0, a0b = f32r.next()
                    a1, a1b = f32r.next()
                    a2, a2b = f32r.next()
                    S.add("dve", lambda e, p0=p0, a0=a0, oc=oc: e.tensor_tensor(
                        out=a0[:, 0:tt], in0=p0[:, 0:tt], in1=bg_[:, oc, 0:tt], op=ALU.mult), [p0b, bgb], [a0b])
                    S.add("dve", lambda e, p1=p1, a1=a1, oc=oc: e.tensor_tensor(
                        out=a1[:, 0:tt], in0=p1[:, 0:tt], in1=bg_[:, 8 + oc, 0:tt], op=ALU.mult), [p1b, bgb], [a1b])
                    S.add("dve", lambda e, p2=p2, a2=a2, oc=oc: e.tensor_tensor(
                        out=a2[:, 0:tt], in0=p2[:, 0:tt], in1=bg_[:, 16 + oc, 0:tt], op=ALU.mult), [p2b, bgb], [a2b])
                    S.add("pool", lambda e, a0=a0, a1=a1: e.tensor_tensor(out=a0[:, 0:tt], in0=a0[:, 0:tt], in1=a1[:, 0:tt], op=ALU.add),
                          [a0b, a1b], [a0b])
                    S.add("pool", lambda e, a0=a0, a2=a2, oc=oc: e.tensor_tensor(
                        out=mg_[:, oc, 0:tt], in0=a0[:, 0:tt], in1=a2[:, 0:tt], op=ALU.add), [a0b, a2b], [mgb])
                if dbg:
                    S.dma(dbg_mg.rearrange("(k p) t -> p k t", p=128)[:, :, t0:t0 + tt], mg_[:, :, 0:tt], reads=[mgb])
                for oc in range(8):
                    w_, wb_ = wor.next()
                    S.dma(w_[:].rearrange("p a b -> p (a b)"), b_wo[oc], reads=[DB["w"]], writes=[wb_], q=wq())
                    po, pob = ebank(4, 2)
                    for k in range(KC):
                        S.add("pe", lambda e, k=k, po=po, w_=w_: e.matmul(po[:, 0:tt], lhsT=w_[:, k, :], rhs=mg_[:, k, 0:tt],
                                                                          start=(k == 0), stop=(k == KC - 1)), [wb_, mgb], [pob])
                    S.add("dve", lambda e, po=po, oc=oc: e.scalar_tensor_tensor(
                        out=x_[:, oc, 0:tt], in0=po[:, 0:tt], scalar=MOD[:, 16 + oc, wsel:wsel + 1], in1=x_[:, oc, 0:tt],
                        op0=ALU.mult, op1=ALU.add), [pob, MB, xb], [xb])
                if dbg:
                    S.dma(dbg_x1.rearrange("(k p) t -> p k t", p=128)[:, :, t0:t0 + tt], x_[:, :, 0:tt], reads=[xb], writes=[xb])
                rs_, rsb = rms(x_, xb, tt)
                h2, h2b = h2r.next()
                for kc in range(KC):
                    tm_, tmb = f32r.next()
                    S.add("dve" if kc % 2 == 0 else "pool", lambda e, kc=kc, tm_=tm_: e.tensor_tensor(
                        out=tm_[:, 0:tt], in0=x_[:, kc, 0:tt], in1=rs_[:, 0:tt], op=ALU.mult), [xb, rsb], [tmb])
                    S.add("act", lambda e, kc=kc, tm_=tm_: e.activation(
                        out=h2[:, kc, 0:tt], in_=tm_[:, 0:tt], func=AF.Identity,
                        scale=G2[:, kc, wsel:wsel + 1], bias=MOD[:, 24 + kc, wsel:wsel + 1]), [tmb, MB], [h2b])
                u_, ub = ur.next()
                for fc in range(NFC):
                    w1_, w1b = wor.next()
                    w3_, w3b = wor.next()
                    S.dma(w1_[:].rearrange("p a b -> p (a b)"), b_w1[fc], reads=[DB["w"]], writes=[w1b], q="sp")
                    S.dma(w3_[:].rearrange("p a b -> p (a b)"), b_w3[fc], reads=[DB["w"]], writes=[w3b], q="sp")
                    pa, pab = ebank(6, 2)
                    for k in range(KC):
                        S.add("pe", lambda e, k=k, pa=pa, w1_=w1_: e.matmul(pa[:, 0:tt], lhsT=w1_[:, k, :], rhs=h2[:, k, 0:tt],
                                                                            start=(k == 0), stop=(k == KC - 1)), [w1b, h2b], [pab])
                    pb3, pb3b = ebank(6, 2)
                    for k in range(KC):
                        S.add("pe", lambda e, k=k, pb3=pb3, w3_=w3_: e.matmul(pb3[:, 0:tt], lhsT=w3_[:, k, :], rhs=h2[:, k, 0:tt],
                                                                              start=(k == 0), stop=(k == KC - 1)), [w3b, h2b], [pb3b])
                    sl, slb = f32r.next()
                    S.add("act", lambda e, pa=pa, sl=sl: e.activation(out=sl[:, 0:tt], in_=pa[:, 0:tt], func=AF.Silu), [pab], [slb])
                    S.add("dve", lambda e, pb3=pb3, sl=sl, fc=fc: e.tensor_tensor(
                        out=u_[:, fc, 0:tt], in0=pb3[:, 0:tt], in1=sl[:, 0:tt], op=ALU.mult), [pb3b, slb], [ub])
                for oc in range(8):
                    w_, wb_ = w2r.next()
                    S.dma(w_[:].rearrange("p a b -> p (a b)"), b_w2[oc], reads=[DB["w"]], writes=[wb_], q=wq())
                    po, pob = ebank(4, 2)
                    for k in range(NFC):
                        S.add("pe", lambda e, k=k, po=po, w_=w_: e.matmul(po[:, 0:tt], lhsT=w_[:, k, :], rhs=u_[:, k, 0:tt],
                                                                          start=(k == 0), stop=(k == NFC - 1)), [wb_, ub], [pob])
                    S.add("dve", lambda e, po=po, oc=oc: e.scalar_tensor_tensor(
                        out=x_[:, oc, 0:tt], in0=po[:, 0:tt], scalar=MOD[:, 40 + oc, wsel:wsel + 1], in1=x_[:, oc, 0:tt],
                        op0=ALU.mult, op1=ALU.add), [pob, MB, xb], [xb])
                if l < depth - 1:
                    S.dma(xs_v[:, :, t0:t0 + tt], x_[:, :, 0:tt], reads=[xb], writes=[DB["xs"]], q="pool")
                else:
                    rs2_, rsb2 = rms(x_, xb, tt)
                    for kc in range(KC):
                        S.add("dve", lambda e, kc=kc: e.scalar_tensor_tensor(
                            out=x_[:, kc, 0:tt], in0=x_[:, kc, 0:tt], scalar=fg_sb[:, kc:kc + 1], in1=rs2_[:, 0:tt],
                            op0=ALU.mult, op1=ALU.mult), [xb, rsb2, FGB], [xb])
                    S.dma(yT.rearrange("(k p) t -> p k t", p=128)[:, :, t0 - 256:t0 - 256 + tt], x_[:, :, 0:tt], reads=[xb], q="pool")

            for (t0_, tt_) in tiles:
                if t0_ == 0 and not ctx_out:
                    continue
                e_tile(t0_, tt_)
            S.barrier()
        lay.close()

    S.emit()
    return nc


def _swap_d(d):
    r = d % 32
    return d + 16 if r < 16 else d - 16


def _fm_cols():
    OFF = dict(mq=0, mk=512, mv=1024, mo=1536, ig=2048, fg=2056, wq=2064, wk=2576, wv=2704, gq=2832, gk=3344, gv=3472, bg=3600)
    chunks = []
    for h in range(4):
        chunks.append([OFF["mq"] + h * 128 + d for d in range(128)])
    for h in range(4):
        chunks.append([OFF["mk"] + h * 128 + d for d in range(128)])
    for pre in ("w", "g"):
        q0, k0 = OFF[pre + "q"], OFF[pre + "k"]
        for sw in (False, True):
            for j in range(4):
                cols = []
                for p in range(128):
                    head = j if p < 64 else 4 + j
                    d = p % 64
                    dd = _swap_d(d) if sw else d
                    cols.append(q0 + head * 64 + dd)
                chunks.append(cols)
        for sw in (False, True):
            cols = []
            for p in range(128):
                head = p // 64
                d = p % 64
                dd = _swap_d(d) if sw else d
                cols.append(k0 + head * 64 + dd)
            chunks.append(cols)
    for c in range(24):
        chunks.append([OFF["bg"] + c * 128 + p for p in range(128)])
    assert len(chunks) == NFM
    tm = (list(range(512, 1024)) + list(range(1024, 1536)) + list(range(1536, 2048)) + list(range(2704, 2832)) +
          list(range(3472, 3600)) + list(range(2048, 2064)))
    assert len(tm) == TMW
    return np.array(chunks), np.array(tm)


def _pack(w, kp):
    K, N = w.shape
    return np.ascontiguousarray(w.reshape(K // kp, kp, N // 128, 128).transpose(2, 1, 0, 3))


def _rope_tables(nlat):
    T = nlat + CTX
    cos = np.ones((128, T), np.float32)
    sin = np.zeros((128, T), np.float32)
    t = np.arange(nlat)
    row = (t // 64).astype(np.float32)
    col = (t % 64).astype(np.float32)
    half = 32
    inv = (10000.0 ** (-np.arange(0, half, 2, dtype=np.float32) / half)).astype(np.float32)
    for p in range(128):
        d = p % 64
        pos = row if d < 32 else col
        ang = (pos * inv[d % 16]).astype(np.float32)
        sign = -1.0 if (d % 32) < 16 else 1.0
        cos[p, CTX:] = np.cos(ang)
        sin[p, CTX:] = sign * np.sin(ang)
    return cos, sin


def prep_shared(inp, nlat, depth=DEPTH):
    fm, tm = _fm_cols()
    sh = {}
    cos, sin = _rope_tables(nlat)
    sh["cos"], sh["sins"] = cos, sin
    sh["fing"] = np.ascontiguousarray(np.asarray(inp["final_g"]).reshape(KC, 128).T)
    swp = np.array([_swap_d(p % 64) for p in range(128)])
    for l in range(depth):
        w_in = np.asarray(inp["w_in"][l])
        b_in = np.asarray(inp["b_in"][l])
        sh[f"wmod{l}"] = _pack(np.asarray(inp["w_mod"][l]), 128)
        sh[f"bmod{l}"] = np.ascontiguousarray(np.asarray(inp["b_mod"][l]).reshape(48, 128).T)
        sh[f"n1g{l}"] = np.ascontiguousarray(np.asarray(inp["norm1_g"][l]).reshape(KC, 128).T)
        sh[f"n2g{l}"] = np.ascontiguousarray(np.asarray(inp["norm2_g"][l]).reshape(KC, 128).T)
        wf = w_in[:, fm.reshape(-1)]
        sh[f"wfm{l}"] = _pack(wf, 128)
        sh[f"bfm{l}"] = np.ascontiguousarray(b_in[fm].T)
        sh[f"wtm{l}"] = np.ascontiguousarray(w_in[:, tm].reshape(KC, 128, TMW).transpose(1, 0, 2))
        sh[f"btm{l}"] = np.ascontiguousarray(np.broadcast_to(b_in[tm][None, :], (128, TMW)))
        sh[f"gml{l}"] = np.ascontiguousarray(np.broadcast_to(np.asarray(inp["ml_norm_g"][l]).reshape(1, 512), (128, 512)))
        snk = np.repeat(np.asarray(inp["win_sink"][l]), 128).reshape(1, 2, 512)
        sh[f"sink{l}"] = np.ascontiguousarray(np.broadcast_to(snk, (128, 2, 512)))
        for nm, key in (("qng", "qn_g"), ("kng", "kn_g")):
            g = np.asarray(inp[key][l])
            sh[f"{nm}{l}"] = np.ascontiguousarray(np.stack([g[np.arange(128) % 64], g[swp]], axis=1))
        w_br = np.asarray(inp["w_br"][l])
        sh[f"wbr0{l}"] = _pack(w_br[0], 128)
        sh[f"wbr1{l}"] = _pack(w_br[1], 64).transpose(0, 1, 2, 3).copy()
        sh[f"wbr2{l}"] = _pack(w_br[2], 64).copy()
        sh[f"wo{l}"] = _pack(np.asarray(inp["w_o"][l]), 128)
        sh[f"w1{l}"] = _pack(np.asarray(inp["w_ff1"][l]), 128)
        sh[f"w3{l}"] = _pack(np.asarray(inp["w_ff3"][l]), 128)
        sh[f"w2{l}"] = _pack(np.asarray(inp["w_ff2"][l]), 128)
    return {k: np.ascontiguousarray(v, dtype=np.float32) for k, v in sh.items()}


def prep_core(inp, b):
    x = np.asarray(inp["x"][b])
    ctx = np.asarray(inp["ctx"][b])
    xT0 = np.ascontiguousarray(np.concatenate([ctx, x], axis=0).T)
    cc = np.stack([np.asarray(inp["c"][b]), np.asarray(inp["c_ctx"])], axis=1)
    cT = np.ascontiguousarray(cc.reshape(KC, 128, 2).transpose(1, 0, 2))
    return {"xT0": xT0.astype(np.float32), "cT": cT.astype(np.float32)}


_CACHE = {}


def kernel(**inp):
    x = np.asarray(inp["x"])
    B, nlat, _ = x.shape
    depth = np.asarray(inp["w_in"]).shape[0]
    key = (nlat, depth)
    if key not in _CACHE:
        _CACHE[key] = build(nlat // 128, depth)
    nc = _CACHE[key]
    sh = prep_shared(inp, nlat, depth)
    in_maps = []
    for b in range(B):
        m = dict(sh)
        m.update(prep_core(inp, b))
        in_maps.append(m)
    res = run_bass_kernel_spmd(nc, in_maps, core_ids=list(range(B)))
    out = np.stack([np.ascontiguousarray(r["yT"].T) for r in res.results], axis=0)
    return out.astype(np.float32)
```
